# Optimizing a Trainium2 kernel written in Bass

```python
import jax
import jax.numpy as jnp
from jax import lax
import numpy as np

D_MODEL = 1024
BATCH = 4
SEQ = 8192
DEPTH = 1

HEAD_DIM = 64
RWKV_WIDTH = D_MODEL // 2
RWKV_HEADS = RWKV_WIDTH // HEAD_DIM
DECAY_LORA = 64
ICLR_LORA = 64
RWKV_GN_EPS = 64e-5
NSA_WIDTH = D_MODEL // 4
NSA_HEADS = NSA_WIDTH // HEAD_DIM
N_BRANCH = 3
CMP_BLOCK = 32
CMP_STRIDE = 16
SEL_BLOCK = 64
SEL_TOPK = 16
WINDOW = 512
MEM_LEN = 256
MEM_WIDTH = D_MODEL // 4
MEM_HEADS = 4
MEM_HEAD_DIM = MEM_WIDTH // MEM_HEADS
MIX_WIDTH = RWKV_WIDTH + NSA_WIDTH + MEM_WIDTH
ROPE_THETA = 500000.0
ROPE_DIM = HEAD_DIM // 4
Q_BLOCK = 128
NORM_EPS = 1e-6
NEG_INF = -1e30
FORCE_SCORE = 1e4

RWKV_SIZES = (RWKV_WIDTH, RWKV_WIDTH, RWKV_WIDTH, RWKV_WIDTH, DECAY_LORA, ICLR_LORA)
NSA_SIZES = (NSA_WIDTH, NSA_WIDTH, NSA_HEADS * N_BRANCH) + (HEAD_DIM,) * 6
MEM_SIZES = (MEM_WIDTH, MEM_WIDTH)
RWKV_COLS = sum(RWKV_SIZES)
NSA_COLS = sum(NSA_SIZES)
MEM_COLS = sum(MEM_SIZES)
IN_COLS = RWKV_COLS + NSA_COLS + MEM_COLS

kernel_name = 'hymba_rwkv7_nsa_memx_layer'


def split_cols(p, sizes):
    offs = [int(o) for o in np.cumsum(sizes)[:-1]]
    return jnp.split(p, offs, axis=-1)


def rms_norm(x, g, eps=NORM_EPS):
    xf = x.astype(jnp.float32)
    y = xf * lax.rsqrt(jnp.mean(xf * xf, axis=-1, keepdims=True) + eps)
    return (y * g.astype(jnp.float32)).astype(x.dtype)


def partial_rope(x, pos):
    half = ROPE_DIM // 2
    inv_freq = ROPE_THETA ** (-jnp.arange(half, dtype=jnp.float32) / half)
    ang = pos.astype(jnp.float32)[:, None] * inv_freq[None, :]
    cos = jnp.cos(ang)[:, None, :]
    sin = jnp.sin(ang)[:, None, :]
    xf = x.astype(jnp.float32)
    x1 = xf[..., :half]
    x2 = xf[..., half:ROPE_DIM]
    out = jnp.concatenate([x1 * cos - x2 * sin, x2 * cos + x1 * sin, xf[..., ROPE_DIM:]], axis=-1)
    return out.astype(x.dtype)


def masked_softmax(s, mask):
    s = jnp.where(mask, s.astype(jnp.float32), NEG_INF)
    m = jnp.max(s, axis=-1, keepdims=True)
    e = jnp.exp(s - m) * mask
    return e / jnp.maximum(jnp.sum(e, axis=-1, keepdims=True), 1e-30)


def head_rms_norm(o, g):
    of = o.astype(jnp.float32)
    y = of * lax.rsqrt(jnp.mean(of * of, axis=-1, keepdims=True) + NORM_EPS)
    return (y * g.astype(jnp.float32)).astype(o.dtype)


def rwkv7_scan(r, decay, k, v, kk, a):
    B, T, H, N = r.shape

    def step(S, inp):
        r_t, w_t, k_t, v_t, kk_t, a_t = inp
        sa = jnp.einsum('bhvk,bhk->bhv', S, -kk_t)
        S = (S * w_t[:, :, None, :]
             + sa[..., None] * (kk_t * a_t)[:, :, None, :]
             + v_t[..., None] * k_t[:, :, None, :])
        y = jnp.einsum('bhvk,bhk->bhv', S, r_t)
        return S, y

    xs = tuple(jnp.moveaxis(u, 1, 0) for u in (r, decay, k, v, kk, a))
    S0 = jnp.zeros((B, H, N, N), jnp.float32)
    _, y = lax.scan(step, S0, xs)
    return jnp.moveaxis(y, 0, 1)


def rwkv7_group(p, mu, w0, w_up, a0, a_up, k_k, k_a, r_k, ln_w, ln_b):
    B, T, _ = p.shape
    H, N = RWKV_HEADS, HEAD_DIM
    pf = p.astype(jnp.float32)
    prev = jnp.pad(pf, ((0, 0), (1, 0), (0, 0)))[:, :-1]
    pf = pf + mu * (prev - pf)
    r, k, v, gate, wd, ad = split_cols(pf, RWKV_SIZES)
    w_log = -jax.nn.softplus(-(w0 + jnp.tanh(wd) @ w_up)) - 0.5
    decay = jnp.exp(-jnp.exp(w_log))
    a = jax.nn.sigmoid(a0 + ad @ a_up)
    kk = k * k_k
    k = k * (1.0 + (a - 1.0) * k_a)
    r, k, v, kk, a, decay = (u.reshape(B, T, H, N) for u in (r, k, v, kk, a, decay))
    kk = kk / jnp.maximum(jnp.sqrt(jnp.sum(kk * kk, axis=-1, keepdims=True)), 1e-12)
    y = rwkv7_scan(r, decay, k, v, kk, a)
    mean = jnp.mean(y, axis=-1, keepdims=True)
    var = jnp.mean(jnp.square(y - mean), axis=-1, keepdims=True)
    y = (y - mean) * lax.rsqrt(var + RWKV_GN_EPS) * ln_w.reshape(H, N) + ln_b.reshape(H, N)
    y = y + jnp.sum(r * k * r_k, axis=-1, keepdims=True) * v
    y = y.reshape(B, T, RWKV_WIDTH) * jax.nn.silu(gate)
    return y.astype(p.dtype)


def nsa_group(p, pos, cmp_pos, ck_w1, ck_w2, cv_w1, cv_w2, gate_b, out_g):
    B, T, _ = p.shape
    Hn, D = NSA_HEADS, HEAD_DIM
    q, gate, glog, kc, vc, ks, vs, kw, vw = split_cols(p, NSA_SIZES)
    q = q.reshape(B, T, Hn, D)
    q_rope = partial_rope(q, pos)
    ks = partial_rope(ks[:, :, None, :], pos)[:, :, 0]
    kw = partial_rope(kw[:, :, None, :], pos)[:, :, 0]
    scale = HEAD_DIM ** -0.5

    n_cmp = (T - CMP_BLOCK) // CMP_STRIDE + 1
    blk = jnp.arange(n_cmp)[:, None] * CMP_STRIDE + jnp.arange(CMP_BLOCK)[None, :]

    def compress(u, w1, w2):
        ub = (u[:, blk] + cmp_pos).reshape(B, n_cmp, CMP_BLOCK * D)
        return jax.nn.silu(ub @ w1) @ w2

    k_cmp = compress(kc, ck_w1, ck_w2)
    v_cmp = compress(vc, cv_w1, cv_w2)
    cmp_end = blk[:, -1]

    n_sel = T // SEL_BLOCK
    n_top = min(SEL_TOPK, n_sel)
    sel_start = jnp.arange(n_sel) * SEL_BLOCK
    overlap = jnp.clip(jnp.minimum(blk[:, -1:] + 1, sel_start[None, :] + SEL_BLOCK)
                       - jnp.maximum(blk[:, :1], sel_start[None, :]), 0, None)
    overlap = overlap.astype(jnp.float32) / CMP_BLOCK
    k_sel_blk = ks.reshape(B, n_sel, SEL_BLOCK, D)
    v_sel_blk = vs.reshape(B, n_sel, SEL_BLOCK, D)
    blk_id = jnp.arange(n_sel)

    kw_pad = jnp.pad(kw, ((0, 0), (WINDOW, 0), (0, 0)))
    vw_pad = jnp.pad(vw, ((0, 0), (WINDOW, 0), (0, 0)))

    def per_block(qb):
        t0 = qb * Q_BLOCK
        tq = t0 + jnp.arange(Q_BLOCK)
        qn = lax.dynamic_slice_in_dim(q, t0, Q_BLOCK, axis=1)
        qr = lax.dynamic_slice_in_dim(q_rope, t0, Q_BLOCK, axis=1)
        s_c = jnp.einsum('bqhd,bcd->bhqc', qn, k_cmp) * scale
        p_c = masked_softmax(s_c, cmp_end[None, :] <= tq[:, None])
        o_c = jnp.einsum('bhqc,bcd->bqhd', p_c.astype(v_cmp.dtype), v_cmp)
        imp = jnp.einsum('bhqc,cs->bqs', p_c, overlap)
        cur = tq // SEL_BLOCK
        valid = blk_id[None, :] <= cur[:, None]
        forced = (blk_id[None, :] == 0) | (blk_id[None, :] == cur[:, None]) | (blk_id[None, :] == cur[:, None] - 1)
        score = jnp.where(forced, FORCE_SCORE, jnp.where(valid, imp, -1.0))
        _, idx = lax.top_k(score, n_top)
        kg = jax.vmap(lambda kb, ib: kb[ib])(k_sel_blk, idx).reshape(B, Q_BLOCK, n_top * SEL_BLOCK, D)
        vg = jax.vmap(lambda vb, ib: vb[ib])(v_sel_blk, idx).reshape(B, Q_BLOCK, n_top * SEL_BLOCK, D)
        kpos = (idx[..., None] * SEL_BLOCK + jnp.arange(SEL_BLOCK)).reshape(B, Q_BLOCK, n_top * SEL_BLOCK)
        s_s = jnp.einsum('bqhd,bqkd->bhqk', qr, kg) * scale
        p_s = masked_softmax(s_s, (kpos <= tq[None, :, None])[:, None])
        o_s = jnp.einsum('bhqk,bqkd->bqhd', p_s.astype(vg.dtype), vg)
        kwb = lax.dynamic_slice_in_dim(kw_pad, t0, Q_BLOCK + WINDOW, axis=1)
        vwb = lax.dynamic_slice_in_dim(vw_pad, t0, Q_BLOCK + WINDOW, axis=1)
        wpos = t0 - WINDOW + jnp.arange(Q_BLOCK + WINDOW)
        diff = tq[:, None] - wpos[None, :]
        m_w = (diff >= 0) & (diff < WINDOW) & (wpos[None, :] >= 0)
        s_w = jnp.einsum('bqhd,bkd->bhqk', qr, kwb) * scale
        p_w = masked_softmax(s_w, m_w)
        o_w = jnp.einsum('bhqk,bkd->bqhd', p_w.astype(vwb.dtype), vwb)
        return o_c, o_s, o_w

    o_c, o_s, o_w = lax.map(per_block, jnp.arange(T // Q_BLOCK))
    o_c, o_s, o_w = (jnp.moveaxis(o, 0, 1).reshape(B, T, Hn, D) for o in (o_c, o_s, o_w))
    g = jax.nn.sigmoid(glog.reshape(B, T, Hn, N_BRANCH) + gate_b.reshape(Hn, N_BRANCH))
    o = g[..., 0:1] * o_c + g[..., 1:2] * o_s + g[..., 2:3] * o_w
    o = head_rms_norm(o, out_g.reshape(Hn, D))
    return o.reshape(B, T, NSA_WIDTH) * jax.nn.silu(gate)


def memory_group(p, mem_k, mem_v, out_g):
    B, T, _ = p.shape
    q, gate = split_cols(p, MEM_SIZES)
    q = q.reshape(B, T, MEM_HEADS, MEM_HEAD_DIM)
    s = jnp.einsum('bthd,bmhd->bhtm', q, mem_k) * (MEM_HEAD_DIM ** -0.5)
    prob = jax.nn.softmax(s.astype(jnp.float32), axis=-1)
    o = jnp.einsum('bhtm,bmhd->bthd', prob.astype(mem_v.dtype), mem_v)
    o = head_rms_norm(o, out_g.reshape(MEM_HEADS, MEM_HEAD_DIM))
    return o.reshape(B, T, MEM_WIDTH) * jax.nn.silu(gate)


def setup_inputs(seed: int = 0) -> dict:
    key = jax.random.key(seed)
    ks = jax.random.split(key, 32)
    L = DEPTH
    f32 = jnp.float32

    def nrm(k, shape, scale):
        return jax.random.normal(k, shape, f32) * scale

    def gain(k, shape):
        return 1.0 + 0.05 * jax.random.normal(k, shape, f32)

    cmp_in = CMP_BLOCK * HEAD_DIM
    return {
        'x': nrm(ks[0], (BATCH, SEQ, D_MODEL), 1.0),
        'mem': nrm(ks[1], (BATCH, MEM_LEN, D_MODEL), 1.0),
        'norm_in_g': gain(ks[2], (L, D_MODEL)),
        'w_in': nrm(ks[3], (L, D_MODEL, IN_COLS), D_MODEL ** -0.5),
        'rwkv_mu': jax.random.uniform(ks[4], (L, RWKV_COLS), f32, 0.2, 0.8),
        'rwkv_w0': jax.random.uniform(ks[5], (L, RWKV_WIDTH), f32, -5.0, -1.0),
        'rwkv_w_up': nrm(ks[6], (L, DECAY_LORA, RWKV_WIDTH), 0.1 * DECAY_LORA ** -0.5),
        'rwkv_a0': nrm(ks[7], (L, RWKV_WIDTH), 0.5),
        'rwkv_a_up': nrm(ks[8], (L, ICLR_LORA, RWKV_WIDTH), ICLR_LORA ** -0.5),
        'rwkv_k_k': 0.85 + 0.05 * jax.random.normal(ks[9], (L, RWKV_WIDTH), f32),
        'rwkv_k_a': 1.0 + 0.05 * jax.random.normal(ks[10], (L, RWKV_WIDTH), f32),
        'rwkv_r_k': nrm(ks[11], (L, RWKV_HEADS, HEAD_DIM), 0.1),
        'rwkv_ln_w': gain(ks[12], (L, RWKV_WIDTH)),
        'rwkv_ln_b': nrm(ks[13], (L, RWKV_WIDTH), 0.02),
        'nsa_cmp_pos': nrm(ks[14], (L, CMP_BLOCK, HEAD_DIM), 0.1),
        'nsa_cmp_k_w1': nrm(ks[15], (L, cmp_in, HEAD_DIM), cmp_in ** -0.5),
        'nsa_cmp_k_w2': nrm(ks[16], (L, HEAD_DIM, HEAD_DIM), HEAD_DIM ** -0.5),
        'nsa_cmp_v_w1': nrm(ks[17], (L, cmp_in, HEAD_DIM), cmp_in ** -0.5),
        'nsa_cmp_v_w2': nrm(ks[18], (L, HEAD_DIM, HEAD_DIM), HEAD_DIM ** -0.5),
        'nsa_gate_b': nrm(ks[19], (L, NSA_HEADS * N_BRANCH), 0.1),
        'nsa_out_g': gain(ks[20], (L, NSA_WIDTH)),
        'mem_norm_g': gain(ks[21], (L, D_MODEL)),
        'w_mem_kv': nrm(ks[22], (L, D_MODEL, 2 * MEM_WIDTH), D_MODEL ** -0.5),
        'mem_out_g': gain(ks[23], (L, MEM_WIDTH)),
        'w_out': nrm(ks[24], (L, MIX_WIDTH, D_MODEL), MIX_WIDTH ** -0.5),
        'norm_final_g': gain(ks[25], (D_MODEL,)),
    }


def reference(x, mem, norm_in_g, w_in, rwkv_mu, rwkv_w0, rwkv_w_up, rwkv_a0, rwkv_a_up,
              rwkv_k_k, rwkv_k_a, rwkv_r_k, rwkv_ln_w, rwkv_ln_b, nsa_cmp_pos, nsa_cmp_k_w1,
              nsa_cmp_k_w2, nsa_cmp_v_w1, nsa_cmp_v_w2, nsa_gate_b, nsa_out_g, mem_norm_g,
              w_mem_kv, mem_out_g, w_out, norm_final_g):
    B, T, _ = x.shape
    M = mem.shape[1]
    pos = jnp.arange(T)
    for l in range(DEPTH):
        h = rms_norm(x, norm_in_g[l])
        p = h @ w_in[l]
        p_rwkv, p_nsa, p_mem = jnp.split(p, [RWKV_COLS, RWKV_COLS + NSA_COLS], axis=-1)
        y_rwkv = rwkv7_group(p_rwkv, rwkv_mu[l], rwkv_w0[l], rwkv_w_up[l], rwkv_a0[l], rwkv_a_up[l],
                             rwkv_k_k[l], rwkv_k_a[l], rwkv_r_k[l], rwkv_ln_w[l], rwkv_ln_b[l])
        y_nsa = nsa_group(p_nsa, pos, nsa_cmp_pos[l], nsa_cmp_k_w1[l], nsa_cmp_k_w2[l],
                          nsa_cmp_v_w1[l], nsa_cmp_v_w2[l], nsa_gate_b[l], nsa_out_g[l])
        kv = rms_norm(mem, mem_norm_g[l]) @ w_mem_kv[l]
        mem_k, mem_v = jnp.split(kv.reshape(B, M, 2, MEM_HEADS, MEM_HEAD_DIM), 2, axis=2)
        y_mem = memory_group(p_mem, mem_k[:, :, 0], mem_v[:, :, 0], mem_out_g[l])
        y = jnp.concatenate([y_rwkv.astype(x.dtype), y_nsa.astype(x.dtype), y_mem.astype(x.dtype)], axis=-1)
        x = x + y @ w_out[l]
    return rms_norm(x, norm_final_g)
```

```python
import numpy as np
import ml_dtypes
from contextlib import ExitStack
import concourse.bass as bass
import concourse.mybir as mybir
from concourse.bass_utils import run_bass_kernel_spmd

F32 = mybir.dt.float32
BF16 = mybir.dt.bfloat16
ALU = mybir.AluOpType
AF = mybir.ActivationFunctionType
AX = mybir.AxisListType

ENGS = ("pe", "act", "dve", "pool", "sp")
N_DMA_SEMS = 24
SAME_ENG_SYNC = True
NEG = -30000.0

D = 1024
NCOL = 29 * 128 + 12
G = 512
PSUM_PREFIX = ("pj", "sc", "oa", "mi", "tp")


class Prog:
    def __init__(self, nc):
        self.nc = nc
        self.lists = {e: [] for e in ENGS}
        self.cnt = {e: 0 for e in ENGS}
        self.seen = {e: {} for e in ENGS}
        self.clock_at = {e: [None] for e in ENGS}
        self.buf = {}
        self.dma_val = [0] * N_DMA_SEMS
        self.dma_clock = [[] for _ in range(N_DMA_SEMS)]
        self.dma_rr = 0
        self.out_toks = []

    def _deps(self, eng, reads, writes):
        need = {}

        def add(tok):
            if tok is None:
                return
            src, val = tok
            if src == eng and (eng == "pe" or not SAME_ENG_SYNC):
                return
            if need.get(src, 0) < val:
                need[src] = val

        for k in reads:
            st = self.buf.get(k)
            if st:
                add(st[0])
                if k[:2] in PSUM_PREFIX:
                    for r in st[1]:
                        if r[0] != eng:
                            add(r)
        for k in writes:
            st = self.buf.get(k)
            if st:
                add(st[0])
                for r in st[1]:
                    add(r)
        seen = self.seen[eng]
        waits = []
        for src, val in need.items():
            if seen.get(src, 0) >= val:
                continue
            waits.append((src, val))
            if isinstance(src, str):
                snap = self.clock_at[src][val]
            else:
                snap = self.dma_clock[src[1]][val // 16 - 1]
            for s2, v2 in snap.items():
                if seen.get(s2, 0) < v2:
                    seen[s2] = v2
            seen[src] = val
        return waits

    def _mark(self, tok, reads, writes):
        for k in reads:
            st = self.buf.setdefault(k, [None, []])
            st[1].append(tok)
            if len(st[1]) > 64:
                st[1] = st[1][-64:]
        for k in writes:
            self.buf[k] = [tok, []]

    def op(self, eng, fn, reads=(), writes=(), self_wait=None):
        waits = self._deps(eng, reads, writes)
        if self_wait is not None and self.seen[eng].get(eng, 0) < self_wait:
            waits.append((eng, self_wait))
            self.seen[eng][eng] = self_wait
        self.cnt[eng] += 1
        c = self.cnt[eng]
        snap = dict(self.seen[eng])
        snap[eng] = c
        self.clock_at[eng].append(snap)
        self.lists[eng].append(("op", fn, waits, None))
        self._mark((eng, c), reads, writes)

    def dma(self, eng, fn, reads=(), writes=(), is_out=False):
        s = self.dma_rr
        self.dma_rr = (self.dma_rr + 1) % N_DMA_SEMS
        waits = self._deps(eng, reads, writes)
        src = ("dma", s)
        prev = self.dma_val[s]
        if prev and self.seen[eng].get(src, 0) < prev:
            waits.append((src, prev))
            self.seen[eng][src] = prev
        val = prev + 16
        self.dma_val[s] = val
        self.dma_clock[s].append(dict(self.seen[eng]))
        self.lists[eng].append(("dma", fn, waits, (s, val)))
        self._mark((src, val), reads, writes)
        if is_out:
            self.out_toks.append((src, val))
        return (src, val)

    def finish(self, eng="sp"):
        waits = []
        for s in range(N_DMA_SEMS):
            if self.dma_val[s]:
                waits.append((("dma", s), self.dma_val[s]))
        self.lists[eng].append(("wait", None, waits, None))

    def emit(self, block, sems, dsems):
        engobj = {"pe": "tensor", "act": "scalar", "dve": "vector", "pool": "gpsimd", "sp": "sync"}

        def semof(src):
            return sems[src] if isinstance(src, str) else dsems[src[1]]

        def make(ename):
            lst = self.lists[ename]

            def body(e):
                for kind, fn, waits, extra in lst:
                    for src, val in waits:
                        e.wait_ge(semof(src), val)
                    if kind == "op":
                        fn(e).then_inc(sems[ename], 1)
                    elif kind == "dma":
                        fn(e).then_inc(dsems[extra[0]], 16)

            return body

        for ename in ENGS:
            if self.lists[ename]:
                getattr(block, engobj[ename])(make(ename))


RW, NSAB, MEMB = 0, 2176, 3084


def _colidx():
    idx = []
    for hp in range(4):
        for base in (0, 512, 1024, 1536):
            idx += list(range(base + 128 * hp, base + 128 * hp + 128))
    idx += list(range(2048, 2176))
    q0, g0 = NSAB, NSAB + 256
    idx += list(range(q0, q0 + 256))
    idx += list(range(g0, g0 + 256))
    kc, vc, ks, vs, kw, vw = (NSAB + 524 + 64 * i for i in range(6))
    idx += list(range(kc, kc + 64)) + list(range(vc, vc + 64))
    idx += list(range(ks, ks + 64)) * 2
    idx += list(range(kw, kw + 64)) * 2
    idx += list(range(vs, vs + 64)) + list(range(vw, vw + 64))
    idx += list(range(MEMB, MEMB + 512))
    idx += list(range(NSAB + 512, NSAB + 524))
    assert len(idx) == NCOL
    return np.array(idx)


def _bf(a):
    return np.ascontiguousarray(a.astype(ml_dtypes.bfloat16))


def host_consts(T):
    c = {}
    c["ident"] = _bf(np.eye(128, dtype=np.float32))
    half = 8
    inv = (np.float32(500000.0) ** (-np.arange(half, dtype=np.float32) / np.float32(half))).astype(np.float32)
    ang = np.arange(T, dtype=np.float32)[None, :] * inv[:, None]
    C = np.ones((64, T), np.float32)
    S = np.zeros((64, T), np.float32)
    C[0:8] = np.cos(ang); C[8:16] = np.cos(ang)
    S[0:8] = -np.sin(ang); S[8:16] = np.sin(ang)
    c["ropeC"] = np.ascontiguousarray(np.concatenate([C, C], 0))
    c["ropeS"] = np.ascontiguousarray(np.concatenate([S, S], 0))
    pm = np.zeros((128, 128), np.float32)
    for hb in (0, 64):
        for d in range(8):
            pm[hb + d + 8, hb + d] = 1.0
            pm[hb + d, hb + d + 8] = 1.0
    c["pm"] = _bf(pm)
    ovl = np.zeros((512, 129), np.float32)
    for i in range(511):
        for sblk in range(128):
            o = min(16 * i + 32, 64 * sblk + 64) - max(16 * i, 64 * sblk)
            if o > 0:
                ovl[i, sblk] = o / 32.0
    ovl[:, 128] = 1.0
    c["ovl"] = _bf(ovl.reshape(4, 128, 129).transpose(1, 0, 2))
    addw = np.zeros((128, 256), np.float32)
    for pp in range(128):
        cur = 1 if pp >= 64 else 0
        for u in range(256):
            sp = u - 126
            valid = sp <= cur
            forced = sp in (cur, cur - 1)
            addw[pp, u] = (0.0 if valid else -1.0) + (1e4 if forced else 0.0)
    c["addw"] = addw
    fsel = np.zeros((64, 16, 128), np.float32)
    for a2 in range(16):
        for key in range(128):
            r = 2 * a2 + (1 if key >= 64 else 0)
            fsel[r, a2, key] = 1.0
            fsel[32 + r, a2, key] = 1.0
    c["fsel"] = _bf(fsel)
    pp = np.arange(128)[:, None]
    ff = np.arange(128)[None, :]
    for nm, m in (("maskL2", pp > ff), ("maskU2", pp < ff), ("maskUI2", pp <= ff), ("ident2", pp == ff)):
        m = m.astype(np.float32)
        c[nm] = _bf(np.stack([m, m], 1))
    blk = (pp // 64 == ff // 64).astype(np.float32)
    c["blk1"] = _bf(blk)
    c["blkavg"] = _bf(blk / 64.0)
    return c


PK = {}
_o = 0
for _n, _w in (("gin", 8), ("gmem", 8), ("mu", 17), ("memg", 2), ("nsag", 2), ("gateb", 1), ("posT", 32), ("w0", 4), ("a0", 4), ("kk", 4), ("ka", 4), ("rk", 4), ("lnw", 4), ("lnb", 4)):
    PK[_n] = (_o, _w)
    _o += _w
NPK = _o


def host_params(inp):
    pk = np.zeros((128, NPK), np.float32)

    def put(name, arr):
        o, w = PK[name]
        pk[:, o:o + w] = arr

    put("gin", inp["norm_in_g"][0].reshape(8, 128).T)
    put("gmem", inp["mem_norm_g"][0].reshape(8, 128).T)
    ci = _colidx()
    put("mu", inp["rwkv_mu"][0][ci[:17 * 128]].reshape(17, 128).T)
    put("memg", inp["mem_out_g"][0].reshape(2, 128).T)
    put("nsag", inp["nsa_out_g"][0].reshape(2, 128).T)
    gb = np.zeros((128, 1), np.float32)
    gb[0:12, 0] = inp["nsa_gate_b"][0]
    put("gateb", gb)
    pt = inp["nsa_cmp_pos"][0].T
    put("posT", np.concatenate([pt, pt], 0))
    for nm, key in (("w0", "rwkv_w0"), ("a0", "rwkv_a0"), ("kk", "rwkv_k_k"), ("ka", "rwkv_k_a"), ("rk", "rwkv_r_k"), ("lnw", "rwkv_ln_w"), ("lnb", "rwkv_ln_b")):
        put(nm, inp[key][0].reshape(4, 128).T)
    return pk


def build_nc(T, dbg=None):
    NG = T // G
    NT = T // 128
    nc = bass.Bass("TRN2", target_bir_lowering=False)
    dram = {}

    def din(name, shape, dt=F32):
        dram[name] = nc.dram_tensor(name, list(shape), dt, kind="ExternalInput").ap()
        return dram[name]

    x_d = din("x", [T, D])
    mem_d = din("mem", [256, D])
    wcat_d = din("wcat", [8, 128, NCOL])
    wout_d = din("wout", [8, 128, D])
    wmkv_d = din("wmkv", [8, 128, 512])
    pk_d = din("pk", [128, NPK])
    gfin_d = din("gfin", [1, D])
    ident_d = din("ident", [128, 128], BF16)
    ropeC_d = din("ropeC", [128, T])
    ropeS_d = din("ropeS", [128, T])
    pm_d = din("pm", [128, 128], BF16)
    ovl_d = din("ovl", [128, 4, 129], BF16)
    addw_d = din("addw", [128, 256])
    fsel_d = din("fsel", [64, 16, 128], BF16)
    w1_d = din("w1", [128, 2048])
    w2_d = din("w2", [128, 192])
    lora_d = din("lora", [128, 512])
    w0row_d = din("w0row", [1, 512])
    maskL2_d = din("maskL2", [128, 2, 128], BF16)
    maskU2_d = din("maskU2", [128, 2, 128], BF16)
    maskUI2_d = din("maskUI2", [128, 2, 128], BF16)
    ident2_d = din("ident2", [128, 2, 128], BF16)
    blk1_d = din("blk1", [128, 128], BF16)
    blkavg_d = din("blkavg", [128, 128], BF16)
    out_d = nc.dram_tensor("out", [T, D], F32, kind="ExternalOutput").ap()
    dbg_d = {}
    if dbg:
        for n, (shape, dt) in dbg.items():
            dbg_d[n] = nc.dram_tensor("dbg_" + n, list(shape), dt, kind="ExternalOutput").ap()

    with ExitStack() as es:
        def sb(name, shape, dt=F32):
            return es.enter_context(nc.sbuf_tensor("sb_" + name, list(shape), dt))

        def ps(name, shape, dt=F32):
            return es.enter_context(nc.psum_tensor("ps_" + name, list(shape), dt))

        wb = sb("wb", [128, 8, NCOL], BF16)
        woutb = sb("woutb", [128, 8, D], BF16)
        wmkvb = sb("wmkvb", [128, 8, 512], BF16)
        pk = sb("pk", [128, NPK])
        gfin = sb("gfin", [128, D])
        ident = sb("ident", [128, 128], BF16)
        xt = [sb("xt%d" % i, [128, D]) for i in range(2)]
        hb0 = sb("hb0", [128, D], BF16)
        hb = [hb0, hb0]
        hT = sb("hT", [128, 8, G], BF16)
        st = sb("st", [128, 16])
        yT = sb("yT", [128, 8, G], BF16)
        memkT = sb("memkT", [128, 2, 256], BF16)
        memvA = sb("memvA", [128, 2, 4, 65], BF16)
        qm = sb("qm", [128, 2, G], BF16)
        gm = sb("gm", [128, 2, G], BF16)
        pT = [sb("pT%d" % i, [128, G], BF16) for i in range(2)]
        oaug = sb("oaug", [128, G], BF16)
        osq = sb("osq", [65, G], BF16)
        cvec = sb("cvec", [65, 128], BF16)
        pm = sb("pm", [128, 128], BF16)
        ovl = sb("ovl", [128, 4, 129], BF16)
        addw = sb("addw", [128, 256])
        fsel = sb("fsel", [64, 16, 128], BF16)
        ones12 = sb("ones12", [12, 128], BF16)
        gsr = sb("gsr", [12, G], BF16)
        e64 = sb("e64", [65, 128], BF16)
        w1b = sb("w1b", [128, 32, 64], BF16)
        w2b = sb("w2b", [64, 192], BF16)
        posTb = sb("posTb", [128, 32], BF16)
        ccst = sb("ccst", [64, 2])
        ropeC = sb("ropeC", [128, G])
        ropeS = sb("ropeS", [128, G])
        qn = sb("qn", [128, 2, G], BF16)
        qr = sb("qr", [128, 2, G], BF16)
        gn = sb("gn", [128, 2, G], BF16)
        kcvc = sb("kcvc", [128, 16 + G], BF16)
        kraw = sb("kraw", [128, G], BF16)
        ksT = sb("ksT", [128, T], BF16)
        kwT = sb("kwT", [128, 1024], BF16)
        vsA = sb("vsA", [128, NT, 65], BF16)
        vwA = sb("vwA", [128, 8, 65], BF16)
        kcmpT = sb("kcmpT", [128, 512], BF16)
        vcmpA = sb("vcmpA", [128, 4, 65], BF16)
        vstg = sb("vstg", [32, 65], BF16)
        h1k = sb("h1k", [64, 32], BF16)
        h1v = sb("h1v", [64, 32], BF16)
        gsig = sb("gsig", [12, G], BF16)
        impS = sb("impS", [128, 4, 128])
        score = sb("score", [128, 128])
        stmp = sb("stmp", [128, 128])
        m8 = sb("m8", [128, 16])
        nbq = sb("nbq", [128, 128], BF16)
        nb = sb("nb", [128, G], BF16)
        nb2 = sb("nb2", [64, G], BF16)
        oab = sb("oab", [65, G], BF16)
        coef = sb("coef", [128, G])
        scl = coef
        rtmp = coef
        acc = sb("acc", [128, 2, G])
        praw = sb("praw", [128, G + 1])
        prevcol = sb("prevcol", [128, 17])
        loraup = sb("loraup", [128, 512], BF16)
        w0hi = sb("w0hi", [1, 512], BF16)
        w0lo = sb("w0lo", [1, 512], BF16)
        onesrow = sb("onesrow", [1, 128], BF16)
        ka1m = sb("ka1m", [128, 4])
        maskL2 = sb("maskL2", [128, 2, 128], BF16)
        maskU2 = sb("maskU2", [128, 2, 128], BF16)
        maskUI2 = sb("maskUI2", [128, 2, 128], BF16)
        ident2 = sb("ident2", [128, 2, 128], BF16)
        blk1 = sb("blk1", [128, 128], BF16)
        blkavg = sb("blkavg", [128, 128], BF16)
        sgtok = sb("sgtok", [128, 4, 128], BF16)
        Pm_ = sb("Pm", [128, 2, 128], BF16)
        PTm = sb("PTm", [128, 2, 128], BF16)
        XTm = sb("XTm", [128, 2, 128], BF16)
        AakT = sb("AakT", [128, 2, 128], BF16)
        ArbT = sb("ArbT", [128, 2, 128], BF16)
        ArkT = sb("ArkT", [128, 2, 128], BF16)
        AkV = sb("AkV", [128, 2, 64], BF16)
        Ahat = sb("Ahat", [128, 2, 64], BF16)
        Uhat = sb("Uhat", [128, 2, 64], BF16)
        RhT = sb("RhT", [64, 2, 128], BF16)
        Msb = sb("Msb", [64, 2, 64], BF16)
        STb = sb("STb", [64, 8, 64], BF16)
        ncL = sb("ncL", [128, 4])
        WL = sb("WL", [128, 4])
        WL0 = sb("WL0", [64, 8])
        pj = [ps("pj%d" % i, [128, 512]) for i in range(2)]
        sc = [ps("sc%d" % i, [128, 512]) for i in range(2)]
        oa = [ps("oa%d" % i, [128, 512]) for i in range(2)]
        mi = ps("mi", [128, 512])
        tp = ps("tp", [128, 1024], BF16)

        pTc = wmkvb[:, 0:4, :]
        sems = {e: es.enter_context(nc.semaphore("s_" + e)) for e in ENGS}
        dsems = [es.enter_context(nc.semaphore("d%d" % i)) for i in range(N_DMA_SEMS)]
        block = es.enter_context(nc.Block())
        P = Prog(nc)
        rr = {"ev": 0}

        def ev_eng():
            rr["ev"] ^= 1
            return "act" if rr["ev"] else "dve"

        def copy(eng, out, in_, reads, writes):
            if eng == "act":
                P.op("act", lambda e: e.copy(out=out, in_=in_), reads, writes)
            else:
                P.op(eng, lambda e: e.tensor_copy(out=out, in_=in_), reads, writes)

        pe_last = {}

        def mm(out, lhsT, rhs, start, stop, reads, writes):
            base, rows = lhsT.base_partition(), lhsT.shape[0]
            sw = None
            for k in writes:
                last = pe_last.get(k)
                if last is not None:
                    c0, b0, r0 = last
                    if b0 + r0 <= base or base + rows <= b0:
                        sw = max(sw or 0, c0)
            P.op("pe", lambda e: e.matmul(out, lhsT, rhs, start=start, stop=stop), reads, writes, self_wait=sw)
            for k in writes:
                pe_last[k] = (P.cnt["pe"], base, rows)

        def dbg_out(name, ap, key):
            if dbg and name in dbg_d:
                P.dma("sp", lambda e: e.dma_start(out=dbg_d[name], in_=ap), reads=[key])

        import os as _os
        KSTOP = int(_os.environ.get("KSTOP", "99"))

        class _Stop(Exception):
            pass

        def stop_at(n):
            if KSTOP == n:
                raise _Stop()

        try:
            P.dma("sp", lambda e: e.dma_start(out=pk[:], in_=pk_d[:, :]), writes=["pk"])
            P.dma("sp", lambda e: e.dma_start(out=ident[:], in_=ident_d[:, :]), writes=["ident"])
            P.dma("sp", lambda e: e.dma_start(out=gfin[:], in_=gfin_d[0:1, :].broadcast_to([128, D])), writes=["gfin"])

            def pkc(name, j=0, rows=128):
                o, w = PK[name]
                return pk[0:rows, o + j:o + j + 1]

            def load_cast(dst3, src_d, ncols, gname, key):
                i = 0
                for k in range(8):
                    for c0 in range(0, ncols, 1024):
                        cw = min(1024, ncols - c0)
                        stg = xt[i % 2]
                        skey = "xt%d" % (i % 2)
                        P.dma("sp" if i % 2 == 0 else "pool",
                              lambda e, stg=stg, k=k, c0=c0, cw=cw: e.dma_start(out=stg[:, 0:cw], in_=src_d[k, :, c0:c0 + cw]),
                              writes=[skey])
                        eng = ev_eng()
                        o = dst3[:, k, c0:c0 + cw]
                        if gname is None:
                            copy(eng, o, stg[:, 0:cw], [skey], [key])
                        elif eng == "act":
                            P.op("act", lambda e, o=o, stg=stg, cw=cw, k=k: e.activation(out=o, in_=stg[:, 0:cw], func=AF.Copy, scale=pkc(gname, k)),
                                 [skey, "pk"], [key])
                        else:
                            P.op("dve", lambda e, o=o, stg=stg, cw=cw, k=k: e.tensor_scalar(out=o, in0=stg[:, 0:cw], scalar1=pkc(gname, k), scalar2=None, op0=ALU.mult),
                                 [skey, "pk"], [key])
                        i += 1

            stop_at(1)
            load_cast(wmkvb, wmkv_d, 512, "gmem", "wmkvb")
            stop_at(2)
            load_cast(wb, wcat_d, NCOL, "gin", "wb")
            load_cast(woutb, wout_d, D, None, "woutb")

            P.op("pool", lambda e: e.memset(cvec[0:64, :], 1.0 / 64), writes=["cvec"])
            P.op("pool", lambda e: e.memset(cvec[64:65, :], 1e-6), writes=["cvec"])

            def norm_transpose(src_tile, skey, dstT, dkey, col0, slot):
                h = hb[slot]
                hk = "hb0"
                ssq = st[:, slot:slot + 1]
                P.op("act", lambda e: e.activation(out=h[:], in_=src_tile[:], func=AF.Square, accum_out=ssq), [skey], [hk, "st%d" % slot])
                P.op("dve", lambda e: e.tensor_scalar(out=ssq, in0=ssq, scalar1=1.0 / D, scalar2=1e-6, op0=ALU.mult, op1=ALU.add), ["st%d" % slot], ["st%d" % slot])
                P.op("dve", lambda e: e.reciprocal(out=ssq, in_=ssq), ["st%d" % slot], ["st%d" % slot])
                P.op("act", lambda e: e.activation(out=ssq, in_=ssq, func=AF.Sqrt), ["st%d" % slot], ["st%d" % slot])
                P.op("dve", lambda e: e.tensor_scalar(out=h[:], in0=src_tile[:], scalar1=ssq, scalar2=None, op0=ALU.mult), [skey, "st%d" % slot], [hk])
                for k in range(8):
                    P.op("pe", lambda e, k=k: e.transpose(out=tp[:, k * 128:(k + 1) * 128], in_=h[:, k * 128:(k + 1) * 128], identity=ident[:]), [hk, "ident"], ["tp"])
                copy(ev_eng(), dstT[:, :, col0:col0 + 128], tp[:].rearrange("p (k t) -> p k t", k=8), ["tp"], [dkey])


            for nm, dst, src in (("pm", pm, pm_d), ("ovl", ovl, ovl_d), ("addw", addw, addw_d),
                                 ("fsel", fsel, fsel_d)):
                P.dma("sp", lambda e, dst=dst, src=src: e.dma_start(out=dst[:], in_=src), writes=[nm])
            P.op("pool", lambda e: e.memset(ones12[:], 1.0), writes=["ones12"])
            P.op("pool", lambda e: e.memset(e64[:], 0.0), writes=["e64"])
            P.op("pool", lambda e: e.memset(e64[64:65, :], 1.0), writes=["e64"])
            P.op("pool", lambda e: e.memset(kcmpT[:], 0.0), writes=["kcmpT"])
            P.op("pool", lambda e: e.memset(vcmpA[:], 0.0), writes=["vcmpA"])
            P.op("pool", lambda e: e.memset(vsA[:], 1.0), writes=["vsA"])
            P.op("pool", lambda e: e.memset(vwA[:], 1.0), writes=["vwA"])
            P.op("pool", lambda e: e.memset(vstg[:], 1.0), writes=["vstg"])
            P.op("pool", lambda e: e.memset(kcvc[:], 0.0), writes=["kcvc"])
            for hlf in range(2):
                P.dma("sp", lambda e, hlf=hlf: e.dma_start(out=xt[hlf][:], in_=w1_d[:, hlf * 1024:(hlf + 1) * 1024]), writes=["xt%d" % hlf])
                copy(ev_eng(), w1b[:, hlf * 16:(hlf + 1) * 16, :], xt[hlf][:].rearrange("p (l e) -> p l e", e=64), ["xt%d" % hlf], ["w1b"])
            P.dma("sp", lambda e: e.dma_start(out=xt[0][:, 0:192], in_=w2_d[:, :]), writes=["xt0"])
            copy("dve", w2b[:], xt[0][0:64, 0:192], ["xt0"], ["w2b"])
            o_, w_ = PK["posT"]
            copy("dve", posTb[:], pk[:, o_:o_ + 32], ["pk"], ["posTb"])
            for kv in range(2):
                b0 = kv * 64
                for l in range(32):
                    mm(mi[0:64, kv:kv + 1], w1b[b0:b0 + 64, l, :], posTb[b0:b0 + 64, l:l + 1], l == 0, l == 31, ["w1b", "posTb"], ["mi"])
                copy("dve", ccst[:, kv:kv + 1], mi[0:64, kv:kv + 1], ["mi"], ["ccst"])

            for nm, dst, src in (("maskL2", maskL2, maskL2_d), ("maskU2", maskU2, maskU2_d), ("maskUI2", maskUI2, maskUI2_d),
                                 ("ident2", ident2, ident2_d), ("blk1", blk1, blk1_d), ("blkavg", blkavg, blkavg_d)):
                P.dma("sp", lambda e, dst=dst, src=src: e.dma_start(out=dst[:], in_=src), writes=[nm])
            P.dma("sp", lambda e: e.dma_start(out=xt[1][:, 0:512], in_=lora_d[:, :]), writes=["xt1"])
            copy("dve", loraup[:], xt[1][:, 0:512], ["xt1"], ["loraup"])
            P.dma("sp", lambda e: e.dma_start(out=praw[0:1, 0:512], in_=w0row_d[:, :]), writes=["praw"])
            copy("dve", w0hi[:], praw[0:1, 0:512], ["praw"], ["w0hi"])
            P.op("dve", lambda e: e.tensor_tensor(out=praw[0:1, 0:512], in0=praw[0:1, 0:512], in1=w0hi[:], op=ALU.subtract), ["praw", "w0hi"], ["praw"])
            copy("dve", w0lo[:], praw[0:1, 0:512], ["praw"], ["w0lo"])
            P.op("pool", lambda e: e.memset(onesrow[:], 1.0), writes=["onesrow"])
            P.op("pool", lambda e: e.memset(prevcol[:], 0.0), writes=["prevcol"])
            P.op("pool", lambda e: e.memset(STb[:], 0.0), writes=["STb"])
            o_, w_ = PK["ka"]
            P.op("dve", lambda e, o_=o_: e.tensor_scalar(out=ka1m[:], in0=pk[:, o_:o_ + 4], scalar1=-1.0, scalar2=1.0, op0=ALU.mult, op1=ALU.add), ["pk"], ["ka1m"])
            tokall = wmkvb[:, 4:8, :].rearrange("p c (w f) -> p c w f", w=4)
            stop_at(3)
            for mt in range(2):
                P.dma("sp", lambda e, mt=mt: e.dma_start(out=xt[mt][:], in_=mem_d[mt * 128:(mt + 1) * 128, :]), writes=["xt%d" % mt])
                norm_transpose(xt[mt], "xt%d" % mt, hT, "hT", mt * 128, mt)
            stop_at(4)
            for pr in range(2):
                for k in range(8):
                    mm(pj[0][:, 0:256], wmkvb[:, k, pr * 128:(pr + 1) * 128], hT[:, k, 0:256], k == 0, k == 7, ["wmkvb", "hT"], ["pj0"])
                copy(ev_eng(), memkT[:, pr, :], pj[0][:, 0:256], ["pj0"], ["memkT"])
            P.op("pool", lambda e: e.memset(memvA[:], 1.0), writes=["memvA"])
            for mt in range(2):
                for k in range(8):
                    mm(pj[1][:, 0:256], hT[:, k, mt * 128:(mt + 1) * 128], wmkvb[:, k, 256:512], k == 0, k == 7, ["wmkvb", "hT"], ["pj1"])
                copy(ev_eng(), memvA[:, mt, :, 0:64], pj[1][:, 0:256].rearrange("p (h d) -> p h d", h=4), ["pj1"], ["memvA"])

            def finalize_head(o_ps, okey, use_den, hb_, gcol_ap, gate_ap, gate_key, dst_ap, dst_key, from_sbuf=False):
                sl = slice(hb_, hb_ + 64)
                if from_sbuf:
                    copy("dve", oaug[sl, :], o_ps, [okey], ["oaug"])
                    P.op("act", lambda e: e.activation(out=osq[0:64, :], in_=o_ps, func=AF.Square), [okey], ["osq"])
                    P.op("pool", lambda e: e.memset(osq[64:65, :], 1.0), [], ["osq"])
                elif use_den:
                    copy("dve", oaug[sl, :], o_ps[0:64, :], [okey], ["oaug"])
                    P.op("act", lambda e: e.activation(out=osq[:], in_=o_ps[0:65, :], func=AF.Square), [okey], ["osq"])
                else:
                    copy("dve", oaug[sl, :], o_ps[0:64, :], [okey], ["oaug"])
                    P.op("act", lambda e: e.activation(out=osq[0:64, :], in_=o_ps[0:64, :], func=AF.Square), [okey], ["osq"])
                    P.op("pool", lambda e: e.memset(osq[64:65, :], 1.0), [], ["osq"])
                mm(mi[:], cvec[:], osq[:], True, True, ["cvec", "osq"], ["mi"])
                P.op("dve", lambda e: e.reciprocal(out=scl[sl, :], in_=mi[sl, :]), ["mi"], ["coef"])
                P.op("act", lambda e: e.activation(out=scl[sl, :], in_=scl[sl, :], func=AF.Sqrt), ["coef"], ["coef"])
                P.op("dve", lambda e: e.scalar_tensor_tensor(out=scl[sl, :], in0=scl[sl, :], scalar=gcol_ap, in1=oaug[sl, :], op0=ALU.mult, op1=ALU.mult), ["coef", "oaug", "pk"], ["coef"])
                P.op("dve", lambda e: e.tensor_tensor(out=dst_ap, in0=scl[sl, :], in1=gate_ap, op=ALU.mult), ["coef", gate_key], [dst_key])

            P.op("pool", lambda e: e.memset(wmkvb[:, 4:8, :], 0.0), ["wmkvb"], ["wmkvb", "tokall"])
            stop_at(5)
            for g in range(NG):
                t0 = g * G
                for i in range(4):
                    P.dma("sp", lambda e, i=i, t0=t0: e.dma_start(out=xt[i % 2][:], in_=x_d[t0 + i * 128:t0 + (i + 1) * 128, :]), writes=["xt%d" % (i % 2)])
                    norm_transpose(xt[i % 2], "xt%d" % (i % 2), hT, "hT", i * 128, i % 2)

                def project(chunk, width=128):
                    pb = pj[chunk % 2]
                    for k in range(8):
                        mm(pb[0:width, :], wb[:, k, chunk * 128:chunk * 128 + width], hT[:, k, :], k == 0, k == 7, ["wb", "hT"], ["pj%d" % (chunk % 2)])
                    return pb, "pj%d" % (chunk % 2)


                C0 = float(np.exp(-0.5))

                def tt(eng, out, in0, in1, op, reads, writes):
                    P.op(eng, lambda e: e.tensor_tensor(out=out, in0=in0, in1=in1, op=op), reads, writes)

                def ts(eng, out, in0, s1, s2, op0, op1, reads, writes):
                    if s2 is None:
                        P.op(eng, lambda e: e.tensor_scalar(out=out, in0=in0, scalar1=s1, scalar2=None, op0=op0), reads, writes)
                    else:
                        P.op(eng, lambda e: e.tensor_scalar(out=out, in0=in0, scalar1=s1, scalar2=s2, op0=op0, op1=op1), reads, writes)

                def stt(eng, out, in0, scalar, in1, op0, op1, reads, writes):
                    P.op(eng, lambda e: e.scalar_tensor_tensor(out=out, in0=in0, scalar=scalar, in1=in1, op0=op0, op1=op1), reads, writes)

                def actf(out, in_, func, reads, writes, bias=None, scale=None):
                    kw = {}
                    if bias is not None:
                        kw["bias"] = bias
                    if scale is not None:
                        kw["scale"] = scale
                    P.op("act", lambda e: e.activation(out=out, in_=in_, func=func, **kw), reads, writes)

                def pcol(name, j):
                    o, w = PK[name]
                    return pk[:, o + j:o + j + 1]

                def lerp_chunk(cidx, dst_ap, dkey):
                    pb, pkey = project(cidx)
                    copy("act", praw[:, 1:G + 1], pb[:], [pkey], ["praw"])
                    copy("dve", praw[:, 0:1], prevcol[:, cidx:cidx + 1], ["prevcol"], ["praw"])
                    copy("dve", prevcol[:, cidx:cidx + 1], praw[:, G:G + 1], ["praw"], ["prevcol"])
                    tt("dve", coef[:], praw[:, 0:G], praw[:, 1:G + 1], ALU.subtract, ["praw"], ["coef"])
                    stt("dve", dst_ap, coef[:], pcol("mu", cidx), praw[:, 1:G + 1], ALU.mult, ALU.add, ["coef", "praw", "pk"], [dkey])

                rT, kmT = qn[:, 0, :], qn[:, 1, :]
                vT, gT = qr[:, 0, :], qr[:, 1, :]
                bT, At = gn[:, 0, :], gn[:, 1, :]
                Rt, Bt = qm[:, 0, :], qm[:, 1, :]
                Kt, Bb = gm[:, 0, :], gm[:, 1, :]
                Kb, kkn = pT[0][:], pT[1][:]
                aT, lin = kraw[:], nb[:]
                kf, yraw = acc[:, 0, :], acc[:, 1, :]
                Etmp, Esq = hb0[:, 0:G], hb0[:, G:2 * G]

                lerp_chunk(16, kf, "acc")
                actf(lin[0:64, :], kf[0:64, :], AF.Tanh, ["acc"], ["nb"])
                copy("dve", lin[64:128, :], kf[64:128, :], ["acc"], ["nb"])

                stop_at(40)
                for hp in range(4):
                    lerp_chunk(4 * hp + 0, rT, "qn")
                    lerp_chunk(4 * hp + 1, kf, "acc")
                    lerp_chunk(4 * hp + 2, vT, "qr")
                    lerp_chunk(4 * hp + 3, gT, "qr")
                    actf(gT, gT, AF.Silu, ["qr"], ["qr"])
                    hc = slice(hp * 128, (hp + 1) * 128)
                    stop_at(41)
                    for c in range(4):
                        reg = mi[:, c * 128:(c + 1) * 128]
                        mm(reg, lin[0:64, c * 128:(c + 1) * 128], loraup[0:64, hc], True, False, ["nb", "loraup"], ["mi"])
                        mm(reg, onesrow[:], w0hi[0:1, hc], False, False, ["onesrow", "w0hi"], ["mi"])
                        mm(reg, onesrow[:], w0lo[0:1, hc], False, True, ["onesrow", "w0lo"], ["mi"])
                    actf(sgtok[:].rearrange("p c f -> p (c f)"), mi[:], AF.Sigmoid, ["mi"], ["sgtok"])
                    for c in range(4):
                        mm(sc[0][:, c * 128:(c + 1) * 128], sgtok[:, c, :], maskUI2[:, 0, :], True, True, ["sgtok", "maskUI2"], ["sc0"])
                    for c in range(4):
                        mm(sc[1][:, c * 128:(c + 1) * 128], sgtok[:, c, :], maskU2[:, 0, :], True, True, ["sgtok", "maskU2"], ["sc1"])
                    stop_at(42)
                    mm(oa[0][:], loraup[64:128, hc], lin[64:128, :], True, True, ["nb", "loraup"], ["oa0"])
                    actf(aT, oa[0][:], AF.Sigmoid, ["oa0", "pk"], ["kraw"], bias=pcol("a0", hp))
                    ts("dve", coef[:], kf, pcol("kk", hp), None, ALU.mult, None, ["acc", "pk"], ["coef"])
                    actf(Esq, coef[:], AF.Square, ["coef"], ["hb0"])
                    mm(oa[1][:], blk1[:], Esq, True, True, ["blk1", "hb0"], ["oa1"])
                    ts("dve", praw[:, 1:G + 1], oa[1][:], 1e-24, None, ALU.max, None, ["oa1"], ["praw"])
                    P.op("dve", lambda e: e.reciprocal(out=praw[:, 1:G + 1], in_=praw[:, 1:G + 1]), ["praw"], ["praw"])
                    actf(praw[:, 1:G + 1], praw[:, 1:G + 1], AF.Sqrt, ["praw"], ["praw"])
                    tt("dve", kkn, coef[:], praw[:, 1:G + 1], ALU.mult, ["coef", "praw"], ["pT1"])
                    ts("dve", coef[:], aT, pcol("ka", hp), ka1m[:, hp:hp + 1], ALU.mult, ALU.add, ["kraw", "pk", "ka1m"], ["coef"])
                    tt("dve", kmT, kf, coef[:], ALU.mult, ["acc", "coef"], ["qn"])
                    tt("pool", bT, kkn, aT, ALU.mult, ["pT1", "kraw"], ["gn"])
                    if g == 0 and hp == 0:
                        dbg_out("rT", rT, "qn"); dbg_out("kmT", kmT, "qn"); dbg_out("vT", vT, "qr"); dbg_out("aT", aT, "kraw"); dbg_out("kkn", kkn, "pT1"); dbg_out("bT", bT, "gn")
                        copy("dve", coef[:], sc[0][:], ["sc0"], ["coef"]); dbg_out("cumI", coef[:], "coef")
                        copy("dve", coef[:], sc[1][:], ["sc1"], ["coef"]); dbg_out("cumE", coef[:], "coef")
                    stop_at(43)
                    actf(Etmp, sc[1][:], AF.Exp, ["sc1"], ["hb0"], scale=-C0)
                    stt("dve", At, Etmp, -1.0, kkn, ALU.mult, ALU.mult, ["hb0", "pT1"], ["gn"])
                    actf(Etmp, sc[0][:], AF.Exp, ["sc0"], ["hb0"], scale=-C0)
                    tt("dve", Rt, rT, Etmp, ALU.mult, ["qn", "hb0"], ["qm"])
                    actf(Esq, sc[0][:], AF.Exp, ["sc0"], ["hb0"], scale=C0)
                    tt("dve", Bt, bT, Esq, ALU.mult, ["gn", "hb0"], ["qm"])
                    tt("pool", Kt, kmT, Esq, ALU.mult, ["qn", "hb0"], ["gm"])
                    ts("dve", ncL[:], sc[0][:, 127:G:128], -C0, None, ALU.mult, None, ["sc0"], ["ncL"])
                    actf(WL[:], ncL[:], AF.Exp, ["ncL"], ["WL"])
                    copy("dve", WL0[:, 0:4], WL[0:64, :], ["WL"], ["WL0"])
                    copy("dve", WL0[:, 4:8], WL[64:128, :], ["WL"], ["WL0"])
                    for c in range(4):
                        actf(Etmp[:, c * 128:(c + 1) * 128], sc[0][:, c * 128:(c + 1) * 128], AF.Exp, ["sc0", "ncL"], ["hb0"], bias=ncL[:, c:c + 1], scale=C0)
                    tt("dve", Bb, bT, Etmp, ALU.mult, ["gn", "hb0"], ["gm"])
                    tt("pool", Kb, kmT, Etmp, ALU.mult, ["qn", "hb0"], ["pT0"])
                    stop_at(44)
                    for c in range(4):
                        cs = slice(c * 128, (c + 1) * 128)
                        half = (c % 2) * 512
                        for wi, (src, skey) in enumerate(((At, "gn"), (vT, "qr"), (Bb, "gm"), (Kb, "pT0"))):
                            P.op("pe", lambda e, src=src, cs=cs, half=half, wi=wi: e.transpose(out=tp[:, half + wi * 128:half + (wi + 1) * 128], in_=src[:, cs], identity=ident[:]), [skey, "ident"], ["tp"])
                        copy(ev_eng(), tokall[:, c, :, :], tp[:, half:half + 512].rearrange("p (w f) -> p w f", w=4), ["tp"], ["tokall"])

                    stop_at(45)
                    for c in range(4):
                        cs = slice(c * 128, (c + 1) * 128)

                        def r2(bank):
                            return bank[:, 0:256].rearrange("p (e f) -> p e f", e=2)

                        def hs(e):
                            return slice(64 * e, 64 * e + 64)

                        for e in range(2):
                            mm(oa[0][:, e * 128:(e + 1) * 128], At[hs(e), cs], Bt[hs(e), cs], True, True, ["gn", "qm"], ["oa0"])
                        for e in range(2):
                            mm(oa[1][:, e * 128:(e + 1) * 128], Bt[hs(e), cs], At[hs(e), cs], True, True, ["gn", "qm"], ["oa1"])
                        tt("dve", Pm_[:], r2(oa[0]), maskL2[:], ALU.mult, ["oa0", "maskL2"], ["Pm"])
                        tt("dve", PTm[:], r2(oa[1]), maskU2[:], ALU.mult, ["oa1", "maskU2"], ["PTm"])
                        tt("pool", XTm[:], PTm[:], ident2[:], ALU.add, ["PTm", "ident2"], ["XTm"])
                        for lvl in range(1, 7):
                            for e in range(2):
                                mm(oa[0][:, e * 128:(e + 1) * 128], PTm[:, e, :], Pm_[:, e, :], True, True, ["PTm", "Pm"], ["oa0"])
                            if lvl < 6:
                                for e in range(2):
                                    mm(oa[1][:, e * 128:(e + 1) * 128], Pm_[:, e, :], PTm[:, e, :], True, True, ["PTm", "Pm"], ["oa1"])
                            copy("act", Pm_[:], r2(oa[0]), ["oa0"], ["Pm"])
                            if lvl < 6:
                                copy("dve", PTm[:], r2(oa[1]), ["oa1"], ["PTm"])
                            for e in range(2):
                                mm(mi[:, e * 128:(e + 1) * 128], Pm_[:, e, :], XTm[:, e, :], True, True, ["Pm", "XTm"], ["mi"])
                            tt("dve", XTm[:], XTm[:], r2(mi), ALU.add, ["XTm", "mi"], ["XTm"])
                        stop_at(46)
                        for e in range(2):
                            mm(oa[0][:, e * 128:(e + 1) * 128], Kt[hs(e), cs], At[hs(e), cs], True, True, ["gm", "gn"], ["oa0"])
                        tt("dve", AakT[:], r2(oa[0]), maskU2[:], ALU.mult, ["oa0", "maskU2"], ["AakT"])
                        for e in range(2):
                            mm(oa[1][:, e * 128:(e + 1) * 128], Bt[hs(e), cs], Rt[hs(e), cs], True, True, ["qm"], ["oa1"])
                        tt("dve", ArbT[:], r2(oa[1]), maskUI2[:], ALU.mult, ["oa1", "maskUI2"], ["ArbT"])
                        for e in range(2):
                            mm(mi[:, e * 128:(e + 1) * 128], Kt[hs(e), cs], Rt[hs(e), cs], True, True, ["gm", "qm"], ["mi"])
                        tt("dve", ArkT[:], r2(mi), maskUI2[:], ALU.mult, ["mi", "maskUI2"], ["ArkT"])
                        for e in range(2):
                            mm(oa[0][:, e * 64:(e + 1) * 64], AakT[:, e, :], tokall[:, c, 1, hs(e)], True, True, ["AakT", "tokall"], ["oa0"])
                        copy("act", AkV[:], oa[0][:, 0:128].rearrange("p (e f) -> p e f", e=2), ["oa0"], ["AkV"])
                        for e in range(2):
                            mm(oa[1][:, e * 64:(e + 1) * 64], XTm[:, e, :], tokall[:, c, 0, hs(e)], True, True, ["XTm", "tokall"], ["oa1"])
                        copy("dve", Ahat[:], oa[1][:, 0:128].rearrange("p (e f) -> p e f", e=2), ["oa1"], ["Ahat"])
                        for e in range(2):
                            mm(mi[:, e * 64:(e + 1) * 64], XTm[:, e, :], AkV[:, e, :], True, True, ["XTm", "AkV"], ["mi"])
                        copy("act", Uhat[:], mi[:, 0:128].rearrange("p (e f) -> p e f", e=2), ["mi"], ["Uhat"])
                        stop_at(47)
                        for e in range(2):
                            mm(oa[0][0:64, e * 128:(e + 1) * 128], Ahat[:, e, :], ArbT[:, e, :], True, False, ["Ahat", "ArbT"], ["oa0"])
                            mm(oa[0][0:64, e * 128:(e + 1) * 128], ident[hs(e), hs(e)], Rt[hs(e), cs], False, True, ["ident", "qm"], ["oa0"])
                        copy("dve", RhT[:], oa[0][0:64, 0:256].rearrange("p (e f) -> p e f", e=2), ["oa0"], ["RhT"])
                        for e in range(2):
                            reg = oa[1][0:64, e * 128:(e + 1) * 128]
                            mm(reg, STb[:, 2 * hp + e, :], RhT[:, e, :], True, False, ["STb", "RhT"], ["oa1"])
                            mm(reg, Uhat[:, e, :], ArbT[:, e, :], False, False, ["Uhat", "ArbT"], ["oa1"])
                            mm(reg, tokall[:, c, 1, hs(e)], ArkT[:, e, :], False, True, ["tokall", "ArkT"], ["oa1"])
                        copy("act", yraw[0:64, cs], oa[1][0:64, 0:128], ["oa1"], ["acc"])
                        copy("dve", yraw[64:128, cs], oa[1][0:64, 128:256], ["oa1"], ["acc"])
                        stop_at(48)
                        for e in range(2):
                            mm(mi[0:64, e * 64:(e + 1) * 64], Ahat[:, e, :], tokall[:, c, 2, hs(e)], True, True, ["Ahat", "tokall"], ["mi"])
                        for e in range(2):
                            stt("dve", Msb[:, e, :], ident[0:64, 0:64], WL0[:, 4 * e + c:4 * e + c + 1], mi[0:64, e * 64:(e + 1) * 64], ALU.mult, ALU.add, ["ident", "WL0", "mi"], ["Msb"])
                        for e in range(2):
                            reg = oa[0][0:64, e * 64:(e + 1) * 64]
                            mm(reg, Msb[:, e, :], STb[:, 2 * hp + e, :], True, False, ["Msb", "STb"], ["oa0"])
                            mm(reg, tokall[:, c, 2, hs(e)], Uhat[:, e, :], False, False, ["tokall", "Uhat"], ["oa0"])
                            mm(reg, tokall[:, c, 3, hs(e)], tokall[:, c, 1, hs(e)], False, True, ["tokall"], ["oa0"])
                        copy("dve", STb[:, 2 * hp:2 * hp + 2, :], oa[0][0:64, 0:128].rearrange("p (e f) -> p e f", e=2), ["oa0"], ["STb"])

                    if g == 0 and hp == 0:
                        dbg_out("yraw", yraw, "acc")
                    stop_at(49)
                    copy("act", Etmp, yraw, ["acc"], ["hb0"])
                    actf(Esq, yraw, AF.Square, ["acc"], ["hb0"])
                    mm(sc[0][:], blkavg[:], Etmp, True, True, ["blkavg", "hb0"], ["sc0"])
                    mm(sc[1][:], blkavg[:], Esq, True, True, ["blkavg", "hb0"], ["sc1"])
                    actf(coef[:], sc[0][:], AF.Square, ["sc0"], ["coef"])
                    tt("dve", coef[:], sc[1][:], coef[:], ALU.subtract, ["sc1", "coef"], ["coef"])
                    ts("dve", coef[:], coef[:], 64e-5, None, ALU.add, None, ["coef"], ["coef"])
                    P.op("dve", lambda e: e.reciprocal(out=coef[:], in_=coef[:]), ["coef"], ["coef"])
                    actf(coef[:], coef[:], AF.Sqrt, ["coef"], ["coef"])
                    tt("dve", yraw, yraw, sc[0][:], ALU.subtract, ["acc", "sc0"], ["acc"])
                    tt("dve", yraw, yraw, coef[:], ALU.mult, ["acc", "coef"], ["acc"])
                    ts("dve", yraw, yraw, pcol("lnw", hp), pcol("lnb", hp), ALU.mult, ALU.add, ["acc", "pk"], ["acc"])
                    tt("pool", Etmp, rT, kmT, ALU.mult, ["qn"], ["hb0"])
                    ts("dve", Esq, Etmp, pcol("rk", hp), None, ALU.mult, None, ["hb0", "pk"], ["hb0"])
                    mm(sc[0][:], blk1[:], Esq, True, True, ["blk1", "hb0"], ["sc0"])
                    tt("dve", coef[:], sc[0][:], vT, ALU.mult, ["sc0", "qr"], ["coef"])
                    tt("dve", yraw, yraw, coef[:], ALU.add, ["acc", "coef"], ["acc"])
                    tt("dve", yT[:, hp, :], yraw, gT, ALU.mult, ["acc", "qr"], ["yT"])

                stop_at(6)
                for pr in range(2):
                    pb, pkey = project(25 + pr)
                    P.op("act", lambda e, pb=pb, pr=pr: e.activation(out=qm[:, pr, :], in_=pb[:], func=AF.Copy, scale=0.125), [pkey], ["qm"])
                    pb, pkey = project(27 + pr)
                    P.op("act", lambda e, pb=pb, pr=pr: e.activation(out=gm[:, pr, :], in_=pb[:], func=AF.Silu), [pkey], ["gm"])
                stop_at(61)
                for h in range(4):
                    pr, hb_ = h // 2, (h % 2) * 64
                    ob = oa[h % 2]
                    okey = "oa%d" % (h % 2)
                    for mc in range(2):
                        sb_ = sc[mc]
                        mm(sb_[:], memkT[hb_:hb_ + 64, pr, mc * 128:(mc + 1) * 128], qm[hb_:hb_ + 64, pr, :], True, True, ["memkT", "qm"], ["sc%d" % mc])
                        P.op("act", lambda e, sb_=sb_, mc=mc: e.activation(out=pT[mc][:], in_=sb_[:], func=AF.Exp), ["sc%d" % mc], ["pT%d" % mc])
                    stop_at(62 if h == 0 else 66)
                    for mc in range(2):
                        mm(ob[0:65, :], memvA[:, mc, h, :], pT[mc][:], mc == 0, mc == 1, ["memvA", "pT%d" % mc], [okey])
                    stop_at(63 if h == 0 else 67)
                    o, w = PK["memg"]
                    finalize_head(ob, okey, True, hb_, pk[hb_:hb_ + 64, o + pr:o + pr + 1], gm[hb_:hb_ + 64, pr, :], "gm", yT[hb_:hb_ + 64, 6 + pr, :], "yT")


                P.dma("sp", lambda e, t0=t0: e.dma_start(out=ropeC[:], in_=ropeC_d[:, t0:t0 + G]), writes=["ropeC"])
                P.dma("sp", lambda e, t0=t0: e.dma_start(out=ropeS[:], in_=ropeS_d[:, t0:t0 + G]), writes=["ropeS"])

                def rope(src_ap, skey, dst_ap, dkey):
                    mm(mi[:], pm[:], src_ap, True, True, ["pm", skey], ["mi"])
                    P.op("dve", lambda e: e.tensor_tensor(out=rtmp[:], in0=mi[:], in1=ropeS[:], op=ALU.mult), ["mi", "ropeS"], ["coef"])
                    P.op("pool", lambda e: e.tensor_tensor(out=dst_ap, in0=src_ap, in1=ropeC[:], op=ALU.mult), [skey, "ropeC"], [dkey])
                    P.op("dve", lambda e: e.tensor_tensor(out=dst_ap, in0=dst_ap, in1=rtmp[:], op=ALU.add), [dkey, "coef"], [dkey])

                for pr in range(2):
                    pb, pkey = project(17 + pr)
                    P.op("act", lambda e, pb=pb, pr=pr: e.activation(out=qn[:, pr, :], in_=pb[:], func=AF.Copy, scale=0.125), [pkey], ["qn"])
                    rope(qn[:, pr, :], "qn", qr[:, pr, :], "qr")
                    pb, pkey = project(19 + pr)
                    P.op("act", lambda e, pb=pb, pr=pr: e.activation(out=gn[:, pr, :], in_=pb[:], func=AF.Silu), [pkey], ["gn"])
                copy("dve", kcvc[:, 0:16], kcvc[:, G:G + 16], ["kcvc"], ["kcvc"])
                pb, pkey = project(21)
                copy("act", kcvc[:, 16:16 + G], pb[:], [pkey], ["kcvc"])
                pb, pkey = project(22)
                copy("act", kraw[:], pb[:], [pkey], ["kraw"])
                rope(kraw[:], "kraw", ksT[:, t0:t0 + G], "ksT")
                pb, pkey = project(23)
                copy("act", kraw[:], pb[:], [pkey], ["kraw"])
                wo = (g % 2) * G
                rope(kraw[:], "kraw", kwT[:, wo:wo + G], "kwT")
                for i in range(4):
                    pbv = pj[i % 2]
                    for k in range(8):
                        mm(pbv[:, 0:128], hT[:, k, i * 128:(i + 1) * 128], wb[:, k, 24 * 128:25 * 128], k == 0, k == 7, ["hT", "wb"], ["pj%d" % (i % 2)])
                    kt = 4 * g + i
                    copy("dve", vsA[:, kt, 0:64], pbv[:, 0:64], ["pj%d" % (i % 2)], ["vsA"])
                    copy("act", vwA[:, kt % 8, 0:64], pbv[:, 64:128], ["pj%d" % (i % 2)], ["vwA"])
                pb, pkey = project(29, 12)
                o_, w_ = PK["gateb"]
                P.op("act", lambda e, pb=pb, o_=o_: e.activation(out=gsig[:], in_=pb[0:12, :], func=AF.Sigmoid, bias=pk[0:12, o_:o_ + 1]), [pkey, "pk"], ["gsig"])

                lo, hi = max(0, 32 * g - 1), 32 * g + 30
                n = hi - lo + 1
                c0 = 16 * lo + 16 - t0
                for kv in range(2):
                    b0 = kv * 64
                    reg = mi[0:64, kv * 64:kv * 64 + n]
                    for l in range(32):
                        mm(reg, w1b[b0:b0 + 64, l, :], kcvc[b0:b0 + 64, c0 + l:c0 + l + 16 * (n - 1) + 1:16], l == 0, l == 31, ["w1b", "kcvc"], ["mi"])
                    dsth = (h1k, h1v)[kv]
                    P.op("act", lambda e, reg=reg, dsth=dsth, kv=kv, n=n: e.activation(out=dsth[:, 0:n], in_=reg, func=AF.Silu, bias=ccst[:, kv:kv + 1]), ["mi", "ccst"], ["h1%d" % kv])
                mm(sc[0][:, 0:n], w2b[:, 0:128], h1k[:, 0:n], True, True, ["w2b", "h10"], ["sc0"])
                copy("dve", kcmpT[:, lo:hi + 1], sc[0][:, 0:n], ["sc0"], ["kcmpT"])
                mm(sc[1][0:n, 0:64], h1v[:, 0:n], w2b[:, 128:192], True, True, ["w2b", "h11"], ["sc1"])
                copy("dve", vstg[0:n, 0:64], sc[1][0:n, 0:64], ["sc1"], ["vstg"])
                i0 = lo
                while i0 <= hi:
                    i1 = min(hi, (i0 // 128) * 128 + 127)
                    P.dma("sp", lambda e, i0=i0, i1=i1, lo=lo: e.dma_start(out=vcmpA[i0 % 128:i1 % 128 + 1, i0 // 128, :], in_=vstg[i0 - lo:i1 - lo + 1, :]), reads=["vstg"], writes=["vcmpA"])
                    i0 = i1 + 1

                def combine(ob, okey, h, b, first):
                    pr, hb_ = h // 2, (h % 2) * 64
                    sl = slice(hb_, hb_ + 64)
                    copy("dve", oab[:], ob[0:65, :], [okey], ["oab"])
                    copy("act", oaug[sl, :], ob[0:64, :], [okey], ["oaug"])
                    mm(sc[0][:], e64[:], oab[:], True, True, ["e64", "oab"], ["sc0"])
                    P.op("dve", lambda e: e.tensor_scalar(out=gsr[:], in0=gsig[:], scalar1=ident[0:12, 3 * h + b:3 * h + b + 1], scalar2=None, op0=ALU.mult), ["gsig", "ident"], ["gsr"])
                    mm(sc[1][:], ones12[:], gsr[:], True, True, ["ones12", "gsr"], ["sc1"])
                    P.op("dve", lambda e: e.tensor_scalar(out=coef[sl, :], in0=sc[0][sl, :], scalar1=1e-30, scalar2=None, op0=ALU.max), ["sc0"], ["coef"])
                    P.op("dve", lambda e: e.reciprocal(out=coef[sl, :], in_=coef[sl, :]), ["coef"], ["coef"])
                    P.op("dve", lambda e: e.tensor_tensor(out=coef[sl, :], in0=coef[sl, :], in1=sc[1][sl, :], op=ALU.mult), ["coef", "sc1"], ["coef"])
                    if first:
                        P.op("dve", lambda e: e.tensor_tensor(out=acc[sl, pr, :], in0=coef[sl, :], in1=oaug[sl, :], op=ALU.mult), ["coef", "oaug"], ["acc"])
                    else:
                        P.op("dve", lambda e: e.tensor_tensor(out=coef[sl, :], in0=coef[sl, :], in1=oaug[sl, :], op=ALU.mult), ["coef", "oaug"], ["coef"])
                        P.op("dve", lambda e: e.tensor_tensor(out=acc[sl, pr, :], in0=acc[sl, pr, :], in1=coef[sl, :], op=ALU.add), ["coef", "acc"], ["acc"])

                def pmask(ap, key, base, cm, step):
                    P.op("pool", lambda e: e.affine_select(out=ap, in_=ap, pattern=[[step, G]], compare_op=ALU.is_ge, fill=0.0, base=base, channel_multiplier=cm), [key], [key])

                def pmask(ap, key, base, cm, step):
                    P.op("pool", lambda e: e.affine_select(out=ap, in_=ap, pattern=[[step, G]], compare_op=ALU.is_ge, fill=0.0, base=base, channel_multiplier=cm), [key], [key])

                P.op("pool", lambda e: e.memset(impS[:], 0.0), [], ["impS"])
                ncc = g // 4 + 1
                for h in range(4):
                    pr, hb_ = h // 2, (h % 2) * 64
                    ob, okey = oa[h % 2], "oa%d" % (h % 2)
                    for ci in range(ncc):
                        dl = 2048 * ci - 512 * g
                        sb_, skey = sc[ci % 2], "sc%d" % (ci % 2)
                        mm(sb_[:], kcmpT[hb_:hb_ + 64, ci * 128:(ci + 1) * 128], qn[hb_:hb_ + 64, pr, :], True, True, ["kcmpT", "qn"], [skey])
                        P.op("act", lambda e, sb_=sb_, ci=ci: e.activation(out=pTc[:, ci, :], in_=sb_[:], func=AF.Exp), [skey], ["wmkvb"])
                        if dl >= -2048:
                            pmask(pTc[:, ci, :], "wmkvb", -31 - dl, -16, 1)
                        mm(ob[0:65, :], vcmpA[:, ci, :], pTc[:, ci, :], ci == 0, ci == ncc - 1, ["vcmpA", "wmkvb"], [okey])
                    for j in range(4):
                        reg = mi[:, (j % 2) * 256:(j % 2) * 256 + 129]
                        for ci in range(ncc):
                            mm(reg, pTc[:, ci, j * 128:(j + 1) * 128], ovl[:, ci, :], ci == 0, ci == ncc - 1, ["wmkvb", "ovl"], ["mi"])
                        P.op("dve", lambda e, reg=reg: e.tensor_scalar(out=m8[:, 0:1], in0=reg[:, 128:129], scalar1=1e-30, scalar2=None, op0=ALU.max), ["mi"], ["m8"])
                        P.op("dve", lambda e: e.reciprocal(out=m8[:, 0:1], in_=m8[:, 0:1]), ["m8"], ["m8"])
                        P.op("dve", lambda e, reg=reg, j=j: e.scalar_tensor_tensor(out=impS[:, j, :], in0=reg[:, 0:128], scalar=m8[:, 0:1], in1=impS[:, j, :], op0=ALU.mult, op1=ALU.add), ["mi", "m8", "impS"], ["impS"])
                    combine(ob, okey, h, 0, True)

                for j in range(4):
                    qt = 4 * g + j
                    u0 = 126 - 2 * qt
                    P.op("dve", lambda e, j=j, u0=u0: e.tensor_tensor(out=score[:], in0=impS[:, j, :], in1=addw[:, u0:u0 + 128], op=ALU.add), ["impS", "addw"], ["score"])
                    P.op("dve", lambda e: e.tensor_scalar(out=score[:, 0:1], in0=score[:, 0:1], scalar1=1e4, scalar2=None, op0=ALU.add), ["score"], ["score"])
                    P.op("dve", lambda e: e.max(out=m8[:, 0:8], in_=score[:]), ["score"], ["m8"])
                    P.op("dve", lambda e: e.match_replace(out=stmp[:], in_to_replace=m8[:, 0:8], in_values=score[:], imm_value=-2.0), ["score", "m8"], ["stmp"])
                    P.op("dve", lambda e: e.max(out=m8[:, 8:16], in_=stmp[:]), ["stmp"], ["m8"])
                    P.op("dve", lambda e: e.tensor_reduce(out=m8[:, 0:1], in_=m8[:, 8:16], axis=AX.X, op=ALU.min), ["m8"], ["m8"])
                    P.op("dve", lambda e: e.tensor_scalar(out=stmp[:], in0=score[:], scalar1=m8[:, 0:1], scalar2=None, op0=ALU.is_ge), ["score", "m8"], ["stmp"])
                    P.op("dve", lambda e: e.tensor_scalar(out=nbq[:], in0=stmp[:], scalar1=1.0, scalar2=-NEG, op0=ALU.subtract, op1=ALU.mult), ["stmp"], ["nbq"])
                    P.op("pe", lambda e: e.transpose(out=tp[:, 0:128], in_=nbq[:], identity=ident[:]), ["nbq", "ident"], ["tp"])
                    copy("dve", nb[:, j * 128:(j + 1) * 128], tp[:, 0:128], ["tp"], ["nb"])
                copy("dve", nb2[:], nb[64:128, :], ["nb"], ["nb2"])

                for h in range(4):
                    pr, hb_ = h // 2, (h % 2) * 64
                    ob, okey = oa[h % 2], "oa%d" % (h % 2)
                    nkt = 4 * g + 4
                    for kt in range(nkt):
                        sb_, skey = sc[kt % 2], "sc%d" % (kt % 2)
                        mm(sb_[:], ksT[hb_:hb_ + 64, kt * 128:(kt + 1) * 128], qr[hb_:hb_ + 64, pr, :], True, False, ["ksT", "qr"], [skey])
                        m_ = (2 * kt) // 32
                        a2 = ((2 * kt) % 32) // 2
                        src, srck = (nb, "nb") if m_ < 2 else (nb2, "nb2")
                        pb_ = 32 * (m_ % 2)
                        diag = kt >= 4 * g
                        mm(sb_[:], fsel[pb_:pb_ + 32, a2, :], src[pb_:pb_ + 32, :], False, True, ["fsel", srck], [skey])
                        P.op("act", lambda e, sb_=sb_, kt=kt: e.activation(out=pT[kt % 2][:], in_=sb_[:], func=AF.Exp), [skey], ["pT%d" % (kt % 2)])
                        if diag:
                            pmask(pT[kt % 2][:], "pT%d" % (kt % 2), -128 * (kt - 4 * g), -1, 1)
                        mm(ob[0:65, :], vsA[:, kt, :], pT[kt % 2][:], kt == 0, kt == nkt - 1, ["vsA", "pT%d" % (kt % 2)], [okey])
                    combine(ob, okey, h, 1, False)

                for h in range(4):
                    pr, hb_ = h // 2, (h % 2) * 64
                    ob, okey = oa[h % 2], "oa%d" % (h % 2)
                    kts = [kt for kt in range(4 * g - 4, 4 * g + 4) if kt >= 0]
                    for ii, kt in enumerate(kts):
                        sb_, skey = sc[kt % 2], "sc%d" % (kt % 2)
                        ro = (kt % 8) * 128
                        mm(sb_[:], kwT[hb_:hb_ + 64, ro:ro + 128], qr[hb_:hb_ + 64, pr, :], True, True, ["kwT", "qr"], [skey])
                        P.op("act", lambda e, sb_=sb_, kt=kt: e.activation(out=pT[kt % 2][:], in_=sb_[:], func=AF.Exp), [skey], ["pT%d" % (kt % 2)])
                        rel = kt - 4 * g
                        if rel >= 0:
                            pmask(pT[kt % 2][:], "pT%d" % (kt % 2), -128 * rel, -1, 1)
                        else:
                            pmask(pT[kt % 2][:], "pT%d" % (kt % 2), 511 + 128 * rel, 1, -1)
                        mm(ob[0:65, :], vwA[:, kt % 8, :], pT[kt % 2][:], ii == 0, ii == len(kts) - 1, ["vwA", "pT%d" % (kt % 2)], [okey])
                    combine(ob, okey, h, 2, False)

                o_, w_ = PK["nsag"]
                for h in range(4):
                    pr, hb_ = h // 2, (h % 2) * 64
                    finalize_head(acc[hb_:hb_ + 64, pr, :], "acc", False, hb_, pk[hb_:hb_ + 64, o_ + pr:o_ + pr + 1], gn[hb_:hb_ + 64, pr, :], "gn", yT[hb_:hb_ + 64, 4 + pr, :], "yT", from_sbuf=True)
                if g == 0:
                    dbg_out("yT", yT[:], "yT")
                stop_at(7)
                for i in range(4):
                    xb, xk = xt[i % 2], "xt%d" % (i % 2)
                    P.dma("sp", lambda e, i=i, t0=t0, xb=xb: e.dma_start(out=xb[:], in_=x_d[t0 + i * 128:t0 + (i + 1) * 128, :]), writes=[xk])
                    for half in range(2):
                        pb = pj[half]
                        for k in range(8):
                            mm(pb[:], yT[:, k, i * 128:(i + 1) * 128], woutb[:, k, half * 512:(half + 1) * 512], k == 0, k == 7, ["yT", "woutb"], ["pj%d" % half])
                        P.op("dve", lambda e, pb=pb, xb=xb, half=half: e.tensor_tensor(out=xb[:, half * 512:(half + 1) * 512], in0=xb[:, half * 512:(half + 1) * 512], in1=pb[:], op=ALU.add),
                             ["pj%d" % half, xk], [xk])
                    sk = "st%d" % (8 + i)
                    ssq = st[:, 8 + i:9 + i]
                    hjk = hb[i % 2]
                    P.op("act", lambda e, xb=xb, ssq=ssq, hjk=hjk: e.activation(out=hjk[:], in_=xb[:], func=AF.Square, accum_out=ssq), [xk], ["hb0", sk])
                    P.op("dve", lambda e, ssq=ssq: e.tensor_scalar(out=ssq, in0=ssq, scalar1=1.0 / D, scalar2=1e-6, op0=ALU.mult, op1=ALU.add), [sk], [sk])
                    P.op("dve", lambda e, ssq=ssq: e.reciprocal(out=ssq, in_=ssq), [sk], [sk])
                    P.op("act", lambda e, ssq=ssq: e.activation(out=ssq, in_=ssq, func=AF.Sqrt), [sk], [sk])
                    P.op("dve", lambda e, xb=xb, ssq=ssq: e.scalar_tensor_tensor(out=xb[:], in0=xb[:], scalar=ssq, in1=gfin[:], op0=ALU.mult, op1=ALU.mult), [xk, sk, "gfin"], [xk])
                    P.dma("pool", lambda e, i=i, t0=t0, xb=xb: e.dma_start(out=out_d[t0 + i * 128:t0 + (i + 1) * 128, :], in_=xb[:]), reads=[xk], is_out=True)
        except _Stop:
            P.dma("pool", lambda e: e.dma_start(out=out_d[0:128, :], in_=xt[0][:]), reads=["xt0"], is_out=True)
        P.finish("sp")
        P.emit(block, sems, dsems)
    return nc


def make_in_maps(inputs, T, batches):
    ci = _colidx()
    w_in = np.asarray(inputs["w_in"][0])
    wcat = np.ascontiguousarray(w_in[:, ci].reshape(8, 128, NCOL))
    wout = np.ascontiguousarray(np.asarray(inputs["w_out"][0]).reshape(8, 128, D))
    wmkv = np.ascontiguousarray(np.asarray(inputs["w_mem_kv"][0]).reshape(8, 128, 512))
    pk = host_params({k: np.asarray(v) for k, v in inputs.items()})
    w1k = np.asarray(inputs["nsa_cmp_k_w1"][0]).reshape(32, 64, 64).transpose(1, 0, 2)
    w1v = np.asarray(inputs["nsa_cmp_v_w1"][0]).reshape(32, 64, 64).transpose(1, 0, 2)
    w1 = np.ascontiguousarray(np.concatenate([w1k, w1v], 0).reshape(128, 2048))
    w2k = np.asarray(inputs["nsa_cmp_k_w2"][0]); w2v = np.asarray(inputs["nsa_cmp_v_w2"][0])
    w2 = np.zeros((128, 192), np.float32)
    w2[0:64] = np.concatenate([w2k, w2k, w2v], 1)
    lora = np.ascontiguousarray(np.concatenate([np.asarray(inputs["rwkv_w_up"][0]), np.asarray(inputs["rwkv_a_up"][0])], 0))
    w0row = np.ascontiguousarray(np.asarray(inputs["rwkv_w0"][0]).reshape(1, 512))
    consts = host_consts(T)
    maps = []
    for b in batches:
        m = {
            "x": np.ascontiguousarray(np.asarray(inputs["x"][b][:T])),
            "mem": np.ascontiguousarray(np.asarray(inputs["mem"][b])),
            "wcat": wcat, "wout": wout, "wmkv": wmkv, "pk": pk, "w1": w1, "w2": w2, "lora": lora, "w0row": w0row,
            "gfin": np.ascontiguousarray(np.asarray(inputs["norm_final_g"]).reshape(1, D)),
        }
        m.update(consts)
        maps.append(m)
    return maps


_NC_CACHE = {}


def kernel(**inputs):
    T = inputs["x"].shape[1]
    B = inputs["x"].shape[0]
    if T not in _NC_CACHE:
        _NC_CACHE[T] = build_nc(T)
    nc = _NC_CACHE[T]
    batches = [i % B for i in range(8)]
    maps = make_in_maps(inputs, T, batches)
    res = run_bass_kernel_spmd(nc, maps, core_ids=list(range(8)))
    out = np.stack([res.results[b]["out"] for b in range(B)], axis=0)
    return out.astype(np.float32)
```

```python
import numpy as np
import ml_dtypes
from contextlib import ExitStack
import concourse.bass as bass
import concourse.mybir as mybir
from concourse.bass_utils import run_bass_kernel_spmd

F32 = mybir.dt.float32
BF16 = mybir.dt.bfloat16
ALU = mybir.AluOpType
AF = mybir.ActivationFunctionType
AX = mybir.AxisListType

ENGS = ("pe", "act", "dve", "pool", "sp")
N_DMA_SEMS = 24
import os as _os0
SAME_ENG_SYNC = _os0.environ.get("KSAME", "1") == "1"
NEG = -30000.0

D = 1024
NCOL = 29 * 128 + 12
G = 512
PSUM_PREFIX = ("pj", "sc", "oa", "mi", "tp")


class Prog:
    def __init__(self, nc):
        self.nc = nc
        self.lists = {e: [] for e in ENGS}
        self.cnt = {e: 0 for e in ENGS}
        self.seen = {e: {} for e in ENGS}
        self.clock_at = {e: [None] for e in ENGS}
        self.buf = {}
        self.dma_val = [0] * N_DMA_SEMS
        self.dma_clock = [[] for _ in range(N_DMA_SEMS)]
        self.dma_rr = 0
        self.out_toks = []
        self.phase = ""
        self.skip = set()

    def _deps(self, eng, reads, writes):
        need = {}

        def add(tok):
            if tok is None:
                return
            src, val = tok
            if src == eng and (eng == "pe" or not SAME_ENG_SYNC):
                return
            if need.get(src, 0) < val:
                need[src] = val

        for k in reads:
            st = self.buf.get(k)
            if st:
                add(st[0])
                if k[:2] in PSUM_PREFIX:
                    for r in st[1]:
                        if r[0] != eng:
                            add(r)
        for k in writes:
            st = self.buf.get(k)
            if st:
                add(st[0])
                for r in st[1]:
                    add(r)
        seen = self.seen[eng]
        waits = []
        for src, val in need.items():
            if seen.get(src, 0) >= val:
                continue
            waits.append((src, val))
            if isinstance(src, str):
                snap = self.clock_at[src][val]
            else:
                snap = self.dma_clock[src[1]][val // 16 - 1]
            for s2, v2 in snap.items():
                if seen.get(s2, 0) < v2:
                    seen[s2] = v2
            seen[src] = val
        return waits

    def _mark(self, tok, reads, writes):
        for k in reads:
            st = self.buf.setdefault(k, [None, []])
            st[1].append(tok)
            if len(st[1]) > 64:
                st[1] = st[1][-64:]
        for k in writes:
            self.buf[k] = [tok, []]

    def op(self, eng, fn, reads=(), writes=(), self_wait=None):
        if self.phase in self.skip:
            return
        waits = self._deps(eng, reads, writes)
        if self_wait is not None and self.seen[eng].get(eng, 0) < self_wait:
            waits.append((eng, self_wait))
            self.seen[eng][eng] = self_wait
        self.cnt[eng] += 1
        c = self.cnt[eng]
        snap = dict(self.seen[eng])
        snap[eng] = c
        self.clock_at[eng].append(snap)
        self.lists[eng].append(("op", fn, waits, None))
        self._mark((eng, c), reads, writes)

    def dma(self, eng, fn, reads=(), writes=(), is_out=False):
        if self.phase in self.skip:
            return None
        s = self.dma_rr
        self.dma_rr = (self.dma_rr + 1) % N_DMA_SEMS
        waits = self._deps(eng, reads, writes)
        src = ("dma", s)
        prev = self.dma_val[s]
        if prev and self.seen[eng].get(src, 0) < prev:
            waits.append((src, prev))
            self.seen[eng][src] = prev
        val = prev + 16
        self.dma_val[s] = val
        self.dma_clock[s].append(dict(self.seen[eng]))
        self.lists[eng].append(("dma", fn, waits, (s, val)))
        self._mark((src, val), reads, writes)
        if is_out:
            self.out_toks.append((src, val))
        return (src, val)

    def finish(self, eng="sp"):
        waits = []
        for s in range(N_DMA_SEMS):
            if self.dma_val[s]:
                waits.append((("dma", s), self.dma_val[s]))
        self.lists[eng].append(("wait", None, waits, None))

    def emit(self, block, sems, dsems):
        engobj = {"pe": "tensor", "act": "scalar", "dve": "vector", "pool": "gpsimd", "sp": "sync"}

        def semof(src):
            return sems[src] if isinstance(src, str) else dsems[src[1]]

        def make(ename):
            lst = self.lists[ename]

            def body(e):
                for kind, fn, waits, extra in lst:
                    for src, val in waits:
                        e.wait_ge(semof(src), val)
                    if kind == "op":
                        fn(e).then_inc(sems[ename], 1)
                    elif kind == "dma":
                        fn(e).then_inc(dsems[extra[0]], 16)

            return body

        for ename in ENGS:
            if self.lists[ename]:
                getattr(block, engobj[ename])(make(ename))


RW, NSAB, MEMB = 0, 2176, 3084


def _colidx():
    idx = []
    for hp in range(4):
        for base in (0, 512, 1024, 1536):
            idx += list(range(base + 128 * hp, base + 128 * hp + 128))
    idx += list(range(2048, 2176))
    q0, g0 = NSAB, NSAB + 256
    idx += list(range(q0, q0 + 256))
    idx += list(range(g0, g0 + 256))
    kc, vc, ks, vs, kw, vw = (NSAB + 524 + 64 * i for i in range(6))
    idx += list(range(kc, kc + 64)) + list(range(vc, vc + 64))
    idx += list(range(ks, ks + 64)) * 2
    idx += list(range(kw, kw + 64)) * 2
    idx += list(range(vs, vs + 64)) + list(range(vw, vw + 64))
    idx += list(range(MEMB, MEMB + 512))
    idx += list(range(NSAB + 512, NSAB + 524))
    assert len(idx) == NCOL
    return np.array(idx)


def _bf(a):
    return np.ascontiguousarray(a.astype(ml_dtypes.bfloat16))


def host_consts(T):
    c = {}
    c["ident"] = _bf(np.eye(128, dtype=np.float32))
    half = 8
    inv = (np.float32(500000.0) ** (-np.arange(half, dtype=np.float32) / np.float32(half))).astype(np.float32)
    ang = np.arange(T, dtype=np.float32)[None, :] * inv[:, None]
    C = np.ones((64, T), np.float32)
    S = np.zeros((64, T), np.float32)
    C[0:8] = np.cos(ang); C[8:16] = np.cos(ang)
    S[0:8] = -np.sin(ang); S[8:16] = np.sin(ang)
    c["ropeC"] = np.ascontiguousarray(np.concatenate([C, C], 0))
    c["ropeS"] = np.ascontiguousarray(np.concatenate([S, S], 0))
    pm = np.zeros((128, 128), np.float32)
    for hb in (0, 64):
        for d in range(8):
            pm[hb + d + 8, hb + d] = 1.0
            pm[hb + d, hb + d + 8] = 1.0
    c["pm"] = _bf(pm)
    ovl = np.zeros((512, 129), np.float32)
    for i in range(511):
        for sblk in range(128):
            o = min(16 * i + 32, 64 * sblk + 64) - max(16 * i, 64 * sblk)
            if o > 0:
                ovl[i, sblk] = o / 32.0
    ovl[:, 128] = 1.0
    c["ovl"] = _bf(ovl.reshape(4, 128, 129).transpose(1, 0, 2))
    addw = np.zeros((128, 256), np.float32)
    for pp in range(128):
        cur = 1 if pp >= 64 else 0
        for u in range(256):
            sp = u - 126
            valid = sp <= cur
            forced = sp in (cur, cur - 1)
            addw[pp, u] = (0.0 if valid else -1.0) + (1e4 if forced else 0.0)
    c["addw"] = addw
    fsel = np.zeros((64, 16, 128), np.float32)
    for a2 in range(16):
        for key in range(128):
            r = 2 * a2 + (1 if key >= 64 else 0)
            fsel[r, a2, key] = 1.0
            fsel[32 + r, a2, key] = 1.0
    c["fsel"] = _bf(fsel)
    pp = np.arange(128)[:, None]
    ff = np.arange(128)[None, :]
    for nm, m in (("maskL2", pp > ff), ("maskU2", pp < ff), ("maskUI2", pp <= ff), ("ident2", pp == ff)):
        m = m.astype(np.float32)
        c[nm] = _bf(np.stack([m, m], 1))
    blk = (pp // 64 == ff // 64).astype(np.float32)
    c["blk1"] = _bf(blk)
    c["blkavg"] = _bf(blk / 64.0)
    return c


PK = {}
_o = 0
for _n, _w in (("gin", 8), ("gmem", 8), ("mu", 17), ("memg", 2), ("nsag", 2), ("gateb", 1), ("posT", 32), ("w0", 4), ("a0", 4), ("kk", 4), ("ka", 4), ("rk", 4), ("lnw", 4), ("lnb", 4)):
    PK[_n] = (_o, _w)
    _o += _w
NPK = _o


def host_params(inp):
    pk = np.zeros((128, NPK), np.float32)

    def put(name, arr):
        o, w = PK[name]
        pk[:, o:o + w] = arr

    put("gin", inp["norm_in_g"][0].reshape(8, 128).T)
    put("gmem", inp["mem_norm_g"][0].reshape(8, 128).T)
    ci = _colidx()
    put("mu", inp["rwkv_mu"][0][ci[:17 * 128]].reshape(17, 128).T)
    put("memg", inp["mem_out_g"][0].reshape(2, 128).T)
    put("nsag", inp["nsa_out_g"][0].reshape(2, 128).T)
    gb = np.zeros((128, 1), np.float32)
    gb[0:12, 0] = inp["nsa_gate_b"][0]
    put("gateb", gb)
    pt = inp["nsa_cmp_pos"][0].T
    put("posT", np.concatenate([pt, pt], 0))
    for nm, key in (("w0", "rwkv_w0"), ("a0", "rwkv_a0"), ("kk", "rwkv_k_k"), ("ka", "rwkv_k_a"), ("rk", "rwkv_r_k"), ("lnw", "rwkv_ln_w"), ("lnb", "rwkv_ln_b")):
        put(nm, inp[key][0].reshape(4, 128).T)
    return pk


def build_nc(T, dbg=None):
    NG = T // G
    NT = T // 128
    nc = bass.Bass("TRN2", target_bir_lowering=False)
    dram = {}

    def din(name, shape, dt=F32):
        dram[name] = nc.dram_tensor(name, list(shape), dt, kind="ExternalInput").ap()
        return dram[name]

    x_d = din("x", [T, D])
    mem_d = din("mem", [256, D])
    wcat_d = din("wcat", [8, 128, NCOL])
    wout_d = din("wout", [8, 128, D])
    wmkv_d = din("wmkv", [8, 128, 512])
    pk_d = din("pk", [128, NPK])
    gfin_d = din("gfin", [1, D])
    ident_d = din("ident", [128, 128], BF16)
    ropeC_d = din("ropeC", [128, T])
    ropeS_d = din("ropeS", [128, T])
    pm_d = din("pm", [128, 128], BF16)
    ovl_d = din("ovl", [128, 4, 129], BF16)
    addw_d = din("addw", [128, 256])
    fsel_d = din("fsel", [64, 16, 128], BF16)
    w1_d = din("w1", [128, 2048])
    w2_d = din("w2", [128, 192])
    lora_d = din("lora", [128, 512])
    w0row_d = din("w0row", [1, 512])
    maskL2_d = din("maskL2", [128, 2, 128], BF16)
    maskU2_d = din("maskU2", [128, 2, 128], BF16)
    maskUI2_d = din("maskUI2", [128, 2, 128], BF16)
    ident2_d = din("ident2", [128, 2, 128], BF16)
    blk1_d = din("blk1", [128, 128], BF16)
    blkavg_d = din("blkavg", [128, 128], BF16)
    out_d = nc.dram_tensor("out", [T, D], F32, kind="ExternalOutput").ap()
    dbg_d = {}
    if dbg:
        for n, (shape, dt) in dbg.items():
            dbg_d[n] = nc.dram_tensor("dbg_" + n, list(shape), dt, kind="ExternalOutput").ap()

    with ExitStack() as es:
        def sb(name, shape, dt=F32):
            return es.enter_context(nc.sbuf_tensor("sb_" + name, list(shape), dt))

        def ps(name, shape, dt=F32):
            return es.enter_context(nc.psum_tensor("ps_" + name, list(shape), dt))

        wb = sb("wb", [128, 8, NCOL], BF16)
        woutb = sb("woutb", [128, 8, D], BF16)
        wmkvb = sb("wmkvb", [128, 8, 512], BF16)
        pk = sb("pk", [128, NPK])
        gfin = sb("gfin", [128, D])
        ident = sb("ident", [128, 128], BF16)
        xt = [sb("xt%d" % i, [128, D]) for i in range(2)]
        hb0 = sb("hb0", [128, D], BF16)
        hb = [hb0, hb0]
        hT = sb("hT", [128, 8, G], BF16)
        st = sb("st", [128, 16])
        yT = sb("yT", [128, 8, G], BF16)
        memkT = sb("memkT", [128, 2, 256], BF16)
        memvA = sb("memvA", [128, 2, 4, 65], BF16)
        qm = sb("qm", [128, 2, G], BF16)
        gm = sb("gm", [128, 2, G], BF16)
        pT = [sb("pT%d" % i, [128, G], BF16) for i in range(2)]
        oaug = sb("oaug", [128, G], BF16)
        osq = sb("osq", [65, G], BF16)
        cvec = sb("cvec", [65, 128], BF16)
        pm = sb("pm", [128, 128], BF16)
        ovl = sb("ovl", [128, 4, 129], BF16)
        addw = sb("addw", [128, 256])
        fsel = sb("fsel", [64, 16, 128], BF16)
        ones12 = sb("ones12", [12, 128], BF16)
        gsr = sb("gsr", [12, G], BF16)
        e64 = sb("e64", [65, 128], BF16)
        w1b = sb("w1b", [128, 32, 64], BF16)
        w2b = sb("w2b", [64, 192], BF16)
        posTb = sb("posTb", [128, 32], BF16)
        ccst = sb("ccst", [64, 2])
        ropeC = sb("ropeC", [128, G])
        ropeS = sb("ropeS", [128, G])
        qn = sb("qn", [128, 2, G], BF16)
        qr = sb("qr", [128, 2, G], BF16)
        gn = sb("gn", [128, 2, G], BF16)
        kcvc = sb("kcvc", [128, 16 + G], BF16)
        kraw = sb("kraw", [128, G], BF16)
        ksT = sb("ksT", [128, T], BF16)
        kwT = sb("kwT", [128, 1024], BF16)
        vsA = sb("vsA", [128, NT, 65], BF16)
        vwA = sb("vwA", [128, 8, 65], BF16)
        kcmpT = sb("kcmpT", [128, 512], BF16)
        vcmpA = sb("vcmpA", [128, 4, 65], BF16)
        vstg = sb("vstg", [32, 65], BF16)
        h1k = sb("h1k", [64, 32], BF16)
        h1v = sb("h1v", [64, 32], BF16)
        gsig = sb("gsig", [12, G], BF16)
        impS = sb("impS", [128, 4, 128])
        score = sb("score", [128, 128])
        stmp = sb("stmp", [128, 128])
        m8 = sb("m8", [128, 16])
        nbq = sb("nbq", [128, 128], BF16)
        nb = sb("nb", [128, G], BF16)
        nb2 = sb("nb2", [64, G], BF16)
        oab = sb("oab", [65, G], BF16)
        coef = sb("coef", [128, G])
        scl = coef
        rtmp = coef
        acc = sb("acc", [128, 2, G])
        praw = sb("praw", [128, G + 1])
        prevcol = sb("prevcol", [128, 17])
        loraup = sb("loraup", [128, 512], BF16)
        w0hi = sb("w0hi", [1, 512], BF16)
        w0lo = sb("w0lo", [1, 512], BF16)
        onesrow = sb("onesrow", [1, 128], BF16)
        ka1m = sb("ka1m", [128, 4])
        maskL2 = sb("maskL2", [128, 2, 128], BF16)
        maskU2 = sb("maskU2", [128, 2, 128], BF16)
        maskUI2 = sb("maskUI2", [128, 2, 128], BF16)
        ident2 = sb("ident2", [128, 2, 128], BF16)
        blk1 = sb("blk1", [128, 128], BF16)
        blkavg = sb("blkavg", [128, 128], BF16)
        sgtok = sb("sgtok", [128, 4, 128], BF16)
        Pm_ = sb("Pm", [128, 2, 128], BF16)
        PTm = sb("PTm", [128, 2, 128], BF16)
        XTm = sb("XTm", [128, 2, 128], BF16)
        AakT = sb("AakT", [128, 2, 128], BF16)
        ArbT = sb("ArbT", [128, 2, 128], BF16)
        ArkT = sb("ArkT", [128, 2, 128], BF16)
        AkV = sb("AkV", [128, 2, 64], BF16)
        Ahat = sb("Ahat", [128, 2, 64], BF16)
        Uhat = sb("Uhat", [128, 2, 64], BF16)
        RhT = sb("RhT", [64, 2, 128], BF16)
        Msb = sb("Msb", [64, 2, 64], BF16)
        STb = sb("STb", [64, 8, 64], BF16)
        ncL = sb("ncL", [128, 4])
        WL = sb("WL", [128, 4])
        WL0 = sb("WL0", [64, 8])
        pj = [ps("pj%d" % i, [128, 512]) for i in range(2)]
        sc = [ps("sc%d" % i, [128, 512]) for i in range(2)]
        oa = [ps("oa%d" % i, [128, 512]) for i in range(2)]
        mi = ps("mi", [128, 512])
        tp = ps("tp", [128, 1024], BF16)

        pTc = wmkvb[:, 0:4, :]
        sems = {e: es.enter_context(nc.semaphore("s_" + e)) for e in ENGS}
        dsems = [es.enter_context(nc.semaphore("d%d" % i)) for i in range(N_DMA_SEMS)]
        block = es.enter_context(nc.Block())
        P = Prog(nc)
        rr = {"ev": 0}

        def ev_eng():
            rr["ev"] ^= 1
            return "act" if rr["ev"] else "dve"

        def copy(eng, out, in_, reads, writes):
            if eng == "act":
                P.op("act", lambda e: e.copy(out=out, in_=in_), reads, writes)
            else:
                P.op(eng, lambda e: e.tensor_copy(out=out, in_=in_), reads, writes)

        pe_last = {}

        def mm(out, lhsT, rhs, start, stop, reads, writes):
            base, rows = lhsT.base_partition(), lhsT.shape[0]
            sw = None
            for k in writes:
                last = pe_last.get(k)
                if last is not None:
                    c0, b0, r0 = last
                    if b0 + r0 <= base or base + rows <= b0:
                        sw = max(sw or 0, c0)
            P.op("pe", lambda e: e.matmul(out, lhsT, rhs, start=start, stop=stop), reads, writes, self_wait=sw)
            for k in writes:
                pe_last[k] = (P.cnt["pe"], base, rows)

        def dbg_out(name, ap, key):
            if dbg and name in dbg_d:
                P.dma("sp", lambda e: e.dma_start(out=dbg_d[name], in_=ap), reads=[key])

        import os as _os
        KSTOP = int(_os.environ.get("KSTOP", "99"))
        KSKIP = _os.environ.get("KSKIP", "")
        P.skip = set(KSKIP.split(",")) if KSKIP else set()

        class _Stop(Exception):
            pass

        def stop_at(n):
            if KSTOP == n:
                raise _Stop()

        try:
            P.dma("sp", lambda e: e.dma_start(out=pk[:], in_=pk_d[:, :]), writes=["pk"])
            P.dma("sp", lambda e: e.dma_start(out=ident[:], in_=ident_d[:, :]), writes=["ident"])
            P.dma("sp", lambda e: e.dma_start(out=gfin[:], in_=gfin_d[0:1, :].broadcast_to([128, D])), writes=["gfin"])

            def pkc(name, j=0, rows=128):
                o, w = PK[name]
                return pk[0:rows, o + j:o + j + 1]

            def load_cast(dst3, src_d, ncols, gname, key):
                i = 0
                for k in range(8):
                    for c0 in range(0, ncols, 1024):
                        cw = min(1024, ncols - c0)
                        stg = xt[i % 2]
                        skey = "xt%d" % (i % 2)
                        P.dma("sp" if i % 2 == 0 else "pool",
                              lambda e, stg=stg, k=k, c0=c0, cw=cw: e.dma_start(out=stg[:, 0:cw], in_=src_d[k, :, c0:c0 + cw]),
                              writes=[skey])
                        eng = ev_eng()
                        o = dst3[:, k, c0:c0 + cw]
                        if gname is None:
                            copy(eng, o, stg[:, 0:cw], [skey], [key])
                        elif eng == "act":
                            P.op("act", lambda e, o=o, stg=stg, cw=cw, k=k: e.activation(out=o, in_=stg[:, 0:cw], func=AF.Copy, scale=pkc(gname, k)),
                                 [skey, "pk"], [key])
                        else:
                            P.op("dve", lambda e, o=o, stg=stg, cw=cw, k=k: e.tensor_scalar(out=o, in0=stg[:, 0:cw], scalar1=pkc(gname, k), scalar2=None, op0=ALU.mult),
                                 [skey, "pk"], [key])
                        i += 1

            stop_at(1)
            load_cast(wmkvb, wmkv_d, 512, "gmem", "wmkvb")
            stop_at(2)
            load_cast(wb, wcat_d, NCOL, "gin", "wb")
            load_cast(woutb, wout_d, D, None, "woutb")

            P.op("pool", lambda e: e.memset(cvec[0:64, :], 1.0 / 64), writes=["cvec"])
            P.op("pool", lambda e: e.memset(cvec[64:65, :], 1e-6), writes=["cvec"])

            def norm_transpose(src_tile, skey, dstT, dkey, col0, slot):
                h = hb[slot]
                hk = "hb0"
                ssq = st[:, slot:slot + 1]
                P.op("act", lambda e: e.activation(out=h[:], in_=src_tile[:], func=AF.Square, accum_out=ssq), [skey], [hk, "st%d" % slot])
                P.op("dve", lambda e: e.tensor_scalar(out=ssq, in0=ssq, scalar1=1.0 / D, scalar2=1e-6, op0=ALU.mult, op1=ALU.add), ["st%d" % slot], ["st%d" % slot])
                P.op("dve", lambda e: e.reciprocal(out=ssq, in_=ssq), ["st%d" % slot], ["st%d" % slot])
                P.op("act", lambda e: e.activation(out=ssq, in_=ssq, func=AF.Sqrt), ["st%d" % slot], ["st%d" % slot])
                P.op("dve", lambda e: e.tensor_scalar(out=h[:], in0=src_tile[:], scalar1=ssq, scalar2=None, op0=ALU.mult), [skey, "st%d" % slot], [hk])
                for k in range(8):
                    P.op("pe", lambda e, k=k: e.transpose(out=tp[:, k * 128:(k + 1) * 128], in_=h[:, k * 128:(k + 1) * 128], identity=ident[:]), [hk, "ident"], ["tp"])
                copy(ev_eng(), dstT[:, :, col0:col0 + 128], tp[:].rearrange("p (k t) -> p k t", k=8), ["tp"], [dkey])


            for nm, dst, src in (("pm", pm, pm_d), ("ovl", ovl, ovl_d), ("addw", addw, addw_d),
                                 ("fsel", fsel, fsel_d)):
                P.dma("sp", lambda e, dst=dst, src=src: e.dma_start(out=dst[:], in_=src), writes=[nm])
            P.op("pool", lambda e: e.memset(ones12[:], 1.0), writes=["ones12"])
            P.op("pool", lambda e: e.memset(e64[:], 0.0), writes=["e64"])
            P.op("pool", lambda e: e.memset(e64[64:65, :], 1.0), writes=["e64"])
            P.op("pool", lambda e: e.memset(kcmpT[:], 0.0), writes=["kcmpT"])
            P.op("pool", lambda e: e.memset(vcmpA[:], 0.0), writes=["vcmpA"])
            P.op("pool", lambda e: e.memset(vsA[:], 1.0), writes=["vsA"])
            P.op("pool", lambda e: e.memset(vwA[:], 1.0), writes=["vwA"])
            P.op("pool", lambda e: e.memset(vstg[:], 1.0), writes=["vstg"])
            P.op("pool", lambda e: e.memset(kcvc[:], 0.0), writes=["kcvc"])
            for hlf in range(2):
                P.dma("sp", lambda e, hlf=hlf: e.dma_start(out=xt[hlf][:], in_=w1_d[:, hlf * 1024:(hlf + 1) * 1024]), writes=["xt%d" % hlf])
                copy(ev_eng(), w1b[:, hlf * 16:(hlf + 1) * 16, :], xt[hlf][:].rearrange("p (l e) -> p l e", e=64), ["xt%d" % hlf], ["w1b"])
            P.dma("sp", lambda e: e.dma_start(out=xt[0][:, 0:192], in_=w2_d[:, :]), writes=["xt0"])
            copy("dve", w2b[:], xt[0][0:64, 0:192], ["xt0"], ["w2b"])
            o_, w_ = PK["posT"]
            copy("dve", posTb[:], pk[:, o_:o_ + 32], ["pk"], ["posTb"])
            for kv in range(2):
                b0 = kv * 64
                for l in range(32):
                    mm(mi[0:64, kv:kv + 1], w1b[b0:b0 + 64, l, :], posTb[b0:b0 + 64, l:l + 1], l == 0, l == 31, ["w1b", "posTb"], ["mi"])
                copy("dve", ccst[:, kv:kv + 1], mi[0:64, kv:kv + 1], ["mi"], ["ccst"])

            for nm, dst, src in (("maskL2", maskL2, maskL2_d), ("maskU2", maskU2, maskU2_d), ("maskUI2", maskUI2, maskUI2_d),
                                 ("ident2", ident2, ident2_d), ("blk1", blk1, blk1_d), ("blkavg", blkavg, blkavg_d)):
                P.dma("sp", lambda e, dst=dst, src=src: e.dma_start(out=dst[:], in_=src), writes=[nm])
            P.dma("sp", lambda e: e.dma_start(out=xt[1][:, 0:512], in_=lora_d[:, :]), writes=["xt1"])
            copy("dve", loraup[:], xt[1][:, 0:512], ["xt1"], ["loraup"])
            P.dma("sp", lambda e: e.dma_start(out=praw[0:1, 0:512], in_=w0row_d[:, :]), writes=["praw"])
            copy("dve", w0hi[:], praw[0:1, 0:512], ["praw"], ["w0hi"])
            P.op("dve", lambda e: e.tensor_tensor(out=praw[0:1, 0:512], in0=praw[0:1, 0:512], in1=w0hi[:], op=ALU.subtract), ["praw", "w0hi"], ["praw"])
            copy("dve", w0lo[:], praw[0:1, 0:512], ["praw"], ["w0lo"])
            P.op("pool", lambda e: e.memset(onesrow[:], 1.0), writes=["onesrow"])
            P.op("pool", lambda e: e.memset(prevcol[:], 0.0), writes=["prevcol"])
            P.op("pool", lambda e: e.memset(STb[:], 0.0), writes=["STb"])
            o_, w_ = PK["ka"]
            P.op("dve", lambda e, o_=o_: e.tensor_scalar(out=ka1m[:], in0=pk[:, o_:o_ + 4], scalar1=-1.0, scalar2=1.0, op0=ALU.mult, op1=ALU.add), ["pk"], ["ka1m"])
            tokall = wmkvb[:, 4:8, :].rearrange("p c (w f) -> p c w f", w=4)
            stop_at(3)
            for mt in range(2):
                P.dma("sp", lambda e, mt=mt: e.dma_start(out=xt[mt][:], in_=mem_d[mt * 128:(mt + 1) * 128, :]), writes=["xt%d" % mt])
                norm_transpose(xt[mt], "xt%d" % mt, hT, "hT", mt * 128, mt)
            stop_at(4)
            for pr in range(2):
                for k in range(8):
                    mm(pj[0][:, 0:256], wmkvb[:, k, pr * 128:(pr + 1) * 128], hT[:, k, 0:256], k == 0, k == 7, ["wmkvb", "hT"], ["pj0"])
                copy(ev_eng(), memkT[:, pr, :], pj[0][:, 0:256], ["pj0"], ["memkT"])
            P.op("pool", lambda e: e.memset(memvA[:], 1.0), writes=["memvA"])
            for mt in range(2):
                for k in range(8):
                    mm(pj[1][:, 0:256], hT[:, k, mt * 128:(mt + 1) * 128], wmkvb[:, k, 256:512], k == 0, k == 7, ["wmkvb", "hT"], ["pj1"])
                copy(ev_eng(), memvA[:, mt, :, 0:64], pj[1][:, 0:256].rearrange("p (h d) -> p h d", h=4), ["pj1"], ["memvA"])

            def finalize_head(o_ps, okey, use_den, hb_, gcol_ap, gate_ap, gate_key, dst_ap, dst_key, from_sbuf=False):
                sl = slice(hb_, hb_ + 64)
                if from_sbuf:
                    copy("dve", oaug[sl, :], o_ps, [okey], ["oaug"])
                    P.op("act", lambda e: e.activation(out=osq[0:64, :], in_=o_ps, func=AF.Square), [okey], ["osq"])
                    P.op("pool", lambda e: e.memset(osq[64:65, :], 1.0), [], ["osq"])
                elif use_den:
                    copy("dve", oaug[sl, :], o_ps[0:64, :], [okey], ["oaug"])
                    P.op("act", lambda e: e.activation(out=osq[:], in_=o_ps[0:65, :], func=AF.Square), [okey], ["osq"])
                else:
                    copy("dve", oaug[sl, :], o_ps[0:64, :], [okey], ["oaug"])
                    P.op("act", lambda e: e.activation(out=osq[0:64, :], in_=o_ps[0:64, :], func=AF.Square), [okey], ["osq"])
                    P.op("pool", lambda e: e.memset(osq[64:65, :], 1.0), [], ["osq"])
                mm(mi[:], cvec[:], osq[:], True, True, ["cvec", "osq"], ["mi"])
                P.op("act", lambda e: e.activation(out=scl[sl, :], in_=mi[sl, :], func=AF.Ln), ["mi"], ["coef"])
                P.op("act", lambda e: e.activation(out=scl[sl, :], in_=scl[sl, :], func=AF.Exp, scale=-0.5), ["coef"], ["coef"])
                P.op("dve", lambda e: e.scalar_tensor_tensor(out=scl[sl, :], in0=scl[sl, :], scalar=gcol_ap, in1=oaug[sl, :], op0=ALU.mult, op1=ALU.mult), ["coef", "oaug", "pk"], ["coef"])
                P.op("dve", lambda e: e.tensor_tensor(out=dst_ap, in0=scl[sl, :], in1=gate_ap, op=ALU.mult), ["coef", gate_key], [dst_key])

            P.op("pool", lambda e: e.memset(wmkvb[:, 4:8, :], 0.0), ["wmkvb"], ["wmkvb", "tokall"])
            stop_at(5)
            for g in range(NG):
                t0 = g * G
                for i in range(4):
                    P.dma("sp", lambda e, i=i, t0=t0: e.dma_start(out=xt[i % 2][:], in_=x_d[t0 + i * 128:t0 + (i + 1) * 128, :]), writes=["xt%d" % (i % 2)])
                    norm_transpose(xt[i % 2], "xt%d" % (i % 2), hT, "hT", i * 128, i % 2)

                def project(chunk, width=128):
                    pb = pj[chunk % 2]
                    for k in range(8):
                        mm(pb[0:width, :], wb[:, k, chunk * 128:chunk * 128 + width], hT[:, k, :], k == 0, k == 7, ["wb", "hT"], ["pj%d" % (chunk % 2)])
                    return pb, "pj%d" % (chunk % 2)


                P.phase = "rwkv"
                C0 = float(np.exp(-0.5))

                def tt(eng, out, in0, in1, op, reads, writes):
                    P.op(eng, lambda e: e.tensor_tensor(out=out, in0=in0, in1=in1, op=op), reads, writes)

                def ts(eng, out, in0, s1, s2, op0, op1, reads, writes):
                    if s2 is None:
                        P.op(eng, lambda e: e.tensor_scalar(out=out, in0=in0, scalar1=s1, scalar2=None, op0=op0), reads, writes)
                    else:
                        P.op(eng, lambda e: e.tensor_scalar(out=out, in0=in0, scalar1=s1, scalar2=s2, op0=op0, op1=op1), reads, writes)

                def stt(eng, out, in0, scalar, in1, op0, op1, reads, writes):
                    P.op(eng, lambda e: e.scalar_tensor_tensor(out=out, in0=in0, scalar=scalar, in1=in1, op0=op0, op1=op1), reads, writes)

                def actf(out, in_, func, reads, writes, bias=None, scale=None):
                    kw = {}
                    if bias is not None:
                        kw["bias"] = bias
                    if scale is not None:
                        kw["scale"] = scale
                    P.op("act", lambda e: e.activation(out=out, in_=in_, func=func, **kw), reads, writes)

                def pcol(name, j):
                    o, w = PK[name]
                    return pk[:, o + j:o + j + 1]

                def lerp_chunk(cidx, dst_ap, dkey):
                    pb, pkey = project(cidx)
                    copy("act", praw[:, 1:G + 1], pb[:], [pkey], ["praw"])
                    copy("dve", praw[:, 0:1], prevcol[:, cidx:cidx + 1], ["prevcol"], ["praw"])
                    copy("dve", prevcol[:, cidx:cidx + 1], praw[:, G:G + 1], ["praw"], ["prevcol"])
                    tt("dve", coef[:], praw[:, 0:G], praw[:, 1:G + 1], ALU.subtract, ["praw"], ["coef"])
                    stt("dve", dst_ap, coef[:], pcol("mu", cidx), praw[:, 1:G + 1], ALU.mult, ALU.add, ["coef", "praw", "pk"], [dkey])

                rT, kmT = qn[:, 0, :], qn[:, 1, :]
                vT, gT = qr[:, 0, :], qr[:, 1, :]
                bT, At = gn[:, 0, :], gn[:, 1, :]
                Rt, Bt = qm[:, 0, :], qm[:, 1, :]
                Kt, Bb = gm[:, 0, :], gm[:, 1, :]
                Kb, kkn = pT[0][:], pT[1][:]
                aT, lin = kraw[:], nb[:]
                kf, yraw = acc[:, 0, :], acc[:, 1, :]
                Etmp, Esq = hb0[:, 0:G], hb0[:, G:2 * G]

                lerp_chunk(16, kf, "acc")
                actf(lin[0:64, :], kf[0:64, :], AF.Tanh, ["acc"], ["nb"])
                copy("dve", lin[64:128, :], kf[64:128, :], ["acc"], ["nb"])

                stop_at(40)
                for hp in range(4):
                    lerp_chunk(4 * hp + 0, rT, "qn")
                    lerp_chunk(4 * hp + 1, kf, "acc")
                    lerp_chunk(4 * hp + 2, vT, "qr")
                    lerp_chunk(4 * hp + 3, gT, "qr")
                    actf(gT, gT, AF.Silu, ["qr"], ["qr"])
                    hc = slice(hp * 128, (hp + 1) * 128)
                    stop_at(41)
                    for c in range(4):
                        reg = mi[:, c * 128:(c + 1) * 128]
                        mm(reg, lin[0:64, c * 128:(c + 1) * 128], loraup[0:64, hc], True, False, ["nb", "loraup"], ["mi"])
                        mm(reg, onesrow[:], w0hi[0:1, hc], False, False, ["onesrow", "w0hi"], ["mi"])
                        mm(reg, onesrow[:], w0lo[0:1, hc], False, True, ["onesrow", "w0lo"], ["mi"])
                    actf(sgtok[:].rearrange("p c f -> p (c f)"), mi[:], AF.Sigmoid, ["mi"], ["sgtok"])
                    for c in range(4):
                        mm(sc[0][:, c * 128:(c + 1) * 128], sgtok[:, c, :], maskUI2[:, 0, :], True, True, ["sgtok", "maskUI2"], ["sc0"])
                    for c in range(4):
                        mm(sc[1][:, c * 128:(c + 1) * 128], sgtok[:, c, :], maskU2[:, 0, :], True, True, ["sgtok", "maskU2"], ["sc1"])
                    stop_at(42)
                    mm(oa[0][:], loraup[64:128, hc], lin[64:128, :], True, True, ["nb", "loraup"], ["oa0"])
                    actf(aT, oa[0][:], AF.Sigmoid, ["oa0", "pk"], ["kraw"], bias=pcol("a0", hp))
                    ts("dve", coef[:], kf, pcol("kk", hp), None, ALU.mult, None, ["acc", "pk"], ["coef"])
                    actf(Esq, coef[:], AF.Square, ["coef"], ["hb0"])
                    mm(oa[1][:], blk1[:], Esq, True, True, ["blk1", "hb0"], ["oa1"])
                    ts("dve", praw[:, 1:G + 1], oa[1][:], 1e-24, None, ALU.max, None, ["oa1"], ["praw"])
                    actf(praw[:, 1:G + 1], praw[:, 1:G + 1], AF.Ln, ["praw"], ["praw"])
                    actf(praw[:, 1:G + 1], praw[:, 1:G + 1], AF.Exp, ["praw"], ["praw"], scale=-0.5)
                    tt("dve", kkn, coef[:], praw[:, 1:G + 1], ALU.mult, ["coef", "praw"], ["pT1"])
                    ts("dve", coef[:], aT, pcol("ka", hp), ka1m[:, hp:hp + 1], ALU.mult, ALU.add, ["kraw", "pk", "ka1m"], ["coef"])
                    tt("dve", kmT, kf, coef[:], ALU.mult, ["acc", "coef"], ["qn"])
                    tt("pool", bT, kkn, aT, ALU.mult, ["pT1", "kraw"], ["gn"])
                    if g == 0 and hp == 0:
                        dbg_out("rT", rT, "qn"); dbg_out("kmT", kmT, "qn"); dbg_out("vT", vT, "qr"); dbg_out("aT", aT, "kraw"); dbg_out("kkn", kkn, "pT1"); dbg_out("bT", bT, "gn")
                        copy("dve", coef[:], sc[0][:], ["sc0"], ["coef"]); dbg_out("cumI", coef[:], "coef")
                        copy("dve", coef[:], sc[1][:], ["sc1"], ["coef"]); dbg_out("cumE", coef[:], "coef")
                    stop_at(43)
                    actf(Etmp, sc[1][:], AF.Exp, ["sc1"], ["hb0"], scale=-C0)
                    stt("dve", At, Etmp, -1.0, kkn, ALU.mult, ALU.mult, ["hb0", "pT1"], ["gn"])
                    actf(Etmp, sc[0][:], AF.Exp, ["sc0"], ["hb0"], scale=-C0)
                    tt("dve", Rt, rT, Etmp, ALU.mult, ["qn", "hb0"], ["qm"])
                    actf(Esq, sc[0][:], AF.Exp, ["sc0"], ["hb0"], scale=C0)
                    tt("dve", Bt, bT, Esq, ALU.mult, ["gn", "hb0"], ["qm"])
                    tt("pool", Kt, kmT, Esq, ALU.mult, ["qn", "hb0"], ["gm"])
                    ts("dve", ncL[:], sc[0][:, 127:G:128], -C0, None, ALU.mult, None, ["sc0"], ["ncL"])
                    actf(WL[:], ncL[:], AF.Exp, ["ncL"], ["WL"])
                    copy("dve", WL0[:, 0:4], WL[0:64, :], ["WL"], ["WL0"])
                    copy("dve", WL0[:, 4:8], WL[64:128, :], ["WL"], ["WL0"])
                    for c in range(4):
                        actf(Etmp[:, c * 128:(c + 1) * 128], sc[0][:, c * 128:(c + 1) * 128], AF.Exp, ["sc0", "ncL"], ["hb0"], bias=ncL[:, c:c + 1], scale=C0)
                    tt("dve", Bb, bT, Etmp, ALU.mult, ["gn", "hb0"], ["gm"])
                    tt("pool", Kb, kmT, Etmp, ALU.mult, ["qn", "hb0"], ["pT0"])
                    stop_at(44)
                    for c in range(4):
                        cs = slice(c * 128, (c + 1) * 128)
                        half = (c % 2) * 512
                        for wi, (src, skey) in enumerate(((At, "gn"), (vT, "qr"), (Bb, "gm"), (Kb, "pT0"))):
                            P.op("pe", lambda e, src=src, cs=cs, half=half, wi=wi: e.transpose(out=tp[:, half + wi * 128:half + (wi + 1) * 128], in_=src[:, cs], identity=ident[:]), [skey, "ident"], ["tp"])
                        copy(ev_eng(), tokall[:, c, :, :], tp[:, half:half + 512].rearrange("p (w f) -> p w f", w=4), ["tp"], ["tokall"])

                    P.phase = "rwkv_chain"
                    stop_at(45)
                    for c in range(4):
                        cs = slice(c * 128, (c + 1) * 128)

                        def r2(bank):
                            return bank[:, 0:256].rearrange("p (e f) -> p e f", e=2)

                        def hs(e):
                            return slice(64 * e, 64 * e + 64)

                        for e in range(2):
                            mm(oa[0][:, e * 128:(e + 1) * 128], At[hs(e), cs], Bt[hs(e), cs], True, True, ["gn", "qm"], ["oa0"])
                        for e in range(2):
                            mm(oa[1][:, e * 128:(e + 1) * 128], Bt[hs(e), cs], At[hs(e), cs], True, True, ["gn", "qm"], ["oa1"])
                        tt("dve", Pm_[:], r2(oa[0]), maskL2[:], ALU.mult, ["oa0", "maskL2"], ["Pm"])
                        tt("dve", PTm[:], r2(oa[1]), maskU2[:], ALU.mult, ["oa1", "maskU2"], ["PTm"])
                        tt("pool", XTm[:], PTm[:], ident2[:], ALU.add, ["PTm", "ident2"], ["XTm"])
                        for lvl in range(1, 7):
                            for e in range(2):
                                mm(oa[0][:, e * 128:(e + 1) * 128], PTm[:, e, :], Pm_[:, e, :], True, True, ["PTm", "Pm"], ["oa0"])
                            if lvl < 6:
                                for e in range(2):
                                    mm(oa[1][:, e * 128:(e + 1) * 128], Pm_[:, e, :], PTm[:, e, :], True, True, ["PTm", "Pm"], ["oa1"])
                            copy("act", Pm_[:], r2(oa[0]), ["oa0"], ["Pm"])
                            if lvl < 6:
                                copy("dve", PTm[:], r2(oa[1]), ["oa1"], ["PTm"])
                            for e in range(2):
                                mm(mi[:, e * 128:(e + 1) * 128], Pm_[:, e, :], XTm[:, e, :], True, True, ["Pm", "XTm"], ["mi"])
                            tt("dve", XTm[:], XTm[:], r2(mi), ALU.add, ["XTm", "mi"], ["XTm"])
                        stop_at(46)
                        for e in range(2):
                            mm(oa[0][:, e * 128:(e + 1) * 128], Kt[hs(e), cs], At[hs(e), cs], True, True, ["gm", "gn"], ["oa0"])
                        tt("dve", AakT[:], r2(oa[0]), maskU2[:], ALU.mult, ["oa0", "maskU2"], ["AakT"])
                        for e in range(2):
                            mm(oa[1][:, e * 128:(e + 1) * 128], Bt[hs(e), cs], Rt[hs(e), cs], True, True, ["qm"], ["oa1"])
                        tt("dve", ArbT[:], r2(oa[1]), maskUI2[:], ALU.mult, ["oa1", "maskUI2"], ["ArbT"])
                        for e in range(2):
                            mm(mi[:, e * 128:(e + 1) * 128], Kt[hs(e), cs], Rt[hs(e), cs], True, True, ["gm", "qm"], ["mi"])
                        tt("dve", ArkT[:], r2(mi), maskUI2[:], ALU.mult, ["mi", "maskUI2"], ["ArkT"])
                        for e in range(2):
                            mm(oa[0][:, e * 64:(e + 1) * 64], AakT[:, e, :], tokall[:, c, 1, hs(e)], True, True, ["AakT", "tokall"], ["oa0"])
                        copy("act", AkV[:], oa[0][:, 0:128].rearrange("p (e f) -> p e f", e=2), ["oa0"], ["AkV"])
                        for e in range(2):
                            mm(oa[1][:, e * 64:(e + 1) * 64], XTm[:, e, :], tokall[:, c, 0, hs(e)], True, True, ["XTm", "tokall"], ["oa1"])
                        copy("dve", Ahat[:], oa[1][:, 0:128].rearrange("p (e f) -> p e f", e=2), ["oa1"], ["Ahat"])
                        for e in range(2):
                            mm(mi[:, e * 64:(e + 1) * 64], XTm[:, e, :], AkV[:, e, :], True, True, ["XTm", "AkV"], ["mi"])
                        copy("act", Uhat[:], mi[:, 0:128].rearrange("p (e f) -> p e f", e=2), ["mi"], ["Uhat"])
                        stop_at(47)
                        for e in range(2):
                            mm(oa[0][0:64, e * 128:(e + 1) * 128], Ahat[:, e, :], ArbT[:, e, :], True, False, ["Ahat", "ArbT"], ["oa0"])
                            mm(oa[0][0:64, e * 128:(e + 1) * 128], ident[hs(e), hs(e)], Rt[hs(e), cs], False, True, ["ident", "qm"], ["oa0"])
                        copy("dve", RhT[:], oa[0][0:64, 0:256].rearrange("p (e f) -> p e f", e=2), ["oa0"], ["RhT"])
                        for e in range(2):
                            reg = oa[1][0:64, e * 128:(e + 1) * 128]
                            mm(reg, STb[:, 2 * hp + e, :], RhT[:, e, :], True, False, ["STb", "RhT"], ["oa1"])
                            mm(reg, Uhat[:, e, :], ArbT[:, e, :], False, False, ["Uhat", "ArbT"], ["oa1"])
                            mm(reg, tokall[:, c, 1, hs(e)], ArkT[:, e, :], False, True, ["tokall", "ArkT"], ["oa1"])
                        copy("act", yraw[0:64, cs], oa[1][0:64, 0:128], ["oa1"], ["acc"])
                        copy("dve", yraw[64:128, cs], oa[1][0:64, 128:256], ["oa1"], ["acc"])
                        stop_at(48)
                        for e in range(2):
                            mm(mi[0:64, e * 64:(e + 1) * 64], Ahat[:, e, :], tokall[:, c, 2, hs(e)], True, True, ["Ahat", "tokall"], ["mi"])
                        for e in range(2):
                            stt("dve", Msb[:, e, :], ident[0:64, 0:64], WL0[:, 4 * e + c:4 * e + c + 1], mi[0:64, e * 64:(e + 1) * 64], ALU.mult, ALU.add, ["ident", "WL0", "mi"], ["Msb"])
                        for e in range(2):
                            reg = oa[0][0:64, e * 64:(e + 1) * 64]
                            mm(reg, Msb[:, e, :], STb[:, 2 * hp + e, :], True, False, ["Msb", "STb"], ["oa0"])
                            mm(reg, tokall[:, c, 2, hs(e)], Uhat[:, e, :], False, False, ["tokall", "Uhat"], ["oa0"])
                            mm(reg, tokall[:, c, 3, hs(e)], tokall[:, c, 1, hs(e)], False, True, ["tokall"], ["oa0"])
                        copy("dve", STb[:, 2 * hp:2 * hp + 2, :], oa[0][0:64, 0:128].rearrange("p (e f) -> p e f", e=2), ["oa0"], ["STb"])

                    if g == 0 and hp == 0:
                        dbg_out("yraw", yraw, "acc")
                    P.phase = "rwkv"
                    stop_at(49)
                    copy("act", Etmp, yraw, ["acc"], ["hb0"])
                    actf(Esq, yraw, AF.Square, ["acc"], ["hb0"])
                    mm(sc[0][:], blkavg[:], Etmp, True, True, ["blkavg", "hb0"], ["sc0"])
                    mm(sc[1][:], blkavg[:], Esq, True, True, ["blkavg", "hb0"], ["sc1"])
                    actf(coef[:], sc[0][:], AF.Square, ["sc0"], ["coef"])
                    tt("dve", coef[:], sc[1][:], coef[:], ALU.subtract, ["sc1", "coef"], ["coef"])
                    ts("dve", coef[:], coef[:], 64e-5, None, ALU.add, None, ["coef"], ["coef"])
                    actf(coef[:], coef[:], AF.Ln, ["coef"], ["coef"])
                    actf(coef[:], coef[:], AF.Exp, ["coef"], ["coef"], scale=-0.5)
                    tt("dve", yraw, yraw, sc[0][:], ALU.subtract, ["acc", "sc0"], ["acc"])
                    tt("dve", yraw, yraw, coef[:], ALU.mult, ["acc", "coef"], ["acc"])
                    ts("dve", yraw, yraw, pcol("lnw", hp), pcol("lnb", hp), ALU.mult, ALU.add, ["acc", "pk"], ["acc"])
                    tt("pool", Etmp, rT, kmT, ALU.mult, ["qn"], ["hb0"])
                    ts("dve", Esq, Etmp, pcol("rk", hp), None, ALU.mult, None, ["hb0", "pk"], ["hb0"])
                    mm(sc[0][:], blk1[:], Esq, True, True, ["blk1", "hb0"], ["sc0"])
                    tt("dve", coef[:], sc[0][:], vT, ALU.mult, ["sc0", "qr"], ["coef"])
                    tt("dve", yraw, yraw, coef[:], ALU.add, ["acc", "coef"], ["acc"])
                    tt("dve", yT[:, hp, :], yraw, gT, ALU.mult, ["acc", "qr"], ["yT"])

                P.phase = "mem"
                stop_at(6)
                for pr in range(2):
                    pb, pkey = project(25 + pr)
                    P.op("act", lambda e, pb=pb, pr=pr: e.activation(out=qm[:, pr, :], in_=pb[:], func=AF.Copy, scale=0.125), [pkey], ["qm"])
                    pb, pkey = project(27 + pr)
                    P.op("act", lambda e, pb=pb, pr=pr: e.activation(out=gm[:, pr, :], in_=pb[:], func=AF.Silu), [pkey], ["gm"])
                stop_at(61)
                for h in range(4):
                    pr, hb_ = h // 2, (h % 2) * 64
                    ob = oa[h % 2]
                    okey = "oa%d" % (h % 2)
                    for mc in range(2):
                        sb_ = sc[mc]
                        mm(sb_[:], memkT[hb_:hb_ + 64, pr, mc * 128:(mc + 1) * 128], qm[hb_:hb_ + 64, pr, :], True, True, ["memkT", "qm"], ["sc%d" % mc])
                        P.op("act", lambda e, sb_=sb_, mc=mc: e.activation(out=pT[mc][:], in_=sb_[:], func=AF.Exp), ["sc%d" % mc], ["pT%d" % mc])
                    stop_at(62 if h == 0 else 66)
                    for mc in range(2):
                        mm(ob[0:65, :], memvA[:, mc, h, :], pT[mc][:], mc == 0, mc == 1, ["memvA", "pT%d" % mc], [okey])
                    stop_at(63 if h == 0 else 67)
                    o, w = PK["memg"]
                    finalize_head(ob, okey, True, hb_, pk[hb_:hb_ + 64, o + pr:o + pr + 1], gm[hb_:hb_ + 64, pr, :], "gm", yT[hb_:hb_ + 64, 6 + pr, :], "yT")


                P.phase = "nsa"
                P.dma("sp", lambda e, t0=t0: e.dma_start(out=ropeC[:], in_=ropeC_d[:, t0:t0 + G]), writes=["ropeC"])
                P.dma("sp", lambda e, t0=t0: e.dma_start(out=ropeS[:], in_=ropeS_d[:, t0:t0 + G]), writes=["ropeS"])

                def rope(src_ap, skey, dst_ap, dkey):
                    mm(mi[:], pm[:], src_ap, True, True, ["pm", skey], ["mi"])
                    P.op("dve", lambda e: e.tensor_tensor(out=rtmp[:], in0=mi[:], in1=ropeS[:], op=ALU.mult), ["mi", "ropeS"], ["coef"])
                    P.op("pool", lambda e: e.tensor_tensor(out=dst_ap, in0=src_ap, in1=ropeC[:], op=ALU.mult), [skey, "ropeC"], [dkey])
                    P.op("dve", lambda e: e.tensor_tensor(out=dst_ap, in0=dst_ap, in1=rtmp[:], op=ALU.add), [dkey, "coef"], [dkey])

                for pr in range(2):
                    pb, pkey = project(17 + pr)
                    P.op("act", lambda e, pb=pb, pr=pr: e.activation(out=qn[:, pr, :], in_=pb[:], func=AF.Copy, scale=0.125), [pkey], ["qn"])
                    rope(qn[:, pr, :], "qn", qr[:, pr, :], "qr")
                    pb, pkey = project(19 + pr)
                    P.op("act", lambda e, pb=pb, pr=pr: e.activation(out=gn[:, pr, :], in_=pb[:], func=AF.Silu), [pkey], ["gn"])
                copy("dve", kcvc[:, 0:16], kcvc[:, G:G + 16], ["kcvc"], ["kcvc"])
                pb, pkey = project(21)
                copy("act", kcvc[:, 16:16 + G], pb[:], [pkey], ["kcvc"])
                pb, pkey = project(22)
                copy("act", kraw[:], pb[:], [pkey], ["kraw"])
                rope(kraw[:], "kraw", ksT[:, t0:t0 + G], "ksT")
                pb, pkey = project(23)
                copy("act", kraw[:], pb[:], [pkey], ["kraw"])
                wo = (g % 2) * G
                rope(kraw[:], "kraw", kwT[:, wo:wo + G], "kwT")
                for i in range(4):
                    pbv = pj[i % 2]
                    for k in range(8):
                        mm(pbv[:, 0:128], hT[:, k, i * 128:(i + 1) * 128], wb[:, k, 24 * 128:25 * 128], k == 0, k == 7, ["hT", "wb"], ["pj%d" % (i % 2)])
                    kt = 4 * g + i
                    copy("dve", vsA[:, kt, 0:64], pbv[:, 0:64], ["pj%d" % (i % 2)], ["vsA"])
                    copy("act", vwA[:, kt % 8, 0:64], pbv[:, 64:128], ["pj%d" % (i % 2)], ["vwA"])
                pb, pkey = project(29, 12)
                o_, w_ = PK["gateb"]
                P.op("act", lambda e, pb=pb, o_=o_: e.activation(out=gsig[:], in_=pb[0:12, :], func=AF.Sigmoid, bias=pk[0:12, o_:o_ + 1]), [pkey, "pk"], ["gsig"])

                lo, hi = max(0, 32 * g - 1), 32 * g + 30
                n = hi - lo + 1
                c0 = 16 * lo + 16 - t0
                for kv in range(2):
                    b0 = kv * 64
                    reg = mi[0:64, kv * 64:kv * 64 + n]
                    for l in range(32):
                        mm(reg, w1b[b0:b0 + 64, l, :], kcvc[b0:b0 + 64, c0 + l:c0 + l + 16 * (n - 1) + 1:16], l == 0, l == 31, ["w1b", "kcvc"], ["mi"])
                    dsth = (h1k, h1v)[kv]
                    P.op("act", lambda e, reg=reg, dsth=dsth, kv=kv, n=n: e.activation(out=dsth[:, 0:n], in_=reg, func=AF.Silu, bias=ccst[:, kv:kv + 1]), ["mi", "ccst"], ["h1%d" % kv])
                mm(sc[0][:, 0:n], w2b[:, 0:128], h1k[:, 0:n], True, True, ["w2b", "h10"], ["sc0"])
                copy("dve", kcmpT[:, lo:hi + 1], sc[0][:, 0:n], ["sc0"], ["kcmpT"])
                mm(sc[1][0:n, 0:64], h1v[:, 0:n], w2b[:, 128:192], True, True, ["w2b", "h11"], ["sc1"])
                copy("dve", vstg[0:n, 0:64], sc[1][0:n, 0:64], ["sc1"], ["vstg"])
                i0 = lo
                while i0 <= hi:
                    i1 = min(hi, (i0 // 128) * 128 + 127)
                    P.dma("sp", lambda e, i0=i0, i1=i1, lo=lo: e.dma_start(out=vcmpA[i0 % 128:i1 % 128 + 1, i0 // 128, :], in_=vstg[i0 - lo:i1 - lo + 1, :]), reads=["vstg"], writes=["vcmpA"])
                    i0 = i1 + 1

                def combine(ob, okey, h, b, first):
                    pr, hb_ = h // 2, (h % 2) * 64
                    sl = slice(hb_, hb_ + 64)
                    copy("dve", oab[:], ob[0:65, :], [okey], ["oab"])
                    copy("act", oaug[sl, :], ob[0:64, :], [okey], ["oaug"])
                    mm(pj[0][:], e64[:], oab[:], True, True, ["e64", "oab"], ["pj0"])
                    P.op("dve", lambda e: e.tensor_scalar(out=gsr[:], in0=gsig[:], scalar1=ident[0:12, 3 * h + b:3 * h + b + 1], scalar2=None, op0=ALU.mult), ["gsig", "ident"], ["gsr"])
                    mm(pj[1][:], ones12[:], gsr[:], True, True, ["ones12", "gsr"], ["pj1"])
                    P.op("dve", lambda e: e.tensor_scalar(out=coef[sl, :], in0=pj[0][sl, :], scalar1=1e-30, scalar2=None, op0=ALU.max), ["pj0"], ["coef"])
                    P.op("act", lambda e: e.activation(out=coef[sl, :], in_=coef[sl, :], func=AF.Ln), ["coef"], ["coef"])
                    P.op("act", lambda e: e.activation(out=coef[sl, :], in_=coef[sl, :], func=AF.Exp, scale=-1.0), ["coef"], ["coef"])
                    P.op("dve", lambda e: e.tensor_tensor(out=coef[sl, :], in0=coef[sl, :], in1=pj[1][sl, :], op=ALU.mult), ["coef", "pj1"], ["coef"])
                    if first:
                        P.op("dve", lambda e: e.tensor_tensor(out=acc[sl, pr, :], in0=coef[sl, :], in1=oaug[sl, :], op=ALU.mult), ["coef", "oaug"], ["acc"])
                    else:
                        P.op("dve", lambda e: e.tensor_tensor(out=coef[sl, :], in0=coef[sl, :], in1=oaug[sl, :], op=ALU.mult), ["coef", "oaug"], ["coef"])
                        P.op("dve", lambda e: e.tensor_tensor(out=acc[sl, pr, :], in0=acc[sl, pr, :], in1=coef[sl, :], op=ALU.add), ["coef", "acc"], ["acc"])

                def pmask(ap, key, base, cm, step):
                    P.op("pool", lambda e: e.affine_select(out=ap, in_=ap, pattern=[[step, G]], compare_op=ALU.is_ge, fill=0.0, base=base, channel_multiplier=cm), [key], [key])

                def pmask(ap, key, base, cm, step):
                    P.op("pool", lambda e: e.affine_select(out=ap, in_=ap, pattern=[[step, G]], compare_op=ALU.is_ge, fill=0.0, base=base, channel_multiplier=cm), [key], [key])

                P.op("pool", lambda e: e.memset(impS[:], 0.0), [], ["impS"])
                ncc = g // 4 + 1
                for h in range(4):
                    pr, hb_ = h // 2, (h % 2) * 64
                    ob, okey = oa[h % 2], "oa%d" % (h % 2)
                    for ci in range(ncc):
                        dl = 2048 * ci - 512 * g
                        sb_, skey = sc[ci % 2], "sc%d" % (ci % 2)
                        mm(sb_[:], kcmpT[hb_:hb_ + 64, ci * 128:(ci + 1) * 128], qn[hb_:hb_ + 64, pr, :], True, True, ["kcmpT", "qn"], [skey])
                        P.op("act", lambda e, sb_=sb_, ci=ci: e.activation(out=pTc[:, ci, :], in_=sb_[:], func=AF.Exp), [skey], ["wmkvb"])
                        if dl >= -2048:
                            pmask(pTc[:, ci, :], "wmkvb", -31 - dl, -16, 1)
                        mm(ob[0:65, :], vcmpA[:, ci, :], pTc[:, ci, :], ci == 0, ci == ncc - 1, ["vcmpA", "wmkvb"], [okey])
                    for j in range(4):
                        reg = mi[:, (j % 2) * 256:(j % 2) * 256 + 129]
                        for ci in range(ncc):
                            mm(reg, pTc[:, ci, j * 128:(j + 1) * 128], ovl[:, ci, :], ci == 0, ci == ncc - 1, ["wmkvb", "ovl"], ["mi"])
                        P.op("dve", lambda e, reg=reg: e.tensor_scalar(out=m8[:, 0:1], in0=reg[:, 128:129], scalar1=1e-30, scalar2=None, op0=ALU.max), ["mi"], ["m8"])
                        P.op("dve", lambda e: e.reciprocal(out=m8[:, 0:1], in_=m8[:, 0:1]), ["m8"], ["m8"])
                        P.op("dve", lambda e, reg=reg, j=j: e.scalar_tensor_tensor(out=impS[:, j, :], in0=reg[:, 0:128], scalar=m8[:, 0:1], in1=impS[:, j, :], op0=ALU.mult, op1=ALU.add), ["mi", "m8", "impS"], ["impS"])
                    combine(ob, okey, h, 0, True)

                for j in range(4):
                    qt = 4 * g + j
                    u0 = 126 - 2 * qt
                    P.op("dve", lambda e, j=j, u0=u0: e.tensor_tensor(out=score[:], in0=impS[:, j, :], in1=addw[:, u0:u0 + 128], op=ALU.add), ["impS", "addw"], ["score"])
                    P.op("dve", lambda e: e.tensor_scalar(out=score[:, 0:1], in0=score[:, 0:1], scalar1=1e4, scalar2=None, op0=ALU.add), ["score"], ["score"])
                    P.op("dve", lambda e: e.max(out=m8[:, 0:8], in_=score[:]), ["score"], ["m8"])
                    P.op("dve", lambda e: e.match_replace(out=stmp[:], in_to_replace=m8[:, 0:8], in_values=score[:], imm_value=-2.0), ["score", "m8"], ["stmp"])
                    P.op("dve", lambda e: e.max(out=m8[:, 8:16], in_=stmp[:]), ["stmp"], ["m8"])
                    P.op("dve", lambda e: e.tensor_reduce(out=m8[:, 0:1], in_=m8[:, 8:16], axis=AX.X, op=ALU.min), ["m8"], ["m8"])
                    P.op("dve", lambda e: e.tensor_scalar(out=stmp[:], in0=score[:], scalar1=m8[:, 0:1], scalar2=None, op0=ALU.is_ge), ["score", "m8"], ["stmp"])
                    P.op("dve", lambda e: e.tensor_scalar(out=nbq[:], in0=stmp[:], scalar1=1.0, scalar2=-NEG, op0=ALU.subtract, op1=ALU.mult), ["stmp"], ["nbq"])
                    P.op("pe", lambda e: e.transpose(out=tp[:, 0:128], in_=nbq[:], identity=ident[:]), ["nbq", "ident"], ["tp"])
                    copy("dve", nb[:, j * 128:(j + 1) * 128], tp[:, 0:128], ["tp"], ["nb"])
                copy("dve", nb2[:], nb[64:128, :], ["nb"], ["nb2"])

                P.phase = "nsa_sel"
                for h in range(4):
                    pr, hb_ = h // 2, (h % 2) * 64
                    ob, okey = oa[h % 2], "oa%d" % (h % 2)
                    nkt = 4 * g + 4

                    def sel_a(kt, pr=pr, hb_=hb_):
                        sb_, skey = sc[kt % 2], "sc%d" % (kt % 2)
                        mm(sb_[:], ksT[hb_:hb_ + 64, kt * 128:(kt + 1) * 128], qr[hb_:hb_ + 64, pr, :], True, False, ["ksT", "qr"], [skey])
                        m_ = (2 * kt) // 32
                        a2 = ((2 * kt) % 32) // 2
                        src, srck = (nb, "nb") if m_ < 2 else (nb2, "nb2")
                        pb_ = 32 * (m_ % 2)
                        mm(sb_[:], fsel[pb_:pb_ + 32, a2, :], src[pb_:pb_ + 32, :], False, True, ["fsel", srck], [skey])
                        P.op("act", lambda e: e.activation(out=pT[kt % 2][:], in_=sb_[:], func=AF.Exp), [skey], ["pT%d" % (kt % 2)])
                        if kt >= 4 * g:
                            pmask(pT[kt % 2][:], "pT%d" % (kt % 2), -128 * (kt - 4 * g), -1, 1)

                    sel_a(0)
                    for kt in range(nkt):
                        if kt + 1 < nkt:
                            sel_a(kt + 1)
                        mm(ob[0:65, :], vsA[:, kt, :], pT[kt % 2][:], kt == 0, kt == nkt - 1, ["vsA", "pT%d" % (kt % 2)], [okey])
                    combine(ob, okey, h, 1, False)

                P.phase = "nsa_win"
                for h in range(4):
                    pr, hb_ = h // 2, (h % 2) * 64
                    ob, okey = oa[h % 2], "oa%d" % (h % 2)
                    kts = [kt for kt in range(4 * g - 4, 4 * g + 4) if kt >= 0]

                    def win_a(kt, pr=pr, hb_=hb_):
                        sb_, skey = sc[kt % 2], "sc%d" % (kt % 2)
                        ro = (kt % 8) * 128
                        mm(sb_[:], kwT[hb_:hb_ + 64, ro:ro + 128], qr[hb_:hb_ + 64, pr, :], True, True, ["kwT", "qr"], [skey])
                        P.op("act", lambda e: e.activation(out=pT[kt % 2][:], in_=sb_[:], func=AF.Exp), [skey], ["pT%d" % (kt % 2)])
                        rel = kt - 4 * g
                        if rel >= 0:
                            pmask(pT[kt % 2][:], "pT%d" % (kt % 2), -128 * rel, -1, 1)
                        else:
                            pmask(pT[kt % 2][:], "pT%d" % (kt % 2), 511 + 128 * rel, 1, -1)

                    win_a(kts[0])
                    for ii, kt in enumerate(kts):
                        if ii + 1 < len(kts):
                            win_a(kts[ii + 1])
                        mm(ob[0:65, :], vwA[:, kt % 8, :], pT[kt % 2][:], ii == 0, ii == len(kts) - 1, ["vwA", "pT%d" % (kt % 2)], [okey])
                    combine(ob, okey, h, 2, False)

                P.phase = "nsa"
                o_, w_ = PK["nsag"]
                for h in range(4):
                    pr, hb_ = h // 2, (h % 2) * 64
                    finalize_head(acc[hb_:hb_ + 64, pr, :], "acc", False, hb_, pk[hb_:hb_ + 64, o_ + pr:o_ + pr + 1], gn[hb_:hb_ + 64, pr, :], "gn", yT[hb_:hb_ + 64, 4 + pr, :], "yT", from_sbuf=True)
                if g == 0:
                    dbg_out("yT", yT[:], "yT")
                P.phase = ""
                stop_at(7)
                for i in range(4):
                    xb, xk = xt[i % 2], "xt%d" % (i % 2)
                    P.dma("sp", lambda e, i=i, t0=t0, xb=xb: e.dma_start(out=xb[:], in_=x_d[t0 + i * 128:t0 + (i + 1) * 128, :]), writes=[xk])
                    for half in range(2):
                        pb = pj[half]
                        for k in range(8):
                            mm(pb[:], yT[:, k, i * 128:(i + 1) * 128], woutb[:, k, half * 512:(half + 1) * 512], k == 0, k == 7, ["yT", "woutb"], ["pj%d" % half])
                        P.op("dve", lambda e, pb=pb, xb=xb, half=half: e.tensor_tensor(out=xb[:, half * 512:(half + 1) * 512], in0=xb[:, half * 512:(half + 1) * 512], in1=pb[:], op=ALU.add),
                             ["pj%d" % half, xk], [xk])
                    sk = "st%d" % (8 + i)
                    ssq = st[:, 8 + i:9 + i]
                    hjk = hb[i % 2]
                    P.op("act", lambda e, xb=xb, ssq=ssq, hjk=hjk: e.activation(out=hjk[:], in_=xb[:], func=AF.Square, accum_out=ssq), [xk], ["hb0", sk])
                    P.op("dve", lambda e, ssq=ssq: e.tensor_scalar(out=ssq, in0=ssq, scalar1=1.0 / D, scalar2=1e-6, op0=ALU.mult, op1=ALU.add), [sk], [sk])
                    P.op("dve", lambda e, ssq=ssq: e.reciprocal(out=ssq, in_=ssq), [sk], [sk])
                    P.op("act", lambda e, ssq=ssq: e.activation(out=ssq, in_=ssq, func=AF.Sqrt), [sk], [sk])
                    P.op("dve", lambda e, xb=xb, ssq=ssq: e.scalar_tensor_tensor(out=xb[:], in0=xb[:], scalar=ssq, in1=gfin[:], op0=ALU.mult, op1=ALU.mult), [xk, sk, "gfin"], [xk])
                    P.dma("pool", lambda e, i=i, t0=t0, xb=xb: e.dma_start(out=out_d[t0 + i * 128:t0 + (i + 1) * 128, :], in_=xb[:]), reads=[xk], is_out=True)
        except _Stop:
            P.dma("pool", lambda e: e.dma_start(out=out_d[0:128, :], in_=xt[0][:]), reads=["xt0"], is_out=True)
        P.finish("sp")
        P.emit(block, sems, dsems)
    return nc


def make_in_maps(inputs, T, batches):
    ci = _colidx()
    w_in = np.asarray(inputs["w_in"][0])
    wcat = np.ascontiguousarray(w_in[:, ci].reshape(8, 128, NCOL))
    wout = np.ascontiguousarray(np.asarray(inputs["w_out"][0]).reshape(8, 128, D))
    wmkv = np.ascontiguousarray(np.asarray(inputs["w_mem_kv"][0]).reshape(8, 128, 512))
    pk = host_params({k: np.asarray(v) for k, v in inputs.items()})
    w1k = np.asarray(inputs["nsa_cmp_k_w1"][0]).reshape(32, 64, 64).transpose(1, 0, 2)
    w1v = np.asarray(inputs["nsa_cmp_v_w1"][0]).reshape(32, 64, 64).transpose(1, 0, 2)
    w1 = np.ascontiguousarray(np.concatenate([w1k, w1v], 0).reshape(128, 2048))
    w2k = np.asarray(inputs["nsa_cmp_k_w2"][0]); w2v = np.asarray(inputs["nsa_cmp_v_w2"][0])
    w2 = np.zeros((128, 192), np.float32)
    w2[0:64] = np.concatenate([w2k, w2k, w2v], 1)
    lora = np.ascontiguousarray(np.concatenate([np.asarray(inputs["rwkv_w_up"][0]), np.asarray(inputs["rwkv_a_up"][0])], 0))
    w0row = np.ascontiguousarray(np.asarray(inputs["rwkv_w0"][0]).reshape(1, 512))
    consts = host_consts(T)
    maps = []
    for b in batches:
        m = {
            "x": np.ascontiguousarray(np.asarray(inputs["x"][b][:T])),
            "mem": np.ascontiguousarray(np.asarray(inputs["mem"][b])),
            "wcat": wcat, "wout": wout, "wmkv": wmkv, "pk": pk, "w1": w1, "w2": w2, "lora": lora, "w0row": w0row,
            "gfin": np.ascontiguousarray(np.asarray(inputs["norm_final_g"]).reshape(1, D)),
        }
        m.update(consts)
        maps.append(m)
    return maps


_NC_CACHE = {}


def kernel(**inputs):
    T = inputs["x"].shape[1]
    B = inputs["x"].shape[0]
    if T not in _NC_CACHE:
        _NC_CACHE[T] = build_nc(T)
    nc = _NC_CACHE[T]
    batches = [i % B for i in range(8)]
    maps = make_in_maps(inputs, T, batches)
    res = run_bass_kernel_spmd(nc, maps, core_ids=list(range(8)))
    out = np.stack([res.results[b]["out"] for b in range(B)], axis=0)
    return out.astype(np.float32)
```

```python
import numpy as np
import ml_dtypes
from contextlib import ExitStack
import concourse.bass as bass
import concourse.mybir as mybir
from concourse.bass_utils import run_bass_kernel_spmd

F32 = mybir.dt.float32
BF16 = mybir.dt.bfloat16
ALU = mybir.AluOpType
AF = mybir.ActivationFunctionType
AX = mybir.AxisListType

ENGS = ("pe", "act", "dve", "pool", "sp")
N_DMA_SEMS = 24
import os as _os0
SAME_ENG_SYNC = _os0.environ.get("KSAME", "1") == "1"
NEG = -30000.0

D = 1024
NCOL = 29 * 128 + 12
G = 512
PSUM_PREFIX = ("pj", "sc", "oa", "mi", "tp")


class Prog:
    def __init__(self, nc):
        self.nc = nc
        self.lists = {e: [] for e in ENGS}
        self.cnt = {e: 0 for e in ENGS}
        self.seen = {e: {} for e in ENGS}
        self.clock_at = {e: [None] for e in ENGS}
        self.buf = {}
        self.dma_val = [0] * N_DMA_SEMS
        self.dma_clock = [[] for _ in range(N_DMA_SEMS)]
        self.dma_rr = 0
        self.out_toks = []
        self.phase = ""
        self.skip = set()
        self.children = {}

    def _deps(self, eng, reads, writes):
        need = {}

        def add(tok):
            if tok is None:
                return
            src, val = tok
            if src == eng and (eng == "pe" or not SAME_ENG_SYNC):
                return
            if need.get(src, 0) < val:
                need[src] = val

        def related(k):
            ks = [k]
            if "/" in k:
                ks.append(k.split("/")[0])
            else:
                ks.extend(self.children.get(k, ()))
            return ks

        for k0 in reads:
            for k in related(k0):
                st = self.buf.get(k)
                if st:
                    add(st[0])
                    if k[:2] in PSUM_PREFIX:
                        for r in st[1]:
                            if r[0] != eng:
                                add(r)
        for k0 in writes:
            for k in related(k0):
                st = self.buf.get(k)
                if st:
                    add(st[0])
                    for r in st[1]:
                        add(r)
        seen = self.seen[eng]
        waits = []
        for src, val in need.items():
            if seen.get(src, 0) >= val:
                continue
            waits.append((src, val))
            if isinstance(src, str):
                snap = self.clock_at[src][val]
            else:
                snap = self.dma_clock[src[1]][val // 16 - 1]
            for s2, v2 in snap.items():
                if seen.get(s2, 0) < v2:
                    seen[s2] = v2
            seen[src] = val
        return waits

    def _mark(self, tok, reads, writes):
        for k in list(reads) + list(writes):
            if "/" in k:
                self.children.setdefault(k.split("/")[0], set()).add(k)
        for k in reads:
            st = self.buf.setdefault(k, [None, []])
            st[1].append(tok)
            if len(st[1]) > 64:
                st[1] = st[1][-64:]
        for k in writes:
            self.buf[k] = [tok, []]
            if "/" not in k:
                for ch in self.children.get(k, ()):
                    self.buf[ch] = [tok, []]

    def op(self, eng, fn, reads=(), writes=(), self_wait=None):
        if self.phase in self.skip:
            return
        waits = self._deps(eng, reads, writes)
        if self_wait is not None and self.seen[eng].get(eng, 0) < self_wait:
            waits.append((eng, self_wait))
            self.seen[eng][eng] = self_wait
        self.cnt[eng] += 1
        c = self.cnt[eng]
        snap = dict(self.seen[eng])
        snap[eng] = c
        self.clock_at[eng].append(snap)
        self.lists[eng].append(("op", fn, waits, None))
        self._mark((eng, c), reads, writes)

    def dma(self, eng, fn, reads=(), writes=(), is_out=False):
        if self.phase in self.skip:
            return None
        s = self.dma_rr
        self.dma_rr = (self.dma_rr + 1) % N_DMA_SEMS
        waits = self._deps(eng, reads, writes)
        src = ("dma", s)
        prev = self.dma_val[s]
        if prev and self.seen[eng].get(src, 0) < prev:
            waits.append((src, prev))
            self.seen[eng][src] = prev
        val = prev + 16
        self.dma_val[s] = val
        self.dma_clock[s].append(dict(self.seen[eng]))
        self.lists[eng].append(("dma", fn, waits, (s, val)))
        self._mark((src, val), reads, writes)
        if is_out:
            self.out_toks.append((src, val))
        return (src, val)

    def finish(self, eng="sp"):
        waits = []
        for s in range(N_DMA_SEMS):
            if self.dma_val[s]:
                waits.append((("dma", s), self.dma_val[s]))
        self.lists[eng].append(("wait", None, waits, None))

    def emit(self, block, sems, dsems):
        engobj = {"pe": "tensor", "act": "scalar", "dve": "vector", "pool": "gpsimd", "sp": "sync"}

        def semof(src):
            return sems[src] if isinstance(src, str) else dsems[src[1]]

        def make(ename):
            lst = self.lists[ename]

            def body(e):
                for kind, fn, waits, extra in lst:
                    for src, val in waits:
                        e.wait_ge(semof(src), val)
                    if kind == "op":
                        fn(e).then_inc(sems[ename], 1)
                    elif kind == "dma":
                        fn(e).then_inc(dsems[extra[0]], 16)

            return body

        for ename in ENGS:
            if self.lists[ename]:
                getattr(block, engobj[ename])(make(ename))


RW, NSAB, MEMB = 0, 2176, 3084


def _colidx():
    idx = []
    for hp in range(4):
        for base in (0, 512, 1024, 1536):
            idx += list(range(base + 128 * hp, base + 128 * hp + 128))
    idx += list(range(2048, 2176))
    q0, g0 = NSAB, NSAB + 256
    idx += list(range(q0, q0 + 256))
    idx += list(range(g0, g0 + 256))
    kc, vc, ks, vs, kw, vw = (NSAB + 524 + 64 * i for i in range(6))
    idx += list(range(kc, kc + 64)) + list(range(vc, vc + 64))
    idx += list(range(ks, ks + 64)) * 2
    idx += list(range(kw, kw + 64)) * 2
    idx += list(range(vs, vs + 64)) + list(range(vw, vw + 64))
    idx += list(range(MEMB, MEMB + 512))
    idx += list(range(NSAB + 512, NSAB + 524))
    assert len(idx) == NCOL
    return np.array(idx)


def _bf(a):
    return np.ascontiguousarray(a.astype(ml_dtypes.bfloat16))


def host_consts(T):
    c = {}
    c["ident"] = _bf(np.eye(128, dtype=np.float32))
    half = 8
    inv = (np.float32(500000.0) ** (-np.arange(half, dtype=np.float32) / np.float32(half))).astype(np.float32)
    ang = np.arange(T, dtype=np.float32)[None, :] * inv[:, None]
    C = np.ones((64, T), np.float32)
    S = np.zeros((64, T), np.float32)
    C[0:8] = np.cos(ang); C[8:16] = np.cos(ang)
    S[0:8] = -np.sin(ang); S[8:16] = np.sin(ang)
    c["ropeC"] = np.ascontiguousarray(np.concatenate([C, C], 0))
    c["ropeS"] = np.ascontiguousarray(np.concatenate([S, S], 0))
    pm = np.zeros((128, 128), np.float32)
    for hb in (0, 64):
        for d in range(8):
            pm[hb + d + 8, hb + d] = 1.0
            pm[hb + d, hb + d + 8] = 1.0
    c["pm"] = _bf(pm)
    ovl = np.zeros((512, 129), np.float32)
    for i in range(511):
        for sblk in range(128):
            o = min(16 * i + 32, 64 * sblk + 64) - max(16 * i, 64 * sblk)
            if o > 0:
                ovl[i, sblk] = o / 32.0
    ovl[:, 128] = 1.0
    c["ovl"] = _bf(ovl.reshape(4, 128, 129).transpose(1, 0, 2))
    addw = np.zeros((128, 256), np.float32)
    for pp in range(128):
        cur = 1 if pp >= 64 else 0
        for u in range(256):
            sp = u - 126
            valid = sp <= cur
            forced = sp in (cur, cur - 1)
            addw[pp, u] = (0.0 if valid else -1.0) + (1e4 if forced else 0.0)
    c["addw"] = addw
    fsel = np.zeros((64, 16, 128), np.float32)
    for a2 in range(16):
        for key in range(128):
            r = 2 * a2 + (1 if key >= 64 else 0)
            fsel[r, a2, key] = 1.0
            fsel[32 + r, a2, key] = 1.0
    c["fsel"] = _bf(fsel)
    pp = np.arange(128)[:, None]
    ff = np.arange(128)[None, :]
    for nm, m in (("maskL2", pp > ff), ("maskU2", pp < ff), ("maskUI2", pp <= ff), ("ident2", pp == ff)):
        m = m.astype(np.float32)
        c[nm] = _bf(np.stack([m, m], 1))
    blk = (pp // 64 == ff // 64).astype(np.float32)
    c["blk1"] = _bf(blk)
    c["blkavg"] = _bf(blk / 64.0)
    return c


PK = {}
_o = 0
for _n, _w in (("gin", 8), ("gmem", 8), ("mu", 17), ("memg", 2), ("nsag", 2), ("gateb", 1), ("posT", 32), ("w0", 4), ("a0", 4), ("kk", 4), ("ka", 4), ("rk", 4), ("lnw", 4), ("lnb", 4)):
    PK[_n] = (_o, _w)
    _o += _w
NPK = _o


def host_params(inp):
    pk = np.zeros((128, NPK), np.float32)

    def put(name, arr):
        o, w = PK[name]
        pk[:, o:o + w] = arr

    put("gin", inp["norm_in_g"][0].reshape(8, 128).T)
    put("gmem", inp["mem_norm_g"][0].reshape(8, 128).T)
    ci = _colidx()
    put("mu", inp["rwkv_mu"][0][ci[:17 * 128]].reshape(17, 128).T)
    put("memg", inp["mem_out_g"][0].reshape(2, 128).T)
    put("nsag", inp["nsa_out_g"][0].reshape(2, 128).T)
    gb = np.zeros((128, 1), np.float32)
    gb[0:12, 0] = inp["nsa_gate_b"][0]
    put("gateb", gb)
    pt = inp["nsa_cmp_pos"][0].T
    put("posT", np.concatenate([pt, pt], 0))
    for nm, key in (("w0", "rwkv_w0"), ("a0", "rwkv_a0"), ("kk", "rwkv_k_k"), ("ka", "rwkv_k_a"), ("rk", "rwkv_r_k"), ("lnw", "rwkv_ln_w"), ("lnb", "rwkv_ln_b")):
        put(nm, inp[key][0].reshape(4, 128).T)
    return pk


def build_nc(T, dbg=None):
    NG = T // G
    NT = T // 128
    nc = bass.Bass("TRN2", target_bir_lowering=False)
    dram = {}

    def din(name, shape, dt=F32):
        dram[name] = nc.dram_tensor(name, list(shape), dt, kind="ExternalInput").ap()
        return dram[name]

    x_d = din("x", [T, D])
    mem_d = din("mem", [256, D])
    wcat_d = din("wcat", [8, 128, NCOL])
    wout_d = din("wout", [8, 128, D])
    wmkv_d = din("wmkv", [8, 128, 512])
    pk_d = din("pk", [128, NPK])
    gfin_d = din("gfin", [1, D])
    ident_d = din("ident", [128, 128], BF16)
    ropeC_d = din("ropeC", [128, T])
    ropeS_d = din("ropeS", [128, T])
    pm_d = din("pm", [128, 128], BF16)
    ovl_d = din("ovl", [128, 4, 129], BF16)
    addw_d = din("addw", [128, 256])
    fsel_d = din("fsel", [64, 16, 128], BF16)
    w1_d = din("w1", [128, 2048])
    w2_d = din("w2", [128, 192])
    lora_d = din("lora", [128, 512])
    w0row_d = din("w0row", [1, 512])
    maskL2_d = din("maskL2", [128, 2, 128], BF16)
    maskU2_d = din("maskU2", [128, 2, 128], BF16)
    maskUI2_d = din("maskUI2", [128, 2, 128], BF16)
    ident2_d = din("ident2", [128, 2, 128], BF16)
    blk1_d = din("blk1", [128, 128], BF16)
    blkavg_d = din("blkavg", [128, 128], BF16)
    out_d = nc.dram_tensor("out", [T, D], F32, kind="ExternalOutput").ap()
    dbg_d = {}
    if dbg:
        for n, (shape, dt) in dbg.items():
            dbg_d[n] = nc.dram_tensor("dbg_" + n, list(shape), dt, kind="ExternalOutput").ap()

    with ExitStack() as es:
        def sb(name, shape, dt=F32):
            return es.enter_context(nc.sbuf_tensor("sb_" + name, list(shape), dt))

        def ps(name, shape, dt=F32):
            return es.enter_context(nc.psum_tensor("ps_" + name, list(shape), dt))

        wb = sb("wb", [128, 8, NCOL], BF16)
        woutb = sb("woutb", [128, 8, D], BF16)
        wmkvb = sb("wmkvb", [128, 8, 512], BF16)
        pk = sb("pk", [128, NPK])
        gfin = sb("gfin", [128, D])
        ident = sb("ident", [128, 128], BF16)
        xt = [sb("xt%d" % i, [128, D]) for i in range(2)]
        hb0 = sb("hb0", [128, D], BF16)
        hb = [hb0, hb0]
        hT = sb("hT", [128, 8, G], BF16)
        st = sb("st", [128, 16])
        yT = sb("yT", [128, 8, G], BF16)
        memkT = sb("memkT", [128, 2, 256], BF16)
        memvA = sb("memvA", [128, 2, 4, 65], BF16)
        qm = sb("qm", [128, 2, G], BF16)
        gm = sb("gm", [128, 2, G], BF16)
        pT = [sb("pT%d" % i, [128, G], BF16) for i in range(2)]
        oaug = sb("oaug", [128, G], BF16)
        osq = sb("osq", [65, G], BF16)
        cvec = sb("cvec", [65, 128], BF16)
        pm = sb("pm", [128, 128], BF16)
        ovl = sb("ovl", [128, 4, 129], BF16)
        addw = sb("addw", [128, 256])
        fsel = sb("fsel", [64, 16, 128], BF16)
        ones12 = sb("ones12", [12, 128], BF16)
        gsr = sb("gsr", [12, G], BF16)
        e64 = sb("e64", [65, 128], BF16)
        w1b = sb("w1b", [128, 32, 64], BF16)
        w2b = sb("w2b", [64, 192], BF16)
        posTb = sb("posTb", [128, 32], BF16)
        ccst = sb("ccst", [64, 2])
        ropeC = sb("ropeC", [128, G])
        ropeS = sb("ropeS", [128, G])
        qn = sb("qn", [128, 2, G], BF16)
        qr = sb("qr", [128, 2, G], BF16)
        gn = sb("gn", [128, 2, G], BF16)
        kcvc = sb("kcvc", [128, 16 + G], BF16)
        kraw = sb("kraw", [128, G], BF16)
        ksT = sb("ksT", [128, T], BF16)
        kwT = sb("kwT", [128, 1024], BF16)
        vsA = sb("vsA", [128, NT, 65], BF16)
        vwA = sb("vwA", [128, 8, 65], BF16)
        kcmpT = sb("kcmpT", [128, 512], BF16)
        vcmpA = sb("vcmpA", [128, 4, 65], BF16)
        vstg = sb("vstg", [32, 65], BF16)
        h1k = sb("h1k", [64, 32], BF16)
        h1v = sb("h1v", [64, 32], BF16)
        gsig = sb("gsig", [12, G], BF16)
        impS = sb("impS", [128, 4, 128])
        score = sb("score", [128, 128])
        stmp = sb("stmp", [128, 128])
        m8 = sb("m8", [128, 16])
        nbq = sb("nbq", [128, 128], BF16)
        nb = sb("nb", [128, G], BF16)
        nb2 = sb("nb2", [64, G], BF16)
        oab = sb("oab", [65, G], BF16)
        coef = sb("coef", [128, G])
        scl = coef
        rtmp = coef
        acc = sb("acc", [128, 2, G])
        praw = sb("praw", [128, G + 1])
        prevcol = sb("prevcol", [128, 17])
        loraup = sb("loraup", [128, 512], BF16)
        w0hi = sb("w0hi", [1, 512], BF16)
        w0lo = sb("w0lo", [1, 512], BF16)
        onesrow = sb("onesrow", [1, 128], BF16)
        ka1m = sb("ka1m", [128, 4])
        maskL2 = sb("maskL2", [128, 2, 128], BF16)
        maskU2 = sb("maskU2", [128, 2, 128], BF16)
        maskUI2 = sb("maskUI2", [128, 2, 128], BF16)
        ident2 = sb("ident2", [128, 2, 128], BF16)
        blk1 = sb("blk1", [128, 128], BF16)
        blkavg = sb("blkavg", [128, 128], BF16)
        sgtok = sb("sgtok", [128, 4, 128], BF16)
        AakT = sb("AakT", [128, 2, 128], BF16)
        ArbT = sb("ArbT", [128, 2, 128], BF16)
        ArkT = sb("ArkT", [128, 2, 128], BF16)
        AkV = sb("AkV", [128, 2, 64], BF16)
        Ahat = sb("Ahat", [128, 2, 64], BF16)
        Uhat = sb("Uhat", [128, 2, 64], BF16)
        RhT = sb("RhT", [64, 2, 128], BF16)
        Msb = sb("Msb", [64, 2, 64], BF16)
        STb = sb("STb", [64, 8, 64], BF16)
        ncL = sb("ncL", [128, 4])
        WL = sb("WL", [128, 4])
        WL0 = sb("WL0", [64, 8])
        pj = [ps("pj%d" % i, [128, 512]) for i in range(2)]
        sc = [ps("sc%d" % i, [128, 512]) for i in range(2)]
        oa = [ps("oa%d" % i, [128, 512]) for i in range(2)]
        mi = ps("mi", [128, 512])
        tp = ps("tp", [128, 1024], BF16)

        pTc = wmkvb[:, 0:4, :]
        sems = {e: es.enter_context(nc.semaphore("s_" + e)) for e in ENGS}
        dsems = [es.enter_context(nc.semaphore("d%d" % i)) for i in range(N_DMA_SEMS)]
        block = es.enter_context(nc.Block())
        P = Prog(nc)
        rr = {"ev": 0}

        def ev_eng():
            rr["ev"] ^= 1
            return "act" if rr["ev"] else "dve"

        def copy(eng, out, in_, reads, writes):
            if eng == "act":
                P.op("act", lambda e: e.copy(out=out, in_=in_), reads, writes)
            else:
                P.op(eng, lambda e: e.tensor_copy(out=out, in_=in_), reads, writes)

        pe_last = {}

        def mm(out, lhsT, rhs, start, stop, reads, writes):
            base, rows = lhsT.base_partition(), lhsT.shape[0]
            sw = None
            for k in writes:
                last = pe_last.get(k)
                if last is not None:
                    c0, b0, r0 = last
                    if b0 + r0 <= base or base + rows <= b0:
                        sw = max(sw or 0, c0)
            P.op("pe", lambda e: e.matmul(out, lhsT, rhs, start=start, stop=stop), reads, writes, self_wait=sw)
            for k in writes:
                pe_last[k] = (P.cnt["pe"], base, rows)

        def dbg_out(name, ap, key):
            if dbg and name in dbg_d:
                P.dma("sp", lambda e: e.dma_start(out=dbg_d[name], in_=ap), reads=[key])

        import os as _os
        KSTOP = int(_os.environ.get("KSTOP", "99"))
        KSKIP = _os.environ.get("KSKIP", "")
        P.skip = set(KSKIP.split(",")) if KSKIP else set()

        class _Stop(Exception):
            pass

        def stop_at(n):
            if KSTOP == n:
                raise _Stop()

        try:
            P.dma("sp", lambda e: e.dma_start(out=pk[:], in_=pk_d[:, :]), writes=["pk"])
            P.dma("sp", lambda e: e.dma_start(out=ident[:], in_=ident_d[:, :]), writes=["ident"])
            P.dma("sp", lambda e: e.dma_start(out=gfin[:], in_=gfin_d[0:1, :].broadcast_to([128, D])), writes=["gfin"])

            def pkc(name, j=0, rows=128):
                o, w = PK[name]
                return pk[0:rows, o + j:o + j + 1]

            def load_cast(dst3, src_d, ncols, gname, key):
                i = 0
                for k in range(8):
                    for c0 in range(0, ncols, 1024):
                        cw = min(1024, ncols - c0)
                        stg = xt[i % 2]
                        skey = "xt%d" % (i % 2)
                        P.dma("sp" if i % 2 == 0 else "pool",
                              lambda e, stg=stg, k=k, c0=c0, cw=cw: e.dma_start(out=stg[:, 0:cw], in_=src_d[k, :, c0:c0 + cw]),
                              writes=[skey])
                        eng = ev_eng()
                        o = dst3[:, k, c0:c0 + cw]
                        if gname is None:
                            copy(eng, o, stg[:, 0:cw], [skey], [key])
                        elif eng == "act":
                            P.op("act", lambda e, o=o, stg=stg, cw=cw, k=k: e.activation(out=o, in_=stg[:, 0:cw], func=AF.Copy, scale=pkc(gname, k)),
                                 [skey, "pk"], [key])
                        else:
                            P.op("dve", lambda e, o=o, stg=stg, cw=cw, k=k: e.tensor_scalar(out=o, in0=stg[:, 0:cw], scalar1=pkc(gname, k), scalar2=None, op0=ALU.mult),
                                 [skey, "pk"], [key])
                        i += 1

            stop_at(1)
            load_cast(wmkvb, wmkv_d, 512, "gmem", "wmkvb")
            stop_at(2)
            load_cast(wb, wcat_d, NCOL, "gin", "wb")
            load_cast(woutb, wout_d, D, None, "woutb")

            P.op("pool", lambda e: e.memset(cvec[0:64, :], 1.0 / 64), writes=["cvec"])
            P.op("pool", lambda e: e.memset(cvec[64:65, :], 1e-6), writes=["cvec"])

            def norm_transpose(src_tile, skey, dstT, dkey, col0, slot):
                h = hb[slot]
                hk = "hb0"
                ssq = st[:, slot:slot + 1]
                P.op("act", lambda e: e.activation(out=h[:], in_=src_tile[:], func=AF.Square, accum_out=ssq), [skey], [hk, "st%d" % slot])
                P.op("dve", lambda e: e.tensor_scalar(out=ssq, in0=ssq, scalar1=1.0 / D, scalar2=1e-6, op0=ALU.mult, op1=ALU.add), ["st%d" % slot], ["st%d" % slot])
                P.op("dve", lambda e: e.reciprocal(out=ssq, in_=ssq), ["st%d" % slot], ["st%d" % slot])
                P.op("act", lambda e: e.activation(out=ssq, in_=ssq, func=AF.Sqrt), ["st%d" % slot], ["st%d" % slot])
                P.op("dve", lambda e: e.tensor_scalar(out=h[:], in0=src_tile[:], scalar1=ssq, scalar2=None, op0=ALU.mult), [skey, "st%d" % slot], [hk])
                for k in range(8):
                    P.op("pe", lambda e, k=k: e.transpose(out=tp[:, k * 128:(k + 1) * 128], in_=h[:, k * 128:(k + 1) * 128], identity=ident[:]), [hk, "ident"], ["tp"])
                copy(ev_eng(), dstT[:, :, col0:col0 + 128], tp[:].rearrange("p (k t) -> p k t", k=8), ["tp"], [dkey])


            for nm, dst, src in (("pm", pm, pm_d), ("ovl", ovl, ovl_d), ("addw", addw, addw_d),
                                 ("fsel", fsel, fsel_d)):
                P.dma("sp", lambda e, dst=dst, src=src: e.dma_start(out=dst[:], in_=src), writes=[nm])
            P.op("pool", lambda e: e.memset(ones12[:], 1.0), writes=["ones12"])
            P.op("pool", lambda e: e.memset(e64[:], 0.0), writes=["e64"])
            P.op("pool", lambda e: e.memset(e64[64:65, :], 1.0), writes=["e64"])
            P.op("pool", lambda e: e.memset(kcmpT[:], 0.0), writes=["kcmpT"])
            P.op("pool", lambda e: e.memset(vcmpA[:], 0.0), writes=["vcmpA"])
            P.op("pool", lambda e: e.memset(vsA[:], 1.0), writes=["vsA"])
            P.op("pool", lambda e: e.memset(vwA[:], 1.0), writes=["vwA"])
            P.op("pool", lambda e: e.memset(vstg[:], 1.0), writes=["vstg"])
            P.op("pool", lambda e: e.memset(kcvc[:], 0.0), writes=["kcvc"])
            for hlf in range(2):
                P.dma("sp", lambda e, hlf=hlf: e.dma_start(out=xt[hlf][:], in_=w1_d[:, hlf * 1024:(hlf + 1) * 1024]), writes=["xt%d" % hlf])
                copy(ev_eng(), w1b[:, hlf * 16:(hlf + 1) * 16, :], xt[hlf][:].rearrange("p (l e) -> p l e", e=64), ["xt%d" % hlf], ["w1b"])
            P.dma("sp", lambda e: e.dma_start(out=xt[0][:, 0:192], in_=w2_d[:, :]), writes=["xt0"])
            copy("dve", w2b[:], xt[0][0:64, 0:192], ["xt0"], ["w2b"])
            o_, w_ = PK["posT"]
            copy("dve", posTb[:], pk[:, o_:o_ + 32], ["pk"], ["posTb"])
            for kv in range(2):
                b0 = kv * 64
                for l in range(32):
                    mm(mi[0:64, kv:kv + 1], w1b[b0:b0 + 64, l, :], posTb[b0:b0 + 64, l:l + 1], l == 0, l == 31, ["w1b", "posTb"], ["mi"])
                copy("dve", ccst[:, kv:kv + 1], mi[0:64, kv:kv + 1], ["mi"], ["ccst"])

            for nm, dst, src in (("maskL2", maskL2, maskL2_d), ("maskU2", maskU2, maskU2_d), ("maskUI2", maskUI2, maskUI2_d),
                                 ("ident2", ident2, ident2_d), ("blk1", blk1, blk1_d), ("blkavg", blkavg, blkavg_d)):
                P.dma("sp", lambda e, dst=dst, src=src: e.dma_start(out=dst[:], in_=src), writes=[nm])
            P.dma("sp", lambda e: e.dma_start(out=xt[1][:, 0:512], in_=lora_d[:, :]), writes=["xt1"])
            copy("dve", loraup[:], xt[1][:, 0:512], ["xt1"], ["loraup"])
            P.dma("sp", lambda e: e.dma_start(out=praw[0:1, 0:512], in_=w0row_d[:, :]), writes=["praw"])
            copy("dve", w0hi[:], praw[0:1, 0:512], ["praw"], ["w0hi"])
            P.op("dve", lambda e: e.tensor_tensor(out=praw[0:1, 0:512], in0=praw[0:1, 0:512], in1=w0hi[:], op=ALU.subtract), ["praw", "w0hi"], ["praw"])
            copy("dve", w0lo[:], praw[0:1, 0:512], ["praw"], ["w0lo"])
            P.op("pool", lambda e: e.memset(onesrow[:], 1.0), writes=["onesrow"])
            P.op("pool", lambda e: e.memset(prevcol[:], 0.0), writes=["prevcol"])
            P.op("pool", lambda e: e.memset(STb[:], 0.0), writes=["STb"])
            o_, w_ = PK["ka"]
            P.op("dve", lambda e, o_=o_: e.tensor_scalar(out=ka1m[:], in0=pk[:, o_:o_ + 4], scalar1=-1.0, scalar2=1.0, op0=ALU.mult, op1=ALU.add), ["pk"], ["ka1m"])
            tokall = wmkvb[:, 4:8, :].rearrange("p c (w f) -> p c w f", w=4)
            stop_at(3)
            for mt in range(2):
                P.dma("sp", lambda e, mt=mt: e.dma_start(out=xt[mt][:], in_=mem_d[mt * 128:(mt + 1) * 128, :]), writes=["xt%d" % mt])
                norm_transpose(xt[mt], "xt%d" % mt, hT, "hT", mt * 128, mt)
            stop_at(4)
            for pr in range(2):
                for k in range(8):
                    mm(pj[0][:, 0:256], wmkvb[:, k, pr * 128:(pr + 1) * 128], hT[:, k, 0:256], k == 0, k == 7, ["wmkvb", "hT"], ["pj0"])
                copy(ev_eng(), memkT[:, pr, :], pj[0][:, 0:256], ["pj0"], ["memkT"])
            P.op("pool", lambda e: e.memset(memvA[:], 1.0), writes=["memvA"])
            for mt in range(2):
                for k in range(8):
                    mm(pj[1][:, 0:256], hT[:, k, mt * 128:(mt + 1) * 128], wmkvb[:, k, 256:512], k == 0, k == 7, ["wmkvb", "hT"], ["pj1"])
                copy(ev_eng(), memvA[:, mt, :, 0:64], pj[1][:, 0:256].rearrange("p (h d) -> p h d", h=4), ["pj1"], ["memvA"])

            def finalize_head(o_ps, okey, use_den, hb_, gcol_ap, gate_ap, gate_key, dst_ap, dst_key, from_sbuf=False):
                sl = slice(hb_, hb_ + 64)
                if from_sbuf:
                    copy("dve", oaug[sl, :], o_ps, [okey], ["oaug"])
                    P.op("act", lambda e: e.activation(out=osq[0:64, :], in_=o_ps, func=AF.Square), [okey], ["osq"])
                    P.op("pool", lambda e: e.memset(osq[64:65, :], 1.0), [], ["osq"])
                elif use_den:
                    copy("dve", oaug[sl, :], o_ps[0:64, :], [okey], ["oaug"])
                    P.op("act", lambda e: e.activation(out=osq[:], in_=o_ps[0:65, :], func=AF.Square), [okey], ["osq"])
                else:
                    copy("dve", oaug[sl, :], o_ps[0:64, :], [okey], ["oaug"])
                    P.op("act", lambda e: e.activation(out=osq[0:64, :], in_=o_ps[0:64, :], func=AF.Square), [okey], ["osq"])
                    P.op("pool", lambda e: e.memset(osq[64:65, :], 1.0), [], ["osq"])
                mm(mi[:], cvec[:], osq[:], True, True, ["cvec", "osq"], ["mi"])
                P.op("act", lambda e: e.activation(out=scl[sl, :], in_=mi[sl, :], func=AF.Ln), ["mi"], ["coef"])
                P.op("act", lambda e: e.activation(out=scl[sl, :], in_=scl[sl, :], func=AF.Exp, scale=-0.5), ["coef"], ["coef"])
                P.op("dve", lambda e: e.scalar_tensor_tensor(out=scl[sl, :], in0=scl[sl, :], scalar=gcol_ap, in1=oaug[sl, :], op0=ALU.mult, op1=ALU.mult), ["coef", "oaug", "pk"], ["coef"])
                P.op("dve", lambda e: e.tensor_tensor(out=dst_ap, in0=scl[sl, :], in1=gate_ap, op=ALU.mult), ["coef", gate_key], [dst_key])

            P.op("pool", lambda e: e.memset(wmkvb[:, 4:8, :], 0.0), ["wmkvb"], ["wmkvb", "tokall"])
            stop_at(5)
            for g in range(NG):
                t0 = g * G
                for i in range(4):
                    P.dma("sp", lambda e, i=i, t0=t0: e.dma_start(out=xt[i % 2][:], in_=x_d[t0 + i * 128:t0 + (i + 1) * 128, :]), writes=["xt%d" % (i % 2)])
                    norm_transpose(xt[i % 2], "xt%d" % (i % 2), hT, "hT", i * 128, i % 2)

                def project(chunk, width=128):
                    pb = pj[chunk % 2]
                    for k in range(8):
                        mm(pb[0:width, :], wb[:, k, chunk * 128:chunk * 128 + width], hT[:, k, :], k == 0, k == 7, ["wb", "hT"], ["pj%d" % (chunk % 2)])
                    return pb, "pj%d" % (chunk % 2)


                P.phase = "rwkv"
                C0 = float(np.exp(-0.5))

                def tt(eng, out, in0, in1, op, reads, writes):
                    P.op(eng, lambda e: e.tensor_tensor(out=out, in0=in0, in1=in1, op=op), reads, writes)

                def ts(eng, out, in0, s1, s2, op0, op1, reads, writes):
                    if s2 is None:
                        P.op(eng, lambda e: e.tensor_scalar(out=out, in0=in0, scalar1=s1, scalar2=None, op0=op0), reads, writes)
                    else:
                        P.op(eng, lambda e: e.tensor_scalar(out=out, in0=in0, scalar1=s1, scalar2=s2, op0=op0, op1=op1), reads, writes)

                def stt(eng, out, in0, scalar, in1, op0, op1, reads, writes):
                    P.op(eng, lambda e: e.scalar_tensor_tensor(out=out, in0=in0, scalar=scalar, in1=in1, op0=op0, op1=op1), reads, writes)

                def actf(out, in_, func, reads, writes, bias=None, scale=None):
                    kw = {}
                    if bias is not None:
                        kw["bias"] = bias
                    if scale is not None:
                        kw["scale"] = scale
                    P.op("act", lambda e: e.activation(out=out, in_=in_, func=func, **kw), reads, writes)

                def pcol(name, j):
                    o, w = PK[name]
                    return pk[:, o + j:o + j + 1]

                def lerp_chunk(cidx, dst_ap, dkey):
                    pb, pkey = project(cidx)
                    copy("act", praw[:, 1:G + 1], pb[:], [pkey], ["praw"])
                    copy("dve", praw[:, 0:1], prevcol[:, cidx:cidx + 1], ["prevcol"], ["praw"])
                    copy("dve", prevcol[:, cidx:cidx + 1], praw[:, G:G + 1], ["praw"], ["prevcol"])
                    tt("dve", coef[:], praw[:, 0:G], praw[:, 1:G + 1], ALU.subtract, ["praw"], ["coef"])
                    stt("dve", dst_ap, coef[:], pcol("mu", cidx), praw[:, 1:G + 1], ALU.mult, ALU.add, ["coef", "praw", "pk"], [dkey])

                rT, kmT = qn[:, 0, :], qn[:, 1, :]
                vT, gT = qr[:, 0, :], qr[:, 1, :]
                bT, At = gn[:, 0, :], gn[:, 1, :]
                Rt, Bt = qm[:, 0, :], qm[:, 1, :]
                Kt, Bb = gm[:, 0, :], gm[:, 1, :]
                Kb, kkn = pT[0][:], pT[1][:]
                aT, lin = kraw[:], nb[:]
                kf, yraw = acc[:, 0, :], acc[:, 1, :]
                Etmp, Esq = hb0[:, 0:G], hb0[:, G:2 * G]

                lerp_chunk(16, kf, "acc")
                actf(lin[0:64, :], kf[0:64, :], AF.Tanh, ["acc"], ["nb"])
                copy("dve", lin[64:128, :], kf[64:128, :], ["acc"], ["nb"])

                stop_at(40)
                for hp in range(4):
                    lerp_chunk(4 * hp + 0, rT, "qn")
                    lerp_chunk(4 * hp + 1, kf, "acc")
                    lerp_chunk(4 * hp + 2, vT, "qr")
                    lerp_chunk(4 * hp + 3, gT, "qr")
                    actf(gT, gT, AF.Silu, ["qr"], ["qr"])
                    hc = slice(hp * 128, (hp + 1) * 128)
                    stop_at(41)
                    for c in range(4):
                        reg = mi[:, c * 128:(c + 1) * 128]
                        mm(reg, lin[0:64, c * 128:(c + 1) * 128], loraup[0:64, hc], True, False, ["nb", "loraup"], ["mi"])
                        mm(reg, onesrow[:], w0hi[0:1, hc], False, False, ["onesrow", "w0hi"], ["mi"])
                        mm(reg, onesrow[:], w0lo[0:1, hc], False, True, ["onesrow", "w0lo"], ["mi"])
                    actf(sgtok[:].rearrange("p c f -> p (c f)"), mi[:], AF.Sigmoid, ["mi"], ["sgtok"])
                    for c in range(4):
                        mm(sc[0][:, c * 128:(c + 1) * 128], sgtok[:, c, :], maskUI2[:, 0, :], True, True, ["sgtok", "maskUI2"], ["sc0"])
                    for c in range(4):
                        mm(sc[1][:, c * 128:(c + 1) * 128], sgtok[:, c, :], maskU2[:, 0, :], True, True, ["sgtok", "maskU2"], ["sc1"])
                    stop_at(42)
                    mm(oa[0][:], loraup[64:128, hc], lin[64:128, :], True, True, ["nb", "loraup"], ["oa0"])
                    actf(aT, oa[0][:], AF.Sigmoid, ["oa0", "pk"], ["kraw"], bias=pcol("a0", hp))
                    ts("dve", coef[:], kf, pcol("kk", hp), None, ALU.mult, None, ["acc", "pk"], ["coef"])
                    actf(Esq, coef[:], AF.Square, ["coef"], ["hb0"])
                    mm(oa[1][:], blk1[:], Esq, True, True, ["blk1", "hb0"], ["oa1"])
                    ts("dve", praw[:, 1:G + 1], oa[1][:], 1e-24, None, ALU.max, None, ["oa1"], ["praw"])
                    actf(praw[:, 1:G + 1], praw[:, 1:G + 1], AF.Ln, ["praw"], ["praw"])
                    actf(praw[:, 1:G + 1], praw[:, 1:G + 1], AF.Exp, ["praw"], ["praw"], scale=-0.5)
                    tt("dve", kkn, coef[:], praw[:, 1:G + 1], ALU.mult, ["coef", "praw"], ["pT1"])
                    ts("dve", coef[:], aT, pcol("ka", hp), ka1m[:, hp:hp + 1], ALU.mult, ALU.add, ["kraw", "pk", "ka1m"], ["coef"])
                    tt("dve", kmT, kf, coef[:], ALU.mult, ["acc", "coef"], ["qn"])
                    tt("pool", bT, kkn, aT, ALU.mult, ["pT1", "kraw"], ["gn"])
                    if g == 0 and hp == 0:
                        dbg_out("rT", rT, "qn"); dbg_out("kmT", kmT, "qn"); dbg_out("vT", vT, "qr"); dbg_out("aT", aT, "kraw"); dbg_out("kkn", kkn, "pT1"); dbg_out("bT", bT, "gn")
                        copy("dve", coef[:], sc[0][:], ["sc0"], ["coef"]); dbg_out("cumI", coef[:], "coef")
                        copy("dve", coef[:], sc[1][:], ["sc1"], ["coef"]); dbg_out("cumE", coef[:], "coef")
                    stop_at(43)
                    actf(Etmp, sc[1][:], AF.Exp, ["sc1"], ["hb0"], scale=-C0)
                    stt("dve", At, Etmp, -1.0, kkn, ALU.mult, ALU.mult, ["hb0", "pT1"], ["gn"])
                    actf(Etmp, sc[0][:], AF.Exp, ["sc0"], ["hb0"], scale=-C0)
                    tt("dve", Rt, rT, Etmp, ALU.mult, ["qn", "hb0"], ["qm"])
                    actf(Esq, sc[0][:], AF.Exp, ["sc0"], ["hb0"], scale=C0)
                    tt("dve", Bt, bT, Esq, ALU.mult, ["gn", "hb0"], ["qm"])
                    tt("pool", Kt, kmT, Esq, ALU.mult, ["qn", "hb0"], ["gm"])
                    ts("dve", ncL[:], sc[0][:, 127:G:128], -C0, None, ALU.mult, None, ["sc0"], ["ncL"])
                    actf(WL[:], ncL[:], AF.Exp, ["ncL"], ["WL"])
                    copy("dve", WL0[:, 0:4], WL[0:64, :], ["WL"], ["WL0"])
                    copy("dve", WL0[:, 4:8], WL[64:128, :], ["WL"], ["WL0"])
                    for c in range(4):
                        actf(Etmp[:, c * 128:(c + 1) * 128], sc[0][:, c * 128:(c + 1) * 128], AF.Exp, ["sc0", "ncL"], ["hb0"], bias=ncL[:, c:c + 1], scale=C0)
                    tt("dve", Bb, bT, Etmp, ALU.mult, ["gn", "hb0"], ["gm"])
                    tt("pool", Kb, kmT, Etmp, ALU.mult, ["qn", "hb0"], ["pT0"])
                    stop_at(44)
                    for c in range(4):
                        cs = slice(c * 128, (c + 1) * 128)
                        half = (c % 2) * 512
                        for wi, (src, skey) in enumerate(((At, "gn"), (vT, "qr"), (Bb, "gm"), (Kb, "pT0"))):
                            P.op("pe", lambda e, src=src, cs=cs, half=half, wi=wi: e.transpose(out=tp[:, half + wi * 128:half + (wi + 1) * 128], in_=src[:, cs], identity=ident[:]), [skey, "ident"], ["tp"])
                        copy(ev_eng(), tokall[:, c, :, :], tp[:, half:half + 512].rearrange("p (w f) -> p w f", w=4), ["tp"], ["tokall"])

                    P.phase = "rwkv_chain"
                    stop_at(45)

                    def r2(bank):
                        return bank[:, 0:256].rearrange("p (e f) -> p e f", e=2)

                    def hs(e):
                        return slice(64 * e, 64 * e + 64)

                    def v4(ap):
                        return ap.rearrange("p (c e f) -> p c e f", c=2, e=2)

                    def bc4(m2):
                        return m2[:, 0:1, :].broadcast_to([128, 4, 128]).rearrange("p (c e) f -> p c e f", c=2)

                    PmH, PmK = [hb0[:, 0:G], hb0[:, G:2 * G]], ["hb0/0", "hb0/1"]
                    PTH, PTK = [pT[0][:], pT[1][:]], ["pT0", "pT1"]
                    XTH, XTK = [gn[:, 0, :], kraw[:]], ["gn/0", "kraw"]
                    bkP = [(oa[0], "oa0"), (oa[1], "oa1")]
                    bkT = [(pj[0], "pj0"), (pj[1], "pj1")]
                    bkX = [(mi, "mi"), (sc[0], "sc0")]

                    def rg(bank, cl, e):
                        o = (cl * 2 + e) * 128
                        return bank[:, o:o + 128]

                    for h2 in range(2):
                        for cl in range(2):
                            cs = slice((2 * h2 + cl) * 128, (2 * h2 + cl + 1) * 128)
                            for e in range(2):
                                mm(rg(bkP[h2][0], cl, e), At[hs(e), cs], Bt[hs(e), cs], True, True, ["gn/1", "qm"], [bkP[h2][1]])
                        for cl in range(2):
                            cs = slice((2 * h2 + cl) * 128, (2 * h2 + cl + 1) * 128)
                            for e in range(2):
                                mm(rg(bkT[h2][0], cl, e), Bt[hs(e), cs], At[hs(e), cs], True, True, ["gn/1", "qm"], [bkT[h2][1]])
                    for h2 in range(2):
                        tt("dve", v4(PmH[h2]), v4(bkP[h2][0][:]), bc4(maskL2), ALU.mult, [bkP[h2][1], "maskL2"], [PmK[h2]])
                        tt("dve", v4(PTH[h2]), v4(bkT[h2][0][:]), bc4(maskU2), ALU.mult, [bkT[h2][1], "maskU2"], [PTK[h2]])
                        tt("pool", v4(XTH[h2]), v4(PTH[h2]), bc4(ident2), ALU.add, [PTK[h2], "ident2"], [XTK[h2]])
                    for lvl in range(1, 7):
                        for h2 in range(2):
                            for cl in range(2):
                                for e in range(2):
                                    mm(rg(bkP[h2][0], cl, e), v4(PTH[h2])[:, cl, e, :], v4(PmH[h2])[:, cl, e, :], True, True, [PTK[h2], PmK[h2]], [bkP[h2][1]])
                            if lvl < 6:
                                for cl in range(2):
                                    for e in range(2):
                                        mm(rg(bkT[h2][0], cl, e), v4(PmH[h2])[:, cl, e, :], v4(PTH[h2])[:, cl, e, :], True, True, [PTK[h2], PmK[h2]], [bkT[h2][1]])
                        for h2 in range(2):
                            copy("act", PmH[h2], bkP[h2][0][:], [bkP[h2][1]], [PmK[h2]])
                            if lvl < 6:
                                copy("dve" if h2 == 0 else "act", PTH[h2], bkT[h2][0][:], [bkT[h2][1]], [PTK[h2]])
                        for h2 in range(2):
                            for cl in range(2):
                                for e in range(2):
                                    mm(rg(bkX[h2][0], cl, e), v4(PmH[h2])[:, cl, e, :], v4(XTH[h2])[:, cl, e, :], True, True, [PmK[h2], XTK[h2]], [bkX[h2][1]])
                        for h2 in range(2):
                            tt("dve", XTH[h2], XTH[h2], bkX[h2][0][:], ALU.add, [XTK[h2], bkX[h2][1]], [XTK[h2]])

                    for c in range(4):
                        cs = slice(c * 128, (c + 1) * 128)
                        XTm = v4(XTH[c // 2])[:, c % 2, :, :]
                        xkey = XTK[c // 2]
                        stop_at(46)
                        for e in range(2):
                            mm(oa[0][:, e * 128:(e + 1) * 128], Kt[hs(e), cs], At[hs(e), cs], True, True, ["gm", "gn"], ["oa0"])
                        tt("dve", AakT[:], r2(oa[0]), maskU2[:], ALU.mult, ["oa0", "maskU2"], ["AakT"])
                        for e in range(2):
                            mm(oa[1][:, e * 128:(e + 1) * 128], Bt[hs(e), cs], Rt[hs(e), cs], True, True, ["qm"], ["oa1"])
                        tt("dve", ArbT[:], r2(oa[1]), maskUI2[:], ALU.mult, ["oa1", "maskUI2"], ["ArbT"])
                        for e in range(2):
                            mm(mi[:, e * 128:(e + 1) * 128], Kt[hs(e), cs], Rt[hs(e), cs], True, True, ["gm", "qm"], ["mi"])
                        tt("dve", ArkT[:], r2(mi), maskUI2[:], ALU.mult, ["mi", "maskUI2"], ["ArkT"])
                        for e in range(2):
                            mm(oa[0][:, e * 64:(e + 1) * 64], AakT[:, e, :], tokall[:, c, 1, hs(e)], True, True, ["AakT", "tokall"], ["oa0"])
                        copy("act", AkV[:], oa[0][:, 0:128].rearrange("p (e f) -> p e f", e=2), ["oa0"], ["AkV"])
                        for e in range(2):
                            mm(oa[1][:, e * 64:(e + 1) * 64], XTm[:, e, :], tokall[:, c, 0, hs(e)], True, True, [xkey, "tokall"], ["oa1"])
                        copy("dve", Ahat[:], oa[1][:, 0:128].rearrange("p (e f) -> p e f", e=2), ["oa1"], ["Ahat"])
                        for e in range(2):
                            mm(mi[:, e * 64:(e + 1) * 64], XTm[:, e, :], AkV[:, e, :], True, True, [xkey, "AkV"], ["mi"])
                        copy("act", Uhat[:], mi[:, 0:128].rearrange("p (e f) -> p e f", e=2), ["mi"], ["Uhat"])
                        stop_at(47)
                        for e in range(2):
                            mm(oa[0][0:64, e * 128:(e + 1) * 128], Ahat[:, e, :], ArbT[:, e, :], True, False, ["Ahat", "ArbT"], ["oa0"])
                            mm(oa[0][0:64, e * 128:(e + 1) * 128], ident[hs(e), hs(e)], Rt[hs(e), cs], False, True, ["ident", "qm"], ["oa0"])
                        copy("dve", RhT[:], oa[0][0:64, 0:256].rearrange("p (e f) -> p e f", e=2), ["oa0"], ["RhT"])
                        for e in range(2):
                            reg = oa[1][0:64, e * 128:(e + 1) * 128]
                            mm(reg, STb[:, 2 * hp + e, :], RhT[:, e, :], True, False, ["STb", "RhT"], ["oa1"])
                            mm(reg, Uhat[:, e, :], ArbT[:, e, :], False, False, ["Uhat", "ArbT"], ["oa1"])
                            mm(reg, tokall[:, c, 1, hs(e)], ArkT[:, e, :], False, True, ["tokall", "ArkT"], ["oa1"])
                        copy("act", yraw[0:64, cs], oa[1][0:64, 0:128], ["oa1"], ["acc"])
                        copy("dve", yraw[64:128, cs], oa[1][0:64, 128:256], ["oa1"], ["acc"])
                        stop_at(48)
                        for e in range(2):
                            mm(mi[0:64, e * 64:(e + 1) * 64], Ahat[:, e, :], tokall[:, c, 2, hs(e)], True, True, ["Ahat", "tokall"], ["mi"])
                        for e in range(2):
                            stt("dve", Msb[:, e, :], ident[0:64, 0:64], WL0[:, 4 * e + c:4 * e + c + 1], mi[0:64, e * 64:(e + 1) * 64], ALU.mult, ALU.add, ["ident", "WL0", "mi"], ["Msb"])
                        for e in range(2):
                            reg = oa[0][0:64, e * 64:(e + 1) * 64]
                            mm(reg, Msb[:, e, :], STb[:, 2 * hp + e, :], True, False, ["Msb", "STb"], ["oa0"])
                            mm(reg, tokall[:, c, 2, hs(e)], Uhat[:, e, :], False, False, ["tokall", "Uhat"], ["oa0"])
                            mm(reg, tokall[:, c, 3, hs(e)], tokall[:, c, 1, hs(e)], False, True, ["tokall"], ["oa0"])
                        copy("dve", STb[:, 2 * hp:2 * hp + 2, :], oa[0][0:64, 0:128].rearrange("p (e f) -> p e f", e=2), ["oa0"], ["STb"])

                    if g == 0 and hp == 0:
                        dbg_out("yraw", yraw, "acc")
                    P.phase = "rwkv"
                    stop_at(49)
                    copy("act", Etmp, yraw, ["acc"], ["hb0"])
                    actf(Esq, yraw, AF.Square, ["acc"], ["hb0"])
                    mm(sc[0][:], blkavg[:], Etmp, True, True, ["blkavg", "hb0"], ["sc0"])
                    mm(sc[1][:], blkavg[:], Esq, True, True, ["blkavg", "hb0"], ["sc1"])
                    actf(coef[:], sc[0][:], AF.Square, ["sc0"], ["coef"])
                    tt("dve", coef[:], sc[1][:], coef[:], ALU.subtract, ["sc1", "coef"], ["coef"])
                    ts("dve", coef[:], coef[:], 64e-5, None, ALU.add, None, ["coef"], ["coef"])
                    actf(coef[:], coef[:], AF.Ln, ["coef"], ["coef"])
                    actf(coef[:], coef[:], AF.Exp, ["coef"], ["coef"], scale=-0.5)
                    tt("dve", yraw, yraw, sc[0][:], ALU.subtract, ["acc", "sc0"], ["acc"])
                    tt("dve", yraw, yraw, coef[:], ALU.mult, ["acc", "coef"], ["acc"])
                    ts("dve", yraw, yraw, pcol("lnw", hp), pcol("lnb", hp), ALU.mult, ALU.add, ["acc", "pk"], ["acc"])
                    tt("pool", Etmp, rT, kmT, ALU.mult, ["qn"], ["hb0"])
                    ts("dve", Esq, Etmp, pcol("rk", hp), None, ALU.mult, None, ["hb0", "pk"], ["hb0"])
                    mm(sc[0][:], blk1[:], Esq, True, True, ["blk1", "hb0"], ["sc0"])
                    tt("dve", coef[:], sc[0][:], vT, ALU.mult, ["sc0", "qr"], ["coef"])
                    tt("dve", yraw, yraw, coef[:], ALU.add, ["acc", "coef"], ["acc"])
                    tt("dve", yT[:, hp, :], yraw, gT, ALU.mult, ["acc", "qr"], ["yT"])

                P.phase = "mem"
                stop_at(6)
                for pr in range(2):
                    pb, pkey = project(25 + pr)
                    P.op("act", lambda e, pb=pb, pr=pr: e.activation(out=qm[:, pr, :], in_=pb[:], func=AF.Copy, scale=0.125), [pkey], ["qm"])
                    pb, pkey = project(27 + pr)
                    P.op("act", lambda e, pb=pb, pr=pr: e.activation(out=gm[:, pr, :], in_=pb[:], func=AF.Silu), [pkey], ["gm"])
                stop_at(61)
                for h in range(4):
                    pr, hb_ = h // 2, (h % 2) * 64
                    ob = oa[h % 2]
                    okey = "oa%d" % (h % 2)
                    for mc in range(2):
                        sb_ = sc[mc]
                        mm(sb_[:], memkT[hb_:hb_ + 64, pr, mc * 128:(mc + 1) * 128], qm[hb_:hb_ + 64, pr, :], True, True, ["memkT", "qm"], ["sc%d" % mc])
                        P.op("act", lambda e, sb_=sb_, mc=mc: e.activation(out=pT[mc][:], in_=sb_[:], func=AF.Exp), ["sc%d" % mc], ["pT%d" % mc])
                    stop_at(62 if h == 0 else 66)
                    for mc in range(2):
                        mm(ob[0:65, :], memvA[:, mc, h, :], pT[mc][:], mc == 0, mc == 1, ["memvA", "pT%d" % mc], [okey])
                    stop_at(63 if h == 0 else 67)
                    o, w = PK["memg"]
                    finalize_head(ob, okey, True, hb_, pk[hb_:hb_ + 64, o + pr:o + pr + 1], gm[hb_:hb_ + 64, pr, :], "gm", yT[hb_:hb_ + 64, 6 + pr, :], "yT")


                P.phase = "nsa"
                P.dma("sp", lambda e, t0=t0: e.dma_start(out=ropeC[:], in_=ropeC_d[:, t0:t0 + G]), writes=["ropeC"])
                P.dma("sp", lambda e, t0=t0: e.dma_start(out=ropeS[:], in_=ropeS_d[:, t0:t0 + G]), writes=["ropeS"])

                def rope(src_ap, skey, dst_ap, dkey):
                    mm(mi[:], pm[:], src_ap, True, True, ["pm", skey], ["mi"])
                    P.op("dve", lambda e: e.tensor_tensor(out=rtmp[:], in0=mi[:], in1=ropeS[:], op=ALU.mult), ["mi", "ropeS"], ["coef"])
                    P.op("pool", lambda e: e.tensor_tensor(out=dst_ap, in0=src_ap, in1=ropeC[:], op=ALU.mult), [skey, "ropeC"], [dkey])
                    P.op("dve", lambda e: e.tensor_tensor(out=dst_ap, in0=dst_ap, in1=rtmp[:], op=ALU.add), [dkey, "coef"], [dkey])

                for pr in range(2):
                    pb, pkey = project(17 + pr)
                    P.op("act", lambda e, pb=pb, pr=pr: e.activation(out=qn[:, pr, :], in_=pb[:], func=AF.Copy, scale=0.125), [pkey], ["qn"])
                    rope(qn[:, pr, :], "qn", qr[:, pr, :], "qr")
                    pb, pkey = project(19 + pr)
                    P.op("act", lambda e, pb=pb, pr=pr: e.activation(out=gn[:, pr, :], in_=pb[:], func=AF.Silu), [pkey], ["gn"])
                copy("dve", kcvc[:, 0:16], kcvc[:, G:G + 16], ["kcvc"], ["kcvc"])
                pb, pkey = project(21)
                copy("act", kcvc[:, 16:16 + G], pb[:], [pkey], ["kcvc"])
                pb, pkey = project(22)
                copy("act", kraw[:], pb[:], [pkey], ["kraw"])
                rope(kraw[:], "kraw", ksT[:, t0:t0 + G], "ksT")
                pb, pkey = project(23)
                copy("act", kraw[:], pb[:], [pkey], ["kraw"])
                wo = (g % 2) * G
                rope(kraw[:], "kraw", kwT[:, wo:wo + G], "kwT")
                for i in range(4):
                    pbv = pj[i % 2]
                    for k in range(8):
                        mm(pbv[:, 0:128], hT[:, k, i * 128:(i + 1) * 128], wb[:, k, 24 * 128:25 * 128], k == 0, k == 7, ["hT", "wb"], ["pj%d" % (i % 2)])
                    kt = 4 * g + i
                    copy("dve", vsA[:, kt, 0:64], pbv[:, 0:64], ["pj%d" % (i % 2)], ["vsA"])
                    copy("act", vwA[:, kt % 8, 0:64], pbv[:, 64:128], ["pj%d" % (i % 2)], ["vwA"])
                pb, pkey = project(29, 12)
                o_, w_ = PK["gateb"]
                P.op("act", lambda e, pb=pb, o_=o_: e.activation(out=gsig[:], in_=pb[0:12, :], func=AF.Sigmoid, bias=pk[0:12, o_:o_ + 1]), [pkey, "pk"], ["gsig"])

                lo, hi = max(0, 32 * g - 1), 32 * g + 30
                n = hi - lo + 1
                c0 = 16 * lo + 16 - t0
                for kv in range(2):
                    b0 = kv * 64
                    reg = mi[0:64, kv * 64:kv * 64 + n]
                    for l in range(32):
                        mm(reg, w1b[b0:b0 + 64, l, :], kcvc[b0:b0 + 64, c0 + l:c0 + l + 16 * (n - 1) + 1:16], l == 0, l == 31, ["w1b", "kcvc"], ["mi"])
                    dsth = (h1k, h1v)[kv]
                    P.op("act", lambda e, reg=reg, dsth=dsth, kv=kv, n=n: e.activation(out=dsth[:, 0:n], in_=reg, func=AF.Silu, bias=ccst[:, kv:kv + 1]), ["mi", "ccst"], ["h1%d" % kv])
                mm(sc[0][:, 0:n], w2b[:, 0:128], h1k[:, 0:n], True, True, ["w2b", "h10"], ["sc0"])
                copy("dve", kcmpT[:, lo:hi + 1], sc[0][:, 0:n], ["sc0"], ["kcmpT"])
                mm(sc[1][0:n, 0:64], h1v[:, 0:n], w2b[:, 128:192], True, True, ["w2b", "h11"], ["sc1"])
                copy("dve", vstg[0:n, 0:64], sc[1][0:n, 0:64], ["sc1"], ["vstg"])
                i0 = lo
                while i0 <= hi:
                    i1 = min(hi, (i0 // 128) * 128 + 127)
                    P.dma("sp", lambda e, i0=i0, i1=i1, lo=lo: e.dma_start(out=vcmpA[i0 % 128:i1 % 128 + 1, i0 // 128, :], in_=vstg[i0 - lo:i1 - lo + 1, :]), reads=["vstg"], writes=["vcmpA"])
                    i0 = i1 + 1

                def combine(ob, okey, h, b, first):
                    pr, hb_ = h // 2, (h % 2) * 64
                    sl = slice(hb_, hb_ + 64)
                    copy("dve", oab[:], ob[0:65, :], [okey], ["oab"])
                    copy("act", oaug[sl, :], ob[0:64, :], [okey], ["oaug"])
                    mm(pj[0][:], e64[:], oab[:], True, True, ["e64", "oab"], ["pj0"])
                    P.op("dve", lambda e: e.tensor_scalar(out=gsr[:], in0=gsig[:], scalar1=ident[0:12, 3 * h + b:3 * h + b + 1], scalar2=None, op0=ALU.mult), ["gsig", "ident"], ["gsr"])
                    mm(pj[1][:], ones12[:], gsr[:], True, True, ["ones12", "gsr"], ["pj1"])
                    P.op("dve", lambda e: e.tensor_scalar(out=coef[sl, :], in0=pj[0][sl, :], scalar1=1e-30, scalar2=None, op0=ALU.max), ["pj0"], ["coef"])
                    P.op("act", lambda e: e.activation(out=coef[sl, :], in_=coef[sl, :], func=AF.Ln), ["coef"], ["coef"])
                    P.op("act", lambda e: e.activation(out=coef[sl, :], in_=coef[sl, :], func=AF.Exp, scale=-1.0), ["coef"], ["coef"])
                    P.op("dve", lambda e: e.tensor_tensor(out=coef[sl, :], in0=coef[sl, :], in1=pj[1][sl, :], op=ALU.mult), ["coef", "pj1"], ["coef"])
                    if first:
                        P.op("dve", lambda e: e.tensor_tensor(out=acc[sl, pr, :], in0=coef[sl, :], in1=oaug[sl, :], op=ALU.mult), ["coef", "oaug"], ["acc"])
                    else:
                        P.op("dve", lambda e: e.tensor_tensor(out=coef[sl, :], in0=coef[sl, :], in1=oaug[sl, :], op=ALU.mult), ["coef", "oaug"], ["coef"])
                        P.op("dve", lambda e: e.tensor_tensor(out=acc[sl, pr, :], in0=acc[sl, pr, :], in1=coef[sl, :], op=ALU.add), ["coef", "acc"], ["acc"])

                def pmask(ap, key, base, cm, step):
                    P.op("pool", lambda e: e.affine_select(out=ap, in_=ap, pattern=[[step, G]], compare_op=ALU.is_ge, fill=0.0, base=base, channel_multiplier=cm), [key], [key])

                def pmask(ap, key, base, cm, step):
                    P.op("pool", lambda e: e.affine_select(out=ap, in_=ap, pattern=[[step, G]], compare_op=ALU.is_ge, fill=0.0, base=base, channel_multiplier=cm), [key], [key])

                P.op("pool", lambda e: e.memset(impS[:], 0.0), [], ["impS"])
                ncc = g // 4 + 1
                for h in range(4):
                    pr, hb_ = h // 2, (h % 2) * 64
                    ob, okey = oa[h % 2], "oa%d" % (h % 2)
                    for ci in range(ncc):
                        dl = 2048 * ci - 512 * g
                        sb_, skey = sc[ci % 2], "sc%d" % (ci % 2)
                        mm(sb_[:], kcmpT[hb_:hb_ + 64, ci * 128:(ci + 1) * 128], qn[hb_:hb_ + 64, pr, :], True, True, ["kcmpT", "qn"], [skey])
                        P.op("act", lambda e, sb_=sb_, ci=ci: e.activation(out=pTc[:, ci, :], in_=sb_[:], func=AF.Exp), [skey], ["wmkvb"])
                        if dl >= -2048:
                            pmask(pTc[:, ci, :], "wmkvb", -31 - dl, -16, 1)
                        mm(ob[0:65, :], vcmpA[:, ci, :], pTc[:, ci, :], ci == 0, ci == ncc - 1, ["vcmpA", "wmkvb"], [okey])
                    for j in range(4):
                        reg = mi[:, (j % 2) * 256:(j % 2) * 256 + 129]
                        for ci in range(ncc):
                            mm(reg, pTc[:, ci, j * 128:(j + 1) * 128], ovl[:, ci, :], ci == 0, ci == ncc - 1, ["wmkvb", "ovl"], ["mi"])
                        P.op("dve", lambda e, reg=reg: e.tensor_scalar(out=m8[:, 0:1], in0=reg[:, 128:129], scalar1=1e-30, scalar2=None, op0=ALU.max), ["mi"], ["m8"])
                        P.op("dve", lambda e: e.reciprocal(out=m8[:, 0:1], in_=m8[:, 0:1]), ["m8"], ["m8"])
                        P.op("dve", lambda e, reg=reg, j=j: e.scalar_tensor_tensor(out=impS[:, j, :], in0=reg[:, 0:128], scalar=m8[:, 0:1], in1=impS[:, j, :], op0=ALU.mult, op1=ALU.add), ["mi", "m8", "impS"], ["impS"])
                    combine(ob, okey, h, 0, True)

                for j in range(4):
                    qt = 4 * g + j
                    u0 = 126 - 2 * qt
                    P.op("dve", lambda e, j=j, u0=u0: e.tensor_tensor(out=score[:], in0=impS[:, j, :], in1=addw[:, u0:u0 + 128], op=ALU.add), ["impS", "addw"], ["score"])
                    P.op("dve", lambda e: e.tensor_scalar(out=score[:, 0:1], in0=score[:, 0:1], scalar1=1e4, scalar2=None, op0=ALU.add), ["score"], ["score"])
                    P.op("dve", lambda e: e.max(out=m8[:, 0:8], in_=score[:]), ["score"], ["m8"])
                    P.op("dve", lambda e: e.match_replace(out=stmp[:], in_to_replace=m8[:, 0:8], in_values=score[:], imm_value=-2.0), ["score", "m8"], ["stmp"])
                    P.op("dve", lambda e: e.max(out=m8[:, 8:16], in_=stmp[:]), ["stmp"], ["m8"])
                    P.op("dve", lambda e: e.tensor_reduce(out=m8[:, 0:1], in_=m8[:, 8:16], axis=AX.X, op=ALU.min), ["m8"], ["m8"])
                    P.op("dve", lambda e: e.tensor_scalar(out=stmp[:], in0=score[:], scalar1=m8[:, 0:1], scalar2=None, op0=ALU.is_ge), ["score", "m8"], ["stmp"])
                    P.op("dve", lambda e: e.tensor_scalar(out=nbq[:], in0=stmp[:], scalar1=1.0, scalar2=-NEG, op0=ALU.subtract, op1=ALU.mult), ["stmp"], ["nbq"])
                    P.op("pe", lambda e: e.transpose(out=tp[:, 0:128], in_=nbq[:], identity=ident[:]), ["nbq", "ident"], ["tp"])
                    copy("dve", nb[:, j * 128:(j + 1) * 128], tp[:, 0:128], ["tp"], ["nb"])
                copy("dve", nb2[:], nb[64:128, :], ["nb"], ["nb2"])

                P.phase = "nsa_sel"
                for h in range(4):
                    pr, hb_ = h // 2, (h % 2) * 64
                    ob, okey = oa[h % 2], "oa%d" % (h % 2)
                    nkt = 4 * g + 4

                    def sel_a(kt, pr=pr, hb_=hb_):
                        sb_, skey = sc[kt % 2], "sc%d" % (kt % 2)
                        mm(sb_[:], ksT[hb_:hb_ + 64, kt * 128:(kt + 1) * 128], qr[hb_:hb_ + 64, pr, :], True, False, ["ksT", "qr"], [skey])
                        m_ = (2 * kt) // 32
                        a2 = ((2 * kt) % 32) // 2
                        src, srck = (nb, "nb") if m_ < 2 else (nb2, "nb2")
                        pb_ = 32 * (m_ % 2)
                        mm(sb_[:], fsel[pb_:pb_ + 32, a2, :], src[pb_:pb_ + 32, :], False, True, ["fsel", srck], [skey])
                        P.op("act", lambda e: e.activation(out=pT[kt % 2][:], in_=sb_[:], func=AF.Exp), [skey], ["pT%d" % (kt % 2)])
                        if kt >= 4 * g:
                            pmask(pT[kt % 2][:], "pT%d" % (kt % 2), -128 * (kt - 4 * g), -1, 1)

                    sel_a(0)
                    for kt in range(nkt):
                        if kt + 1 < nkt:
                            sel_a(kt + 1)
                        mm(ob[0:65, :], vsA[:, kt, :], pT[kt % 2][:], kt == 0, kt == nkt - 1, ["vsA", "pT%d" % (kt % 2)], [okey])
                    combine(ob, okey, h, 1, False)

                P.phase = "nsa_win"
                for h in range(4):
                    pr, hb_ = h // 2, (h % 2) * 64
                    ob, okey = oa[h % 2], "oa%d" % (h % 2)
                    kts = [kt for kt in range(4 * g - 4, 4 * g + 4) if kt >= 0]

                    def win_a(kt, pr=pr, hb_=hb_):
                        sb_, skey = sc[kt % 2], "sc%d" % (kt % 2)
                        ro = (kt % 8) * 128
                        mm(sb_[:], kwT[hb_:hb_ + 64, ro:ro + 128], qr[hb_:hb_ + 64, pr, :], True, True, ["kwT", "qr"], [skey])
                        P.op("act", lambda e: e.activation(out=pT[kt % 2][:], in_=sb_[:], func=AF.Exp), [skey], ["pT%d" % (kt % 2)])
                        rel = kt - 4 * g
                        if rel >= 0:
                            pmask(pT[kt % 2][:], "pT%d" % (kt % 2), -128 * rel, -1, 1)
                        else:
                            pmask(pT[kt % 2][:], "pT%d" % (kt % 2), 511 + 128 * rel, 1, -1)

                    win_a(kts[0])
                    for ii, kt in enumerate(kts):
                        if ii + 1 < len(kts):
                            win_a(kts[ii + 1])
                        mm(ob[0:65, :], vwA[:, kt % 8, :], pT[kt % 2][:], ii == 0, ii == len(kts) - 1, ["vwA", "pT%d" % (kt % 2)], [okey])
                    combine(ob, okey, h, 2, False)

                P.phase = "nsa"
                o_, w_ = PK["nsag"]
                for h in range(4):
                    pr, hb_ = h // 2, (h % 2) * 64
                    finalize_head(acc[hb_:hb_ + 64, pr, :], "acc", False, hb_, pk[hb_:hb_ + 64, o_ + pr:o_ + pr + 1], gn[hb_:hb_ + 64, pr, :], "gn", yT[hb_:hb_ + 64, 4 + pr, :], "yT", from_sbuf=True)
                if g == 0:
                    dbg_out("yT", yT[:], "yT")
                P.phase = ""
                stop_at(7)
                for i in range(4):
                    xb, xk = xt[i % 2], "xt%d" % (i % 2)
                    P.dma("sp", lambda e, i=i, t0=t0, xb=xb: e.dma_start(out=xb[:], in_=x_d[t0 + i * 128:t0 + (i + 1) * 128, :]), writes=[xk])
                    for half in range(2):
                        pb = pj[half]
                        for k in range(8):
                            mm(pb[:], yT[:, k, i * 128:(i + 1) * 128], woutb[:, k, half * 512:(half + 1) * 512], k == 0, k == 7, ["yT", "woutb"], ["pj%d" % half])
                        P.op("dve", lambda e, pb=pb, xb=xb, half=half: e.tensor_tensor(out=xb[:, half * 512:(half + 1) * 512], in0=xb[:, half * 512:(half + 1) * 512], in1=pb[:], op=ALU.add),
                             ["pj%d" % half, xk], [xk])
                    sk = "st%d" % (8 + i)
                    ssq = st[:, 8 + i:9 + i]
                    hjk = hb[i % 2]
                    P.op("act", lambda e, xb=xb, ssq=ssq, hjk=hjk: e.activation(out=hjk[:], in_=xb[:], func=AF.Square, accum_out=ssq), [xk], ["hb0", sk])
                    P.op("dve", lambda e, ssq=ssq: e.tensor_scalar(out=ssq, in0=ssq, scalar1=1.0 / D, scalar2=1e-6, op0=ALU.mult, op1=ALU.add), [sk], [sk])
                    P.op("dve", lambda e, ssq=ssq: e.reciprocal(out=ssq, in_=ssq), [sk], [sk])
                    P.op("act", lambda e, ssq=ssq: e.activation(out=ssq, in_=ssq, func=AF.Sqrt), [sk], [sk])
                    P.op("dve", lambda e, xb=xb, ssq=ssq: e.scalar_tensor_tensor(out=xb[:], in0=xb[:], scalar=ssq, in1=gfin[:], op0=ALU.mult, op1=ALU.mult), [xk, sk, "gfin"], [xk])
                    P.dma("pool", lambda e, i=i, t0=t0, xb=xb: e.dma_start(out=out_d[t0 + i * 128:t0 + (i + 1) * 128, :], in_=xb[:]), reads=[xk], is_out=True)
        except _Stop:
            P.dma("pool", lambda e: e.dma_start(out=out_d[0:128, :], in_=xt[0][:]), reads=["xt0"], is_out=True)
        P.finish("sp")
        P.emit(block, sems, dsems)
    return nc


def make_in_maps(inputs, T, batches):
    ci = _colidx()
    w_in = np.asarray(inputs["w_in"][0])
    wcat = np.ascontiguousarray(w_in[:, ci].reshape(8, 128, NCOL))
    wout = np.ascontiguousarray(np.asarray(inputs["w_out"][0]).reshape(8, 128, D))
    wmkv = np.ascontiguousarray(np.asarray(inputs["w_mem_kv"][0]).reshape(8, 128, 512))
    pk = host_params({k: np.asarray(v) for k, v in inputs.items()})
    w1k = np.asarray(inputs["nsa_cmp_k_w1"][0]).reshape(32, 64, 64).transpose(1, 0, 2)
    w1v = np.asarray(inputs["nsa_cmp_v_w1"][0]).reshape(32, 64, 64).transpose(1, 0, 2)
    w1 = np.ascontiguousarray(np.concatenate([w1k, w1v], 0).reshape(128, 2048))
    w2k = np.asarray(inputs["nsa_cmp_k_w2"][0]); w2v = np.asarray(inputs["nsa_cmp_v_w2"][0])
    w2 = np.zeros((128, 192), np.float32)
    w2[0:64] = np.concatenate([w2k, w2k, w2v], 1)
    lora = np.ascontiguousarray(np.concatenate([np.asarray(inputs["rwkv_w_up"][0]), np.asarray(inputs["rwkv_a_up"][0])], 0))
    w0row = np.ascontiguousarray(np.asarray(inputs["rwkv_w0"][0]).reshape(1, 512))
    consts = host_consts(T)
    maps = []
    for b in batches:
        m = {
            "x": np.ascontiguousarray(np.asarray(inputs["x"][b][:T])),
            "mem": np.ascontiguousarray(np.asarray(inputs["mem"][b])),
            "wcat": wcat, "wout": wout, "wmkv": wmkv, "pk": pk, "w1": w1, "w2": w2, "lora": lora, "w0row": w0row,
            "gfin": np.ascontiguousarray(np.asarray(inputs["norm_final_g"]).reshape(1, D)),
        }
        m.update(consts)
        maps.append(m)
    return maps


_NC_CACHE = {}


def kernel(**inputs):
    T = inputs["x"].shape[1]
    B = inputs["x"].shape[0]
    if T not in _NC_CACHE:
        _NC_CACHE[T] = build_nc(T)
    nc = _NC_CACHE[T]
    batches = [i % B for i in range(8)]
    maps = make_in_maps(inputs, T, batches)
    res = run_bass_kernel_spmd(nc, maps, core_ids=list(range(8)))
    out = np.stack([res.results[b]["out"] for b in range(B)], axis=0)
    return out.astype(np.float32)
```

```python
import numpy as np
import ml_dtypes
from contextlib import ExitStack
import concourse.bass as bass
import concourse.mybir as mybir
from concourse.bass_utils import run_bass_kernel_spmd

F32 = mybir.dt.float32
BF16 = mybir.dt.bfloat16
ALU = mybir.AluOpType
AF = mybir.ActivationFunctionType
AX = mybir.AxisListType

ENGS = ("pe", "act", "dve", "pool", "sp")
N_DMA_SEMS = 24
import os as _os0
SAME_ENG_SYNC = _os0.environ.get("KSAME", "1") == "1"
NEG = -30000.0

D = 1024
NCOL = 29 * 128 + 12
G = 512
PSUM_PREFIX = ("pj", "sc", "oa", "mi", "tp")


class Prog:
    def __init__(self, nc):
        self.nc = nc
        self.lists = {e: [] for e in ENGS}
        self.cnt = {e: 0 for e in ENGS}
        self.seen = {e: {} for e in ENGS}
        self.clock_at = {e: [None] for e in ENGS}
        self.buf = {}
        self.dma_val = [0] * N_DMA_SEMS
        self.dma_clock = [[] for _ in range(N_DMA_SEMS)]
        self.dma_rr = 0
        self.out_toks = []
        self.phase = ""
        self.skip = set()
        self.children = {}

    def _deps(self, eng, reads, writes):
        need = {}

        def add(tok):
            if tok is None:
                return
            src, val = tok
            if src == eng and (eng == "pe" or not SAME_ENG_SYNC):
                return
            if need.get(src, 0) < val:
                need[src] = val

        def related(k):
            ks = [k]
            if "/" in k:
                ks.append(k.split("/")[0])
            else:
                ks.extend(self.children.get(k, ()))
            return ks

        for k0 in reads:
            for k in related(k0):
                st = self.buf.get(k)
                if st:
                    add(st[0])
                    if k[:2] in PSUM_PREFIX:
                        for r in st[1]:
                            if r[0] != eng:
                                add(r)
        for k0 in writes:
            for k in related(k0):
                st = self.buf.get(k)
                if st:
                    add(st[0])
                    for r in st[1]:
                        add(r)
        seen = self.seen[eng]
        waits = []
        for src, val in need.items():
            if seen.get(src, 0) >= val:
                continue
            waits.append((src, val))
            if isinstance(src, str):
                snap = self.clock_at[src][val]
            else:
                snap = self.dma_clock[src[1]][val // 16 - 1]
            for s2, v2 in snap.items():
                if seen.get(s2, 0) < v2:
                    seen[s2] = v2
            seen[src] = val
        return waits

    def _mark(self, tok, reads, writes):
        for k in list(reads) + list(writes):
            if "/" in k:
                self.children.setdefault(k.split("/")[0], set()).add(k)
        for k in reads:
            st = self.buf.setdefault(k, [None, []])
            st[1].append(tok)
            if len(st[1]) > 64:
                st[1] = st[1][-64:]
        for k in writes:
            self.buf[k] = [tok, []]
            if "/" not in k:
                for ch in self.children.get(k, ()):
                    self.buf[ch] = [tok, []]

    def op(self, eng, fn, reads=(), writes=(), self_wait=None):
        if self.phase in self.skip:
            return
        waits = self._deps(eng, reads, writes)
        if self_wait is not None and self.seen[eng].get(eng, 0) < self_wait:
            waits.append((eng, self_wait))
            self.seen[eng][eng] = self_wait
        self.cnt[eng] += 1
        c = self.cnt[eng]
        snap = dict(self.seen[eng])
        snap[eng] = c
        self.clock_at[eng].append(snap)
        self.lists[eng].append(("op", fn, waits, None))
        self._mark((eng, c), reads, writes)

    def dma(self, eng, fn, reads=(), writes=(), is_out=False):
        if self.phase in self.skip:
            return None
        s = self.dma_rr
        self.dma_rr = (self.dma_rr + 1) % N_DMA_SEMS
        waits = self._deps(eng, reads, writes)
        src = ("dma", s)
        prev = self.dma_val[s]
        if prev and self.seen[eng].get(src, 0) < prev:
            waits.append((src, prev))
            self.seen[eng][src] = prev
        val = prev + 16
        self.dma_val[s] = val
        self.dma_clock[s].append(dict(self.seen[eng]))
        self.lists[eng].append(("dma", fn, waits, (s, val)))
        self._mark((src, val), reads, writes)
        if is_out:
            self.out_toks.append((src, val))
        return (src, val)

    def finish(self, eng="sp"):
        waits = []
        for s in range(N_DMA_SEMS):
            if self.dma_val[s]:
                waits.append((("dma", s), self.dma_val[s]))
        self.lists[eng].append(("wait", None, waits, None))

    def emit(self, block, sems, dsems):
        engobj = {"pe": "tensor", "act": "scalar", "dve": "vector", "pool": "gpsimd", "sp": "sync"}

        def semof(src):
            return sems[src] if isinstance(src, str) else dsems[src[1]]

        def make(ename):
            lst = self.lists[ename]

            def body(e):
                for kind, fn, waits, extra in lst:
                    for src, val in waits:
                        e.wait_ge(semof(src), val)
                    if kind == "op":
                        fn(e).then_inc(sems[ename], 1)
                    elif kind == "dma":
                        fn(e).then_inc(dsems[extra[0]], 16)

            return body

        for ename in ENGS:
            if self.lists[ename]:
                getattr(block, engobj[ename])(make(ename))


RW, NSAB, MEMB = 0, 2176, 3084


def _colidx():
    idx = []
    for hp in range(4):
        for base in (0, 512, 1024, 1536):
            idx += list(range(base + 128 * hp, base + 128 * hp + 128))
    idx += list(range(2048, 2176))
    q0, g0 = NSAB, NSAB + 256
    idx += list(range(q0, q0 + 256))
    idx += list(range(g0, g0 + 256))
    kc, vc, ks, vs, kw, vw = (NSAB + 524 + 64 * i for i in range(6))
    idx += list(range(kc, kc + 64)) + list(range(vc, vc + 64))
    idx += list(range(ks, ks + 64)) * 2
    idx += list(range(kw, kw + 64)) * 2
    idx += list(range(vs, vs + 64)) + list(range(vw, vw + 64))
    idx += list(range(MEMB, MEMB + 512))
    idx += list(range(NSAB + 512, NSAB + 524))
    assert len(idx) == NCOL
    return np.array(idx)


def _bf(a):
    return np.ascontiguousarray(a.astype(ml_dtypes.bfloat16))


def host_consts(T):
    c = {}
    c["ident"] = _bf(np.eye(128, dtype=np.float32))
    half = 8
    inv = (np.float32(500000.0) ** (-np.arange(half, dtype=np.float32) / np.float32(half))).astype(np.float32)
    ang = np.arange(T, dtype=np.float32)[None, :] * inv[:, None]
    C = np.ones((64, T), np.float32)
    S = np.zeros((64, T), np.float32)
    C[0:8] = np.cos(ang); C[8:16] = np.cos(ang)
    S[0:8] = -np.sin(ang); S[8:16] = np.sin(ang)
    c["ropeC"] = np.ascontiguousarray(np.concatenate([C, C], 0))
    c["ropeS"] = np.ascontiguousarray(np.concatenate([S, S], 0))
    pm = np.zeros((128, 128), np.float32)
    for hb in (0, 64):
        for d in range(8):
            pm[hb + d + 8, hb + d] = 1.0
            pm[hb + d, hb + d + 8] = 1.0
    c["pm"] = _bf(pm)
    ovl = np.zeros((512, 129), np.float32)
    for i in range(511):
        for sblk in range(128):
            o = min(16 * i + 32, 64 * sblk + 64) - max(16 * i, 64 * sblk)
            if o > 0:
                ovl[i, sblk] = o / 32.0
    ovl[:, 128] = 1.0
    c["ovl"] = _bf(ovl.reshape(4, 128, 129).transpose(1, 0, 2))
    addw = np.zeros((128, 256), np.float32)
    for pp in range(128):
        cur = 1 if pp >= 64 else 0
        for u in range(256):
            sp = u - 126
            valid = sp <= cur
            forced = sp in (cur, cur - 1)
            addw[pp, u] = (0.0 if valid else -1.0) + (1e4 if forced else 0.0)
    c["addw"] = addw
    selpat = np.zeros((64, 32, 128), np.float32)
    for j in range(32):
        selpat[2 * j, j, 0:64] = 1.0
        selpat[2 * j + 1, j, 64:128] = 1.0
    c["selpat"] = _bf(selpat.reshape(64, 4096))
    pp = np.arange(128)[:, None]
    ff = np.arange(128)[None, :]
    for nm, m in (("maskL2", pp > ff), ("maskU2", pp < ff), ("maskUI2", pp <= ff), ("ident2", pp == ff)):
        m = m.astype(np.float32)
        c[nm] = _bf(np.stack([m, m], 1))
    blk = (pp // 64 == ff // 64).astype(np.float32)
    c["blk1"] = _bf(blk)
    c["blkavg"] = _bf(blk / 64.0)
    return c


PK = {}
_o = 0
for _n, _w in (("gin", 8), ("gmem", 8), ("mu", 17), ("memg", 2), ("nsag", 2), ("gateb", 1), ("posT", 32), ("w0", 4), ("a0", 4), ("kk", 4), ("ka", 4), ("rk", 4), ("lnw", 4), ("lnb", 4)):
    PK[_n] = (_o, _w)
    _o += _w
NPK = _o


def host_params(inp):
    pk = np.zeros((128, NPK), np.float32)

    def put(name, arr):
        o, w = PK[name]
        pk[:, o:o + w] = arr

    put("gin", inp["norm_in_g"][0].reshape(8, 128).T)
    put("gmem", inp["mem_norm_g"][0].reshape(8, 128).T)
    ci = _colidx()
    put("mu", inp["rwkv_mu"][0][ci[:17 * 128]].reshape(17, 128).T)
    put("memg", inp["mem_out_g"][0].reshape(2, 128).T)
    put("nsag", inp["nsa_out_g"][0].reshape(2, 128).T)
    gb = np.zeros((128, 1), np.float32)
    gb[0:12, 0] = inp["nsa_gate_b"][0]
    put("gateb", gb)
    pt = inp["nsa_cmp_pos"][0].T
    put("posT", np.concatenate([pt, pt], 0))
    for nm, key in (("w0", "rwkv_w0"), ("a0", "rwkv_a0"), ("kk", "rwkv_k_k"), ("ka", "rwkv_k_a"), ("rk", "rwkv_r_k"), ("lnw", "rwkv_ln_w"), ("lnb", "rwkv_ln_b")):
        put(nm, inp[key][0].reshape(4, 128).T)
    return pk


def build_nc(T, dbg=None):
    NG = T // G
    NT = T // 128
    nc = bass.Bass("TRN2", target_bir_lowering=False)
    dram = {}

    def din(name, shape, dt=F32):
        dram[name] = nc.dram_tensor(name, list(shape), dt, kind="ExternalInput").ap()
        return dram[name]

    x_d = din("x", [T, D])
    mem_d = din("mem", [256, D])
    wcat_d = din("wcat", [8, 128, NCOL])
    wout_d = din("wout", [8, 128, D])
    wmkv_d = din("wmkv", [8, 128, 512])
    pk_d = din("pk", [128, NPK])
    gfin_d = din("gfin", [1, D])
    ident_d = din("ident", [128, 128], BF16)
    ropeC_d = din("ropeC", [128, T])
    ropeS_d = din("ropeS", [128, T])
    pm_d = din("pm", [128, 128], BF16)
    ovl_d = din("ovl", [128, 4, 129], BF16)
    addw_d = din("addw", [128, 256])
    selpat_d = din("selpat", [64, 4096], BF16)
    w1_d = din("w1", [128, 2048])
    w2_d = din("w2", [128, 192])
    lora_d = din("lora", [128, 512])
    w0row_d = din("w0row", [1, 512])
    maskL2_d = din("maskL2", [128, 2, 128], BF16)
    maskU2_d = din("maskU2", [128, 2, 128], BF16)
    maskUI2_d = din("maskUI2", [128, 2, 128], BF16)
    ident2_d = din("ident2", [128, 2, 128], BF16)
    blk1_d = din("blk1", [128, 128], BF16)
    blkavg_d = din("blkavg", [128, 128], BF16)
    out_d = nc.dram_tensor("out", [T, D], F32, kind="ExternalOutput").ap()
    dbg_d = {}
    if dbg:
        for n, (shape, dt) in dbg.items():
            dbg_d[n] = nc.dram_tensor("dbg_" + n, list(shape), dt, kind="ExternalOutput").ap()

    with ExitStack() as es:
        def sb(name, shape, dt=F32):
            return es.enter_context(nc.sbuf_tensor("sb_" + name, list(shape), dt))

        def ps(name, shape, dt=F32):
            return es.enter_context(nc.psum_tensor("ps_" + name, list(shape), dt))

        wb = sb("wb", [128, 8, NCOL], BF16)
        woutb = sb("woutb", [128, 8, D], BF16)
        wmkvb = sb("wmkvb", [128, 8, 512], BF16)
        pk = sb("pk", [128, NPK])
        gfin = sb("gfin", [128, D])
        ident = sb("ident", [128, 128], BF16)
        xt = [sb("xt%d" % i, [128, D]) for i in range(2)]
        hb0 = sb("hb0", [128, D], BF16)
        hb = [hb0, hb0]
        hT = sb("hT", [128, 8, G], BF16)
        st = sb("st", [128, 16])
        yT = sb("yT", [128, 8, G], BF16)
        memkT = sb("memkT", [128, 2, 256], BF16)
        memvA = sb("memvA", [128, 2, 4, 65], BF16)
        qm = sb("qm", [128, 2, G], BF16)
        gm = sb("gm", [128, 2, G], BF16)
        pT = [sb("pT%d" % i, [128, G], BF16) for i in range(2)]
        oaug = sb("oaug", [128, G], BF16)
        osq = sb("osq", [65, G], BF16)
        cvec = sb("cvec", [65, 128], BF16)
        pm = sb("pm", [128, 128], BF16)
        ovl = sb("ovl", [128, 4, 129], BF16)
        addw = sb("addw", [128, 256])
        qcat = sb("qcat", [128, 4, G], BF16)
        ones12 = sb("ones12", [12, 128], BF16)
        gsr = sb("gsr", [12, G], BF16)
        e64 = sb("e64", [65, 128], BF16)
        w1b = sb("w1b", [128, 32, 64], BF16)
        w2b = sb("w2b", [64, 192], BF16)
        posTb = sb("posTb", [128, 32], BF16)
        ccst = sb("ccst", [64, 2])
        ropeC = sb("ropeC", [128, G])
        ropeS = sb("ropeS", [128, G])
        qn = sb("qn", [128, 2, G], BF16)
        qr = sb("qr", [128, 2, G], BF16)
        gn = sb("gn", [128, 2, G], BF16)
        kcvc = sb("kcvc", [128, 16 + G], BF16)
        kraw = sb("kraw", [128, G], BF16)
        ksT = sb("ksT", [128, T], BF16)
        kwT = sb("kwT", [128, 1024], BF16)
        vsA = sb("vsA", [128, NT, 65], BF16)
        vwA = sb("vwA", [128, 8, 65], BF16)
        kcmpT = sb("kcmpT", [128, 512], BF16)
        vcmpA = sb("vcmpA", [128, 4, 65], BF16)
        vstg = sb("vstg", [32, 65], BF16)
        h1k = sb("h1k", [64, 32], BF16)
        h1v = sb("h1v", [64, 32], BF16)
        gsig = sb("gsig", [12, G], BF16)
        impS = sb("impS", [128, 4, 128])
        score = sb("score", [128, 128])
        stmp = sb("stmp", [128, 128])
        m8 = sb("m8", [128, 16])
        nbq = sb("nbq", [128, 128], BF16)
        nb = sb("nb", [128, G], BF16)
        oab = sb("oab", [65, G], BF16)
        coef = sb("coef", [128, G])
        scl = coef
        rtmp = coef
        acc = sb("acc", [128, 2, G])
        praw = sb("praw", [128, G + 1])
        prevcol = sb("prevcol", [128, 17])
        loraup = sb("loraup", [128, 512], BF16)
        w0hi = sb("w0hi", [1, 512], BF16)
        w0lo = sb("w0lo", [1, 512], BF16)
        onesrow = sb("onesrow", [1, 128], BF16)
        ka1m = sb("ka1m", [128, 4])
        maskL2 = sb("maskL2", [128, 2, 128], BF16)
        maskU2 = sb("maskU2", [128, 2, 128], BF16)
        maskUI2 = sb("maskUI2", [128, 2, 128], BF16)
        ident2 = sb("ident2", [128, 2, 128], BF16)
        blk1 = sb("blk1", [128, 128], BF16)
        blkavg = sb("blkavg", [128, 128], BF16)
        sgtok = sb("sgtok", [128, 4, 128], BF16)
        AakT4 = sb("AakT4", [128, 2, G], BF16)
        AkV4 = sb("AkV4", [128, G], BF16)
        STb = sb("STb", [64, 8, 64], BF16)
        ncL = sb("ncL", [128, 4])
        WL = sb("WL", [128, 4])
        WL0 = sb("WL0", [64, 8])
        pj = [ps("pj%d" % i, [128, 512]) for i in range(2)]
        sc = [ps("sc%d" % i, [128, 512]) for i in range(2)]
        oa = [ps("oa%d" % i, [128, 512]) for i in range(2)]
        mi = ps("mi", [128, 512])
        tp = ps("tp", [128, 1024], BF16)

        pTc = wmkvb[:, 0:4, :]
        sems = {e: es.enter_context(nc.semaphore("s_" + e)) for e in ENGS}
        dsems = [es.enter_context(nc.semaphore("d%d" % i)) for i in range(N_DMA_SEMS)]
        block = es.enter_context(nc.Block())
        P = Prog(nc)
        rr = {"ev": 0}

        def ev_eng():
            rr["ev"] ^= 1
            return "act" if rr["ev"] else "dve"

        def copy(eng, out, in_, reads, writes):
            if eng == "act":
                P.op("act", lambda e: e.copy(out=out, in_=in_), reads, writes)
            else:
                P.op(eng, lambda e: e.tensor_copy(out=out, in_=in_), reads, writes)

        pe_last = {}

        def mm(out, lhsT, rhs, start, stop, reads, writes):
            base, rows = lhsT.base_partition(), lhsT.shape[0]
            sw = None
            for k in writes:
                last = pe_last.get(k)
                if last is not None:
                    c0, b0, r0 = last
                    if b0 + r0 <= base or base + rows <= b0:
                        sw = max(sw or 0, c0)
            P.op("pe", lambda e: e.matmul(out, lhsT, rhs, start=start, stop=stop), reads, writes, self_wait=sw)
            for k in writes:
                pe_last[k] = (P.cnt["pe"], base, rows)

        def dbg_out(name, ap, key):
            if dbg and name in dbg_d:
                P.dma("sp", lambda e: e.dma_start(out=dbg_d[name], in_=ap), reads=[key])

        import os as _os
        KSTOP = int(_os.environ.get("KSTOP", "99"))
        KSKIP = _os.environ.get("KSKIP", "")
        P.skip = set(KSKIP.split(",")) if KSKIP else set()

        class _Stop(Exception):
            pass

        def stop_at(n):
            if KSTOP == n:
                raise _Stop()

        try:
            P.dma("sp", lambda e: e.dma_start(out=pk[:], in_=pk_d[:, :]), writes=["pk"])
            P.dma("sp", lambda e: e.dma_start(out=ident[:], in_=ident_d[:, :]), writes=["ident"])
            P.dma("sp", lambda e: e.dma_start(out=gfin[:], in_=gfin_d[0:1, :].broadcast_to([128, D])), writes=["gfin"])

            def pkc(name, j=0, rows=128):
                o, w = PK[name]
                return pk[0:rows, o + j:o + j + 1]

            def load_cast(dst3, src_d, ncols, gname, key):
                i = 0
                for k in range(8):
                    for c0 in range(0, ncols, 1024):
                        cw = min(1024, ncols - c0)
                        stg = xt[i % 2]
                        skey = "xt%d" % (i % 2)
                        P.dma("sp" if i % 2 == 0 else "pool",
                              lambda e, stg=stg, k=k, c0=c0, cw=cw: e.dma_start(out=stg[:, 0:cw], in_=src_d[k, :, c0:c0 + cw]),
                              writes=[skey])
                        eng = ev_eng()
                        o = dst3[:, k, c0:c0 + cw]
                        if gname is None:
                            copy(eng, o, stg[:, 0:cw], [skey], [key])
                        elif eng == "act":
                            P.op("act", lambda e, o=o, stg=stg, cw=cw, k=k: e.activation(out=o, in_=stg[:, 0:cw], func=AF.Copy, scale=pkc(gname, k)),
                                 [skey, "pk"], [key])
                        else:
                            P.op("dve", lambda e, o=o, stg=stg, cw=cw, k=k: e.tensor_scalar(out=o, in0=stg[:, 0:cw], scalar1=pkc(gname, k), scalar2=None, op0=ALU.mult),
                                 [skey, "pk"], [key])
                        i += 1

            stop_at(1)
            load_cast(wmkvb, wmkv_d, 512, "gmem", "wmkvb")
            stop_at(2)
            load_cast(wb, wcat_d, NCOL, "gin", "wb")
            load_cast(woutb, wout_d, D, None, "woutb")

            P.op("pool", lambda e: e.memset(cvec[0:64, :], 1.0 / 64), writes=["cvec"])
            P.op("pool", lambda e: e.memset(cvec[64:65, :], 1e-6), writes=["cvec"])

            def norm_transpose(src_tile, skey, dstT, dkey, col0, slot):
                h = hb[slot]
                hk = "hb0"
                ssq = st[:, slot:slot + 1]
                P.op("act", lambda e: e.activation(out=h[:], in_=src_tile[:], func=AF.Square, accum_out=ssq), [skey], [hk, "st%d" % slot])
                P.op("dve", lambda e: e.tensor_scalar(out=ssq, in0=ssq, scalar1=1.0 / D, scalar2=1e-6, op0=ALU.mult, op1=ALU.add), ["st%d" % slot], ["st%d" % slot])
                P.op("dve", lambda e: e.reciprocal(out=ssq, in_=ssq), ["st%d" % slot], ["st%d" % slot])
                P.op("act", lambda e: e.activation(out=ssq, in_=ssq, func=AF.Sqrt), ["st%d" % slot], ["st%d" % slot])
                P.op("dve", lambda e: e.tensor_scalar(out=h[:], in0=src_tile[:], scalar1=ssq, scalar2=None, op0=ALU.mult), [skey, "st%d" % slot], [hk])
                for k in range(8):
                    P.op("pe", lambda e, k=k: e.transpose(out=tp[:, k * 128:(k + 1) * 128], in_=h[:, k * 128:(k + 1) * 128], identity=ident[:]), [hk, "ident"], ["tp"])
                copy(ev_eng(), dstT[:, :, col0:col0 + 128], tp[:].rearrange("p (k t) -> p k t", k=8), ["tp"], [dkey])


            for nm, dst, src in (("pm", pm, pm_d), ("ovl", ovl, ovl_d), ("addw", addw, addw_d),
                                 ):
                P.dma("sp", lambda e, dst=dst, src=src: e.dma_start(out=dst[:], in_=src), writes=[nm])
            P.op("pool", lambda e: e.memset(ones12[:], 1.0), writes=["ones12"])
            P.op("pool", lambda e: e.memset(e64[:], 0.0), writes=["e64"])
            P.op("pool", lambda e: e.memset(e64[64:65, :], 1.0), writes=["e64"])
            P.op("pool", lambda e: e.memset(kcmpT[:], 0.0), writes=["kcmpT"])
            P.op("pool", lambda e: e.memset(vcmpA[:], 0.0), writes=["vcmpA"])
            P.op("pool", lambda e: e.memset(vsA[:], 1.0), writes=["vsA"])
            P.op("pool", lambda e: e.memset(vwA[:], 1.0), writes=["vwA"])
            P.op("pool", lambda e: e.memset(kwT[:], 0.0), writes=["kwT"])
            for c0 in range(0, T, 4096):
                cw = min(4096, T - c0)
                P.dma("sp", lambda e, c0=c0, cw=cw: e.dma_start(out=ksT[64:128, c0:c0 + cw], in_=selpat_d[:, 0:cw]), writes=["ksT"])
            P.op("pool", lambda e: e.memset(vstg[:], 1.0), writes=["vstg"])
            P.op("pool", lambda e: e.memset(kcvc[:], 0.0), writes=["kcvc"])
            for hlf in range(2):
                P.dma("sp", lambda e, hlf=hlf: e.dma_start(out=xt[hlf][:], in_=w1_d[:, hlf * 1024:(hlf + 1) * 1024]), writes=["xt%d" % hlf])
                copy(ev_eng(), w1b[:, hlf * 16:(hlf + 1) * 16, :], xt[hlf][:].rearrange("p (l e) -> p l e", e=64), ["xt%d" % hlf], ["w1b"])
            P.dma("sp", lambda e: e.dma_start(out=xt[0][:, 0:192], in_=w2_d[:, :]), writes=["xt0"])
            copy("dve", w2b[:], xt[0][0:64, 0:192], ["xt0"], ["w2b"])
            o_, w_ = PK["posT"]
            copy("dve", posTb[:], pk[:, o_:o_ + 32], ["pk"], ["posTb"])
            for kv in range(2):
                b0 = kv * 64
                for l in range(32):
                    mm(mi[0:64, kv:kv + 1], w1b[b0:b0 + 64, l, :], posTb[b0:b0 + 64, l:l + 1], l == 0, l == 31, ["w1b", "posTb"], ["mi"])
                copy("dve", ccst[:, kv:kv + 1], mi[0:64, kv:kv + 1], ["mi"], ["ccst"])

            for nm, dst, src in (("maskL2", maskL2, maskL2_d), ("maskU2", maskU2, maskU2_d), ("maskUI2", maskUI2, maskUI2_d),
                                 ("ident2", ident2, ident2_d), ("blk1", blk1, blk1_d), ("blkavg", blkavg, blkavg_d)):
                P.dma("sp", lambda e, dst=dst, src=src: e.dma_start(out=dst[:], in_=src), writes=[nm])
            P.dma("sp", lambda e: e.dma_start(out=xt[1][:, 0:512], in_=lora_d[:, :]), writes=["xt1"])
            copy("dve", loraup[:], xt[1][:, 0:512], ["xt1"], ["loraup"])
            P.dma("sp", lambda e: e.dma_start(out=praw[0:1, 0:512], in_=w0row_d[:, :]), writes=["praw"])
            copy("dve", w0hi[:], praw[0:1, 0:512], ["praw"], ["w0hi"])
            P.op("dve", lambda e: e.tensor_tensor(out=praw[0:1, 0:512], in0=praw[0:1, 0:512], in1=w0hi[:], op=ALU.subtract), ["praw", "w0hi"], ["praw"])
            copy("dve", w0lo[:], praw[0:1, 0:512], ["praw"], ["w0lo"])
            P.op("pool", lambda e: e.memset(onesrow[:], 1.0), writes=["onesrow"])
            P.op("pool", lambda e: e.memset(prevcol[:], 0.0), writes=["prevcol"])
            P.op("pool", lambda e: e.memset(STb[:], 0.0), writes=["STb"])
            o_, w_ = PK["ka"]
            P.op("dve", lambda e, o_=o_: e.tensor_scalar(out=ka1m[:], in0=pk[:, o_:o_ + 4], scalar1=-1.0, scalar2=1.0, op0=ALU.mult, op1=ALU.add), ["pk"], ["ka1m"])
            tokall = wmkvb[:, 4:8, :].rearrange("p c (w f) -> p c w f", w=4)
            stop_at(3)
            for mt in range(2):
                P.dma("sp", lambda e, mt=mt: e.dma_start(out=xt[mt][:], in_=mem_d[mt * 128:(mt + 1) * 128, :]), writes=["xt%d" % mt])
                norm_transpose(xt[mt], "xt%d" % mt, hT, "hT", mt * 128, mt)
            stop_at(4)
            for pr in range(2):
                for k in range(8):
                    mm(pj[0][:, 0:256], wmkvb[:, k, pr * 128:(pr + 1) * 128], hT[:, k, 0:256], k == 0, k == 7, ["wmkvb", "hT"], ["pj0"])
                copy(ev_eng(), memkT[:, pr, :], pj[0][:, 0:256], ["pj0"], ["memkT"])
            P.op("pool", lambda e: e.memset(memvA[:], 1.0), writes=["memvA"])
            for mt in range(2):
                for k in range(8):
                    mm(pj[1][:, 0:256], hT[:, k, mt * 128:(mt + 1) * 128], wmkvb[:, k, 256:512], k == 0, k == 7, ["wmkvb", "hT"], ["pj1"])
                copy(ev_eng(), memvA[:, mt, :, 0:64], pj[1][:, 0:256].rearrange("p (h d) -> p h d", h=4), ["pj1"], ["memvA"])

            def finalize_head(o_ps, okey, use_den, hb_, gcol_ap, gate_ap, gate_key, dst_ap, dst_key, from_sbuf=False):
                sl = slice(hb_, hb_ + 64)
                if from_sbuf:
                    copy("dve", oaug[sl, :], o_ps, [okey], ["oaug"])
                    P.op("act", lambda e: e.activation(out=osq[0:64, :], in_=o_ps, func=AF.Square), [okey], ["osq"])
                    P.op("pool", lambda e: e.memset(osq[64:65, :], 1.0), [], ["osq"])
                elif use_den:
                    copy("dve", oaug[sl, :], o_ps[0:64, :], [okey], ["oaug"])
                    P.op("act", lambda e: e.activation(out=osq[:], in_=o_ps[0:65, :], func=AF.Square), [okey], ["osq"])
                else:
                    copy("dve", oaug[sl, :], o_ps[0:64, :], [okey], ["oaug"])
                    P.op("act", lambda e: e.activation(out=osq[0:64, :], in_=o_ps[0:64, :], func=AF.Square), [okey], ["osq"])
                    P.op("pool", lambda e: e.memset(osq[64:65, :], 1.0), [], ["osq"])
                mm(mi[:], cvec[:], osq[:], True, True, ["cvec", "osq"], ["mi"])
                P.op("act", lambda e: e.activation(out=scl[sl, :], in_=mi[sl, :], func=AF.Ln), ["mi"], ["coef"])
                P.op("act", lambda e: e.activation(out=scl[sl, :], in_=scl[sl, :], func=AF.Exp, scale=-0.5), ["coef"], ["coef"])
                P.op("dve", lambda e: e.scalar_tensor_tensor(out=scl[sl, :], in0=scl[sl, :], scalar=gcol_ap, in1=oaug[sl, :], op0=ALU.mult, op1=ALU.mult), ["coef", "oaug", "pk"], ["coef"])
                P.op("dve", lambda e: e.tensor_tensor(out=dst_ap, in0=scl[sl, :], in1=gate_ap, op=ALU.mult), ["coef", gate_key], [dst_key])

            P.op("pool", lambda e: e.memset(wmkvb[:, 4:8, :], 0.0), ["wmkvb"], ["wmkvb", "tokall"])
            stop_at(5)
            for g in range(NG):
                t0 = g * G
                for i in range(4):
                    P.dma("sp", lambda e, i=i, t0=t0: e.dma_start(out=xt[i % 2][:], in_=x_d[t0 + i * 128:t0 + (i + 1) * 128, :]), writes=["xt%d" % (i % 2)])
                    norm_transpose(xt[i % 2], "xt%d" % (i % 2), hT, "hT", i * 128, i % 2)

                def project(chunk, width=128):
                    pb = pj[chunk % 2]
                    for k in range(8):
                        mm(pb[0:width, :], wb[:, k, chunk * 128:chunk * 128 + width], hT[:, k, :], k == 0, k == 7, ["wb", "hT"], ["pj%d" % (chunk % 2)])
                    return pb, "pj%d" % (chunk % 2)


                P.phase = "rwkv"
                C0 = float(np.exp(-0.5))

                def tt(eng, out, in0, in1, op, reads, writes):
                    P.op(eng, lambda e: e.tensor_tensor(out=out, in0=in0, in1=in1, op=op), reads, writes)

                def ts(eng, out, in0, s1, s2, op0, op1, reads, writes):
                    if s2 is None:
                        P.op(eng, lambda e: e.tensor_scalar(out=out, in0=in0, scalar1=s1, scalar2=None, op0=op0), reads, writes)
                    else:
                        P.op(eng, lambda e: e.tensor_scalar(out=out, in0=in0, scalar1=s1, scalar2=s2, op0=op0, op1=op1), reads, writes)

                def stt(eng, out, in0, scalar, in1, op0, op1, reads, writes):
                    P.op(eng, lambda e: e.scalar_tensor_tensor(out=out, in0=in0, scalar=scalar, in1=in1, op0=op0, op1=op1), reads, writes)

                def actf(out, in_, func, reads, writes, bias=None, scale=None):
                    kw = {}
                    if bias is not None:
                        kw["bias"] = bias
                    if scale is not None:
                        kw["scale"] = scale
                    P.op("act", lambda e: e.activation(out=out, in_=in_, func=func, **kw), reads, writes)

                def pcol(name, j):
                    o, w = PK[name]
                    return pk[:, o + j:o + j + 1]

                def lerp_chunk(cidx, dst_ap, dkey):
                    pb, pkey = project(cidx)
                    copy("act", praw[:, 1:G + 1], pb[:], [pkey], ["praw"])
                    copy("dve", praw[:, 0:1], prevcol[:, cidx:cidx + 1], ["prevcol"], ["praw"])
                    copy("dve", prevcol[:, cidx:cidx + 1], praw[:, G:G + 1], ["praw"], ["prevcol"])
                    tt("dve", coef[:], praw[:, 0:G], praw[:, 1:G + 1], ALU.subtract, ["praw"], ["coef"])
                    stt("dve", dst_ap, coef[:], pcol("mu", cidx), praw[:, 1:G + 1], ALU.mult, ALU.add, ["coef", "praw", "pk"], [dkey])

                rT, kmT = qn[:, 0, :], qn[:, 1, :]
                vT, gT = qr[:, 0, :], qr[:, 1, :]
                bT, At = gn[:, 0, :], gn[:, 1, :]
                Rt, Bt = qm[:, 0, :], qm[:, 1, :]
                Kt, Bb = gm[:, 0, :], gm[:, 1, :]
                Kb, kkn = pT[0][:], pT[1][:]
                aT, lin = kraw[:], nb[:]
                kf, yraw = acc[:, 0, :], acc[:, 1, :]
                Etmp, Esq = hb0[:, 0:G], hb0[:, G:2 * G]

                lerp_chunk(16, kf, "acc")
                actf(lin[0:64, :], kf[0:64, :], AF.Tanh, ["acc"], ["nb"])
                copy("dve", lin[64:128, :], kf[64:128, :], ["acc"], ["nb"])

                stop_at(40)
                for hp in range(4):
                    lerp_chunk(4 * hp + 0, rT, "qn")
                    lerp_chunk(4 * hp + 1, kf, "acc")
                    lerp_chunk(4 * hp + 2, vT, "qr")
                    lerp_chunk(4 * hp + 3, gT, "qr")
                    actf(gT, gT, AF.Silu, ["qr"], ["qr"])
                    hc = slice(hp * 128, (hp + 1) * 128)
                    stop_at(41)
                    for c in range(4):
                        reg = mi[:, c * 128:(c + 1) * 128]
                        mm(reg, lin[0:64, c * 128:(c + 1) * 128], loraup[0:64, hc], True, False, ["nb", "loraup"], ["mi"])
                        mm(reg, onesrow[:], w0hi[0:1, hc], False, False, ["onesrow", "w0hi"], ["mi"])
                        mm(reg, onesrow[:], w0lo[0:1, hc], False, True, ["onesrow", "w0lo"], ["mi"])
                    actf(sgtok[:].rearrange("p c f -> p (c f)"), mi[:], AF.Sigmoid, ["mi"], ["sgtok"])
                    for c in range(4):
                        mm(sc[0][:, c * 128:(c + 1) * 128], sgtok[:, c, :], maskUI2[:, 0, :], True, True, ["sgtok", "maskUI2"], ["sc0"])
                    for c in range(4):
                        mm(sc[1][:, c * 128:(c + 1) * 128], sgtok[:, c, :], maskU2[:, 0, :], True, True, ["sgtok", "maskU2"], ["sc1"])
                    stop_at(42)
                    mm(oa[0][:], loraup[64:128, hc], lin[64:128, :], True, True, ["nb", "loraup"], ["oa0"])
                    actf(aT, oa[0][:], AF.Sigmoid, ["oa0", "pk"], ["kraw"], bias=pcol("a0", hp))
                    ts("dve", coef[:], kf, pcol("kk", hp), None, ALU.mult, None, ["acc", "pk"], ["coef"])
                    actf(Esq, coef[:], AF.Square, ["coef"], ["hb0"])
                    mm(oa[1][:], blk1[:], Esq, True, True, ["blk1", "hb0"], ["oa1"])
                    ts("dve", praw[:, 1:G + 1], oa[1][:], 1e-24, None, ALU.max, None, ["oa1"], ["praw"])
                    actf(praw[:, 1:G + 1], praw[:, 1:G + 1], AF.Ln, ["praw"], ["praw"])
                    actf(praw[:, 1:G + 1], praw[:, 1:G + 1], AF.Exp, ["praw"], ["praw"], scale=-0.5)
                    tt("dve", kkn, coef[:], praw[:, 1:G + 1], ALU.mult, ["coef", "praw"], ["pT1"])
                    ts("dve", coef[:], aT, pcol("ka", hp), ka1m[:, hp:hp + 1], ALU.mult, ALU.add, ["kraw", "pk", "ka1m"], ["coef"])
                    tt("dve", kmT, kf, coef[:], ALU.mult, ["acc", "coef"], ["qn"])
                    tt("pool", bT, kkn, aT, ALU.mult, ["pT1", "kraw"], ["gn"])
                    if g == 0 and hp == 0:
                        dbg_out("rT", rT, "qn"); dbg_out("kmT", kmT, "qn"); dbg_out("vT", vT, "qr"); dbg_out("aT", aT, "kraw"); dbg_out("kkn", kkn, "pT1"); dbg_out("bT", bT, "gn")
                        copy("dve", coef[:], sc[0][:], ["sc0"], ["coef"]); dbg_out("cumI", coef[:], "coef")
                        copy("dve", coef[:], sc[1][:], ["sc1"], ["coef"]); dbg_out("cumE", coef[:], "coef")
                    stop_at(43)
                    actf(Etmp, sc[1][:], AF.Exp, ["sc1"], ["hb0"], scale=-C0)
                    stt("dve", At, Etmp, -1.0, kkn, ALU.mult, ALU.mult, ["hb0", "pT1"], ["gn"])
                    actf(Etmp, sc[0][:], AF.Exp, ["sc0"], ["hb0"], scale=-C0)
                    tt("dve", Rt, rT, Etmp, ALU.mult, ["qn", "hb0"], ["qm"])
                    actf(Esq, sc[0][:], AF.Exp, ["sc0"], ["hb0"], scale=C0)
                    tt("dve", Bt, bT, Esq, ALU.mult, ["gn", "hb0"], ["qm"])
                    tt("pool", Kt, kmT, Esq, ALU.mult, ["qn", "hb0"], ["gm"])
                    ts("dve", ncL[:], sc[0][:, 127:G:128], -C0, None, ALU.mult, None, ["sc0"], ["ncL"])
                    actf(WL[:], ncL[:], AF.Exp, ["ncL"], ["WL"])
                    copy("dve", WL0[:, 0:4], WL[0:64, :], ["WL"], ["WL0"])
                    copy("dve", WL0[:, 4:8], WL[64:128, :], ["WL"], ["WL0"])
                    for c in range(4):
                        actf(Etmp[:, c * 128:(c + 1) * 128], sc[0][:, c * 128:(c + 1) * 128], AF.Exp, ["sc0", "ncL"], ["hb0"], bias=ncL[:, c:c + 1], scale=C0)
                    tt("dve", Bb, bT, Etmp, ALU.mult, ["gn", "hb0"], ["gm"])
                    tt("pool", Kb, kmT, Etmp, ALU.mult, ["qn", "hb0"], ["pT0"])
                    stop_at(44)
                    for c in range(4):
                        cs = slice(c * 128, (c + 1) * 128)
                        half = (c % 2) * 512
                        for wi, (src, skey) in enumerate(((At, "gn"), (vT, "qr"), (Bb, "gm"), (Kb, "pT0"))):
                            P.op("pe", lambda e, src=src, cs=cs, half=half, wi=wi: e.transpose(out=tp[:, half + wi * 128:half + (wi + 1) * 128], in_=src[:, cs], identity=ident[:]), [skey, "ident"], ["tp"])
                        copy(ev_eng(), tokall[:, c, :, :], tp[:, half:half + 512].rearrange("p (w f) -> p w f", w=4), ["tp"], ["tokall"])

                    P.phase = "rwkv_chain"
                    stop_at(45)

                    def r2(bank):
                        return bank[:, 0:256].rearrange("p (e f) -> p e f", e=2)

                    def hs(e):
                        return slice(64 * e, 64 * e + 64)

                    def v4(ap):
                        return ap.rearrange("p (c e f) -> p c e f", c=2, e=2)

                    def bc4(m2):
                        return m2[:, 0:1, :].broadcast_to([128, 4, 128]).rearrange("p (c e) f -> p c e f", c=2)

                    PmH, PmK = [hb0[:, 0:G], hb0[:, G:2 * G]], ["hb0/0", "hb0/1"]
                    PTH, PTK = [pT[0][:], pT[1][:]], ["pT0", "pT1"]
                    XTH, XTK = [gn[:, 0, :], kraw[:]], ["gn/0", "kraw"]
                    bkP = [(oa[0], "oa0"), (oa[1], "oa1")]
                    bkT = [(pj[0], "pj0"), (pj[1], "pj1")]
                    bkX = [(mi, "mi"), (sc[0], "sc0")]

                    def rg(bank, cl, e):
                        o = (cl * 2 + e) * 128
                        return bank[:, o:o + 128]

                    for h2 in range(2):
                        for cl in range(2):
                            cs = slice((2 * h2 + cl) * 128, (2 * h2 + cl + 1) * 128)
                            for e in range(2):
                                mm(rg(bkP[h2][0], cl, e), At[hs(e), cs], Bt[hs(e), cs], True, True, ["gn/1", "qm"], [bkP[h2][1]])
                        for cl in range(2):
                            cs = slice((2 * h2 + cl) * 128, (2 * h2 + cl + 1) * 128)
                            for e in range(2):
                                mm(rg(bkT[h2][0], cl, e), Bt[hs(e), cs], At[hs(e), cs], True, True, ["gn/1", "qm"], [bkT[h2][1]])
                    for h2 in range(2):
                        tt("dve", v4(PmH[h2]), v4(bkP[h2][0][:]), bc4(maskL2), ALU.mult, [bkP[h2][1], "maskL2"], [PmK[h2]])
                        tt("dve", v4(PTH[h2]), v4(bkT[h2][0][:]), bc4(maskU2), ALU.mult, [bkT[h2][1], "maskU2"], [PTK[h2]])
                        tt("pool", v4(XTH[h2]), v4(PTH[h2]), bc4(ident2), ALU.add, [PTK[h2], "ident2"], [XTK[h2]])
                    for lvl in range(1, 7):
                        for h2 in range(2):
                            for cl in range(2):
                                for e in range(2):
                                    mm(rg(bkP[h2][0], cl, e), v4(PTH[h2])[:, cl, e, :], v4(PmH[h2])[:, cl, e, :], True, True, [PTK[h2], PmK[h2]], [bkP[h2][1]])
                            if lvl < 6:
                                for cl in range(2):
                                    for e in range(2):
                                        mm(rg(bkT[h2][0], cl, e), v4(PmH[h2])[:, cl, e, :], v4(PTH[h2])[:, cl, e, :], True, True, [PTK[h2], PmK[h2]], [bkT[h2][1]])
                        for h2 in range(2):
                            copy("act", PmH[h2], bkP[h2][0][:], [bkP[h2][1]], [PmK[h2]])
                            if lvl < 6:
                                copy("dve" if h2 == 0 else "act", PTH[h2], bkT[h2][0][:], [bkT[h2][1]], [PTK[h2]])
                        for h2 in range(2):
                            for cl in range(2):
                                for e in range(2):
                                    mm(rg(bkX[h2][0], cl, e), v4(PmH[h2])[:, cl, e, :], v4(XTH[h2])[:, cl, e, :], True, True, [PmK[h2], XTK[h2]], [bkX[h2][1]])
                        for h2 in range(2):
                            tt("dve", XTH[h2], XTH[h2], bkX[h2][0][:], ALU.add, [XTK[h2], bkX[h2][1]], [XTK[h2]])

                    stop_at(46)
                    def v8(ap):
                        return ap.rearrange("p (c e f) -> p c e f", c=4, e=2)

                    ArbH, ArbK = [hb0[:, 0:G], hb0[:, G:2 * G]], ["hb0/0", "hb0/1"]
                    ArkH, ArkK = [pT[0][:], pT[1][:]], ["pT0", "pT1"]
                    AakH, AakK = [AakT4[:, 0, :], AakT4[:, 1, :]], ["AakT4/0", "AakT4/1"]
                    Ahat4, AhK = v8(gm[:, 1, :]), "gm/1"
                    Uhat4, UhK = v8(sgtok[:].rearrange("p c f -> p (c f)")), "sgtok"
                    AkV4v = v8(AkV4[:])
                    RhH, RhK = [oab[0:64, :], osq[0:64, :]], ["oab", "osq"]
                    Msb4 = v8(qcat[0:64, 0, :])

                    def XT(c):
                        return v4(XTH[c // 2])[:, c % 2, :, :], XTK[c // 2]

                    for h2 in range(2):
                        for cl in range(2):
                            cs = slice((2 * h2 + cl) * 128, (2 * h2 + cl + 1) * 128)
                            for e in range(2):
                                mm(rg(bkP[h2][0], cl, e), Kt[hs(e), cs], At[hs(e), cs], True, True, ["gm/0", "gn/1"], [bkP[h2][1]])
                        for cl in range(2):
                            cs = slice((2 * h2 + cl) * 128, (2 * h2 + cl + 1) * 128)
                            for e in range(2):
                                mm(rg(bkT[h2][0], cl, e), Bt[hs(e), cs], Rt[hs(e), cs], True, True, ["qm"], [bkT[h2][1]])
                        for cl in range(2):
                            cs = slice((2 * h2 + cl) * 128, (2 * h2 + cl + 1) * 128)
                            for e in range(2):
                                mm(rg(bkX[h2][0], cl, e), Kt[hs(e), cs], Rt[hs(e), cs], True, True, ["gm/0", "qm"], [bkX[h2][1]])
                    for h2 in range(2):
                        tt("dve", v4(AakH[h2]), v4(bkP[h2][0][:]), bc4(maskU2), ALU.mult, [bkP[h2][1], "maskU2"], [AakK[h2]])
                        tt("dve", v4(ArbH[h2]), v4(bkT[h2][0][:]), bc4(maskUI2), ALU.mult, [bkT[h2][1], "maskUI2"], [ArbK[h2]])
                        tt("dve", v4(ArkH[h2]), v4(bkX[h2][0][:]), bc4(maskUI2), ALU.mult, [bkX[h2][1], "maskUI2"], [ArkK[h2]])
                    for c in range(4):
                        for e in range(2):
                            o = (c * 2 + e) * 64
                            mm(oa[0][:, o:o + 64], v4(AakH[c // 2])[:, c % 2, e, :], tokall[:, c, 1, hs(e)], True, True, [AakK[c // 2], "tokall"], ["oa0"])
                    copy("act", AkV4[:], oa[0][:], ["oa0"], ["AkV4"])
                    for c in range(4):
                        xt_, xk_ = XT(c)
                        for e in range(2):
                            o = (c * 2 + e) * 64
                            mm(oa[1][:, o:o + 64], xt_[:, e, :], tokall[:, c, 0, hs(e)], True, True, [xk_, "tokall"], ["oa1"])
                    copy("dve", gm[:, 1, :], oa[1][:], ["oa1"], [AhK])
                    for c in range(4):
                        xt_, xk_ = XT(c)
                        for e in range(2):
                            o = (c * 2 + e) * 64
                            mm(pj[0][:, o:o + 64], xt_[:, e, :], AkV4v[:, c, e, :], True, True, [xk_, "AkV4"], ["pj0"])
                    copy("act", sgtok[:].rearrange("p c f -> p (c f)"), pj[0][:], ["pj0"], [UhK])
                    for h2 in range(2):
                        for cl in range(2):
                            c = 2 * h2 + cl
                            cs = slice(c * 128, (c + 1) * 128)
                            for e in range(2):
                                reg = rg(bkX[h2][0], cl, e)[0:64, :]
                                mm(reg, Ahat4[:, c, e, :], v4(ArbH[h2])[:, cl, e, :], True, False, [AhK, ArbK[h2]], [bkX[h2][1]])
                                mm(reg, ident[hs(e), hs(e)], Rt[hs(e), cs], False, True, ["ident", "qm"], [bkX[h2][1]])
                        copy("dve" if h2 == 0 else "act", RhH[h2], bkX[h2][0][0:64, :], [bkX[h2][1]], [RhK[h2]])
                    for c in range(4):
                        for e in range(2):
                            o = (c * 2 + e) * 64
                            mm(pj[1][0:64, o:o + 64], Ahat4[:, c, e, :], tokall[:, c, 2, hs(e)], True, True, [AhK, "tokall"], ["pj1"])
                    copy("act", qcat[0:64, 0, :], pj[1][0:64, :], ["pj1"], ["qcat"])
                    stop_at(47)
                    for c in range(4):
                        cs = slice(c * 128, (c + 1) * 128)
                        h2, cl = c // 2, c % 2
                        rh = v4(RhH[h2])
                        for e in range(2):
                            reg = sc[1][0:64, e * 128:(e + 1) * 128]
                            mm(reg, STb[:, 2 * hp + e, :], rh[:, cl, e, :], True, False, ["STb", RhK[h2]], ["sc1"])
                            mm(reg, Uhat4[:, c, e, :], v4(ArbH[h2])[:, cl, e, :], False, False, [UhK, ArbK[h2]], ["sc1"])
                            mm(reg, tokall[:, c, 1, hs(e)], v4(ArkH[h2])[:, cl, e, :], False, True, ["tokall", ArkK[h2]], ["sc1"])
                        copy("act", yraw[0:64, cs], sc[1][0:64, 0:128], ["sc1"], ["acc"])
                        copy("act", yraw[64:128, cs], sc[1][0:64, 128:256], ["sc1"], ["acc"])
                        ob_, obk = oa[c % 2], "oa%d" % (c % 2)
                        for e in range(2):
                            reg = ob_[0:64, e * 64:(e + 1) * 64]
                            mm(reg, tokall[:, c, 2, hs(e)], Uhat4[:, c, e, :], True, False, ["tokall", UhK], [obk])
                            mm(reg, tokall[:, c, 3, hs(e)], tokall[:, c, 1, hs(e)], False, False, ["tokall"], [obk])
                            mm(reg, Msb4[:, c, e, :], STb[:, 2 * hp + e, :], False, True, ["qcat", "STb"], [obk])
                        for e in range(2):
                            stt("dve", STb[:, 2 * hp + e, :], STb[:, 2 * hp + e, :], WL0[:, 4 * e + c:4 * e + c + 1], ob_[0:64, e * 64:(e + 1) * 64], ALU.mult, ALU.add, ["STb", "WL0", obk], ["STb"])

                    if g == 0 and hp == 0:
                        dbg_out("yraw", yraw, "acc")
                    P.phase = "rwkv"
                    stop_at(49)
                    copy("act", Etmp, yraw, ["acc"], ["hb0"])
                    actf(Esq, yraw, AF.Square, ["acc"], ["hb0"])
                    mm(sc[0][:], blkavg[:], Etmp, True, True, ["blkavg", "hb0"], ["sc0"])
                    mm(sc[1][:], blkavg[:], Esq, True, True, ["blkavg", "hb0"], ["sc1"])
                    actf(coef[:], sc[0][:], AF.Square, ["sc0"], ["coef"])
                    tt("dve", coef[:], sc[1][:], coef[:], ALU.subtract, ["sc1", "coef"], ["coef"])
                    ts("dve", coef[:], coef[:], 64e-5, None, ALU.add, None, ["coef"], ["coef"])
                    actf(coef[:], coef[:], AF.Ln, ["coef"], ["coef"])
                    actf(coef[:], coef[:], AF.Exp, ["coef"], ["coef"], scale=-0.5)
                    tt("dve", yraw, yraw, sc[0][:], ALU.subtract, ["acc", "sc0"], ["acc"])
                    tt("dve", yraw, yraw, coef[:], ALU.mult, ["acc", "coef"], ["acc"])
                    ts("dve", yraw, yraw, pcol("lnw", hp), pcol("lnb", hp), ALU.mult, ALU.add, ["acc", "pk"], ["acc"])
                    tt("pool", Etmp, rT, kmT, ALU.mult, ["qn"], ["hb0"])
                    ts("dve", Esq, Etmp, pcol("rk", hp), None, ALU.mult, None, ["hb0", "pk"], ["hb0"])
                    mm(sc[0][:], blk1[:], Esq, True, True, ["blk1", "hb0"], ["sc0"])
                    tt("dve", coef[:], sc[0][:], vT, ALU.mult, ["sc0", "qr"], ["coef"])
                    tt("dve", yraw, yraw, coef[:], ALU.add, ["acc", "coef"], ["acc"])
                    tt("dve", yT[:, hp, :], yraw, gT, ALU.mult, ["acc", "qr"], ["yT"])

                P.phase = "mem"
                stop_at(6)
                for pr in range(2):
                    pb, pkey = project(25 + pr)
                    P.op("act", lambda e, pb=pb, pr=pr: e.activation(out=qm[:, pr, :], in_=pb[:], func=AF.Copy, scale=0.125), [pkey], ["qm"])
                    pb, pkey = project(27 + pr)
                    P.op("act", lambda e, pb=pb, pr=pr: e.activation(out=gm[:, pr, :], in_=pb[:], func=AF.Silu), [pkey], ["gm"])
                stop_at(61)
                for h in range(4):
                    pr, hb_ = h // 2, (h % 2) * 64
                    ob = oa[h % 2]
                    okey = "oa%d" % (h % 2)
                    for mc in range(2):
                        sb_ = sc[mc]
                        mm(sb_[:], memkT[hb_:hb_ + 64, pr, mc * 128:(mc + 1) * 128], qm[hb_:hb_ + 64, pr, :], True, True, ["memkT", "qm"], ["sc%d" % mc])
                        P.op("act", lambda e, sb_=sb_, mc=mc: e.activation(out=pT[mc][:], in_=sb_[:], func=AF.Exp), ["sc%d" % mc], ["pT%d" % mc])
                    stop_at(62 if h == 0 else 66)
                    for mc in range(2):
                        mm(ob[0:65, :], memvA[:, mc, h, :], pT[mc][:], mc == 0, mc == 1, ["memvA", "pT%d" % mc], [okey])
                    stop_at(63 if h == 0 else 67)
                    o, w = PK["memg"]
                    finalize_head(ob, okey, True, hb_, pk[hb_:hb_ + 64, o + pr:o + pr + 1], gm[hb_:hb_ + 64, pr, :], "gm", yT[hb_:hb_ + 64, 6 + pr, :], "yT")


                P.phase = "nsa"
                P.dma("sp", lambda e, t0=t0: e.dma_start(out=ropeC[:], in_=ropeC_d[:, t0:t0 + G]), writes=["ropeC"])
                P.dma("sp", lambda e, t0=t0: e.dma_start(out=ropeS[:], in_=ropeS_d[:, t0:t0 + G]), writes=["ropeS"])

                def rope(src_t, skey, dst_ap, dkey, nr=128):
                    mm(mi[:], pm[:], src_t, True, True, ["pm", skey], ["mi"])
                    P.op("dve", lambda e: e.tensor_tensor(out=rtmp[0:nr, :], in0=mi[0:nr, :], in1=ropeS[0:nr, :], op=ALU.mult), ["mi", "ropeS"], ["coef"])
                    P.op("pool", lambda e: e.tensor_tensor(out=dst_ap, in0=src_t[0:nr, :], in1=ropeC[0:nr, :], op=ALU.mult), [skey, "ropeC"], [dkey])
                    P.op("dve", lambda e: e.tensor_tensor(out=dst_ap, in0=dst_ap, in1=rtmp[0:nr, :], op=ALU.add), [dkey, "coef"], [dkey])

                for pr in range(2):
                    pb, pkey = project(17 + pr)
                    P.op("act", lambda e, pb=pb, pr=pr: e.activation(out=qn[:, pr, :], in_=pb[:], func=AF.Copy, scale=0.125), [pkey], ["qn"])
                    rope(qn[:, pr, :], "qn", qr[:, pr, :], "qr")
                    pb, pkey = project(19 + pr)
                    P.op("act", lambda e, pb=pb, pr=pr: e.activation(out=gn[:, pr, :], in_=pb[:], func=AF.Silu), [pkey], ["gn"])
                copy("dve", kcvc[:, 0:16], kcvc[:, G:G + 16], ["kcvc"], ["kcvc"])
                pb, pkey = project(21)
                copy("act", kcvc[:, 16:16 + G], pb[:], [pkey], ["kcvc"])
                pb, pkey = project(22)
                copy("act", kraw[:], pb[:], [pkey], ["kraw"])
                rope(kraw[:], "kraw", ksT[0:64, t0:t0 + G], "ksT", 64)
                pb, pkey = project(23)
                copy("act", kraw[:], pb[:], [pkey], ["kraw"])
                wo = (g % 2) * G
                rope(kraw[:], "kraw", kwT[0:64, wo:wo + G], "kwT", 64)
                for i in range(4):
                    pbv = pj[i % 2]
                    for k in range(8):
                        mm(pbv[:, 0:128], hT[:, k, i * 128:(i + 1) * 128], wb[:, k, 24 * 128:25 * 128], k == 0, k == 7, ["hT", "wb"], ["pj%d" % (i % 2)])
                    kt = 4 * g + i
                    copy("dve", vsA[:, kt, 0:64], pbv[:, 0:64], ["pj%d" % (i % 2)], ["vsA"])
                    copy("act", vwA[:, kt % 8, 0:64], pbv[:, 64:128], ["pj%d" % (i % 2)], ["vwA"])
                pb, pkey = project(29, 12)
                o_, w_ = PK["gateb"]
                P.op("act", lambda e, pb=pb, o_=o_: e.activation(out=gsig[:], in_=pb[0:12, :], func=AF.Sigmoid, bias=pk[0:12, o_:o_ + 1]), [pkey, "pk"], ["gsig"])

                lo, hi = max(0, 32 * g - 1), 32 * g + 30
                n = hi - lo + 1
                c0 = 16 * lo + 16 - t0
                for kv in range(2):
                    b0 = kv * 64
                    reg = mi[0:64, kv * 64:kv * 64 + n]
                    for l in range(32):
                        mm(reg, w1b[b0:b0 + 64, l, :], kcvc[b0:b0 + 64, c0 + l:c0 + l + 16 * (n - 1) + 1:16], l == 0, l == 31, ["w1b", "kcvc"], ["mi"])
                    dsth = (h1k, h1v)[kv]
                    P.op("act", lambda e, reg=reg, dsth=dsth, kv=kv, n=n: e.activation(out=dsth[:, 0:n], in_=reg, func=AF.Silu, bias=ccst[:, kv:kv + 1]), ["mi", "ccst"], ["h1%d" % kv])
                mm(sc[0][:, 0:n], w2b[:, 0:128], h1k[:, 0:n], True, True, ["w2b", "h10"], ["sc0"])
                copy("dve", kcmpT[:, lo:hi + 1], sc[0][:, 0:n], ["sc0"], ["kcmpT"])
                mm(sc[1][0:n, 0:64], h1v[:, 0:n], w2b[:, 128:192], True, True, ["w2b", "h11"], ["sc1"])
                copy("dve", vstg[0:n, 0:64], sc[1][0:n, 0:64], ["sc1"], ["vstg"])
                i0 = lo
                while i0 <= hi:
                    i1 = min(hi, (i0 // 128) * 128 + 127)
                    P.dma("sp", lambda e, i0=i0, i1=i1, lo=lo: e.dma_start(out=vcmpA[i0 % 128:i1 % 128 + 1, i0 // 128, :], in_=vstg[i0 - lo:i1 - lo + 1, :]), reads=["vstg"], writes=["vcmpA"])
                    i0 = i1 + 1

                def combine(ob, okey, h, b, first):
                    pr, hb_ = h // 2, (h % 2) * 64
                    sl = slice(hb_, hb_ + 64)
                    copy("dve", oab[:], ob[0:65, :], [okey], ["oab"])
                    copy("act", oaug[sl, :], ob[0:64, :], [okey], ["oaug"])
                    mm(pj[0][:], e64[:], oab[:], True, True, ["e64", "oab"], ["pj0"])
                    P.op("dve", lambda e: e.tensor_scalar(out=gsr[:], in0=gsig[:], scalar1=ident[0:12, 3 * h + b:3 * h + b + 1], scalar2=None, op0=ALU.mult), ["gsig", "ident"], ["gsr"])
                    mm(pj[1][:], ones12[:], gsr[:], True, True, ["ones12", "gsr"], ["pj1"])
                    P.op("dve", lambda e: e.tensor_scalar(out=coef[sl, :], in0=pj[0][sl, :], scalar1=1e-30, scalar2=None, op0=ALU.max), ["pj0"], ["coef"])
                    P.op("act", lambda e: e.activation(out=coef[sl, :], in_=coef[sl, :], func=AF.Ln), ["coef"], ["coef"])
                    P.op("act", lambda e: e.activation(out=coef[sl, :], in_=coef[sl, :], func=AF.Exp, scale=-1.0), ["coef"], ["coef"])
                    P.op("dve", lambda e: e.tensor_tensor(out=coef[sl, :], in0=coef[sl, :], in1=pj[1][sl, :], op=ALU.mult), ["coef", "pj1"], ["coef"])
                    if first:
                        P.op("dve", lambda e: e.tensor_tensor(out=acc[sl, pr, :], in0=coef[sl, :], in1=oaug[sl, :], op=ALU.mult), ["coef", "oaug"], ["acc"])
                    else:
                        P.op("dve", lambda e: e.tensor_tensor(out=coef[sl, :], in0=coef[sl, :], in1=oaug[sl, :], op=ALU.mult), ["coef", "oaug"], ["coef"])
                        P.op("dve", lambda e: e.tensor_tensor(out=acc[sl, pr, :], in0=acc[sl, pr, :], in1=coef[sl, :], op=ALU.add), ["coef", "acc"], ["acc"])

                def pmask(ap, key, base, cm, step):
                    P.op("pool", lambda e: e.affine_select(out=ap, in_=ap, pattern=[[step, G]], compare_op=ALU.is_ge, fill=0.0, base=base, channel_multiplier=cm), [key], [key])

                def pmask(ap, key, base, cm, step):
                    P.op("pool", lambda e: e.affine_select(out=ap, in_=ap, pattern=[[step, G]], compare_op=ALU.is_ge, fill=0.0, base=base, channel_multiplier=cm), [key], [key])

                P.op("pool", lambda e: e.memset(impS[:], 0.0), [], ["impS"])
                ncc = g // 4 + 1
                for h in range(4):
                    pr, hb_ = h // 2, (h % 2) * 64
                    ob, okey = oa[h % 2], "oa%d" % (h % 2)
                    for ci in range(ncc):
                        dl = 2048 * ci - 512 * g
                        sb_, skey = sc[ci % 2], "sc%d" % (ci % 2)
                        mm(sb_[:], kcmpT[hb_:hb_ + 64, ci * 128:(ci + 1) * 128], qn[hb_:hb_ + 64, pr, :], True, True, ["kcmpT", "qn"], [skey])
                        P.op("act", lambda e, sb_=sb_, ci=ci: e.activation(out=pTc[:, ci, :], in_=sb_[:], func=AF.Exp), [skey], ["wmkvb"])
                        if dl >= -2048:
                            pmask(pTc[:, ci, :], "wmkvb", -31 - dl, -16, 1)
                        mm(ob[0:65, :], vcmpA[:, ci, :], pTc[:, ci, :], ci == 0, ci == ncc - 1, ["vcmpA", "wmkvb"], [okey])
                    for j in range(4):
                        reg = mi[:, (j % 2) * 256:(j % 2) * 256 + 129]
                        for ci in range(ncc):
                            mm(reg, pTc[:, ci, j * 128:(j + 1) * 128], ovl[:, ci, :], ci == 0, ci == ncc - 1, ["wmkvb", "ovl"], ["mi"])
                        P.op("dve", lambda e, reg=reg: e.tensor_scalar(out=m8[:, 0:1], in0=reg[:, 128:129], scalar1=1e-30, scalar2=None, op0=ALU.max), ["mi"], ["m8"])
                        P.op("dve", lambda e: e.reciprocal(out=m8[:, 0:1], in_=m8[:, 0:1]), ["m8"], ["m8"])
                        P.op("dve", lambda e, reg=reg, j=j: e.scalar_tensor_tensor(out=impS[:, j, :], in0=reg[:, 0:128], scalar=m8[:, 0:1], in1=impS[:, j, :], op0=ALU.mult, op1=ALU.add), ["mi", "m8", "impS"], ["impS"])
                    combine(ob, okey, h, 0, True)

                for j in range(4):
                    qt = 4 * g + j
                    u0 = 126 - 2 * qt
                    P.op("dve", lambda e, j=j, u0=u0: e.tensor_tensor(out=score[:], in0=impS[:, j, :], in1=addw[:, u0:u0 + 128], op=ALU.add), ["impS", "addw"], ["score"])
                    P.op("dve", lambda e: e.tensor_scalar(out=score[:, 0:1], in0=score[:, 0:1], scalar1=1e4, scalar2=None, op0=ALU.add), ["score"], ["score"])
                    P.op("dve", lambda e: e.max(out=m8[:, 0:8], in_=score[:]), ["score"], ["m8"])
                    P.op("dve", lambda e: e.match_replace(out=stmp[:], in_to_replace=m8[:, 0:8], in_values=score[:], imm_value=-2.0), ["score", "m8"], ["stmp"])
                    P.op("dve", lambda e: e.max(out=m8[:, 8:16], in_=stmp[:]), ["stmp"], ["m8"])
                    P.op("dve", lambda e: e.tensor_reduce(out=m8[:, 0:1], in_=m8[:, 8:16], axis=AX.X, op=ALU.min), ["m8"], ["m8"])
                    P.op("dve", lambda e: e.tensor_scalar(out=stmp[:], in0=score[:], scalar1=m8[:, 0:1], scalar2=None, op0=ALU.is_ge), ["score", "m8"], ["stmp"])
                    P.op("dve", lambda e: e.tensor_scalar(out=nbq[:], in0=stmp[:], scalar1=1.0, scalar2=-NEG, op0=ALU.subtract, op1=ALU.mult), ["stmp"], ["nbq"])
                    P.op("pe", lambda e: e.transpose(out=tp[:, 0:128], in_=nbq[:], identity=ident[:]), ["nbq", "ident"], ["tp"])
                    copy("dve", nb[:, j * 128:(j + 1) * 128], tp[:, 0:128], ["tp"], ["nb"])

                P.phase = "nsa_sel"
                scA = [(sc[0], "sc0"), (sc[1], "sc1")]
                scB = [(pj[0], "pj0"), (pj[1], "pj1")]
                pTA = [(pT[0][:], "pT0"), (pT[1][:], "pT1")]
                pTB = [(AakT4[:, 0, :], "AakT4/0"), (AakT4[:, 1, :], "AakT4/1")]
                nslab = (4 * g + 3) // 32 + 1
                for pr in range(2):
                    for hl in range(2):
                        for M in range(nslab):
                            copy(("act", "dve")[(hl + M) % 2], qcat[0:64, 2 * hl + M, :], qr[64 * hl:64 * hl + 64, pr, :], ["qr"], ["qcat"])
                            copy(("dve", "pool")[hl], qcat[64:128, 2 * hl + M, :], nb[64 * M:64 * M + 64, :], ["nb"], ["qcat"])
                    nkt = 4 * g + 4

                    def sel_a(kt):
                        (sa, ska), (sb2, skb) = scA[kt % 2], scB[kt % 2]
                        (pa, pka), (pb2, pkb) = pTA[kt % 2], pTB[kt % 2]
                        M = kt // 32
                        ks_ = slice(kt * 128, (kt + 1) * 128)
                        mm(sa[:], ksT[:, ks_], qcat[:, M, :], True, True, ["ksT", "qcat"], [ska])
                        mm(sb2[:], ksT[:, ks_], qcat[:, 2 + M, :], True, True, ["ksT", "qcat"], [skb])
                        P.op("act", lambda e: e.activation(out=pa, in_=sa[:], func=AF.Exp), [ska], [pka])
                        P.op("act", lambda e: e.activation(out=pb2, in_=sb2[:], func=AF.Exp), [skb], [pkb])
                        if kt >= 4 * g:
                            pmask(pa, pka, -128 * (kt - 4 * g), -1, 1)
                            pmask(pb2, pkb, -128 * (kt - 4 * g), -1, 1)

                    sel_a(0)
                    for kt in range(nkt):
                        if kt + 1 < nkt:
                            sel_a(kt + 1)
                        mm(oa[0][0:65, :], vsA[:, kt, :], pTA[kt % 2][0], kt == 0, kt == nkt - 1, ["vsA", pTA[kt % 2][1]], ["oa0"])
                        mm(oa[1][0:65, :], vsA[:, kt, :], pTB[kt % 2][0], kt == 0, kt == nkt - 1, ["vsA", pTB[kt % 2][1]], ["oa1"])
                    combine(oa[0], "oa0", 2 * pr, 1, False)
                    combine(oa[1], "oa1", 2 * pr + 1, 1, False)

                    P.phase = "nsa_win"
                    kts = [kt for kt in range(4 * g - 4, 4 * g + 4) if kt >= 0]

                    def win_a(kt):
                        (sa, ska), (sb2, skb) = scA[kt % 2], scB[kt % 2]
                        (pa, pka), (pb2, pkb) = pTA[kt % 2], pTB[kt % 2]
                        ro = (kt % 8) * 128
                        mm(sa[:], kwT[:, ro:ro + 128], qcat[:, 0, :], True, True, ["kwT", "qcat"], [ska])
                        mm(sb2[:], kwT[:, ro:ro + 128], qcat[:, 2, :], True, True, ["kwT", "qcat"], [skb])
                        P.op("act", lambda e: e.activation(out=pa, in_=sa[:], func=AF.Exp), [ska], [pka])
                        P.op("act", lambda e: e.activation(out=pb2, in_=sb2[:], func=AF.Exp), [skb], [pkb])
                        rel = kt - 4 * g
                        for p_, k_ in ((pa, pka), (pb2, pkb)):
                            if rel >= 0:
                                pmask(p_, k_, -128 * rel, -1, 1)
                            else:
                                pmask(p_, k_, 511 + 128 * rel, 1, -1)

                    win_a(kts[0])
                    for ii, kt in enumerate(kts):
                        if ii + 1 < len(kts):
                            win_a(kts[ii + 1])
                        mm(oa[0][0:65, :], vwA[:, kt % 8, :], pTA[kt % 2][0], ii == 0, ii == len(kts) - 1, ["vwA", pTA[kt % 2][1]], ["oa0"])
                        mm(oa[1][0:65, :], vwA[:, kt % 8, :], pTB[kt % 2][0], ii == 0, ii == len(kts) - 1, ["vwA", pTB[kt % 2][1]], ["oa1"])
                    combine(oa[0], "oa0", 2 * pr, 2, False)
                    combine(oa[1], "oa1", 2 * pr + 1, 2, False)
                    P.phase = "nsa_sel"

                P.phase = "nsa"
                o_, w_ = PK["nsag"]
                for h in range(4):
                    pr, hb_ = h // 2, (h % 2) * 64
                    finalize_head(acc[hb_:hb_ + 64, pr, :], "acc", False, hb_, pk[hb_:hb_ + 64, o_ + pr:o_ + pr + 1], gn[hb_:hb_ + 64, pr, :], "gn", yT[hb_:hb_ + 64, 4 + pr, :], "yT", from_sbuf=True)
                if g == 0:
                    dbg_out("yT", yT[:], "yT")
                P.phase = ""
                stop_at(7)
                for i in range(4):
                    xb, xk = xt[i % 2], "xt%d" % (i % 2)
                    P.dma("sp", lambda e, i=i, t0=t0, xb=xb: e.dma_start(out=xb[:], in_=x_d[t0 + i * 128:t0 + (i + 1) * 128, :]), writes=[xk])
                    for half in range(2):
                        pb = pj[half]
                        for k in range(8):
                            mm(pb[:], yT[:, k, i * 128:(i + 1) * 128], woutb[:, k, half * 512:(half + 1) * 512], k == 0, k == 7, ["yT", "woutb"], ["pj%d" % half])
                        P.op("dve", lambda e, pb=pb, xb=xb, half=half: e.tensor_tensor(out=xb[:, half * 512:(half + 1) * 512], in0=xb[:, half * 512:(half + 1) * 512], in1=pb[:], op=ALU.add),
                             ["pj%d" % half, xk], [xk])
                    sk = "st%d" % (8 + i)
                    ssq = st[:, 8 + i:9 + i]
                    hjk = hb[i % 2]
                    P.op("act", lambda e, xb=xb, ssq=ssq, hjk=hjk: e.activation(out=hjk[:], in_=xb[:], func=AF.Square, accum_out=ssq), [xk], ["hb0", sk])
                    P.op("dve", lambda e, ssq=ssq: e.tensor_scalar(out=ssq, in0=ssq, scalar1=1.0 / D, scalar2=1e-6, op0=ALU.mult, op1=ALU.add), [sk], [sk])
                    P.op("dve", lambda e, ssq=ssq: e.reciprocal(out=ssq, in_=ssq), [sk], [sk])
                    P.op("act", lambda e, ssq=ssq: e.activation(out=ssq, in_=ssq, func=AF.Sqrt), [sk], [sk])
                    P.op("dve", lambda e, xb=xb, ssq=ssq: e.scalar_tensor_tensor(out=xb[:], in0=xb[:], scalar=ssq, in1=gfin[:], op0=ALU.mult, op1=ALU.mult), [xk, sk, "gfin"], [xk])
                    P.dma("pool", lambda e, i=i, t0=t0, xb=xb: e.dma_start(out=out_d[t0 + i * 128:t0 + (i + 1) * 128, :], in_=xb[:]), reads=[xk], is_out=True)
        except _Stop:
            P.dma("pool", lambda e: e.dma_start(out=out_d[0:128, :], in_=xt[0][:]), reads=["xt0"], is_out=True)
        P.finish("sp")
        P.emit(block, sems, dsems)
    return nc


def make_in_maps(inputs, T, batches):
    ci = _colidx()
    w_in = np.asarray(inputs["w_in"][0])
    wcat = np.ascontiguousarray(w_in[:, ci].reshape(8, 128, NCOL))
    wout = np.ascontiguousarray(np.asarray(inputs["w_out"][0]).reshape(8, 128, D))
    wmkv = np.ascontiguousarray(np.asarray(inputs["w_mem_kv"][0]).reshape(8, 128, 512))
    pk = host_params({k: np.asarray(v) for k, v in inputs.items()})
    w1k = np.asarray(inputs["nsa_cmp_k_w1"][0]).reshape(32, 64, 64).transpose(1, 0, 2)
    w1v = np.asarray(inputs["nsa_cmp_v_w1"][0]).reshape(32, 64, 64).transpose(1, 0, 2)
    w1 = np.ascontiguousarray(np.concatenate([w1k, w1v], 0).reshape(128, 2048))
    w2k = np.asarray(inputs["nsa_cmp_k_w2"][0]); w2v = np.asarray(inputs["nsa_cmp_v_w2"][0])
    w2 = np.zeros((128, 192), np.float32)
    w2[0:64] = np.concatenate([w2k, w2k, w2v], 1)
    lora = np.ascontiguousarray(np.concatenate([np.asarray(inputs["rwkv_w_up"][0]), np.asarray(inputs["rwkv_a_up"][0])], 0))
    w0row = np.ascontiguousarray(np.asarray(inputs["rwkv_w0"][0]).reshape(1, 512))
    consts = host_consts(T)
    maps = []
    for b in batches:
        m = {
            "x": np.ascontiguousarray(np.asarray(inputs["x"][b][:T])),
            "mem": np.ascontiguousarray(np.asarray(inputs["mem"][b])),
            "wcat": wcat, "wout": wout, "wmkv": wmkv, "pk": pk, "w1": w1, "w2": w2, "lora": lora, "w0row": w0row,
            "gfin": np.ascontiguousarray(np.asarray(inputs["norm_final_g"]).reshape(1, D)),
        }
        m.update(consts)
        maps.append(m)
    return maps


_NC_CACHE = {}


def kernel(**inputs):
    T = inputs["x"].shape[1]
    B = inputs["x"].shape[0]
    if T not in _NC_CACHE:
        _NC_CACHE[T] = build_nc(T)
    nc = _NC_CACHE[T]
    batches = [i % B for i in range(8)]
    maps = make_in_maps(inputs, T, batches)
    res = run_bass_kernel_spmd(nc, maps, core_ids=list(range(8)))
    out = np.stack([res.results[b]["out"] for b in range(B)], axis=0)
    return out.astype(np.float32)
```

```python
import numpy as np
import ml_dtypes
from contextlib import ExitStack
import concourse.bass as bass
import concourse.mybir as mybir
from concourse.bass_utils import run_bass_kernel_spmd

F32 = mybir.dt.float32
BF16 = mybir.dt.bfloat16
ALU = mybir.AluOpType
AF = mybir.ActivationFunctionType
AX = mybir.AxisListType

ENGS = ("pe", "act", "dve", "pool", "sp")
N_DMA_SEMS = 24
import os as _os0
SAME_ENG_SYNC = _os0.environ.get("KSAME", "1") == "1"
NEG = -30000.0

D = 1024
NCOL = 29 * 128 + 12
G = 512
PSUM_PREFIX = ("pj", "sc", "oa", "mi", "tp")


class Prog:
    def __init__(self, nc):
        self.nc = nc
        self.lists = {e: [] for e in ENGS}
        self.cnt = {e: 0 for e in ENGS}
        self.seen = {e: {} for e in ENGS}
        self.clock_at = {e: [None] for e in ENGS}
        self.buf = {}
        self.dma_val = [0] * N_DMA_SEMS
        self.dma_clock = [[] for _ in range(N_DMA_SEMS)]
        self.dma_rr = 0
        self.out_toks = []
        self.phase = ""
        self.skip = set()
        self.children = {}

    def _deps(self, eng, reads, writes):
        need = {}

        def add(tok):
            if tok is None:
                return
            src, val = tok
            if src == eng and (eng == "pe" or not SAME_ENG_SYNC):
                return
            if need.get(src, 0) < val:
                need[src] = val

        def related(k):
            ks = [k]
            if "/" in k:
                ks.append(k.split("/")[0])
            else:
                ks.extend(self.children.get(k, ()))
            return ks

        for k0 in reads:
            for k in related(k0):
                st = self.buf.get(k)
                if st:
                    add(st[0])
                    if k[:2] in PSUM_PREFIX:
                        for r in st[1]:
                            if r[0] != eng:
                                add(r)
        for k0 in writes:
            for k in related(k0):
                st = self.buf.get(k)
                if st:
                    add(st[0])
                    for r in st[1]:
                        add(r)
        seen = self.seen[eng]
        waits = []
        for src, val in need.items():
            if seen.get(src, 0) >= val:
                continue
            waits.append((src, val))
            if isinstance(src, str):
                snap = self.clock_at[src][val]
            else:
                snap = self.dma_clock[src[1]][val // 16 - 1]
            for s2, v2 in snap.items():
                if seen.get(s2, 0) < v2:
                    seen[s2] = v2
            seen[src] = val
        return waits

    def _mark(self, tok, reads, writes):
        for k in list(reads) + list(writes):
            if "/" in k:
                self.children.setdefault(k.split("/")[0], set()).add(k)
        for k in reads:
            st = self.buf.setdefault(k, [None, []])
            st[1].append(tok)
            if len(st[1]) > 64:
                st[1] = st[1][-64:]
        for k in writes:
            self.buf[k] = [tok, []]
            if "/" not in k:
                for ch in self.children.get(k, ()):
                    self.buf[ch] = [tok, []]

    def op(self, eng, fn, reads=(), writes=(), self_wait=None):
        if self.phase in self.skip:
            return
        waits = self._deps(eng, reads, writes)
        if self_wait is not None and self.seen[eng].get(eng, 0) < self_wait:
            waits.append((eng, self_wait))
            self.seen[eng][eng] = self_wait
        self.cnt[eng] += 1
        c = self.cnt[eng]
        snap = dict(self.seen[eng])
        snap[eng] = c
        self.clock_at[eng].append(snap)
        self.lists[eng].append(("op", fn, waits, None))
        self._mark((eng, c), reads, writes)

    def dma(self, eng, fn, reads=(), writes=(), is_out=False):
        if self.phase in self.skip:
            return None
        s = self.dma_rr
        self.dma_rr = (self.dma_rr + 1) % N_DMA_SEMS
        waits = self._deps(eng, reads, writes)
        src = ("dma", s)
        prev = self.dma_val[s]
        if prev and self.seen[eng].get(src, 0) < prev:
            waits.append((src, prev))
            self.seen[eng][src] = prev
        val = prev + 16
        self.dma_val[s] = val
        self.dma_clock[s].append(dict(self.seen[eng]))
        self.lists[eng].append(("dma", fn, waits, (s, val)))
        self._mark((src, val), reads, writes)
        if is_out:
            self.out_toks.append((src, val))
        return (src, val)

    def finish(self, eng="sp"):
        waits = []
        for s in range(N_DMA_SEMS):
            if self.dma_val[s]:
                waits.append((("dma", s), self.dma_val[s]))
        self.lists[eng].append(("wait", None, waits, None))

    def emit(self, block, sems, dsems):
        engobj = {"pe": "tensor", "act": "scalar", "dve": "vector", "pool": "gpsimd", "sp": "sync"}

        def semof(src):
            return sems[src] if isinstance(src, str) else dsems[src[1]]

        def make(ename):
            lst = self.lists[ename]

            def body(e):
                for kind, fn, waits, extra in lst:
                    for src, val in waits:
                        e.wait_ge(semof(src), val)
                    if kind == "op":
                        fn(e).then_inc(sems[ename], 1)
                    elif kind == "dma":
                        fn(e).then_inc(dsems[extra[0]], 16)

            return body

        for ename in ENGS:
            if self.lists[ename]:
                getattr(block, engobj[ename])(make(ename))


RW, NSAB, MEMB = 0, 2176, 3084


def _colidx():
    idx = []
    for hp in range(4):
        for base in (0, 512, 1024, 1536):
            idx += list(range(base + 128 * hp, base + 128 * hp + 128))
    idx += list(range(2048, 2176))
    q0, g0 = NSAB, NSAB + 256
    idx += list(range(q0, q0 + 256))
    idx += list(range(g0, g0 + 256))
    kc, vc, ks, vs, kw, vw = (NSAB + 524 + 64 * i for i in range(6))
    idx += list(range(kc, kc + 64)) + list(range(vc, vc + 64))
    idx += list(range(ks, ks + 64)) * 2
    idx += list(range(kw, kw + 64)) * 2
    idx += list(range(vs, vs + 64)) + list(range(vw, vw + 64))
    idx += list(range(MEMB, MEMB + 512))
    idx += list(range(NSAB + 512, NSAB + 524))
    assert len(idx) == NCOL
    return np.array(idx)


def _bf(a):
    return np.ascontiguousarray(a.astype(ml_dtypes.bfloat16))


def host_consts(T):
    c = {}
    c["ident"] = _bf(np.eye(128, dtype=np.float32))
    half = 8
    inv = (np.float32(500000.0) ** (-np.arange(half, dtype=np.float32) / np.float32(half))).astype(np.float32)
    ang = np.arange(T, dtype=np.float32)[None, :] * inv[:, None]
    C = np.ones((64, T), np.float32)
    S = np.zeros((64, T), np.float32)
    C[0:8] = np.cos(ang); C[8:16] = np.cos(ang)
    S[0:8] = -np.sin(ang); S[8:16] = np.sin(ang)
    c["ropeC"] = np.ascontiguousarray(np.concatenate([C, C], 0))
    c["ropeS"] = np.ascontiguousarray(np.concatenate([S, S], 0))
    pm = np.zeros((128, 128), np.float32)
    for hb in (0, 64):
        for d in range(8):
            pm[hb + d + 8, hb + d] = 1.0
            pm[hb + d, hb + d + 8] = 1.0
    c["pm"] = _bf(pm)
    ovl = np.zeros((512, 129), np.float32)
    for i in range(511):
        for sblk in range(128):
            o = min(16 * i + 32, 64 * sblk + 64) - max(16 * i, 64 * sblk)
            if o > 0:
                ovl[i, sblk] = o / 32.0
    ovl[:, 128] = 1.0
    c["ovl"] = _bf(ovl.reshape(4, 128, 129).transpose(1, 0, 2))
    addw = np.zeros((128, 256), np.float32)
    for pp in range(128):
        cur = 1 if pp >= 64 else 0
        for u in range(256):
            sp = u - 126
            valid = sp <= cur
            forced = sp in (cur, cur - 1)
            addw[pp, u] = (0.0 if valid else -1.0) + (1e4 if forced else 0.0)
    c["addw"] = addw
    selpat = np.zeros((64, 32, 128), np.float32)
    for j in range(32):
        selpat[2 * j, j, 0:64] = 1.0
        selpat[2 * j + 1, j, 64:128] = 1.0
    c["selpat"] = _bf(selpat.reshape(64, 4096))
    gselp = np.zeros((12, 6, 128), np.float32)
    for pr_ in range(2):
        for b_ in range(3):
            gselp[3 * (2 * pr_) + b_, pr_ * 3 + b_, 0:64] = 1.0
            gselp[3 * (2 * pr_ + 1) + b_, pr_ * 3 + b_, 64:128] = 1.0
    c["gselp"] = _bf(gselp)
    dsel = np.zeros((33, 128), np.float32)
    dsel[0, 0:64] = 1.0
    dsel[32, 64:128] = 1.0
    c["dsel"] = _bf(dsel)
    pp = np.arange(128)[:, None]
    ff = np.arange(128)[None, :]
    for nm, m in (("maskL2", pp > ff), ("maskU2", pp < ff), ("maskUI2", pp <= ff), ("ident2", pp == ff)):
        m = m.astype(np.float32)
        c[nm] = _bf(np.stack([m, m], 1))
    blk = (pp // 64 == ff // 64).astype(np.float32)
    c["blk1"] = _bf(blk)
    c["blkavg"] = _bf(blk / 64.0)
    return c


PK = {}
_o = 0
for _n, _w in (("gin", 8), ("gmem", 8), ("mu", 17), ("memg", 2), ("nsag", 2), ("gateb", 1), ("posT", 32), ("w0", 4), ("a0", 4), ("kk", 4), ("ka", 4), ("rk", 4), ("lnw", 4), ("lnb", 4)):
    PK[_n] = (_o, _w)
    _o += _w
NPK = _o


def host_params(inp):
    pk = np.zeros((128, NPK), np.float32)

    def put(name, arr):
        o, w = PK[name]
        pk[:, o:o + w] = arr

    put("gin", inp["norm_in_g"][0].reshape(8, 128).T)
    put("gmem", inp["mem_norm_g"][0].reshape(8, 128).T)
    ci = _colidx()
    put("mu", inp["rwkv_mu"][0][ci[:17 * 128]].reshape(17, 128).T)
    put("memg", inp["mem_out_g"][0].reshape(2, 128).T)
    put("nsag", inp["nsa_out_g"][0].reshape(2, 128).T)
    gb = np.zeros((128, 1), np.float32)
    gb[0:12, 0] = inp["nsa_gate_b"][0]
    put("gateb", gb)
    pt = inp["nsa_cmp_pos"][0].T
    put("posT", np.concatenate([pt, pt], 0))
    for nm, key in (("w0", "rwkv_w0"), ("a0", "rwkv_a0"), ("kk", "rwkv_k_k"), ("ka", "rwkv_k_a"), ("rk", "rwkv_r_k"), ("lnw", "rwkv_ln_w"), ("lnb", "rwkv_ln_b")):
        put(nm, inp[key][0].reshape(4, 128).T)
    return pk


def build_nc(T, dbg=None):
    NG = T // G
    NT = T // 128
    nc = bass.Bass("TRN2", target_bir_lowering=False)
    dram = {}

    def din(name, shape, dt=F32):
        dram[name] = nc.dram_tensor(name, list(shape), dt, kind="ExternalInput").ap()
        return dram[name]

    x_d = din("x", [T, D])
    mem_d = din("mem", [256, D])
    wcat_d = din("wcat", [8, 128, NCOL])
    wout_d = din("wout", [8, 128, D])
    wmkv_d = din("wmkv", [8, 128, 512])
    pk_d = din("pk", [128, NPK])
    gfin_d = din("gfin", [1, D])
    ident_d = din("ident", [128, 128], BF16)
    ropeC_d = din("ropeC", [128, T])
    ropeS_d = din("ropeS", [128, T])
    pm_d = din("pm", [128, 128], BF16)
    ovl_d = din("ovl", [128, 4, 129], BF16)
    addw_d = din("addw", [128, 256])
    selpat_d = din("selpat", [64, 4096], BF16)
    gselp_d = din("gselp", [12, 6, 128], BF16)
    dsel_d = din("dsel", [33, 128], BF16)
    w1_d = din("w1", [128, 2048])
    w2_d = din("w2", [128, 192])
    lora_d = din("lora", [128, 512])
    w0row_d = din("w0row", [1, 512])
    maskL2_d = din("maskL2", [128, 2, 128], BF16)
    maskU2_d = din("maskU2", [128, 2, 128], BF16)
    maskUI2_d = din("maskUI2", [128, 2, 128], BF16)
    ident2_d = din("ident2", [128, 2, 128], BF16)
    blk1_d = din("blk1", [128, 128], BF16)
    blkavg_d = din("blkavg", [128, 128], BF16)
    out_d = nc.dram_tensor("out", [T, D], F32, kind="ExternalOutput").ap()
    dbg_d = {}
    if dbg:
        for n, (shape, dt) in dbg.items():
            dbg_d[n] = nc.dram_tensor("dbg_" + n, list(shape), dt, kind="ExternalOutput").ap()

    with ExitStack() as es:
        def sb(name, shape, dt=F32):
            return es.enter_context(nc.sbuf_tensor("sb_" + name, list(shape), dt))

        def ps(name, shape, dt=F32):
            return es.enter_context(nc.psum_tensor("ps_" + name, list(shape), dt))

        wb = sb("wb", [128, 8, NCOL], BF16)
        woutb = sb("woutb", [128, 8, D], BF16)
        wmkvb = sb("wmkvb", [128, 8, 512], BF16)
        pk = sb("pk", [128, NPK])
        gfin = sb("gfin", [128, D])
        ident = sb("ident", [128, 128], BF16)
        xt = [sb("xt%d" % i, [128, D]) for i in range(2)]
        hb0 = sb("hb0", [128, D], BF16)
        hb = [hb0, hb0]
        hT = sb("hT", [128, 8, G], BF16)
        st = sb("st", [128, 16])
        yT = sb("yT", [128, 8, G], BF16)
        memkT = sb("memkT", [128, 2, 256], BF16)
        memvA = sb("memvA", [128, 2, 4, 65], BF16)
        qm = sb("qm", [128, 2, G], BF16)
        gm = sb("gm", [128, 2, G], BF16)
        pT = [sb("pT%d" % i, [128, G], BF16) for i in range(2)]
        oaug = sb("oaug", [128, G], BF16)
        osq = sb("osq", [65, G], BF16)
        cvec = sb("cvec", [65, 128], BF16)
        pm = sb("pm", [128, 128], BF16)
        ovl = sb("ovl", [128, 4, 129], BF16)
        addw = sb("addw", [128, 256])
        qcat = sb("qcat", [128, 4, G], BF16)
        gselp = sb("gselp", [12, 6, 128], BF16)
        dsel = sb("dsel", [33, 128], BF16)
        epsc = sb("epsc", [128, 1])
        e64 = sb("e64", [65, 128], BF16)
        w1b = sb("w1b", [128, 32, 64], BF16)
        w2b = sb("w2b", [64, 192], BF16)
        posTb = sb("posTb", [128, 32], BF16)
        ccst = sb("ccst", [64, 2])
        ropeC = sb("ropeC", [128, G])
        ropeS = sb("ropeS", [128, G])
        qn = sb("qn", [128, 2, G], BF16)
        qr = sb("qr", [128, 2, G], BF16)
        gn = sb("gn", [128, 2, G], BF16)
        kcvc = sb("kcvc", [128, 16 + G], BF16)
        kraw = sb("kraw", [128, G], BF16)
        ksT = sb("ksT", [128, T], BF16)
        kwT = sb("kwT", [128, 1024], BF16)
        vsA = sb("vsA", [128, NT, 65], BF16)
        vwA = sb("vwA", [128, 8, 65], BF16)
        kcmpT = sb("kcmpT", [128, 512], BF16)
        vcmpA = sb("vcmpA", [128, 4, 65], BF16)
        vstg = sb("vstg", [32, 65], BF16)
        h1k = sb("h1k", [64, 32], BF16)
        h1v = sb("h1v", [64, 32], BF16)
        gsig = sb("gsig", [12, G], BF16)
        impS = sb("impS", [128, 4, 128])
        score = sb("score", [128, 128])
        stmp = sb("stmp", [128, 128])
        m8 = sb("m8", [128, 16])
        nbq = sb("nbq", [128, 128], BF16)
        nb = sb("nb", [128, G], BF16)
        oab = sb("oab", [65, G], BF16)
        coef = sb("coef", [128, G])
        scl = coef
        rtmp = coef
        acc = sb("acc", [128, 2, G])
        ltb = [sb("lt%d" % i, [128, G + 2], BF16) for i in range(2)]
        mu1m = sb("mu1m", [128, 17])
        prevcol = sb("prevcol", [128, 17])
        loraup = sb("loraup", [128, 512], BF16)
        w0hi = sb("w0hi", [1, 512], BF16)
        w0lo = sb("w0lo", [1, 512], BF16)
        onesrow = sb("onesrow", [1, 128], BF16)
        ka1m = sb("ka1m", [128, 4])
        maskL2 = sb("maskL2", [128, 2, 128], BF16)
        maskU2 = sb("maskU2", [128, 2, 128], BF16)
        maskUI2 = sb("maskUI2", [128, 2, 128], BF16)
        ident2 = sb("ident2", [128, 2, 128], BF16)
        blk1 = sb("blk1", [128, 128], BF16)
        blkavg = sb("blkavg", [128, 128], BF16)
        sgtok = sb("sgtok", [128, 4, 128], BF16)
        AakT4 = sb("AakT4", [128, 2, G], BF16)
        AkV4 = sb("AkV4", [128, G], BF16)
        STb = sb("STb", [64, 8, 64], BF16)
        ncL = sb("ncL", [128, 4])
        WL = sb("WL", [128, 4])
        WL0 = sb("WL0", [64, 8])
        pj = [ps("pj%d" % i, [128, 512]) for i in range(2)]
        sc = [ps("sc%d" % i, [128, 512]) for i in range(2)]
        oa = [ps("oa%d" % i, [128, 512]) for i in range(2)]
        mi = ps("mi", [128, 512])
        tp = ps("tp", [128, 1024], BF16)

        pTc = wmkvb[:, 0:4, :]
        sems = {e: es.enter_context(nc.semaphore("s_" + e)) for e in ENGS}
        dsems = [es.enter_context(nc.semaphore("d%d" % i)) for i in range(N_DMA_SEMS)]
        block = es.enter_context(nc.Block())
        P = Prog(nc)
        rr = {"ev": 0}

        def ev_eng():
            rr["ev"] ^= 1
            return "act" if rr["ev"] else "dve"

        def copy(eng, out, in_, reads, writes):
            if eng == "act":
                P.op("act", lambda e: e.copy(out=out, in_=in_), reads, writes)
            else:
                P.op(eng, lambda e: e.tensor_copy(out=out, in_=in_), reads, writes)

        pe_last = {}

        def mm(out, lhsT, rhs, start, stop, reads, writes):
            base, rows = lhsT.base_partition(), lhsT.shape[0]
            sw = None
            for k in writes:
                last = pe_last.get(k)
                if last is not None:
                    c0, b0, r0 = last
                    if b0 + r0 <= base or base + rows <= b0:
                        sw = max(sw or 0, c0)
            P.op("pe", lambda e: e.matmul(out, lhsT, rhs, start=start, stop=stop), reads, writes, self_wait=sw)
            for k in writes:
                pe_last[k] = (P.cnt["pe"], base, rows)

        def dbg_out(name, ap, key):
            if dbg and name in dbg_d:
                P.dma("sp", lambda e: e.dma_start(out=dbg_d[name], in_=ap), reads=[key])

        import os as _os
        KSTOP = int(_os.environ.get("KSTOP", "99"))
        KSKIP = _os.environ.get("KSKIP", "")
        P.skip = set(KSKIP.split(",")) if KSKIP else set()

        class _Stop(Exception):
            pass

        def stop_at(n):
            if KSTOP == n:
                raise _Stop()

        try:
            P.dma("sp", lambda e: e.dma_start(out=pk[:], in_=pk_d[:, :]), writes=["pk"])
            P.dma("sp", lambda e: e.dma_start(out=ident[:], in_=ident_d[:, :]), writes=["ident"])
            P.dma("sp", lambda e: e.dma_start(out=gfin[:], in_=gfin_d[0:1, :].broadcast_to([128, D])), writes=["gfin"])

            def pkc(name, j=0, rows=128):
                o, w = PK[name]
                return pk[0:rows, o + j:o + j + 1]

            def load_cast(dst3, src_d, ncols, gname, key):
                i = 0
                for k in range(8):
                    for c0 in range(0, ncols, 1024):
                        cw = min(1024, ncols - c0)
                        stg = xt[i % 2]
                        skey = "xt%d" % (i % 2)
                        P.dma("sp" if i % 2 == 0 else "pool",
                              lambda e, stg=stg, k=k, c0=c0, cw=cw: e.dma_start(out=stg[:, 0:cw], in_=src_d[k, :, c0:c0 + cw]),
                              writes=[skey])
                        eng = ev_eng()
                        o = dst3[:, k, c0:c0 + cw]
                        if gname is None:
                            copy(eng, o, stg[:, 0:cw], [skey], [key])
                        elif eng == "act":
                            P.op("act", lambda e, o=o, stg=stg, cw=cw, k=k: e.activation(out=o, in_=stg[:, 0:cw], func=AF.Copy, scale=pkc(gname, k)),
                                 [skey, "pk"], [key])
                        else:
                            P.op("dve", lambda e, o=o, stg=stg, cw=cw, k=k: e.tensor_scalar(out=o, in0=stg[:, 0:cw], scalar1=pkc(gname, k), scalar2=None, op0=ALU.mult),
                                 [skey, "pk"], [key])
                        i += 1

            stop_at(1)
            load_cast(wmkvb, wmkv_d, 512, "gmem", "wmkvb")
            stop_at(2)
            load_cast(wb, wcat_d, NCOL, "gin", "wb")
            load_cast(woutb, wout_d, D, None, "woutb")

            P.op("pool", lambda e: e.memset(cvec[0:64, :], 1.0 / 64), writes=["cvec"])
            P.op("pool", lambda e: e.memset(cvec[64:65, :], 1e-6), writes=["cvec"])

            def norm_transpose(src_tile, skey, dstT, dkey, col0, slot):
                h = hb[slot]
                hk = "hb0"
                ssq = st[:, slot:slot + 1]
                P.op("act", lambda e: e.activation(out=h[:], in_=src_tile[:], func=AF.Square, accum_out=ssq), [skey], [hk, "st%d" % slot])
                P.op("dve", lambda e: e.tensor_scalar(out=ssq, in0=ssq, scalar1=1.0 / D, scalar2=1e-6, op0=ALU.mult, op1=ALU.add), ["st%d" % slot], ["st%d" % slot])
                P.op("dve", lambda e: e.reciprocal(out=ssq, in_=ssq), ["st%d" % slot], ["st%d" % slot])
                P.op("act", lambda e: e.activation(out=ssq, in_=ssq, func=AF.Sqrt), ["st%d" % slot], ["st%d" % slot])
                P.op("dve", lambda e: e.tensor_scalar(out=h[:], in0=src_tile[:], scalar1=ssq, scalar2=None, op0=ALU.mult), [skey, "st%d" % slot], [hk])
                for k in range(8):
                    P.op("pe", lambda e, k=k: e.transpose(out=tp[:, k * 128:(k + 1) * 128], in_=h[:, k * 128:(k + 1) * 128], identity=ident[:]), [hk, "ident"], ["tp"])
                copy(ev_eng(), dstT[:, :, col0:col0 + 128], tp[:].rearrange("p (k t) -> p k t", k=8), ["tp"], [dkey])


            for nm, dst, src in (("pm", pm, pm_d), ("ovl", ovl, ovl_d), ("addw", addw, addw_d),
                                 ):
                P.dma("sp", lambda e, dst=dst, src=src: e.dma_start(out=dst[:], in_=src), writes=[nm])
            P.dma("sp", lambda e: e.dma_start(out=gselp[:], in_=gselp_d), writes=["gselp"])
            P.dma("sp", lambda e: e.dma_start(out=dsel[:], in_=dsel_d), writes=["dsel"])
            P.op("pool", lambda e: e.memset(epsc[:], 1e-6), writes=["epsc"])
            P.op("pool", lambda e: e.memset(oab[:], 0.0), writes=["oab"])
            P.op("pool", lambda e: e.memset(e64[:], 0.0), writes=["e64"])
            P.op("pool", lambda e: e.memset(e64[64:65, :], 1.0), writes=["e64"])
            P.op("pool", lambda e: e.memset(kcmpT[:], 0.0), writes=["kcmpT"])
            P.op("pool", lambda e: e.memset(vcmpA[:], 0.0), writes=["vcmpA"])
            P.op("pool", lambda e: e.memset(vsA[:], 1.0), writes=["vsA"])
            P.op("pool", lambda e: e.memset(vwA[:], 1.0), writes=["vwA"])
            P.op("pool", lambda e: e.memset(kwT[:], 0.0), writes=["kwT"])
            for c0 in range(0, T, 4096):
                cw = min(4096, T - c0)
                P.dma("sp", lambda e, c0=c0, cw=cw: e.dma_start(out=ksT[64:128, c0:c0 + cw], in_=selpat_d[:, 0:cw]), writes=["ksT"])
            P.op("pool", lambda e: e.memset(vstg[:], 1.0), writes=["vstg"])
            P.op("pool", lambda e: e.memset(kcvc[:], 0.0), writes=["kcvc"])
            for hlf in range(2):
                P.dma("sp", lambda e, hlf=hlf: e.dma_start(out=xt[hlf][:], in_=w1_d[:, hlf * 1024:(hlf + 1) * 1024]), writes=["xt%d" % hlf])
                copy(ev_eng(), w1b[:, hlf * 16:(hlf + 1) * 16, :], xt[hlf][:].rearrange("p (l e) -> p l e", e=64), ["xt%d" % hlf], ["w1b"])
            P.dma("sp", lambda e: e.dma_start(out=xt[0][:, 0:192], in_=w2_d[:, :]), writes=["xt0"])
            copy("dve", w2b[:], xt[0][0:64, 0:192], ["xt0"], ["w2b"])
            o_, w_ = PK["posT"]
            copy("dve", posTb[:], pk[:, o_:o_ + 32], ["pk"], ["posTb"])
            for kv in range(2):
                b0 = kv * 64
                for l in range(32):
                    mm(mi[0:64, kv:kv + 1], w1b[b0:b0 + 64, l, :], posTb[b0:b0 + 64, l:l + 1], l == 0, l == 31, ["w1b", "posTb"], ["mi"])
                copy("dve", ccst[:, kv:kv + 1], mi[0:64, kv:kv + 1], ["mi"], ["ccst"])

            for nm, dst, src in (("maskL2", maskL2, maskL2_d), ("maskU2", maskU2, maskU2_d), ("maskUI2", maskUI2, maskUI2_d),
                                 ("ident2", ident2, ident2_d), ("blk1", blk1, blk1_d), ("blkavg", blkavg, blkavg_d)):
                P.dma("sp", lambda e, dst=dst, src=src: e.dma_start(out=dst[:], in_=src), writes=[nm])
            P.dma("sp", lambda e: e.dma_start(out=xt[1][:, 0:512], in_=lora_d[:, :]), writes=["xt1"])
            copy("dve", loraup[:], xt[1][:, 0:512], ["xt1"], ["loraup"])
            P.dma("sp", lambda e: e.dma_start(out=coef[0:1, 0:512], in_=w0row_d[:, :]), writes=["coef"])
            copy("dve", w0hi[:], coef[0:1, 0:512], ["coef"], ["w0hi"])
            P.op("dve", lambda e: e.tensor_tensor(out=coef[0:1, 0:512], in0=coef[0:1, 0:512], in1=w0hi[:], op=ALU.subtract), ["coef", "w0hi"], ["coef"])
            copy("dve", w0lo[:], coef[0:1, 0:512], ["coef"], ["w0lo"])
            o_, w_ = PK["mu"]
            P.op("dve", lambda e, o_=o_: e.tensor_scalar(out=mu1m[:], in0=pk[:, o_:o_ + 17], scalar1=-1.0, scalar2=1.0, op0=ALU.mult, op1=ALU.add), ["pk"], ["mu1m"])
            P.op("pool", lambda e: e.memset(onesrow[:], 1.0), writes=["onesrow"])
            P.op("pool", lambda e: e.memset(prevcol[:], 0.0), writes=["prevcol"])
            P.op("pool", lambda e: e.memset(STb[:], 0.0), writes=["STb"])
            o_, w_ = PK["ka"]
            P.op("dve", lambda e, o_=o_: e.tensor_scalar(out=ka1m[:], in0=pk[:, o_:o_ + 4], scalar1=-1.0, scalar2=1.0, op0=ALU.mult, op1=ALU.add), ["pk"], ["ka1m"])
            tokall = wmkvb[:, 4:8, :].rearrange("p c (w f) -> p c w f", w=4)
            stop_at(3)
            for mt in range(2):
                P.dma("sp", lambda e, mt=mt: e.dma_start(out=xt[mt][:], in_=mem_d[mt * 128:(mt + 1) * 128, :]), writes=["xt%d" % mt])
                norm_transpose(xt[mt], "xt%d" % mt, hT, "hT", mt * 128, mt)
            stop_at(4)
            for pr in range(2):
                for k in range(8):
                    mm(pj[0][:, 0:256], wmkvb[:, k, pr * 128:(pr + 1) * 128], hT[:, k, 0:256], k == 0, k == 7, ["wmkvb", "hT"], ["pj0"])
                copy(ev_eng(), memkT[:, pr, :], pj[0][:, 0:256], ["pj0"], ["memkT"])
            P.op("pool", lambda e: e.memset(memvA[:], 1.0), writes=["memvA"])
            for mt in range(2):
                for k in range(8):
                    mm(pj[1][:, 0:256], hT[:, k, mt * 128:(mt + 1) * 128], wmkvb[:, k, 256:512], k == 0, k == 7, ["wmkvb", "hT"], ["pj1"])
                copy(ev_eng(), memvA[:, mt, :, 0:64], pj[1][:, 0:256].rearrange("p (h d) -> p h d", h=4), ["pj1"], ["memvA"])

            def finalize_head(o_ps, okey, use_den, hb_, gcol_ap, gate_ap, gate_key, dst_ap, dst_key, from_sbuf=False):
                sl = slice(hb_, hb_ + 64)
                if from_sbuf:
                    copy("dve", oaug[sl, :], o_ps, [okey], ["oaug"])
                    P.op("act", lambda e: e.activation(out=osq[0:64, :], in_=o_ps, func=AF.Square), [okey], ["osq"])
                    P.op("pool", lambda e: e.memset(osq[64:65, :], 1.0), [], ["osq"])
                elif use_den:
                    copy("dve", oaug[sl, :], o_ps[0:64, :], [okey], ["oaug"])
                    P.op("act", lambda e: e.activation(out=osq[:], in_=o_ps[0:65, :], func=AF.Square), [okey], ["osq"])
                else:
                    copy("dve", oaug[sl, :], o_ps[0:64, :], [okey], ["oaug"])
                    P.op("act", lambda e: e.activation(out=osq[0:64, :], in_=o_ps[0:64, :], func=AF.Square), [okey], ["osq"])
                    P.op("pool", lambda e: e.memset(osq[64:65, :], 1.0), [], ["osq"])
                mm(mi[:], cvec[:], osq[:], True, True, ["cvec", "osq"], ["mi"])
                P.op("act", lambda e: e.activation(out=scl[sl, :], in_=mi[sl, :], func=AF.Ln), ["mi"], ["coef"])
                P.op("act", lambda e: e.activation(out=scl[sl, :], in_=scl[sl, :], func=AF.Exp, scale=-0.5), ["coef"], ["coef"])
                P.op("dve", lambda e: e.scalar_tensor_tensor(out=scl[sl, :], in0=scl[sl, :], scalar=gcol_ap, in1=oaug[sl, :], op0=ALU.mult, op1=ALU.mult), ["coef", "oaug", "pk"], ["coef"])
                P.op("dve", lambda e: e.tensor_tensor(out=dst_ap, in0=scl[sl, :], in1=gate_ap, op=ALU.mult), ["coef", gate_key], [dst_key])

            P.op("pool", lambda e: e.memset(wmkvb[:, 4:8, :], 0.0), ["wmkvb"], ["wmkvb", "tokall"])
            stop_at(5)
            for g in range(NG):
                t0 = g * G
                for i in range(4):
                    P.dma("sp", lambda e, i=i, t0=t0: e.dma_start(out=xt[i % 2][:], in_=x_d[t0 + i * 128:t0 + (i + 1) * 128, :]), writes=["xt%d" % (i % 2)])
                    norm_transpose(xt[i % 2], "xt%d" % (i % 2), hT, "hT", i * 128, i % 2)

                def project(chunk, width=128):
                    pb = pj[chunk % 2]
                    for k in range(8):
                        mm(pb[0:width, :], wb[:, k, chunk * 128:chunk * 128 + width], hT[:, k, :], k == 0, k == 7, ["wb", "hT"], ["pj%d" % (chunk % 2)])
                    return pb, "pj%d" % (chunk % 2)


                P.phase = "rwkv"
                C0 = float(np.exp(-0.5))

                def tt(eng, out, in0, in1, op, reads, writes):
                    P.op(eng, lambda e: e.tensor_tensor(out=out, in0=in0, in1=in1, op=op), reads, writes)

                def ts(eng, out, in0, s1, s2, op0, op1, reads, writes):
                    if s2 is None:
                        P.op(eng, lambda e: e.tensor_scalar(out=out, in0=in0, scalar1=s1, scalar2=None, op0=op0), reads, writes)
                    else:
                        P.op(eng, lambda e: e.tensor_scalar(out=out, in0=in0, scalar1=s1, scalar2=s2, op0=op0, op1=op1), reads, writes)

                def stt(eng, out, in0, scalar, in1, op0, op1, reads, writes):
                    P.op(eng, lambda e: e.scalar_tensor_tensor(out=out, in0=in0, scalar=scalar, in1=in1, op0=op0, op1=op1), reads, writes)

                def actf(out, in_, func, reads, writes, bias=None, scale=None):
                    kw = {}
                    if bias is not None:
                        kw["bias"] = bias
                    if scale is not None:
                        kw["scale"] = scale
                    P.op("act", lambda e: e.activation(out=out, in_=in_, func=func, **kw), reads, writes)

                def pcol(name, j):
                    o, w = PK[name]
                    return pk[:, o + j:o + j + 1]

                def lerp_chunk(cidx, dst_ap, dkey):
                    pb, pkey = project(cidx)
                    lt, lk = ltb[cidx % 2], "lt%d" % (cidx % 2)
                    actf(lt[:, 1:G + 1], pb[:], AF.Copy, [pkey, "pk"], [lk], scale=pcol("mu", cidx))
                    copy("dve", lt[:, 0:1], prevcol[:, cidx:cidx + 1], ["prevcol"], [lk])
                    copy("dve", prevcol[:, cidx:cidx + 1], lt[:, G:G + 1], [lk], ["prevcol"])
                    stt("dve", dst_ap, pb[:], mu1m[:, cidx:cidx + 1], lt[:, 0:G], ALU.mult, ALU.add, [pkey, "mu1m", lk], [dkey])

                rT, kmT = qn[:, 0, :], qn[:, 1, :]
                vT, gT = qr[:, 0, :], qr[:, 1, :]
                bT, At = gn[:, 0, :], gn[:, 1, :]
                Rt, Bt = qm[:, 0, :], qm[:, 1, :]
                Kt, Bb = gm[:, 0, :], gm[:, 1, :]
                Kb, kkn = pT[0][:], pT[1][:]
                aT, lin = kraw[:], nb[:]
                kf, yraw = acc[:, 0, :], acc[:, 1, :]
                Etmp, Esq = hb0[:, 0:G], hb0[:, G:2 * G]

                lerp_chunk(16, kf, "acc")
                actf(lin[0:64, :], kf[0:64, :], AF.Tanh, ["acc"], ["nb"])
                copy("dve", lin[64:128, :], kf[64:128, :], ["acc"], ["nb"])

                stop_at(40)
                for hp in range(4):
                    lerp_chunk(4 * hp + 0, rT, "qn")
                    lerp_chunk(4 * hp + 1, kf, "acc")
                    lerp_chunk(4 * hp + 2, vT, "qr")
                    lerp_chunk(4 * hp + 3, gT, "qr")
                    actf(gT, gT, AF.Silu, ["qr"], ["qr"])
                    hc = slice(hp * 128, (hp + 1) * 128)
                    stop_at(41)
                    for c in range(4):
                        reg = mi[:, c * 128:(c + 1) * 128]
                        mm(reg, lin[0:64, c * 128:(c + 1) * 128], loraup[0:64, hc], True, False, ["nb", "loraup"], ["mi"])
                        mm(reg, onesrow[:], w0hi[0:1, hc], False, False, ["onesrow", "w0hi"], ["mi"])
                        mm(reg, onesrow[:], w0lo[0:1, hc], False, True, ["onesrow", "w0lo"], ["mi"])
                    actf(sgtok[:].rearrange("p c f -> p (c f)"), mi[:], AF.Sigmoid, ["mi"], ["sgtok"])
                    for c in range(4):
                        mm(sc[0][:, c * 128:(c + 1) * 128], sgtok[:, c, :], maskUI2[:, 0, :], True, True, ["sgtok", "maskUI2"], ["sc0"])
                    for c in range(4):
                        mm(sc[1][:, c * 128:(c + 1) * 128], sgtok[:, c, :], maskU2[:, 0, :], True, True, ["sgtok", "maskU2"], ["sc1"])
                    stop_at(42)
                    mm(oa[0][:], loraup[64:128, hc], lin[64:128, :], True, True, ["nb", "loraup"], ["oa0"])
                    actf(aT, oa[0][:], AF.Sigmoid, ["oa0", "pk"], ["kraw"], bias=pcol("a0", hp))
                    ts("dve", coef[:], kf, pcol("kk", hp), None, ALU.mult, None, ["acc", "pk"], ["coef"])
                    actf(Esq, coef[:], AF.Square, ["coef"], ["hb0"])
                    mm(oa[1][:], blk1[:], Esq, True, True, ["blk1", "hb0"], ["oa1"])
                    ts("dve", yraw, oa[1][:], 1e-24, None, ALU.max, None, ["oa1"], ["acc/1"])
                    actf(yraw, yraw, AF.Ln, ["acc/1"], ["acc/1"])
                    actf(yraw, yraw, AF.Exp, ["acc/1"], ["acc/1"], scale=-0.5)
                    tt("dve", kkn, coef[:], yraw, ALU.mult, ["coef", "acc/1"], ["pT1"])
                    ts("dve", coef[:], aT, pcol("ka", hp), ka1m[:, hp:hp + 1], ALU.mult, ALU.add, ["kraw", "pk", "ka1m"], ["coef"])
                    tt("dve", kmT, kf, coef[:], ALU.mult, ["acc", "coef"], ["qn"])
                    tt("pool", bT, kkn, aT, ALU.mult, ["pT1", "kraw"], ["gn"])
                    if g == 0 and hp == 0:
                        dbg_out("rT", rT, "qn"); dbg_out("kmT", kmT, "qn"); dbg_out("vT", vT, "qr"); dbg_out("aT", aT, "kraw"); dbg_out("kkn", kkn, "pT1"); dbg_out("bT", bT, "gn")
                        copy("dve", coef[:], sc[0][:], ["sc0"], ["coef"]); dbg_out("cumI", coef[:], "coef")
                        copy("dve", coef[:], sc[1][:], ["sc1"], ["coef"]); dbg_out("cumE", coef[:], "coef")
                    stop_at(43)
                    actf(Etmp, sc[1][:], AF.Exp, ["sc1"], ["hb0"], scale=-C0)
                    stt("dve", At, Etmp, -1.0, kkn, ALU.mult, ALU.mult, ["hb0", "pT1"], ["gn"])
                    actf(Etmp, sc[0][:], AF.Exp, ["sc0"], ["hb0"], scale=-C0)
                    tt("dve", Rt, rT, Etmp, ALU.mult, ["qn", "hb0"], ["qm"])
                    actf(Esq, sc[0][:], AF.Exp, ["sc0"], ["hb0"], scale=C0)
                    tt("dve", Bt, bT, Esq, ALU.mult, ["gn", "hb0"], ["qm"])
                    tt("pool", Kt, kmT, Esq, ALU.mult, ["qn", "hb0"], ["gm"])
                    ts("dve", ncL[:], sc[0][:, 127:G:128], -C0, None, ALU.mult, None, ["sc0"], ["ncL"])
                    actf(WL[:], ncL[:], AF.Exp, ["ncL"], ["WL"])
                    copy("dve", WL0[:, 0:4], WL[0:64, :], ["WL"], ["WL0"])
                    copy("dve", WL0[:, 4:8], WL[64:128, :], ["WL"], ["WL0"])
                    for c in range(4):
                        actf(Etmp[:, c * 128:(c + 1) * 128], sc[0][:, c * 128:(c + 1) * 128], AF.Exp, ["sc0", "ncL"], ["hb0"], bias=ncL[:, c:c + 1], scale=C0)
                    tt("dve", Bb, bT, Etmp, ALU.mult, ["gn", "hb0"], ["gm"])
                    tt("pool", Kb, kmT, Etmp, ALU.mult, ["qn", "hb0"], ["pT0"])
                    stop_at(44)
                    for c in range(4):
                        cs = slice(c * 128, (c + 1) * 128)
                        half = (c % 2) * 512
                        for wi, (src, skey) in enumerate(((At, "gn"), (vT, "qr"), (Bb, "gm"), (Kb, "pT0"))):
                            P.op("pe", lambda e, src=src, cs=cs, half=half, wi=wi: e.transpose(out=tp[:, half + wi * 128:half + (wi + 1) * 128], in_=src[:, cs], identity=ident[:]), [skey, "ident"], ["tp"])
                        copy(ev_eng(), tokall[:, c, :, :], tp[:, half:half + 512].rearrange("p (w f) -> p w f", w=4), ["tp"], ["tokall"])

                    P.phase = "rwkv_chain"
                    stop_at(45)

                    def r2(bank):
                        return bank[:, 0:256].rearrange("p (e f) -> p e f", e=2)

                    def hs(e):
                        return slice(64 * e, 64 * e + 64)

                    def v4(ap):
                        return ap.rearrange("p (c e f) -> p c e f", c=2, e=2)

                    def bc4(m2):
                        return m2[:, 0:1, :].broadcast_to([128, 4, 128]).rearrange("p (c e) f -> p c e f", c=2)

                    PmH, PmK = [hb0[:, 0:G], hb0[:, G:2 * G]], ["hb0/0", "hb0/1"]
                    PTH, PTK = [pT[0][:], pT[1][:]], ["pT0", "pT1"]
                    XTH, XTK = [gn[:, 0, :], kraw[:]], ["gn/0", "kraw"]
                    bkP = [(oa[0], "oa0"), (oa[1], "oa1")]
                    bkT = [(pj[0], "pj0"), (pj[1], "pj1")]
                    bkX = [(mi, "mi"), (sc[0], "sc0")]

                    def rg(bank, cl, e):
                        o = (cl * 2 + e) * 128
                        return bank[:, o:o + 128]

                    for h2 in range(2):
                        for cl in range(2):
                            cs = slice((2 * h2 + cl) * 128, (2 * h2 + cl + 1) * 128)
                            for e in range(2):
                                mm(rg(bkP[h2][0], cl, e), At[hs(e), cs], Bt[hs(e), cs], True, True, ["gn/1", "qm"], [bkP[h2][1]])
                        for cl in range(2):
                            cs = slice((2 * h2 + cl) * 128, (2 * h2 + cl + 1) * 128)
                            for e in range(2):
                                mm(rg(bkT[h2][0], cl, e), Bt[hs(e), cs], At[hs(e), cs], True, True, ["gn/1", "qm"], [bkT[h2][1]])
                    for h2 in range(2):
                        tt("dve", v4(PmH[h2]), v4(bkP[h2][0][:]), bc4(maskL2), ALU.mult, [bkP[h2][1], "maskL2"], [PmK[h2]])
                        tt("dve", v4(PTH[h2]), v4(bkT[h2][0][:]), bc4(maskU2), ALU.mult, [bkT[h2][1], "maskU2"], [PTK[h2]])
                        tt("pool", v4(XTH[h2]), v4(PTH[h2]), bc4(ident2), ALU.add, [PTK[h2], "ident2"], [XTK[h2]])
                    for lvl in range(1, 7):
                        for h2 in range(2):
                            for cl in range(2):
                                for e in range(2):
                                    mm(rg(bkP[h2][0], cl, e), v4(PTH[h2])[:, cl, e, :], v4(PmH[h2])[:, cl, e, :], True, True, [PTK[h2], PmK[h2]], [bkP[h2][1]])
                            if lvl < 6:
                                for cl in range(2):
                                    for e in range(2):
                                        mm(rg(bkT[h2][0], cl, e), v4(PmH[h2])[:, cl, e, :], v4(PTH[h2])[:, cl, e, :], True, True, [PTK[h2], PmK[h2]], [bkT[h2][1]])
                        for h2 in range(2):
                            copy("act", PmH[h2], bkP[h2][0][:], [bkP[h2][1]], [PmK[h2]])
                            if lvl < 6:
                                copy("dve" if h2 == 0 else "act", PTH[h2], bkT[h2][0][:], [bkT[h2][1]], [PTK[h2]])
                        for h2 in range(2):
                            for cl in range(2):
                                for e in range(2):
                                    mm(rg(bkX[h2][0], cl, e), v4(PmH[h2])[:, cl, e, :], v4(XTH[h2])[:, cl, e, :], True, True, [PmK[h2], XTK[h2]], [bkX[h2][1]])
                        for h2 in range(2):
                            tt("dve", XTH[h2], XTH[h2], bkX[h2][0][:], ALU.add, [XTK[h2], bkX[h2][1]], [XTK[h2]])

                    stop_at(46)
                    def v8(ap):
                        return ap.rearrange("p (c e f) -> p c e f", c=4, e=2)

                    ArbH, ArbK = [hb0[:, 0:G], hb0[:, G:2 * G]], ["hb0/0", "hb0/1"]
                    ArkH, ArkK = [pT[0][:], pT[1][:]], ["pT0", "pT1"]
                    AakH, AakK = [AakT4[:, 0, :], AakT4[:, 1, :]], ["AakT4/0", "AakT4/1"]
                    Ahat4, AhK = v8(gm[:, 1, :]), "gm/1"
                    Uhat4, UhK = v8(sgtok[:].rearrange("p c f -> p (c f)")), "sgtok"
                    AkV4v = v8(AkV4[:])
                    RhH, RhK = [oab[0:64, :], osq[0:64, :]], ["oab", "osq"]
                    Msb4 = v8(qcat[0:64, 0, :])

                    def XT(c):
                        return v4(XTH[c // 2])[:, c % 2, :, :], XTK[c // 2]

                    for h2 in range(2):
                        for cl in range(2):
                            cs = slice((2 * h2 + cl) * 128, (2 * h2 + cl + 1) * 128)
                            for e in range(2):
                                mm(rg(bkP[h2][0], cl, e), Kt[hs(e), cs], At[hs(e), cs], True, True, ["gm/0", "gn/1"], [bkP[h2][1]])
                        for cl in range(2):
                            cs = slice((2 * h2 + cl) * 128, (2 * h2 + cl + 1) * 128)
                            for e in range(2):
                                mm(rg(bkT[h2][0], cl, e), Bt[hs(e), cs], Rt[hs(e), cs], True, True, ["qm"], [bkT[h2][1]])
                        for cl in range(2):
                            cs = slice((2 * h2 + cl) * 128, (2 * h2 + cl + 1) * 128)
                            for e in range(2):
                                mm(rg(bkX[h2][0], cl, e), Kt[hs(e), cs], Rt[hs(e), cs], True, True, ["gm/0", "qm"], [bkX[h2][1]])
                    for h2 in range(2):
                        tt("dve", v4(AakH[h2]), v4(bkP[h2][0][:]), bc4(maskU2), ALU.mult, [bkP[h2][1], "maskU2"], [AakK[h2]])
                        tt("dve", v4(ArbH[h2]), v4(bkT[h2][0][:]), bc4(maskUI2), ALU.mult, [bkT[h2][1], "maskUI2"], [ArbK[h2]])
                        tt("dve", v4(ArkH[h2]), v4(bkX[h2][0][:]), bc4(maskUI2), ALU.mult, [bkX[h2][1], "maskUI2"], [ArkK[h2]])
                    for c in range(4):
                        for e in range(2):
                            o = (c * 2 + e) * 64
                            mm(oa[0][:, o:o + 64], v4(AakH[c // 2])[:, c % 2, e, :], tokall[:, c, 1, hs(e)], True, True, [AakK[c // 2], "tokall"], ["oa0"])
                    copy("act", AkV4[:], oa[0][:], ["oa0"], ["AkV4"])
                    for c in range(4):
                        xt_, xk_ = XT(c)
                        for e in range(2):
                            o = (c * 2 + e) * 64
                            mm(oa[1][:, o:o + 64], xt_[:, e, :], tokall[:, c, 0, hs(e)], True, True, [xk_, "tokall"], ["oa1"])
                    copy("dve", gm[:, 1, :], oa[1][:], ["oa1"], [AhK])
                    for c in range(4):
                        xt_, xk_ = XT(c)
                        for e in range(2):
                            o = (c * 2 + e) * 64
                            mm(pj[0][:, o:o + 64], xt_[:, e, :], AkV4v[:, c, e, :], True, True, [xk_, "AkV4"], ["pj0"])
                    copy("act", sgtok[:].rearrange("p c f -> p (c f)"), pj[0][:], ["pj0"], [UhK])
                    for h2 in range(2):
                        for cl in range(2):
                            c = 2 * h2 + cl
                            cs = slice(c * 128, (c + 1) * 128)
                            for e in range(2):
                                reg = rg(bkX[h2][0], cl, e)[0:64, :]
                                mm(reg, Ahat4[:, c, e, :], v4(ArbH[h2])[:, cl, e, :], True, False, [AhK, ArbK[h2]], [bkX[h2][1]])
                                mm(reg, ident[hs(e), hs(e)], Rt[hs(e), cs], False, True, ["ident", "qm"], [bkX[h2][1]])
                        copy("dve" if h2 == 0 else "act", RhH[h2], bkX[h2][0][0:64, :], [bkX[h2][1]], [RhK[h2]])
                    for c in range(4):
                        for e in range(2):
                            o = (c * 2 + e) * 64
                            mm(pj[1][0:64, o:o + 64], Ahat4[:, c, e, :], tokall[:, c, 2, hs(e)], True, True, [AhK, "tokall"], ["pj1"])
                    copy("act", qcat[0:64, 0, :], pj[1][0:64, :], ["pj1"], ["qcat"])
                    stop_at(47)
                    for c in range(4):
                        cs = slice(c * 128, (c + 1) * 128)
                        h2, cl = c // 2, c % 2
                        rh = v4(RhH[h2])
                        for e in range(2):
                            reg = sc[1][0:64, e * 128:(e + 1) * 128]
                            mm(reg, STb[:, 2 * hp + e, :], rh[:, cl, e, :], True, False, ["STb", RhK[h2]], ["sc1"])
                            mm(reg, Uhat4[:, c, e, :], v4(ArbH[h2])[:, cl, e, :], False, False, [UhK, ArbK[h2]], ["sc1"])
                            mm(reg, tokall[:, c, 1, hs(e)], v4(ArkH[h2])[:, cl, e, :], False, True, ["tokall", ArkK[h2]], ["sc1"])
                        copy("act", yraw[0:64, cs], sc[1][0:64, 0:128], ["sc1"], ["acc"])
                        copy("act", yraw[64:128, cs], sc[1][0:64, 128:256], ["sc1"], ["acc"])
                        ob_, obk = oa[c % 2], "oa%d" % (c % 2)
                        for e in range(2):
                            reg = ob_[0:64, e * 64:(e + 1) * 64]
                            mm(reg, tokall[:, c, 2, hs(e)], Uhat4[:, c, e, :], True, False, ["tokall", UhK], [obk])
                            mm(reg, tokall[:, c, 3, hs(e)], tokall[:, c, 1, hs(e)], False, False, ["tokall"], [obk])
                            mm(reg, Msb4[:, c, e, :], STb[:, 2 * hp + e, :], False, True, ["qcat", "STb"], [obk])
                        for e in range(2):
                            stt("dve", STb[:, 2 * hp + e, :], STb[:, 2 * hp + e, :], WL0[:, 4 * e + c:4 * e + c + 1], ob_[0:64, e * 64:(e + 1) * 64], ALU.mult, ALU.add, ["STb", "WL0", obk], ["STb"])

                    if g == 0 and hp == 0:
                        dbg_out("yraw", yraw, "acc")
                    P.phase = "rwkv"
                    stop_at(49)
                    copy("act", Etmp, yraw, ["acc"], ["hb0"])
                    actf(Esq, yraw, AF.Square, ["acc"], ["hb0"])
                    mm(sc[0][:], blkavg[:], Etmp, True, True, ["blkavg", "hb0"], ["sc0"])
                    mm(sc[1][:], blkavg[:], Esq, True, True, ["blkavg", "hb0"], ["sc1"])
                    actf(coef[:], sc[0][:], AF.Square, ["sc0"], ["coef"])
                    tt("dve", coef[:], sc[1][:], coef[:], ALU.subtract, ["sc1", "coef"], ["coef"])
                    ts("dve", coef[:], coef[:], 64e-5, None, ALU.add, None, ["coef"], ["coef"])
                    actf(coef[:], coef[:], AF.Ln, ["coef"], ["coef"])
                    actf(coef[:], coef[:], AF.Exp, ["coef"], ["coef"], scale=-0.5)
                    tt("dve", yraw, yraw, sc[0][:], ALU.subtract, ["acc", "sc0"], ["acc"])
                    tt("dve", yraw, yraw, coef[:], ALU.mult, ["acc", "coef"], ["acc"])
                    ts("dve", yraw, yraw, pcol("lnw", hp), pcol("lnb", hp), ALU.mult, ALU.add, ["acc", "pk"], ["acc"])
                    tt("pool", Etmp, rT, kmT, ALU.mult, ["qn"], ["hb0"])
                    ts("dve", Esq, Etmp, pcol("rk", hp), None, ALU.mult, None, ["hb0", "pk"], ["hb0"])
                    mm(sc[0][:], blk1[:], Esq, True, True, ["blk1", "hb0"], ["sc0"])
                    tt("dve", coef[:], sc[0][:], vT, ALU.mult, ["sc0", "qr"], ["coef"])
                    tt("dve", yraw, yraw, coef[:], ALU.add, ["acc", "coef"], ["acc"])
                    tt("dve", yT[:, hp, :], yraw, gT, ALU.mult, ["acc", "qr"], ["yT"])

                P.phase = "mem"
                stop_at(6)
                for pr in range(2):
                    pb, pkey = project(25 + pr)
                    P.op("act", lambda e, pb=pb, pr=pr: e.activation(out=qm[:, pr, :], in_=pb[:], func=AF.Copy, scale=0.125), [pkey], ["qm"])
                    pb, pkey = project(27 + pr)
                    P.op("act", lambda e, pb=pb, pr=pr: e.activation(out=gm[:, pr, :], in_=pb[:], func=AF.Silu), [pkey], ["gm"])
                stop_at(61)
                for h in range(4):
                    pr, hb_ = h // 2, (h % 2) * 64
                    ob = oa[h % 2]
                    okey = "oa%d" % (h % 2)
                    for mc in range(2):
                        sb_ = sc[mc]
                        mm(sb_[:], memkT[hb_:hb_ + 64, pr, mc * 128:(mc + 1) * 128], qm[hb_:hb_ + 64, pr, :], True, True, ["memkT", "qm"], ["sc%d" % mc])
                        P.op("act", lambda e, sb_=sb_, mc=mc: e.activation(out=pT[mc][:], in_=sb_[:], func=AF.Exp), ["sc%d" % mc], ["pT%d" % mc])
                    stop_at(62 if h == 0 else 66)
                    for mc in range(2):
                        mm(ob[0:65, :], memvA[:, mc, h, :], pT[mc][:], mc == 0, mc == 1, ["memvA", "pT%d" % mc], [okey])
                    stop_at(63 if h == 0 else 67)
                    o, w = PK["memg"]
                    finalize_head(ob, okey, True, hb_, pk[hb_:hb_ + 64, o + pr:o + pr + 1], gm[hb_:hb_ + 64, pr, :], "gm", yT[hb_:hb_ + 64, 6 + pr, :], "yT")


                P.phase = "nsa"
                P.dma("sp", lambda e, t0=t0: e.dma_start(out=ropeC[:], in_=ropeC_d[:, t0:t0 + G]), writes=["ropeC"])
                P.dma("sp", lambda e, t0=t0: e.dma_start(out=ropeS[:], in_=ropeS_d[:, t0:t0 + G]), writes=["ropeS"])

                def rope(src_t, skey, dst_ap, dkey, nr=128):
                    mm(mi[:], pm[:], src_t, True, True, ["pm", skey], ["mi"])
                    P.op("dve", lambda e: e.tensor_tensor(out=rtmp[0:nr, :], in0=mi[0:nr, :], in1=ropeS[0:nr, :], op=ALU.mult), ["mi", "ropeS"], ["coef"])
                    P.op("pool", lambda e: e.tensor_tensor(out=dst_ap, in0=src_t[0:nr, :], in1=ropeC[0:nr, :], op=ALU.mult), [skey, "ropeC"], [dkey])
                    P.op("dve", lambda e: e.tensor_tensor(out=dst_ap, in0=dst_ap, in1=rtmp[0:nr, :], op=ALU.add), [dkey, "coef"], [dkey])

                for pr in range(2):
                    pb, pkey = project(17 + pr)
                    P.op("act", lambda e, pb=pb, pr=pr: e.activation(out=qn[:, pr, :], in_=pb[:], func=AF.Copy, scale=0.125), [pkey], ["qn"])
                    rope(qn[:, pr, :], "qn", qr[:, pr, :], "qr")
                    pb, pkey = project(19 + pr)
                    P.op("act", lambda e, pb=pb, pr=pr: e.activation(out=gn[:, pr, :], in_=pb[:], func=AF.Silu), [pkey], ["gn"])
                copy("dve", kcvc[:, 0:16], kcvc[:, G:G + 16], ["kcvc"], ["kcvc"])
                pb, pkey = project(21)
                copy("act", kcvc[:, 16:16 + G], pb[:], [pkey], ["kcvc"])
                pb, pkey = project(22)
                copy("act", kraw[:], pb[:], [pkey], ["kraw"])
                rope(kraw[:], "kraw", ksT[0:64, t0:t0 + G], "ksT", 64)
                pb, pkey = project(23)
                copy("act", kraw[:], pb[:], [pkey], ["kraw"])
                wo = (g % 2) * G
                rope(kraw[:], "kraw", kwT[0:64, wo:wo + G], "kwT", 64)
                for i in range(4):
                    pbv = pj[i % 2]
                    for k in range(8):
                        mm(pbv[:, 0:128], hT[:, k, i * 128:(i + 1) * 128], wb[:, k, 24 * 128:25 * 128], k == 0, k == 7, ["hT", "wb"], ["pj%d" % (i % 2)])
                    kt = 4 * g + i
                    copy("dve", vsA[:, kt, 0:64], pbv[:, 0:64], ["pj%d" % (i % 2)], ["vsA"])
                    copy("act", vwA[:, kt % 8, 0:64], pbv[:, 64:128], ["pj%d" % (i % 2)], ["vwA"])
                pb, pkey = project(29, 12)
                o_, w_ = PK["gateb"]
                P.op("act", lambda e, pb=pb, o_=o_: e.activation(out=gsig[:], in_=pb[0:12, :], func=AF.Sigmoid, bias=pk[0:12, o_:o_ + 1]), [pkey, "pk"], ["gsig"])

                lo, hi = max(0, 32 * g - 1), 32 * g + 30
                n = hi - lo + 1
                c0 = 16 * lo + 16 - t0
                for kv in range(2):
                    b0 = kv * 64
                    reg = mi[0:64, kv * 64:kv * 64 + n]
                    for l in range(32):
                        mm(reg, w1b[b0:b0 + 64, l, :], kcvc[b0:b0 + 64, c0 + l:c0 + l + 16 * (n - 1) + 1:16], l == 0, l == 31, ["w1b", "kcvc"], ["mi"])
                    dsth = (h1k, h1v)[kv]
                    P.op("act", lambda e, reg=reg, dsth=dsth, kv=kv, n=n: e.activation(out=dsth[:, 0:n], in_=reg, func=AF.Silu, bias=ccst[:, kv:kv + 1]), ["mi", "ccst"], ["h1%d" % kv])
                mm(sc[0][:, 0:n], w2b[:, 0:128], h1k[:, 0:n], True, True, ["w2b", "h10"], ["sc0"])
                copy("dve", kcmpT[:, lo:hi + 1], sc[0][:, 0:n], ["sc0"], ["kcmpT"])
                mm(sc[1][0:n, 0:64], h1v[:, 0:n], w2b[:, 128:192], True, True, ["w2b", "h11"], ["sc1"])
                copy("dve", vstg[0:n, 0:64], sc[1][0:n, 0:64], ["sc1"], ["vstg"])
                i0 = lo
                while i0 <= hi:
                    i1 = min(hi, (i0 // 128) * 128 + 127)
                    P.dma("sp", lambda e, i0=i0, i1=i1, lo=lo: e.dma_start(out=vcmpA[i0 % 128:i1 % 128 + 1, i0 // 128, :], in_=vstg[i0 - lo:i1 - lo + 1, :]), reads=["vstg"], writes=["vcmpA"])
                    i0 = i1 + 1

                def combine_pair(pr, b, first):
                    copy("act", oaug[0:64, :], oa[0][0:64, :], ["oa0"], ["oaug"])
                    copy("dve", oaug[64:128, :], oa[1][0:64, :], ["oa1"], ["oaug"])
                    copy("act", oab[0:1, :], oa[0][64:65, :], ["oa0"], ["oab"])
                    copy("dve", oab[32:33, :], oa[1][64:65, :], ["oa1"], ["oab"])
                    mm(pj[0][:], dsel[:], oab[0:33, :], True, True, ["dsel", "oab"], ["pj0"])
                    mm(pj[1][:], gselp[:, pr * 3 + b, :], gsig[:], True, True, ["gselp", "gsig"], ["pj1"])
                    P.op("dve", lambda e: e.tensor_scalar(out=coef[:], in0=pj[0][:], scalar1=1e-30, scalar2=None, op0=ALU.max), ["pj0"], ["coef"])
                    P.op("act", lambda e: e.activation(out=coef[:], in_=coef[:], func=AF.Ln), ["coef"], ["coef"])
                    P.op("act", lambda e: e.activation(out=coef[:], in_=coef[:], func=AF.Exp, scale=-1.0), ["coef"], ["coef"])
                    P.op("dve", lambda e: e.tensor_tensor(out=coef[:], in0=coef[:], in1=pj[1][:], op=ALU.mult), ["coef", "pj1"], ["coef"])
                    if first:
                        P.op("dve", lambda e: e.tensor_tensor(out=acc[:, pr, :], in0=coef[:], in1=oaug[:], op=ALU.mult), ["coef", "oaug"], ["acc"])
                    else:
                        P.op("dve", lambda e: e.tensor_tensor(out=coef[:], in0=coef[:], in1=oaug[:], op=ALU.mult), ["coef", "oaug"], ["coef"])
                        P.op("dve", lambda e: e.tensor_tensor(out=acc[:, pr, :], in0=acc[:, pr, :], in1=coef[:], op=ALU.add), ["coef", "acc"], ["acc"])

                def pmask(ap, key, base, cm, step):
                    P.op("pool", lambda e: e.affine_select(out=ap, in_=ap, pattern=[[step, G]], compare_op=ALU.is_ge, fill=0.0, base=base, channel_multiplier=cm), [key], [key])

                def pmask(ap, key, base, cm, step):
                    P.op("pool", lambda e: e.affine_select(out=ap, in_=ap, pattern=[[step, G]], compare_op=ALU.is_ge, fill=0.0, base=base, channel_multiplier=cm), [key], [key])

                P.op("pool", lambda e: e.memset(impS[:], 0.0), [], ["impS"])
                ncc = g // 4 + 1
                for h in range(4):
                    pr, hb_ = h // 2, (h % 2) * 64
                    ob, okey = oa[h % 2], "oa%d" % (h % 2)
                    for ci in range(ncc):
                        dl = 2048 * ci - 512 * g
                        sb_, skey = sc[ci % 2], "sc%d" % (ci % 2)
                        mm(sb_[:], kcmpT[hb_:hb_ + 64, ci * 128:(ci + 1) * 128], qn[hb_:hb_ + 64, pr, :], True, True, ["kcmpT", "qn"], [skey])
                        P.op("act", lambda e, sb_=sb_, ci=ci: e.activation(out=pTc[:, ci, :], in_=sb_[:], func=AF.Exp), [skey], ["wmkvb"])
                        if dl >= -2048:
                            pmask(pTc[:, ci, :], "wmkvb", -31 - dl, -16, 1)
                        mm(ob[0:65, :], vcmpA[:, ci, :], pTc[:, ci, :], ci == 0, ci == ncc - 1, ["vcmpA", "wmkvb"], [okey])
                    for j in range(4):
                        reg = mi[:, (j % 2) * 256:(j % 2) * 256 + 129]
                        for ci in range(ncc):
                            mm(reg, pTc[:, ci, j * 128:(j + 1) * 128], ovl[:, ci, :], ci == 0, ci == ncc - 1, ["wmkvb", "ovl"], ["mi"])
                        P.op("dve", lambda e, reg=reg: e.tensor_scalar(out=m8[:, 0:1], in0=reg[:, 128:129], scalar1=1e-30, scalar2=None, op0=ALU.max), ["mi"], ["m8"])
                        P.op("dve", lambda e: e.reciprocal(out=m8[:, 0:1], in_=m8[:, 0:1]), ["m8"], ["m8"])
                        P.op("dve", lambda e, reg=reg, j=j: e.scalar_tensor_tensor(out=impS[:, j, :], in0=reg[:, 0:128], scalar=m8[:, 0:1], in1=impS[:, j, :], op0=ALU.mult, op1=ALU.add), ["mi", "m8", "impS"], ["impS"])
                    if h % 2 == 1:
                        combine_pair(h // 2, 0, True)

                for j in range(4):
                    qt = 4 * g + j
                    u0 = 126 - 2 * qt
                    P.op("dve", lambda e, j=j, u0=u0: e.tensor_tensor(out=score[:], in0=impS[:, j, :], in1=addw[:, u0:u0 + 128], op=ALU.add), ["impS", "addw"], ["score"])
                    P.op("dve", lambda e: e.tensor_scalar(out=score[:, 0:1], in0=score[:, 0:1], scalar1=1e4, scalar2=None, op0=ALU.add), ["score"], ["score"])
                    P.op("dve", lambda e: e.max(out=m8[:, 0:8], in_=score[:]), ["score"], ["m8"])
                    P.op("dve", lambda e: e.match_replace(out=stmp[:], in_to_replace=m8[:, 0:8], in_values=score[:], imm_value=-2.0), ["score", "m8"], ["stmp"])
                    P.op("dve", lambda e: e.max(out=m8[:, 8:16], in_=stmp[:]), ["stmp"], ["m8"])
                    P.op("dve", lambda e: e.tensor_reduce(out=m8[:, 0:1], in_=m8[:, 8:16], axis=AX.X, op=ALU.min), ["m8"], ["m8"])
                    P.op("dve", lambda e: e.tensor_scalar(out=stmp[:], in0=score[:], scalar1=m8[:, 0:1], scalar2=None, op0=ALU.is_ge), ["score", "m8"], ["stmp"])
                    P.op("dve", lambda e: e.tensor_scalar(out=nbq[:], in0=stmp[:], scalar1=1.0, scalar2=-NEG, op0=ALU.subtract, op1=ALU.mult), ["stmp"], ["nbq"])
                    P.op("pe", lambda e: e.transpose(out=tp[:, 0:128], in_=nbq[:], identity=ident[:]), ["nbq", "ident"], ["tp"])
                    copy("dve", nb[:, j * 128:(j + 1) * 128], tp[:, 0:128], ["tp"], ["nb"])

                P.phase = "nsa_sel"
                scA = [(sc[0], "sc0"), (sc[1], "sc1")]
                scB = [(pj[0], "pj0"), (pj[1], "pj1")]
                pTA = [(pT[0][:], "pT0"), (pT[1][:], "pT1")]
                pTB = [(AakT4[:, 0, :], "AakT4/0"), (AakT4[:, 1, :], "AakT4/1")]
                nslab = (4 * g + 3) // 32 + 1
                for pr in range(2):
                    for hl in range(2):
                        for M in range(nslab):
                            copy(("act", "dve")[(hl + M) % 2], qcat[0:64, 2 * hl + M, :], qr[64 * hl:64 * hl + 64, pr, :], ["qr"], ["qcat"])
                            copy(("dve", "pool")[hl], qcat[64:128, 2 * hl + M, :], nb[64 * M:64 * M + 64, :], ["nb"], ["qcat"])
                    nkt = 4 * g + 4

                    def sel_a(kt):
                        (sa, ska), (sb2, skb) = scA[kt % 2], scB[kt % 2]
                        (pa, pka), (pb2, pkb) = pTA[kt % 2], pTB[kt % 2]
                        M = kt // 32
                        ks_ = slice(kt * 128, (kt + 1) * 128)
                        mm(sa[:], ksT[:, ks_], qcat[:, M, :], True, True, ["ksT", "qcat"], [ska])
                        mm(sb2[:], ksT[:, ks_], qcat[:, 2 + M, :], True, True, ["ksT", "qcat"], [skb])
                        P.op("act", lambda e: e.activation(out=pa, in_=sa[:], func=AF.Exp), [ska], [pka])
                        P.op("act", lambda e: e.activation(out=pb2, in_=sb2[:], func=AF.Exp), [skb], [pkb])
                        if kt >= 4 * g:
                            pmask(pa, pka, -128 * (kt - 4 * g), -1, 1)
                            pmask(pb2, pkb, -128 * (kt - 4 * g), -1, 1)

                    sel_a(0)
                    for kt in range(nkt):
                        if kt + 1 < nkt:
                            sel_a(kt + 1)
                        mm(oa[0][0:65, :], vsA[:, kt, :], pTA[kt % 2][0], kt == 0, kt == nkt - 1, ["vsA", pTA[kt % 2][1]], ["oa0"])
                        mm(oa[1][0:65, :], vsA[:, kt, :], pTB[kt % 2][0], kt == 0, kt == nkt - 1, ["vsA", pTB[kt % 2][1]], ["oa1"])
                    combine_pair(pr, 1, False)

                    P.phase = "nsa_win"
                    kts = [kt for kt in range(4 * g - 4, 4 * g + 4) if kt >= 0]

                    def win_a(kt):
                        (sa, ska), (sb2, skb) = scA[kt % 2], scB[kt % 2]
                        (pa, pka), (pb2, pkb) = pTA[kt % 2], pTB[kt % 2]
                        ro = (kt % 8) * 128
                        mm(sa[:], kwT[:, ro:ro + 128], qcat[:, 0, :], True, True, ["kwT", "qcat"], [ska])
                        mm(sb2[:], kwT[:, ro:ro + 128], qcat[:, 2, :], True, True, ["kwT", "qcat"], [skb])
                        P.op("act", lambda e: e.activation(out=pa, in_=sa[:], func=AF.Exp), [ska], [pka])
                        P.op("act", lambda e: e.activation(out=pb2, in_=sb2[:], func=AF.Exp), [skb], [pkb])
                        rel = kt - 4 * g
                        for p_, k_ in ((pa, pka), (pb2, pkb)):
                            if rel >= 0:
                                pmask(p_, k_, -128 * rel, -1, 1)
                            else:
                                pmask(p_, k_, 511 + 128 * rel, 1, -1)

                    win_a(kts[0])
                    for ii, kt in enumerate(kts):
                        if ii + 1 < len(kts):
                            win_a(kts[ii + 1])
                        mm(oa[0][0:65, :], vwA[:, kt % 8, :], pTA[kt % 2][0], ii == 0, ii == len(kts) - 1, ["vwA", pTA[kt % 2][1]], ["oa0"])
                        mm(oa[1][0:65, :], vwA[:, kt % 8, :], pTB[kt % 2][0], ii == 0, ii == len(kts) - 1, ["vwA", pTB[kt % 2][1]], ["oa1"])
                    combine_pair(pr, 2, False)
                    P.phase = "nsa_sel"

                P.phase = "nsa"
                o_, w_ = PK["nsag"]
                for pr in range(2):
                    src = acc[:, pr, :]
                    P.op("act", lambda e, src=src: e.activation(out=hb0[:, 0:G], in_=src, func=AF.Square), ["acc"], ["hb0"])
                    mm(mi[:], blkavg[:], hb0[:, 0:G], True, True, ["blkavg", "hb0"], ["mi"])
                    P.op("act", lambda e: e.activation(out=coef[:], in_=mi[:], func=AF.Ln, bias=epsc[:]), ["mi", "epsc"], ["coef"])
                    P.op("act", lambda e: e.activation(out=coef[:], in_=coef[:], func=AF.Exp, scale=-0.5), ["coef"], ["coef"])
                    P.op("dve", lambda e, src=src, pr=pr, o_=o_: e.scalar_tensor_tensor(out=coef[:], in0=coef[:], scalar=pk[:, o_ + pr:o_ + pr + 1], in1=src, op0=ALU.mult, op1=ALU.mult), ["coef", "acc", "pk"], ["coef"])
                    P.op("dve", lambda e, pr=pr: e.tensor_tensor(out=yT[:, 4 + pr, :], in0=coef[:], in1=gn[:, pr, :], op=ALU.mult), ["coef", "gn"], ["yT"])
                P.phase = ""
                stop_at(7)
                for i in range(4):
                    xb, xk = xt[i % 2], "xt%d" % (i % 2)
                    P.dma("sp", lambda e, i=i, t0=t0, xb=xb: e.dma_start(out=xb[:], in_=x_d[t0 + i * 128:t0 + (i + 1) * 128, :]), writes=[xk])
                    for half in range(2):
                        pb = pj[half]
                        for k in range(8):
                            mm(pb[:], yT[:, k, i * 128:(i + 1) * 128], woutb[:, k, half * 512:(half + 1) * 512], k == 0, k == 7, ["yT", "woutb"], ["pj%d" % half])
                        P.op("dve", lambda e, pb=pb, xb=xb, half=half: e.tensor_tensor(out=xb[:, half * 512:(half + 1) * 512], in0=xb[:, half * 512:(half + 1) * 512], in1=pb[:], op=ALU.add),
                             ["pj%d" % half, xk], [xk])
                    sk = "st%d" % (8 + i)
                    ssq = st[:, 8 + i:9 + i]
                    hjk = hb[i % 2]
                    P.op("act", lambda e, xb=xb, ssq=ssq, hjk=hjk: e.activation(out=hjk[:], in_=xb[:], func=AF.Square, accum_out=ssq), [xk], ["hb0", sk])
                    P.op("dve", lambda e, ssq=ssq: e.tensor_scalar(out=ssq, in0=ssq, scalar1=1.0 / D, scalar2=1e-6, op0=ALU.mult, op1=ALU.add), [sk], [sk])
                    P.op("dve", lambda e, ssq=ssq: e.reciprocal(out=ssq, in_=ssq), [sk], [sk])
                    P.op("act", lambda e, ssq=ssq: e.activation(out=ssq, in_=ssq, func=AF.Sqrt), [sk], [sk])
                    P.op("dve", lambda e, xb=xb, ssq=ssq: e.scalar_tensor_tensor(out=xb[:], in0=xb[:], scalar=ssq, in1=gfin[:], op0=ALU.mult, op1=ALU.mult), [xk, sk, "gfin"], [xk])
                    P.dma("pool", lambda e, i=i, t0=t0, xb=xb: e.dma_start(out=out_d[t0 + i * 128:t0 + (i + 1) * 128, :], in_=xb[:]), reads=[xk], is_out=True)
        except _Stop:
            P.dma("pool", lambda e: e.dma_start(out=out_d[0:128, :], in_=xt[0][:]), reads=["xt0"], is_out=True)
        P.finish("sp")
        P.emit(block, sems, dsems)
    return nc


def make_in_maps(inputs, T, batches):
    ci = _colidx()
    w_in = np.asarray(inputs["w_in"][0])
    wcat = np.ascontiguousarray(w_in[:, ci].reshape(8, 128, NCOL))
    wout = np.ascontiguousarray(np.asarray(inputs["w_out"][0]).reshape(8, 128, D))
    wmkv = np.ascontiguousarray(np.asarray(inputs["w_mem_kv"][0]).reshape(8, 128, 512))
    pk = host_params({k: np.asarray(v) for k, v in inputs.items()})
    w1k = np.asarray(inputs["nsa_cmp_k_w1"][0]).reshape(32, 64, 64).transpose(1, 0, 2)
    w1v = np.asarray(inputs["nsa_cmp_v_w1"][0]).reshape(32, 64, 64).transpose(1, 0, 2)
    w1 = np.ascontiguousarray(np.concatenate([w1k, w1v], 0).reshape(128, 2048))
    w2k = np.asarray(inputs["nsa_cmp_k_w2"][0]); w2v = np.asarray(inputs["nsa_cmp_v_w2"][0])
    w2 = np.zeros((128, 192), np.float32)
    w2[0:64] = np.concatenate([w2k, w2k, w2v], 1)
    lora = np.ascontiguousarray(np.concatenate([np.asarray(inputs["rwkv_w_up"][0]), np.asarray(inputs["rwkv_a_up"][0])], 0))
    w0row = np.ascontiguousarray(np.asarray(inputs["rwkv_w0"][0]).reshape(1, 512))
    consts = host_consts(T)
    maps = []
    for b in batches:
        m = {
            "x": np.ascontiguousarray(np.asarray(inputs["x"][b][:T])),
            "mem": np.ascontiguousarray(np.asarray(inputs["mem"][b])),
            "wcat": wcat, "wout": wout, "wmkv": wmkv, "pk": pk, "w1": w1, "w2": w2, "lora": lora, "w0row": w0row,
            "gfin": np.ascontiguousarray(np.asarray(inputs["norm_final_g"]).reshape(1, D)),
        }
        m.update(consts)
        maps.append(m)
    return maps


_NC_CACHE = {}


def kernel(**inputs):
    T = inputs["x"].shape[1]
    B = inputs["x"].shape[0]
    if T not in _NC_CACHE:
        _NC_CACHE[T] = build_nc(T)
    nc = _NC_CACHE[T]
    batches = [i % B for i in range(8)]
    maps = make_in_maps(inputs, T, batches)
    res = run_bass_kernel_spmd(nc, maps, core_ids=list(range(8)))
    out = np.stack([res.results[b]["out"] for b in range(B)], axis=0)
    return out.astype(np.float32)
```

```python
import numpy as np
import ml_dtypes
from contextlib import ExitStack
import concourse.bass as bass
import concourse.mybir as mybir
from concourse.bass_utils import run_bass_kernel_spmd

F32 = mybir.dt.float32
BF16 = mybir.dt.bfloat16
ALU = mybir.AluOpType
AF = mybir.ActivationFunctionType
AX = mybir.AxisListType

ENGS = ("pe", "act", "dve", "pool", "sp")
N_DMA_SEMS = 24
import os as _os0
SAME_ENG_SYNC = _os0.environ.get("KSAME", "1") == "1"
NEG = -30000.0

D = 1024
NCOL = 29 * 128 + 12
G = 512
PSUM_PREFIX = ("pj", "sc", "oa", "mi", "tp")


class Prog:
    def __init__(self, nc):
        self.nc = nc
        self.lists = {e: [] for e in ENGS}
        self.cnt = {e: 0 for e in ENGS}
        self.seen = {e: {} for e in ENGS}
        self.clock_at = {e: [None] for e in ENGS}
        self.buf = {}
        self.dma_val = [0] * N_DMA_SEMS
        self.dma_clock = [[] for _ in range(N_DMA_SEMS)]
        self.dma_rr = 0
        self.out_toks = []
        self.phase = ""
        self.skip = set()
        self.children = {}

    def _deps(self, eng, reads, writes):
        need = {}

        def add(tok):
            if tok is None:
                return
            src, val = tok
            if src == eng and (eng == "pe" or not SAME_ENG_SYNC):
                return
            if need.get(src, 0) < val:
                need[src] = val

        def related(k):
            ks = [k]
            if "/" in k:
                ks.append(k.split("/")[0])
            else:
                ks.extend(self.children.get(k, ()))
            return ks

        for k0 in reads:
            for k in related(k0):
                st = self.buf.get(k)
                if st:
                    add(st[0])
                    if k[:2] in PSUM_PREFIX:
                        for r in st[1]:
                            if r[0] != eng:
                                add(r)
        for k0 in writes:
            for k in related(k0):
                st = self.buf.get(k)
                if st:
                    add(st[0])
                    for r in st[1]:
                        add(r)
        seen = self.seen[eng]
        waits = []
        for src, val in need.items():
            if seen.get(src, 0) >= val:
                continue
            waits.append((src, val))
            if isinstance(src, str):
                snap = self.clock_at[src][val]
            else:
                snap = self.dma_clock[src[1]][val // 16 - 1]
            for s2, v2 in snap.items():
                if seen.get(s2, 0) < v2:
                    seen[s2] = v2
            seen[src] = val
        return waits

    def _mark(self, tok, reads, writes):
        for k in list(reads) + list(writes):
            if "/" in k:
                self.children.setdefault(k.split("/")[0], set()).add(k)
        for k in reads:
            st = self.buf.setdefault(k, [None, []])
            st[1].append(tok)
            if len(st[1]) > 64:
                st[1] = st[1][-64:]
        for k in writes:
            self.buf[k] = [tok, []]
            if "/" not in k:
                for ch in self.children.get(k, ()):
                    self.buf[ch] = [tok, []]

    def op(self, eng, fn, reads=(), writes=(), self_wait=None):
        if self.phase in self.skip:
            return
        waits = self._deps(eng, reads, writes)
        if self_wait is not None and self.seen[eng].get(eng, 0) < self_wait:
            waits.append((eng, self_wait))
            self.seen[eng][eng] = self_wait
        self.cnt[eng] += 1
        c = self.cnt[eng]
        snap = dict(self.seen[eng])
        snap[eng] = c
        self.clock_at[eng].append(snap)
        self.lists[eng].append(("op", fn, waits, None))
        self._mark((eng, c), reads, writes)

    def dma(self, eng, fn, reads=(), writes=(), is_out=False):
        if self.phase in self.skip:
            return None
        s = self.dma_rr
        self.dma_rr = (self.dma_rr + 1) % N_DMA_SEMS
        waits = self._deps(eng, reads, writes)
        src = ("dma", s)
        prev = self.dma_val[s]
        if prev and self.seen[eng].get(src, 0) < prev:
            waits.append((src, prev))
            self.seen[eng][src] = prev
        val = prev + 16
        self.dma_val[s] = val
        self.dma_clock[s].append(dict(self.seen[eng]))
        self.lists[eng].append(("dma", fn, waits, (s, val)))
        self._mark((src, val), reads, writes)
        if is_out:
            self.out_toks.append((src, val))
        return (src, val)

    def finish(self, eng="sp"):
        waits = []
        for s in range(N_DMA_SEMS):
            if self.dma_val[s]:
                waits.append((("dma", s), self.dma_val[s]))
        self.lists[eng].append(("wait", None, waits, None))

    def emit(self, block, sems, dsems):
        engobj = {"pe": "tensor", "act": "scalar", "dve": "vector", "pool": "gpsimd", "sp": "sync"}

        def semof(src):
            return sems[src] if isinstance(src, str) else dsems[src[1]]

        def make(ename):
            lst = self.lists[ename]

            def body(e):
                for kind, fn, waits, extra in lst:
                    for src, val in waits:
                        e.wait_ge(semof(src), val)
                    if kind == "op":
                        fn(e).then_inc(sems[ename], 1)
                    elif kind == "dma":
                        fn(e).then_inc(dsems[extra[0]], 16)

            return body

        for ename in ENGS:
            if self.lists[ename]:
                getattr(block, engobj[ename])(make(ename))


RW, NSAB, MEMB = 0, 2176, 3084


def _colidx():
    idx = []
    for hp in range(4):
        for base in (0, 512, 1024, 1536):
            idx += list(range(base + 128 * hp, base + 128 * hp + 128))
    idx += list(range(2048, 2176))
    q0, g0 = NSAB, NSAB + 256
    idx += list(range(q0, q0 + 256))
    idx += list(range(g0, g0 + 256))
    kc, vc, ks, vs, kw, vw = (NSAB + 524 + 64 * i for i in range(6))
    idx += list(range(kc, kc + 64)) + list(range(vc, vc + 64))
    idx += list(range(ks, ks + 64)) * 2
    idx += list(range(kw, kw + 64)) * 2
    idx += list(range(vs, vs + 64)) + list(range(vw, vw + 64))
    idx += list(range(MEMB, MEMB + 512))
    idx += list(range(NSAB + 512, NSAB + 524))
    assert len(idx) == NCOL
    return np.array(idx)


def _bf(a):
    return np.ascontiguousarray(a.astype(ml_dtypes.bfloat16))


def host_consts(T):
    c = {}
    c["ident"] = _bf(np.eye(128, dtype=np.float32))
    half = 8
    inv = (np.float32(500000.0) ** (-np.arange(half, dtype=np.float32) / np.float32(half))).astype(np.float32)
    ang = np.arange(T, dtype=np.float32)[None, :] * inv[:, None]
    C = np.ones((64, T), np.float32)
    S = np.zeros((64, T), np.float32)
    C[0:8] = np.cos(ang); C[8:16] = np.cos(ang)
    S[0:8] = -np.sin(ang); S[8:16] = np.sin(ang)
    c["ropeC"] = np.ascontiguousarray(np.concatenate([C, C], 0))
    c["ropeS"] = np.ascontiguousarray(np.concatenate([S, S], 0))
    pm = np.zeros((128, 128), np.float32)
    for hb in (0, 64):
        for d in range(8):
            pm[hb + d + 8, hb + d] = 1.0
            pm[hb + d, hb + d + 8] = 1.0
    c["pm"] = _bf(pm)
    ovl = np.zeros((512, 129), np.float32)
    for i in range(511):
        for sblk in range(128):
            o = min(16 * i + 32, 64 * sblk + 64) - max(16 * i, 64 * sblk)
            if o > 0:
                ovl[i, sblk] = o / 32.0
    ovl[:, 128] = 1.0
    c["ovl"] = _bf(ovl.reshape(4, 128, 129).transpose(1, 0, 2))
    addw = np.zeros((128, 256), np.float32)
    for pp in range(128):
        cur = 1 if pp >= 64 else 0
        for u in range(256):
            sp = u - 126
            valid = sp <= cur
            forced = sp in (cur, cur - 1)
            addw[pp, u] = (0.0 if valid else -1.0) + (1e4 if forced else 0.0)
    c["addw"] = addw
    selpat = np.zeros((64, 32, 128), np.float32)
    for j in range(32):
        selpat[2 * j, j, 0:64] = 1.0
        selpat[2 * j + 1, j, 64:128] = 1.0
    c["selpat"] = _bf(selpat.reshape(64, 4096))
    gselp = np.zeros((12, 6, 128), np.float32)
    for pr_ in range(2):
        for b_ in range(3):
            gselp[3 * (2 * pr_) + b_, pr_ * 3 + b_, 0:64] = 1.0
            gselp[3 * (2 * pr_ + 1) + b_, pr_ * 3 + b_, 64:128] = 1.0
    c["gselp"] = _bf(gselp)
    dsel = np.zeros((33, 128), np.float32)
    dsel[0, 0:64] = 1.0
    dsel[32, 64:128] = 1.0
    c["dsel"] = _bf(dsel)
    pp = np.arange(128)[:, None]
    ff = np.arange(128)[None, :]
    for nm, m in (("maskL2", pp > ff), ("maskU2", pp < ff), ("maskUI2", pp <= ff), ("ident2", pp == ff)):
        m = m.astype(np.float32)
        c[nm] = _bf(np.stack([m, m], 1))
    blk = (pp // 64 == ff // 64).astype(np.float32)
    c["blk1"] = _bf(blk)
    c["blkavg"] = _bf(blk / 64.0)
    return c


PK = {}
_o = 0
for _n, _w in (("gin", 8), ("gmem", 8), ("mu", 17), ("memg", 2), ("nsag", 2), ("gateb", 1), ("posT", 32), ("w0", 4), ("a0", 4), ("kk", 4), ("ka", 4), ("rk", 4), ("lnw", 4), ("lnb", 4)):
    PK[_n] = (_o, _w)
    _o += _w
NPK = _o


def host_params(inp):
    pk = np.zeros((128, NPK), np.float32)

    def put(name, arr):
        o, w = PK[name]
        pk[:, o:o + w] = arr

    put("gin", inp["norm_in_g"][0].reshape(8, 128).T)
    put("gmem", inp["mem_norm_g"][0].reshape(8, 128).T)
    ci = _colidx()
    put("mu", inp["rwkv_mu"][0][ci[:17 * 128]].reshape(17, 128).T)
    put("memg", inp["mem_out_g"][0].reshape(2, 128).T)
    put("nsag", inp["nsa_out_g"][0].reshape(2, 128).T)
    gb = np.zeros((128, 1), np.float32)
    gb[0:12, 0] = inp["nsa_gate_b"][0]
    put("gateb", gb)
    pt = inp["nsa_cmp_pos"][0].T
    put("posT", np.concatenate([pt, pt], 0))
    for nm, key in (("w0", "rwkv_w0"), ("a0", "rwkv_a0"), ("kk", "rwkv_k_k"), ("ka", "rwkv_k_a"), ("rk", "rwkv_r_k"), ("lnw", "rwkv_ln_w"), ("lnb", "rwkv_ln_b")):
        put(nm, inp[key][0].reshape(4, 128).T)
    return pk


def build_nc(T, dbg=None):
    NG = T // G
    NT = T // 128
    nc = bass.Bass("TRN2", target_bir_lowering=False)
    dram = {}

    def din(name, shape, dt=F32):
        dram[name] = nc.dram_tensor(name, list(shape), dt, kind="ExternalInput").ap()
        return dram[name]

    x_d = din("x", [T, D])
    mem_d = din("mem", [256, D])
    wcat_d = din("wcat", [8, 128, NCOL])
    wout_d = din("wout", [8, 128, D])
    wmkv_d = din("wmkv", [8, 128, 512])
    pk_d = din("pk", [128, NPK])
    gfin_d = din("gfin", [1, D])
    ident_d = din("ident", [128, 128], BF16)
    ropeC_d = din("ropeC", [128, T])
    ropeS_d = din("ropeS", [128, T])
    pm_d = din("pm", [128, 128], BF16)
    ovl_d = din("ovl", [128, 4, 129], BF16)
    addw_d = din("addw", [128, 256])
    selpat_d = din("selpat", [64, 4096], BF16)
    gselp_d = din("gselp", [12, 6, 128], BF16)
    dsel_d = din("dsel", [33, 128], BF16)
    w1_d = din("w1", [128, 2048])
    w2_d = din("w2", [128, 192])
    lora_d = din("lora", [128, 512])
    w0row_d = din("w0row", [1, 512])
    maskL2_d = din("maskL2", [128, 2, 128], BF16)
    maskU2_d = din("maskU2", [128, 2, 128], BF16)
    maskUI2_d = din("maskUI2", [128, 2, 128], BF16)
    ident2_d = din("ident2", [128, 2, 128], BF16)
    blk1_d = din("blk1", [128, 128], BF16)
    blkavg_d = din("blkavg", [128, 128], BF16)
    out_d = nc.dram_tensor("out", [T, D], F32, kind="ExternalOutput").ap()
    dbg_d = {}
    if dbg:
        for n, (shape, dt) in dbg.items():
            dbg_d[n] = nc.dram_tensor("dbg_" + n, list(shape), dt, kind="ExternalOutput").ap()

    with ExitStack() as es:
        def sb(name, shape, dt=F32):
            return es.enter_context(nc.sbuf_tensor("sb_" + name, list(shape), dt))

        def ps(name, shape, dt=F32):
            return es.enter_context(nc.psum_tensor("ps_" + name, list(shape), dt))

        wb = sb("wb", [128, 8, NCOL], BF16)
        woutb = sb("woutb", [128, 8, D], BF16)
        wmkvb = sb("wmkvb", [128, 8, 512], BF16)
        pk = sb("pk", [128, NPK])
        gfin = sb("gfin", [128, D])
        ident = sb("ident", [128, 128], BF16)
        xt = [sb("xt%d" % i, [128, D]) for i in range(2)]
        hb0 = sb("hb0", [128, D], BF16)
        hb = [hb0, hb0]
        hT = sb("hT", [128, 8, G], BF16)
        st = sb("st", [128, 16])
        yT = sb("yT", [128, 8, G], BF16)
        memkT = sb("memkT", [128, 2, 256], BF16)
        memvA = sb("memvA", [128, 2, 4, 65], BF16)
        qm = sb("qm", [128, 2, G], BF16)
        gm = sb("gm", [128, 2, G], BF16)
        pT = [sb("pT%d" % i, [128, G], BF16) for i in range(2)]
        oaug = sb("oaug", [128, G], BF16)
        osq = sb("osq", [65, G], BF16)
        cvec = sb("cvec", [65, 128], BF16)
        pm = sb("pm", [128, 128], BF16)
        ovl = sb("ovl", [128, 4, 129], BF16)
        addw = sb("addw", [128, 256])
        qcat = sb("qcat", [128, 4, G], BF16)
        gselp = sb("gselp", [12, 6, 128], BF16)
        dsel = sb("dsel", [33, 128], BF16)
        epsc = sb("epsc", [128, 1])
        e64 = sb("e64", [65, 128], BF16)
        w1b = sb("w1b", [128, 32, 64], BF16)
        w2b = sb("w2b", [64, 192], BF16)
        posTb = sb("posTb", [128, 32], BF16)
        ccst = sb("ccst", [64, 2])
        ropeC = sb("ropeC", [128, G])
        ropeS = sb("ropeS", [128, G])
        qn = sb("qn", [128, 2, G], BF16)
        qr = sb("qr", [128, 2, G], BF16)
        gn = sb("gn", [128, 2, G], BF16)
        kcvc = sb("kcvc", [128, 16 + G], BF16)
        kraw = sb("kraw", [128, G], BF16)
        ksT = sb("ksT", [128, T], BF16)
        kwT = sb("kwT", [128, 1024], BF16)
        vsA = sb("vsA", [128, NT, 65], BF16)
        vwA = sb("vwA", [128, 8, 65], BF16)
        kcmpT = sb("kcmpT", [128, 512], BF16)
        vcmpA = sb("vcmpA", [128, 4, 65], BF16)
        vstg = sb("vstg", [32, 65], BF16)
        h1k = sb("h1k", [64, 32], BF16)
        h1v = sb("h1v", [64, 32], BF16)
        gsig = sb("gsig", [12, G], BF16)
        impS = sb("impS", [128, 4, 128])
        score = sb("score", [128, 128])
        stmp = sb("stmp", [128, 128])
        m8 = sb("m8", [128, 16])
        nbq = sb("nbq", [128, 128], BF16)
        nb = sb("nb", [128, G], BF16)
        oab = sb("oab", [65, G], BF16)
        coef = sb("coef", [128, G])
        scl = coef
        rtmp = coef
        acc = sb("acc", [128, 2, G])
        ltb = [sb("lt%d" % i, [128, G + 2], BF16) for i in range(2)]
        mu1m = sb("mu1m", [128, 17])
        prevcol = sb("prevcol", [128, 17])
        loraup = sb("loraup", [128, 512], BF16)
        w0hi = sb("w0hi", [1, 512], BF16)
        w0lo = sb("w0lo", [1, 512], BF16)
        onesrow = sb("onesrow", [1, 128], BF16)
        ka1m = sb("ka1m", [128, 4])
        maskL2 = sb("maskL2", [128, 2, 128], BF16)
        maskU2 = sb("maskU2", [128, 2, 128], BF16)
        maskUI2 = sb("maskUI2", [128, 2, 128], BF16)
        ident2 = sb("ident2", [128, 2, 128], BF16)
        blk1 = sb("blk1", [128, 128], BF16)
        blkavg = sb("blkavg", [128, 128], BF16)
        sgtok = sb("sgtok", [128, 4, 128], BF16)
        AakT4 = sb("AakT4", [128, 2, G], BF16)
        AkV4 = sb("AkV4", [128, G], BF16)
        STb = sb("STb", [64, 8, 64], BF16)
        STn = sb("STn", [64, 2, 64], BF16)
        ncL = sb("ncL", [128, 4])
        WL = sb("WL", [128, 4])
        WL0 = sb("WL0", [64, 8])
        pj = [ps("pj%d" % i, [128, 512]) for i in range(2)]
        sc = [ps("sc%d" % i, [128, 512]) for i in range(2)]
        oa = [ps("oa%d" % i, [128, 512]) for i in range(2)]
        mi = ps("mi", [128, 512])
        tp = ps("tp", [128, 1024], BF16)

        pTc = wmkvb[:, 0:4, :]
        sems = {e: es.enter_context(nc.semaphore("s_" + e)) for e in ENGS}
        dsems = [es.enter_context(nc.semaphore("d%d" % i)) for i in range(N_DMA_SEMS)]
        block = es.enter_context(nc.Block())
        P = Prog(nc)
        rr = {"ev": 0}

        def ev_eng():
            rr["ev"] ^= 1
            return "act" if rr["ev"] else "dve"

        def copy(eng, out, in_, reads, writes):
            if eng == "act":
                P.op("act", lambda e: e.copy(out=out, in_=in_), reads, writes)
            else:
                P.op(eng, lambda e: e.tensor_copy(out=out, in_=in_), reads, writes)

        pe_last = {}

        def mm(out, lhsT, rhs, start, stop, reads, writes):
            base, rows = lhsT.base_partition(), lhsT.shape[0]
            sw = None
            for k in writes:
                last = pe_last.get(k)
                if last is not None:
                    c0, b0, r0 = last
                    if b0 + r0 <= base or base + rows <= b0:
                        sw = max(sw or 0, c0)
            P.op("pe", lambda e: e.matmul(out, lhsT, rhs, start=start, stop=stop), reads, writes, self_wait=sw)
            for k in writes:
                pe_last[k] = (P.cnt["pe"], base, rows)

        def dbg_out(name, ap, key):
            if dbg and name in dbg_d:
                P.dma("sp", lambda e: e.dma_start(out=dbg_d[name], in_=ap), reads=[key])

        import os as _os
        KSTOP = int(_os.environ.get("KSTOP", "99"))
        KSKIP = _os.environ.get("KSKIP", "")
        P.skip = set(KSKIP.split(",")) if KSKIP else set()

        class _Stop(Exception):
            pass

        def stop_at(n):
            if KSTOP == n:
                raise _Stop()

        try:
            P.dma("sp", lambda e: e.dma_start(out=pk[:], in_=pk_d[:, :]), writes=["pk"])
            P.dma("sp", lambda e: e.dma_start(out=ident[:], in_=ident_d[:, :]), writes=["ident"])
            P.dma("sp", lambda e: e.dma_start(out=gfin[:], in_=gfin_d[0:1, :].broadcast_to([128, D])), writes=["gfin"])

            def pkc(name, j=0, rows=128):
                o, w = PK[name]
                return pk[0:rows, o + j:o + j + 1]

            def load_cast(dst3, src_d, ncols, gname, key):
                i = 0
                for k in range(8):
                    for c0 in range(0, ncols, 1024):
                        cw = min(1024, ncols - c0)
                        stg = xt[i % 2]
                        skey = "xt%d" % (i % 2)
                        P.dma("sp" if i % 2 == 0 else "pool",
                              lambda e, stg=stg, k=k, c0=c0, cw=cw: e.dma_start(out=stg[:, 0:cw], in_=src_d[k, :, c0:c0 + cw]),
                              writes=[skey])
                        eng = ev_eng()
                        o = dst3[:, k, c0:c0 + cw]
                        if gname is None:
                            copy(eng, o, stg[:, 0:cw], [skey], [key])
                        elif eng == "act":
                            P.op("act", lambda e, o=o, stg=stg, cw=cw, k=k: e.activation(out=o, in_=stg[:, 0:cw], func=AF.Copy, scale=pkc(gname, k)),
                                 [skey, "pk"], [key])
                        else:
                            P.op("dve", lambda e, o=o, stg=stg, cw=cw, k=k: e.tensor_scalar(out=o, in0=stg[:, 0:cw], scalar1=pkc(gname, k), scalar2=None, op0=ALU.mult),
                                 [skey, "pk"], [key])
                        i += 1

            stop_at(1)
            load_cast(wmkvb, wmkv_d, 512, "gmem", "wmkvb")
            stop_at(2)
            load_cast(wb, wcat_d, NCOL, "gin", "wb")
            load_cast(woutb, wout_d, D, None, "woutb")

            P.op("pool", lambda e: e.memset(cvec[0:64, :], 1.0 / 64), writes=["cvec"])
            P.op("pool", lambda e: e.memset(cvec[64:65, :], 1e-6), writes=["cvec"])

            def norm_transpose(src_tile, skey, dstT, dkey, col0, slot):
                h = hb[slot]
                hk = "hb0"
                ssq = st[:, slot:slot + 1]
                P.op("act", lambda e: e.activation(out=h[:], in_=src_tile[:], func=AF.Square, accum_out=ssq), [skey], [hk, "st%d" % slot])
                P.op("dve", lambda e: e.tensor_scalar(out=ssq, in0=ssq, scalar1=1.0 / D, scalar2=1e-6, op0=ALU.mult, op1=ALU.add), ["st%d" % slot], ["st%d" % slot])
                P.op("dve", lambda e: e.reciprocal(out=ssq, in_=ssq), ["st%d" % slot], ["st%d" % slot])
                P.op("act", lambda e: e.activation(out=ssq, in_=ssq, func=AF.Sqrt), ["st%d" % slot], ["st%d" % slot])
                P.op("dve", lambda e: e.tensor_scalar(out=h[:], in0=src_tile[:], scalar1=ssq, scalar2=None, op0=ALU.mult), [skey, "st%d" % slot], [hk])
                for k in range(8):
                    P.op("pe", lambda e, k=k: e.transpose(out=tp[:, k * 128:(k + 1) * 128], in_=h[:, k * 128:(k + 1) * 128], identity=ident[:]), [hk, "ident"], ["tp"])
                copy(ev_eng(), dstT[:, :, col0:col0 + 128], tp[:].rearrange("p (k t) -> p k t", k=8), ["tp"], [dkey])


            for nm, dst, src in (("pm", pm, pm_d), ("ovl", ovl, ovl_d), ("addw", addw, addw_d),
                                 ):
                P.dma("sp", lambda e, dst=dst, src=src: e.dma_start(out=dst[:], in_=src), writes=[nm])
            P.dma("sp", lambda e: e.dma_start(out=gselp[:], in_=gselp_d), writes=["gselp"])
            P.dma("sp", lambda e: e.dma_start(out=dsel[:], in_=dsel_d), writes=["dsel"])
            P.op("pool", lambda e: e.memset(epsc[:], 1e-6), writes=["epsc"])
            P.op("pool", lambda e: e.memset(oab[:], 0.0), writes=["oab"])
            P.op("pool", lambda e: e.memset(e64[:], 0.0), writes=["e64"])
            P.op("pool", lambda e: e.memset(e64[64:65, :], 1.0), writes=["e64"])
            P.op("pool", lambda e: e.memset(kcmpT[:], 0.0), writes=["kcmpT"])
            P.op("pool", lambda e: e.memset(vcmpA[:], 0.0), writes=["vcmpA"])
            P.op("pool", lambda e: e.memset(vsA[:], 1.0), writes=["vsA"])
            P.op("pool", lambda e: e.memset(vwA[:], 1.0), writes=["vwA"])
            P.op("pool", lambda e: e.memset(kwT[:], 0.0), writes=["kwT"])
            for c0 in range(0, T, 4096):
                cw = min(4096, T - c0)
                P.dma("sp", lambda e, c0=c0, cw=cw: e.dma_start(out=ksT[64:128, c0:c0 + cw], in_=selpat_d[:, 0:cw]), writes=["ksT"])
            P.op("pool", lambda e: e.memset(vstg[:], 1.0), writes=["vstg"])
            P.op("pool", lambda e: e.memset(kcvc[:], 0.0), writes=["kcvc"])
            for hlf in range(2):
                P.dma("sp", lambda e, hlf=hlf: e.dma_start(out=xt[hlf][:], in_=w1_d[:, hlf * 1024:(hlf + 1) * 1024]), writes=["xt%d" % hlf])
                copy(ev_eng(), w1b[:, hlf * 16:(hlf + 1) * 16, :], xt[hlf][:].rearrange("p (l e) -> p l e", e=64), ["xt%d" % hlf], ["w1b"])
            P.dma("sp", lambda e: e.dma_start(out=xt[0][:, 0:192], in_=w2_d[:, :]), writes=["xt0"])
            copy("dve", w2b[:], xt[0][0:64, 0:192], ["xt0"], ["w2b"])
            o_, w_ = PK["posT"]
            copy("dve", posTb[:], pk[:, o_:o_ + 32], ["pk"], ["posTb"])
            for kv in range(2):
                b0 = kv * 64
                for l in range(32):
                    mm(mi[0:64, kv:kv + 1], w1b[b0:b0 + 64, l, :], posTb[b0:b0 + 64, l:l + 1], l == 0, l == 31, ["w1b", "posTb"], ["mi"])
                copy("dve", ccst[:, kv:kv + 1], mi[0:64, kv:kv + 1], ["mi"], ["ccst"])

            for nm, dst, src in (("maskL2", maskL2, maskL2_d), ("maskU2", maskU2, maskU2_d), ("maskUI2", maskUI2, maskUI2_d),
                                 ("ident2", ident2, ident2_d), ("blk1", blk1, blk1_d), ("blkavg", blkavg, blkavg_d)):
                P.dma("sp", lambda e, dst=dst, src=src: e.dma_start(out=dst[:], in_=src), writes=[nm])
            P.dma("sp", lambda e: e.dma_start(out=xt[1][:, 0:512], in_=lora_d[:, :]), writes=["xt1"])
            copy("dve", loraup[:], xt[1][:, 0:512], ["xt1"], ["loraup"])
            P.dma("sp", lambda e: e.dma_start(out=coef[0:1, 0:512], in_=w0row_d[:, :]), writes=["coef"])
            copy("dve", w0hi[:], coef[0:1, 0:512], ["coef"], ["w0hi"])
            P.op("dve", lambda e: e.tensor_tensor(out=coef[0:1, 0:512], in0=coef[0:1, 0:512], in1=w0hi[:], op=ALU.subtract), ["coef", "w0hi"], ["coef"])
            copy("dve", w0lo[:], coef[0:1, 0:512], ["coef"], ["w0lo"])
            o_, w_ = PK["mu"]
            P.op("dve", lambda e, o_=o_: e.tensor_scalar(out=mu1m[:], in0=pk[:, o_:o_ + 17], scalar1=-1.0, scalar2=1.0, op0=ALU.mult, op1=ALU.add), ["pk"], ["mu1m"])
            P.op("pool", lambda e: e.memset(onesrow[:], 1.0), writes=["onesrow"])
            P.op("pool", lambda e: e.memset(prevcol[:], 0.0), writes=["prevcol"])
            P.op("pool", lambda e: e.memset(STb[:], 0.0), writes=["STb"])
            o_, w_ = PK["ka"]
            P.op("dve", lambda e, o_=o_: e.tensor_scalar(out=ka1m[:], in0=pk[:, o_:o_ + 4], scalar1=-1.0, scalar2=1.0, op0=ALU.mult, op1=ALU.add), ["pk"], ["ka1m"])
            tokall = wmkvb[:, 4:8, :].rearrange("p c (w f) -> p c w f", w=4)
            stop_at(3)
            for mt in range(2):
                P.dma("sp", lambda e, mt=mt: e.dma_start(out=xt[mt][:], in_=mem_d[mt * 128:(mt + 1) * 128, :]), writes=["xt%d" % mt])
                norm_transpose(xt[mt], "xt%d" % mt, hT, "hT", mt * 128, mt)
            stop_at(4)
            for pr in range(2):
                for k in range(8):
                    mm(pj[0][:, 0:256], wmkvb[:, k, pr * 128:(pr + 1) * 128], hT[:, k, 0:256], k == 0, k == 7, ["wmkvb", "hT"], ["pj0"])
                copy(ev_eng(), memkT[:, pr, :], pj[0][:, 0:256], ["pj0"], ["memkT"])
            P.op("pool", lambda e: e.memset(memvA[:], 1.0), writes=["memvA"])
            for mt in range(2):
                for k in range(8):
                    mm(pj[1][:, 0:256], hT[:, k, mt * 128:(mt + 1) * 128], wmkvb[:, k, 256:512], k == 0, k == 7, ["wmkvb", "hT"], ["pj1"])
                copy(ev_eng(), memvA[:, mt, :, 0:64], pj[1][:, 0:256].rearrange("p (h d) -> p h d", h=4), ["pj1"], ["memvA"])

            def finalize_head(o_ps, okey, use_den, hb_, gcol_ap, gate_ap, gate_key, dst_ap, dst_key, from_sbuf=False):
                sl = slice(hb_, hb_ + 64)
                if from_sbuf:
                    copy("dve", oaug[sl, :], o_ps, [okey], ["oaug"])
                    P.op("act", lambda e: e.activation(out=osq[0:64, :], in_=o_ps, func=AF.Square), [okey], ["osq"])
                    P.op("pool", lambda e: e.memset(osq[64:65, :], 1.0), [], ["osq"])
                elif use_den:
                    copy("dve", oaug[sl, :], o_ps[0:64, :], [okey], ["oaug"])
                    P.op("act", lambda e: e.activation(out=osq[:], in_=o_ps[0:65, :], func=AF.Square), [okey], ["osq"])
                else:
                    copy("dve", oaug[sl, :], o_ps[0:64, :], [okey], ["oaug"])
                    P.op("act", lambda e: e.activation(out=osq[0:64, :], in_=o_ps[0:64, :], func=AF.Square), [okey], ["osq"])
                    P.op("pool", lambda e: e.memset(osq[64:65, :], 1.0), [], ["osq"])
                mm(mi[:], cvec[:], osq[:], True, True, ["cvec", "osq"], ["mi"])
                P.op("act", lambda e: e.activation(out=scl[sl, :], in_=mi[sl, :], func=AF.Ln), ["mi"], ["coef"])
                P.op("act", lambda e: e.activation(out=scl[sl, :], in_=scl[sl, :], func=AF.Exp, scale=-0.5), ["coef"], ["coef"])
                P.op("dve", lambda e: e.scalar_tensor_tensor(out=scl[sl, :], in0=scl[sl, :], scalar=gcol_ap, in1=oaug[sl, :], op0=ALU.mult, op1=ALU.mult), ["coef", "oaug", "pk"], ["coef"])
                P.op("dve", lambda e: e.tensor_tensor(out=dst_ap, in0=scl[sl, :], in1=gate_ap, op=ALU.mult), ["coef", gate_key], [dst_key])

            P.op("pool", lambda e: e.memset(wmkvb[:, 4:8, :], 0.0), ["wmkvb"], ["wmkvb", "tokall"])
            stop_at(5)
            for g in range(NG):
                t0 = g * G
                for i in range(4):
                    P.dma("sp", lambda e, i=i, t0=t0: e.dma_start(out=xt[i % 2][:], in_=x_d[t0 + i * 128:t0 + (i + 1) * 128, :]), writes=["xt%d" % (i % 2)])
                    norm_transpose(xt[i % 2], "xt%d" % (i % 2), hT, "hT", i * 128, i % 2)

                pj_rot = {"i": 0}

                def project(chunk, width=128):
                    if P.phase in ("rwkv", "nsa", "mem"):
                        banks = ((pj[0], "pj0"), (pj[1], "pj1"), (sc[0], "sc0"), (sc[1], "sc1"))
                    else:
                        banks = ((pj[0], "pj0"), (pj[1], "pj1"))
                    pb, pkey = banks[pj_rot["i"] % len(banks)]
                    pj_rot["i"] += 1
                    for k in range(8):
                        mm(pb[0:width, :], wb[:, k, chunk * 128:chunk * 128 + width], hT[:, k, :], k == 0, k == 7, ["wb", "hT"], [pkey])
                    return pb, pkey


                P.phase = "rwkv"
                C0 = float(np.exp(-0.5))

                def tt(eng, out, in0, in1, op, reads, writes):
                    P.op(eng, lambda e: e.tensor_tensor(out=out, in0=in0, in1=in1, op=op), reads, writes)

                def ts(eng, out, in0, s1, s2, op0, op1, reads, writes):
                    if s2 is None:
                        P.op(eng, lambda e: e.tensor_scalar(out=out, in0=in0, scalar1=s1, scalar2=None, op0=op0), reads, writes)
                    else:
                        P.op(eng, lambda e: e.tensor_scalar(out=out, in0=in0, scalar1=s1, scalar2=s2, op0=op0, op1=op1), reads, writes)

                def stt(eng, out, in0, scalar, in1, op0, op1, reads, writes):
                    P.op(eng, lambda e: e.scalar_tensor_tensor(out=out, in0=in0, scalar=scalar, in1=in1, op0=op0, op1=op1), reads, writes)

                def actf(out, in_, func, reads, writes, bias=None, scale=None):
                    kw = {}
                    if bias is not None:
                        kw["bias"] = bias
                    if scale is not None:
                        kw["scale"] = scale
                    P.op("act", lambda e: e.activation(out=out, in_=in_, func=func, **kw), reads, writes)

                def pcol(name, j):
                    o, w = PK[name]
                    return pk[:, o + j:o + j + 1]

                def lerp_chunk(cidx, dst_ap, dkey):
                    pb, pkey = project(cidx)
                    lt, lk = ltb[cidx % 2], "lt%d" % (cidx % 2)
                    actf(lt[:, 1:G + 1], pb[:], AF.Copy, [pkey, "pk"], [lk], scale=pcol("mu", cidx))
                    copy("dve", lt[:, 0:1], prevcol[:, cidx:cidx + 1], ["prevcol"], [lk])
                    copy("dve", prevcol[:, cidx:cidx + 1], lt[:, G:G + 1], [lk], ["prevcol"])
                    stt("dve", dst_ap, pb[:], mu1m[:, cidx:cidx + 1], lt[:, 0:G], ALU.mult, ALU.add, [pkey, "mu1m", lk], [dkey])

                rT, kmT = qn[:, 0, :], qn[:, 1, :]
                vT, gT = qr[:, 0, :], qr[:, 1, :]
                bT, At = gn[:, 0, :], gn[:, 1, :]
                Rt, Bt = qm[:, 0, :], qm[:, 1, :]
                Kt, Bb = gm[:, 0, :], gm[:, 1, :]
                Kb, kkn = pT[0][:], pT[1][:]
                aT, lin = kraw[:], nb[:]
                kf, yraw = acc[:, 0, :], acc[:, 1, :]
                Etmp, Esq = hb0[:, 0:G], hb0[:, G:2 * G]

                lerp_chunk(16, kf, "acc")
                actf(lin[0:64, :], kf[0:64, :], AF.Tanh, ["acc"], ["nb"])
                copy("dve", lin[64:128, :], kf[64:128, :], ["acc"], ["nb"])

                stop_at(40)
                for hp in range(4):
                    lerp_chunk(4 * hp + 0, rT, "qn")
                    lerp_chunk(4 * hp + 1, kf, "acc")
                    lerp_chunk(4 * hp + 2, vT, "qr")
                    lerp_chunk(4 * hp + 3, gT, "qr")
                    actf(gT, gT, AF.Silu, ["qr"], ["qr"])
                    hc = slice(hp * 128, (hp + 1) * 128)
                    stop_at(41)
                    for c in range(4):
                        reg = mi[:, c * 128:(c + 1) * 128]
                        mm(reg, lin[0:64, c * 128:(c + 1) * 128], loraup[0:64, hc], True, False, ["nb", "loraup"], ["mi"])
                        mm(reg, onesrow[:], w0hi[0:1, hc], False, False, ["onesrow", "w0hi"], ["mi"])
                        mm(reg, onesrow[:], w0lo[0:1, hc], False, True, ["onesrow", "w0lo"], ["mi"])
                    actf(sgtok[:].rearrange("p c f -> p (c f)"), mi[:], AF.Sigmoid, ["mi"], ["sgtok"])
                    for c in range(4):
                        mm(sc[0][:, c * 128:(c + 1) * 128], sgtok[:, c, :], maskUI2[:, 0, :], True, True, ["sgtok", "maskUI2"], ["sc0"])
                    for c in range(4):
                        mm(sc[1][:, c * 128:(c + 1) * 128], sgtok[:, c, :], maskU2[:, 0, :], True, True, ["sgtok", "maskU2"], ["sc1"])
                    stop_at(42)
                    mm(oa[0][:], loraup[64:128, hc], lin[64:128, :], True, True, ["nb", "loraup"], ["oa0"])
                    actf(aT, oa[0][:], AF.Sigmoid, ["oa0", "pk"], ["kraw"], bias=pcol("a0", hp))
                    ts("dve", coef[:], kf, pcol("kk", hp), None, ALU.mult, None, ["acc", "pk"], ["coef"])
                    actf(Esq, coef[:], AF.Square, ["coef"], ["hb0"])
                    mm(oa[1][:], blk1[:], Esq, True, True, ["blk1", "hb0"], ["oa1"])
                    ts("dve", yraw, oa[1][:], 1e-24, None, ALU.max, None, ["oa1"], ["acc/1"])
                    actf(yraw, yraw, AF.Ln, ["acc/1"], ["acc/1"])
                    actf(yraw, yraw, AF.Exp, ["acc/1"], ["acc/1"], scale=-0.5)
                    tt("dve", kkn, coef[:], yraw, ALU.mult, ["coef", "acc/1"], ["pT1"])
                    ts("dve", coef[:], aT, pcol("ka", hp), ka1m[:, hp:hp + 1], ALU.mult, ALU.add, ["kraw", "pk", "ka1m"], ["coef"])
                    tt("dve", kmT, kf, coef[:], ALU.mult, ["acc", "coef"], ["qn"])
                    tt("pool", bT, kkn, aT, ALU.mult, ["pT1", "kraw"], ["gn"])
                    if g == 0 and hp == 0:
                        dbg_out("rT", rT, "qn"); dbg_out("kmT", kmT, "qn"); dbg_out("vT", vT, "qr"); dbg_out("aT", aT, "kraw"); dbg_out("kkn", kkn, "pT1"); dbg_out("bT", bT, "gn")
                        copy("dve", coef[:], sc[0][:], ["sc0"], ["coef"]); dbg_out("cumI", coef[:], "coef")
                        copy("dve", coef[:], sc[1][:], ["sc1"], ["coef"]); dbg_out("cumE", coef[:], "coef")
                    stop_at(43)
                    actf(Etmp, sc[1][:], AF.Exp, ["sc1"], ["hb0"], scale=-C0)
                    stt("dve", At, Etmp, -1.0, kkn, ALU.mult, ALU.mult, ["hb0", "pT1"], ["gn"])
                    actf(Etmp, sc[0][:], AF.Exp, ["sc0"], ["hb0"], scale=-C0)
                    tt("dve", Rt, rT, Etmp, ALU.mult, ["qn", "hb0"], ["qm"])
                    actf(Esq, sc[0][:], AF.Exp, ["sc0"], ["hb0"], scale=C0)
                    tt("dve", Bt, bT, Esq, ALU.mult, ["gn", "hb0"], ["qm"])
                    tt("pool", Kt, kmT, Esq, ALU.mult, ["qn", "hb0"], ["gm"])
                    ts("dve", ncL[:], sc[0][:, 127:G:128], -C0, None, ALU.mult, None, ["sc0"], ["ncL"])
                    actf(WL[:], ncL[:], AF.Exp, ["ncL"], ["WL"])
                    copy("dve", WL0[:, 0:4], WL[0:64, :], ["WL"], ["WL0"])
                    copy("dve", WL0[:, 4:8], WL[64:128, :], ["WL"], ["WL0"])
                    for c in range(4):
                        actf(Etmp[:, c * 128:(c + 1) * 128], sc[0][:, c * 128:(c + 1) * 128], AF.Exp, ["sc0", "ncL"], ["hb0"], bias=ncL[:, c:c + 1], scale=C0)
                    tt("dve", Bb, bT, Etmp, ALU.mult, ["gn", "hb0"], ["gm"])
                    tt("pool", Kb, kmT, Etmp, ALU.mult, ["qn", "hb0"], ["pT0"])
                    stop_at(44)
                    for c in range(4):
                        cs = slice(c * 128, (c + 1) * 128)
                        half = (c % 2) * 512
                        for wi, (src, skey) in enumerate(((At, "gn"), (vT, "qr"), (Bb, "gm"), (Kb, "pT0"))):
                            P.op("pe", lambda e, src=src, cs=cs, half=half, wi=wi: e.transpose(out=tp[:, half + wi * 128:half + (wi + 1) * 128], in_=src[:, cs], identity=ident[:]), [skey, "ident"], ["tp"])
                        copy(ev_eng(), tokall[:, c, :, :], tp[:, half:half + 512].rearrange("p (w f) -> p w f", w=4), ["tp"], ["tokall"])

                    P.phase = "rwkv_chain"
                    stop_at(45)

                    def r2(bank):
                        return bank[:, 0:256].rearrange("p (e f) -> p e f", e=2)

                    def hs(e):
                        return slice(64 * e, 64 * e + 64)

                    def v4(ap):
                        return ap.rearrange("p (c e f) -> p c e f", c=2, e=2)

                    def bc4(m2):
                        return m2[:, 0:1, :].broadcast_to([128, 4, 128]).rearrange("p (c e) f -> p c e f", c=2)

                    PmH, PmK = [hb0[:, 0:G], hb0[:, G:2 * G]], ["hb0/0", "hb0/1"]
                    PTH, PTK = [pT[0][:], pT[1][:]], ["pT0", "pT1"]
                    XTH, XTK = [gn[:, 0, :], kraw[:]], ["gn/0", "kraw"]
                    bkP = [(oa[0], "oa0"), (oa[1], "oa1")]
                    bkT = [(pj[0], "pj0"), (pj[1], "pj1")]
                    bkX = [(mi, "mi"), (sc[0], "sc0")]

                    def rg(bank, cl, e):
                        o = (cl * 2 + e) * 128
                        return bank[:, o:o + 128]

                    for h2 in range(2):
                        for cl in range(2):
                            cs = slice((2 * h2 + cl) * 128, (2 * h2 + cl + 1) * 128)
                            for e in range(2):
                                mm(rg(bkP[h2][0], cl, e), At[hs(e), cs], Bt[hs(e), cs], True, True, ["gn/1", "qm"], [bkP[h2][1]])
                        for cl in range(2):
                            cs = slice((2 * h2 + cl) * 128, (2 * h2 + cl + 1) * 128)
                            for e in range(2):
                                mm(rg(bkT[h2][0], cl, e), Bt[hs(e), cs], At[hs(e), cs], True, True, ["gn/1", "qm"], [bkT[h2][1]])
                    for h2 in range(2):
                        tt("dve", v4(PmH[h2]), v4(bkP[h2][0][:]), bc4(maskL2), ALU.mult, [bkP[h2][1], "maskL2"], [PmK[h2]])
                        tt("dve", v4(PTH[h2]), v4(bkT[h2][0][:]), bc4(maskU2), ALU.mult, [bkT[h2][1], "maskU2"], [PTK[h2]])
                        tt("pool", v4(XTH[h2]), v4(PTH[h2]), bc4(ident2), ALU.add, [PTK[h2], "ident2"], [XTK[h2]])
                    for lvl in range(1, 7):
                        for h2 in range(2):
                            for cl in range(2):
                                for e in range(2):
                                    mm(rg(bkP[h2][0], cl, e), v4(PTH[h2])[:, cl, e, :], v4(PmH[h2])[:, cl, e, :], True, True, [PTK[h2], PmK[h2]], [bkP[h2][1]])
                            if lvl < 6:
                                for cl in range(2):
                                    for e in range(2):
                                        mm(rg(bkT[h2][0], cl, e), v4(PmH[h2])[:, cl, e, :], v4(PTH[h2])[:, cl, e, :], True, True, [PTK[h2], PmK[h2]], [bkT[h2][1]])
                        for h2 in range(2):
                            copy("act", PmH[h2], bkP[h2][0][:], [bkP[h2][1]], [PmK[h2]])
                            if lvl < 6:
                                copy("dve" if h2 == 0 else "act", PTH[h2], bkT[h2][0][:], [bkT[h2][1]], [PTK[h2]])
                        for h2 in range(2):
                            for cl in range(2):
                                for e in range(2):
                                    mm(rg(bkX[h2][0], cl, e), v4(PmH[h2])[:, cl, e, :], v4(XTH[h2])[:, cl, e, :], True, True, [PmK[h2], XTK[h2]], [bkX[h2][1]])
                        for h2 in range(2):
                            tt("dve", XTH[h2], XTH[h2], bkX[h2][0][:], ALU.add, [XTK[h2], bkX[h2][1]], [XTK[h2]])

                    stop_at(46)
                    def v8(ap):
                        return ap.rearrange("p (c e f) -> p c e f", c=4, e=2)

                    ArbH, ArbK = [hb0[:, 0:G], hb0[:, G:2 * G]], ["hb0/0", "hb0/1"]
                    ArkH, ArkK = [pT[0][:], pT[1][:]], ["pT0", "pT1"]
                    AakH, AakK = [AakT4[:, 0, :], AakT4[:, 1, :]], ["AakT4/0", "AakT4/1"]
                    Ahat4, AhK = v8(gm[:, 1, :]), "gm/1"
                    Uhat4, UhK = v8(sgtok[:].rearrange("p c f -> p (c f)")), "sgtok"
                    AkV4v = v8(AkV4[:])
                    RhH, RhK = [oab[0:64, :], osq[0:64, :]], ["oab", "osq"]
                    Msb4 = v8(qcat[0:64, 0, :])

                    def XT(c):
                        return v4(XTH[c // 2])[:, c % 2, :, :], XTK[c // 2]

                    for h2 in range(2):
                        for cl in range(2):
                            cs = slice((2 * h2 + cl) * 128, (2 * h2 + cl + 1) * 128)
                            for e in range(2):
                                mm(rg(bkP[h2][0], cl, e), Kt[hs(e), cs], At[hs(e), cs], True, True, ["gm/0", "gn/1"], [bkP[h2][1]])
                        for cl in range(2):
                            cs = slice((2 * h2 + cl) * 128, (2 * h2 + cl + 1) * 128)
                            for e in range(2):
                                mm(rg(bkT[h2][0], cl, e), Bt[hs(e), cs], Rt[hs(e), cs], True, True, ["qm"], [bkT[h2][1]])
                        for cl in range(2):
                            cs = slice((2 * h2 + cl) * 128, (2 * h2 + cl + 1) * 128)
                            for e in range(2):
                                mm(rg(bkX[h2][0], cl, e), Kt[hs(e), cs], Rt[hs(e), cs], True, True, ["gm/0", "qm"], [bkX[h2][1]])
                    for h2 in range(2):
                        tt("dve", v4(AakH[h2]), v4(bkP[h2][0][:]), bc4(maskU2), ALU.mult, [bkP[h2][1], "maskU2"], [AakK[h2]])
                        tt("dve", v4(ArbH[h2]), v4(bkT[h2][0][:]), bc4(maskUI2), ALU.mult, [bkT[h2][1], "maskUI2"], [ArbK[h2]])
                        tt("dve", v4(ArkH[h2]), v4(bkX[h2][0][:]), bc4(maskUI2), ALU.mult, [bkX[h2][1], "maskUI2"], [ArkK[h2]])
                    for c in range(4):
                        for e in range(2):
                            o = (c * 2 + e) * 64
                            mm(oa[0][:, o:o + 64], v4(AakH[c // 2])[:, c % 2, e, :], tokall[:, c, 1, hs(e)], True, True, [AakK[c // 2], "tokall"], ["oa0"])
                    copy("act", AkV4[:], oa[0][:], ["oa0"], ["AkV4"])
                    for c in range(4):
                        xt_, xk_ = XT(c)
                        for e in range(2):
                            o = (c * 2 + e) * 64
                            mm(oa[1][:, o:o + 64], xt_[:, e, :], tokall[:, c, 0, hs(e)], True, True, [xk_, "tokall"], ["oa1"])
                    copy("dve", gm[:, 1, :], oa[1][:], ["oa1"], [AhK])
                    for c in range(4):
                        xt_, xk_ = XT(c)
                        for e in range(2):
                            o = (c * 2 + e) * 64
                            mm(pj[0][:, o:o + 64], xt_[:, e, :], AkV4v[:, c, e, :], True, True, [xk_, "AkV4"], ["pj0"])
                    copy("act", sgtok[:].rearrange("p c f -> p (c f)"), pj[0][:], ["pj0"], [UhK])
                    for h2 in range(2):
                        for cl in range(2):
                            c = 2 * h2 + cl
                            cs = slice(c * 128, (c + 1) * 128)
                            for e in range(2):
                                reg = rg(bkX[h2][0], cl, e)[0:64, :]
                                mm(reg, Ahat4[:, c, e, :], v4(ArbH[h2])[:, cl, e, :], True, False, [AhK, ArbK[h2]], [bkX[h2][1]])
                                mm(reg, ident[hs(e), hs(e)], Rt[hs(e), cs], False, True, ["ident", "qm"], [bkX[h2][1]])
                        copy("dve" if h2 == 0 else "act", RhH[h2], bkX[h2][0][0:64, :], [bkX[h2][1]], [RhK[h2]])
                    for c in range(4):
                        for e in range(2):
                            o = (c * 2 + e) * 64
                            mm(pj[1][0:64, o:o + 64], Ahat4[:, c, e, :], tokall[:, c, 2, hs(e)], True, True, [AhK, "tokall"], ["pj1"])
                    copy("act", qcat[0:64, 0, :], pj[1][0:64, :], ["pj1"], ["qcat"])
                    stop_at(47)
                    for c in range(4):
                        cs = slice(c * 128, (c + 1) * 128)
                        h2, cl = c // 2, c % 2
                        rh = v4(RhH[h2])

                        def s_src(e):
                            return (STb[:, 2 * hp + e, :], "STb") if c % 2 == 0 else (STn[:, e, :], "STn")

                        def s_dst(e):
                            return (STn[:, e, :], "STn") if c % 2 == 0 else (STb[:, 2 * hp + e, :], "STb")

                        ob_, obk = oa[c % 2], "oa%d" % (c % 2)
                        for e in range(2):
                            reg = ob_[0:64, e * 64:(e + 1) * 64]
                            mm(reg, tokall[:, c, 2, hs(e)], Uhat4[:, c, e, :], True, False, ["tokall", UhK], [obk])
                            mm(reg, tokall[:, c, 3, hs(e)], tokall[:, c, 1, hs(e)], False, False, ["tokall"], [obk])
                            mm(reg, Msb4[:, c, e, :], s_src(e)[0], False, True, ["qcat", s_src(e)[1]], [obk])
                        for e in range(2):
                            stt("dve", s_dst(e)[0], s_src(e)[0], WL0[:, 4 * e + c:4 * e + c + 1], ob_[0:64, e * 64:(e + 1) * 64], ALU.mult, ALU.add, [s_src(e)[1], "WL0", obk], [s_dst(e)[1]])
                        for e in range(2):
                            reg = sc[1][0:64, e * 128:(e + 1) * 128]
                            mm(reg, Uhat4[:, c, e, :], v4(ArbH[h2])[:, cl, e, :], True, False, [UhK, ArbK[h2]], ["sc1"])
                            mm(reg, tokall[:, c, 1, hs(e)], v4(ArkH[h2])[:, cl, e, :], False, False, ["tokall", ArkK[h2]], ["sc1"])
                            mm(reg, s_src(e)[0], rh[:, cl, e, :], False, True, [s_src(e)[1], RhK[h2]], ["sc1"])
                        copy("act", yraw[0:64, cs], sc[1][0:64, 0:128], ["sc1"], ["acc"])
                        copy("act", yraw[64:128, cs], sc[1][0:64, 128:256], ["sc1"], ["acc"])

                    if g == 0 and hp == 0:
                        dbg_out("yraw", yraw, "acc")
                    P.phase = "rwkv"
                    stop_at(49)
                    copy("act", Etmp, yraw, ["acc"], ["hb0"])
                    actf(Esq, yraw, AF.Square, ["acc"], ["hb0"])
                    mm(sc[0][:], blkavg[:], Etmp, True, True, ["blkavg", "hb0"], ["sc0"])
                    mm(sc[1][:], blkavg[:], Esq, True, True, ["blkavg", "hb0"], ["sc1"])
                    actf(coef[:], sc[0][:], AF.Square, ["sc0"], ["coef"])
                    tt("dve", coef[:], sc[1][:], coef[:], ALU.subtract, ["sc1", "coef"], ["coef"])
                    ts("dve", coef[:], coef[:], 64e-5, None, ALU.add, None, ["coef"], ["coef"])
                    actf(coef[:], coef[:], AF.Ln, ["coef"], ["coef"])
                    actf(coef[:], coef[:], AF.Exp, ["coef"], ["coef"], scale=-0.5)
                    tt("dve", yraw, yraw, sc[0][:], ALU.subtract, ["acc", "sc0"], ["acc"])
                    tt("dve", yraw, yraw, coef[:], ALU.mult, ["acc", "coef"], ["acc"])
                    ts("dve", yraw, yraw, pcol("lnw", hp), pcol("lnb", hp), ALU.mult, ALU.add, ["acc", "pk"], ["acc"])
                    tt("pool", Etmp, rT, kmT, ALU.mult, ["qn"], ["hb0"])
                    ts("dve", Esq, Etmp, pcol("rk", hp), None, ALU.mult, None, ["hb0", "pk"], ["hb0"])
                    mm(sc[0][:], blk1[:], Esq, True, True, ["blk1", "hb0"], ["sc0"])
                    tt("dve", coef[:], sc[0][:], vT, ALU.mult, ["sc0", "qr"], ["coef"])
                    tt("dve", yraw, yraw, coef[:], ALU.add, ["acc", "coef"], ["acc"])
                    tt("dve", yT[:, hp, :], yraw, gT, ALU.mult, ["acc", "qr"], ["yT"])

                P.phase = "mem"
                stop_at(6)
                for pr in range(2):
                    pb, pkey = project(25 + pr)
                    P.op("act", lambda e, pb=pb, pr=pr: e.activation(out=qm[:, pr, :], in_=pb[:], func=AF.Copy, scale=0.125), [pkey], ["qm"])
                    pb, pkey = project(27 + pr)
                    P.op("act", lambda e, pb=pb, pr=pr: e.activation(out=gm[:, pr, :], in_=pb[:], func=AF.Silu), [pkey], ["gm"])
                stop_at(61)
                for h in range(4):
                    pr, hb_ = h // 2, (h % 2) * 64
                    ob = oa[h % 2]
                    okey = "oa%d" % (h % 2)
                    for mc in range(2):
                        sb_ = sc[mc]
                        mm(sb_[:], memkT[hb_:hb_ + 64, pr, mc * 128:(mc + 1) * 128], qm[hb_:hb_ + 64, pr, :], True, True, ["memkT", "qm"], ["sc%d" % mc])
                        P.op("act", lambda e, sb_=sb_, mc=mc: e.activation(out=pT[mc][:], in_=sb_[:], func=AF.Exp), ["sc%d" % mc], ["pT%d" % mc])
                    stop_at(62 if h == 0 else 66)
                    for mc in range(2):
                        mm(ob[0:65, :], memvA[:, mc, h, :], pT[mc][:], mc == 0, mc == 1, ["memvA", "pT%d" % mc], [okey])
                    stop_at(63 if h == 0 else 67)
                    o, w = PK["memg"]
                    finalize_head(ob, okey, True, hb_, pk[hb_:hb_ + 64, o + pr:o + pr + 1], gm[hb_:hb_ + 64, pr, :], "gm", yT[hb_:hb_ + 64, 6 + pr, :], "yT")


                P.phase = "nsa"
                P.dma("sp", lambda e, t0=t0: e.dma_start(out=ropeC[:], in_=ropeC_d[:, t0:t0 + G]), writes=["ropeC"])
                P.dma("sp", lambda e, t0=t0: e.dma_start(out=ropeS[:], in_=ropeS_d[:, t0:t0 + G]), writes=["ropeS"])

                def rope(src_t, skey, dst_ap, dkey, nr=128):
                    mm(mi[:], pm[:], src_t, True, True, ["pm", skey], ["mi"])
                    P.op("dve", lambda e: e.tensor_tensor(out=rtmp[0:nr, :], in0=mi[0:nr, :], in1=ropeS[0:nr, :], op=ALU.mult), ["mi", "ropeS"], ["coef"])
                    P.op("pool", lambda e: e.tensor_tensor(out=dst_ap, in0=src_t[0:nr, :], in1=ropeC[0:nr, :], op=ALU.mult), [skey, "ropeC"], [dkey])
                    P.op("dve", lambda e: e.tensor_tensor(out=dst_ap, in0=dst_ap, in1=rtmp[0:nr, :], op=ALU.add), [dkey, "coef"], [dkey])

                for pr in range(2):
                    pb, pkey = project(17 + pr)
                    P.op("act", lambda e, pb=pb, pr=pr: e.activation(out=qn[:, pr, :], in_=pb[:], func=AF.Copy, scale=0.125), [pkey], ["qn"])
                    rope(qn[:, pr, :], "qn", qr[:, pr, :], "qr")
                    pb, pkey = project(19 + pr)
                    P.op("act", lambda e, pb=pb, pr=pr: e.activation(out=gn[:, pr, :], in_=pb[:], func=AF.Silu), [pkey], ["gn"])
                copy("dve", kcvc[:, 0:16], kcvc[:, G:G + 16], ["kcvc"], ["kcvc"])
                pb, pkey = project(21)
                copy("act", kcvc[:, 16:16 + G], pb[:], [pkey], ["kcvc"])
                pb, pkey = project(22)
                copy("act", kraw[:], pb[:], [pkey], ["kraw"])
                rope(kraw[:], "kraw", ksT[0:64, t0:t0 + G], "ksT", 64)
                pb, pkey = project(23)
                copy("act", kraw[:], pb[:], [pkey], ["kraw"])
                wo = (g % 2) * G
                rope(kraw[:], "kraw", kwT[0:64, wo:wo + G], "kwT", 64)
                for i in range(4):
                    pbv = pj[i % 2]
                    for k in range(8):
                        mm(pbv[:, 0:128], hT[:, k, i * 128:(i + 1) * 128], wb[:, k, 24 * 128:25 * 128], k == 0, k == 7, ["hT", "wb"], ["pj%d" % (i % 2)])
                    kt = 4 * g + i
                    copy("dve", vsA[:, kt, 0:64], pbv[:, 0:64], ["pj%d" % (i % 2)], ["vsA"])
                    copy("act", vwA[:, kt % 8, 0:64], pbv[:, 64:128], ["pj%d" % (i % 2)], ["vwA"])
                pb, pkey = project(29, 12)
                o_, w_ = PK["gateb"]
                P.op("act", lambda e, pb=pb, o_=o_: e.activation(out=gsig[:], in_=pb[0:12, :], func=AF.Sigmoid, bias=pk[0:12, o_:o_ + 1]), [pkey, "pk"], ["gsig"])

                lo, hi = max(0, 32 * g - 1), 32 * g + 30
                n = hi - lo + 1
                c0 = 16 * lo + 16 - t0
                for kv in range(2):
                    b0 = kv * 64
                    reg = mi[0:64, kv * 64:kv * 64 + n]
                    for l in range(32):
                        mm(reg, w1b[b0:b0 + 64, l, :], kcvc[b0:b0 + 64, c0 + l:c0 + l + 16 * (n - 1) + 1:16], l == 0, l == 31, ["w1b", "kcvc"], ["mi"])
                    dsth = (h1k, h1v)[kv]
                    P.op("act", lambda e, reg=reg, dsth=dsth, kv=kv, n=n: e.activation(out=dsth[:, 0:n], in_=reg, func=AF.Silu, bias=ccst[:, kv:kv + 1]), ["mi", "ccst"], ["h1%d" % kv])
                mm(sc[0][:, 0:n], w2b[:, 0:128], h1k[:, 0:n], True, True, ["w2b", "h10"], ["sc0"])
                copy("dve", kcmpT[:, lo:hi + 1], sc[0][:, 0:n], ["sc0"], ["kcmpT"])
                mm(sc[1][0:n, 0:64], h1v[:, 0:n], w2b[:, 128:192], True, True, ["w2b", "h11"], ["sc1"])
                copy("dve", vstg[0:n, 0:64], sc[1][0:n, 0:64], ["sc1"], ["vstg"])
                i0 = lo
                while i0 <= hi:
                    i1 = min(hi, (i0 // 128) * 128 + 127)
                    P.dma("sp", lambda e, i0=i0, i1=i1, lo=lo: e.dma_start(out=vcmpA[i0 % 128:i1 % 128 + 1, i0 // 128, :], in_=vstg[i0 - lo:i1 - lo + 1, :]), reads=["vstg"], writes=["vcmpA"])
                    i0 = i1 + 1

                def combine_pair(pr, b, first):
                    copy("act", oaug[0:64, :], oa[0][0:64, :], ["oa0"], ["oaug"])
                    copy("dve", oaug[64:128, :], oa[1][0:64, :], ["oa1"], ["oaug"])
                    copy("act", oab[0:1, :], oa[0][64:65, :], ["oa0"], ["oab"])
                    copy("dve", oab[32:33, :], oa[1][64:65, :], ["oa1"], ["oab"])
                    mm(pj[0][:], dsel[:], oab[0:33, :], True, True, ["dsel", "oab"], ["pj0"])
                    mm(pj[1][:], gselp[:, pr * 3 + b, :], gsig[:], True, True, ["gselp", "gsig"], ["pj1"])
                    P.op("dve", lambda e: e.tensor_scalar(out=coef[:], in0=pj[0][:], scalar1=1e-30, scalar2=None, op0=ALU.max), ["pj0"], ["coef"])
                    P.op("act", lambda e: e.activation(out=coef[:], in_=coef[:], func=AF.Ln), ["coef"], ["coef"])
                    P.op("act", lambda e: e.activation(out=coef[:], in_=coef[:], func=AF.Exp, scale=-1.0), ["coef"], ["coef"])
                    P.op("dve", lambda e: e.tensor_tensor(out=coef[:], in0=coef[:], in1=pj[1][:], op=ALU.mult), ["coef", "pj1"], ["coef"])
                    if first:
                        P.op("dve", lambda e: e.tensor_tensor(out=acc[:, pr, :], in0=coef[:], in1=oaug[:], op=ALU.mult), ["coef", "oaug"], ["acc"])
                    else:
                        P.op("dve", lambda e: e.tensor_tensor(out=coef[:], in0=coef[:], in1=oaug[:], op=ALU.mult), ["coef", "oaug"], ["coef"])
                        P.op("dve", lambda e: e.tensor_tensor(out=acc[:, pr, :], in0=acc[:, pr, :], in1=coef[:], op=ALU.add), ["coef", "acc"], ["acc"])

                def pmask(ap, key, base, cm, step):
                    P.op("pool", lambda e: e.affine_select(out=ap, in_=ap, pattern=[[step, G]], compare_op=ALU.is_ge, fill=0.0, base=base, channel_multiplier=cm), [key], [key])

                def pmask(ap, key, base, cm, step):
                    P.op("pool", lambda e: e.affine_select(out=ap, in_=ap, pattern=[[step, G]], compare_op=ALU.is_ge, fill=0.0, base=base, channel_multiplier=cm), [key], [key])

                P.op("pool", lambda e: e.memset(impS[:], 0.0), [], ["impS"])
                ncc = g // 4 + 1
                for h in range(4):
                    pr, hb_ = h // 2, (h % 2) * 64
                    ob, okey = oa[h % 2], "oa%d" % (h % 2)
                    for ci in range(ncc):
                        dl = 2048 * ci - 512 * g
                        sb_, skey = sc[ci % 2], "sc%d" % (ci % 2)
                        mm(sb_[:], kcmpT[hb_:hb_ + 64, ci * 128:(ci + 1) * 128], qn[hb_:hb_ + 64, pr, :], True, True, ["kcmpT", "qn"], [skey])
                        P.op("act", lambda e, sb_=sb_, ci=ci: e.activation(out=pTc[:, ci, :], in_=sb_[:], func=AF.Exp), [skey], ["wmkvb"])
                        if dl >= -2048:
                            pmask(pTc[:, ci, :], "wmkvb", -31 - dl, -16, 1)
                        mm(ob[0:65, :], vcmpA[:, ci, :], pTc[:, ci, :], ci == 0, ci == ncc - 1, ["vcmpA", "wmkvb"], [okey])
                    for j in range(4):
                        reg = mi[:, (j % 2) * 256:(j % 2) * 256 + 129]
                        for ci in range(ncc):
                            mm(reg, pTc[:, ci, j * 128:(j + 1) * 128], ovl[:, ci, :], ci == 0, ci == ncc - 1, ["wmkvb", "ovl"], ["mi"])
                        P.op("dve", lambda e, reg=reg: e.tensor_scalar(out=m8[:, 0:1], in0=reg[:, 128:129], scalar1=1e-30, scalar2=None, op0=ALU.max), ["mi"], ["m8"])
                        P.op("dve", lambda e: e.reciprocal(out=m8[:, 0:1], in_=m8[:, 0:1]), ["m8"], ["m8"])
                        P.op("dve", lambda e, reg=reg, j=j: e.scalar_tensor_tensor(out=impS[:, j, :], in0=reg[:, 0:128], scalar=m8[:, 0:1], in1=impS[:, j, :], op0=ALU.mult, op1=ALU.add), ["mi", "m8", "impS"], ["impS"])
                    if h % 2 == 1:
                        combine_pair(h // 2, 0, True)

                for j in range(4):
                    qt = 4 * g + j
                    u0 = 126 - 2 * qt
                    P.op("dve", lambda e, j=j, u0=u0: e.tensor_tensor(out=score[:], in0=impS[:, j, :], in1=addw[:, u0:u0 + 128], op=ALU.add), ["impS", "addw"], ["score"])
                    P.op("dve", lambda e: e.tensor_scalar(out=score[:, 0:1], in0=score[:, 0:1], scalar1=1e4, scalar2=None, op0=ALU.add), ["score"], ["score"])
                    P.op("dve", lambda e: e.max(out=m8[:, 0:8], in_=score[:]), ["score"], ["m8"])
                    P.op("dve", lambda e: e.match_replace(out=stmp[:], in_to_replace=m8[:, 0:8], in_values=score[:], imm_value=-2.0), ["score", "m8"], ["stmp"])
                    P.op("dve", lambda e: e.max(out=m8[:, 8:16], in_=stmp[:]), ["stmp"], ["m8"])
                    P.op("dve", lambda e: e.tensor_reduce(out=m8[:, 0:1], in_=m8[:, 8:16], axis=AX.X, op=ALU.min), ["m8"], ["m8"])
                    P.op("dve", lambda e: e.tensor_scalar(out=stmp[:], in0=score[:], scalar1=m8[:, 0:1], scalar2=None, op0=ALU.is_ge), ["score", "m8"], ["stmp"])
                    P.op("dve", lambda e: e.tensor_scalar(out=nbq[:], in0=stmp[:], scalar1=1.0, scalar2=-NEG, op0=ALU.subtract, op1=ALU.mult), ["stmp"], ["nbq"])
                    P.op("pe", lambda e: e.transpose(out=tp[:, 0:128], in_=nbq[:], identity=ident[:]), ["nbq", "ident"], ["tp"])
                    copy("dve", nb[:, j * 128:(j + 1) * 128], tp[:, 0:128], ["tp"], ["nb"])

                P.phase = "nsa_sel"
                scA = [(sc[0], "sc0"), (sc[1], "sc1")]
                scB = [(pj[0], "pj0"), (pj[1], "pj1")]
                pTA = [(pT[0][:], "pT0"), (pT[1][:], "pT1")]
                pTB = [(AakT4[:, 0, :], "AakT4/0"), (AakT4[:, 1, :], "AakT4/1")]
                nslab = (4 * g + 3) // 32 + 1
                for pr in range(2):
                    for hl in range(2):
                        for M in range(nslab):
                            copy(("act", "dve")[(hl + M) % 2], qcat[0:64, 2 * hl + M, :], qr[64 * hl:64 * hl + 64, pr, :], ["qr"], ["qcat"])
                            copy(("dve", "pool")[hl], qcat[64:128, 2 * hl + M, :], nb[64 * M:64 * M + 64, :], ["nb"], ["qcat"])
                    nkt = 4 * g + 4

                    def sel_a(kt):
                        (sa, ska), (sb2, skb) = scA[kt % 2], scB[kt % 2]
                        (pa, pka), (pb2, pkb) = pTA[kt % 2], pTB[kt % 2]
                        M = kt // 32
                        ks_ = slice(kt * 128, (kt + 1) * 128)
                        mm(sa[:], ksT[:, ks_], qcat[:, M, :], True, True, ["ksT", "qcat"], [ska])
                        mm(sb2[:], ksT[:, ks_], qcat[:, 2 + M, :], True, True, ["ksT", "qcat"], [skb])
                        P.op("act", lambda e: e.activation(out=pa, in_=sa[:], func=AF.Exp), [ska], [pka])
                        P.op("act", lambda e: e.activation(out=pb2, in_=sb2[:], func=AF.Exp), [skb], [pkb])
                        if kt >= 4 * g:
                            pmask(pa, pka, -128 * (kt - 4 * g), -1, 1)
                            pmask(pb2, pkb, -128 * (kt - 4 * g), -1, 1)

                    sel_a(0)
                    for kt in range(nkt):
                        if kt + 1 < nkt:
                            sel_a(kt + 1)
                        mm(oa[0][0:65, :], vsA[:, kt, :], pTA[kt % 2][0], kt == 0, kt == nkt - 1, ["vsA", pTA[kt % 2][1]], ["oa0"])
                        mm(oa[1][0:65, :], vsA[:, kt, :], pTB[kt % 2][0], kt == 0, kt == nkt - 1, ["vsA", pTB[kt % 2][1]], ["oa1"])
                    combine_pair(pr, 1, False)

                    P.phase = "nsa_win"
                    kts = [kt for kt in range(4 * g - 4, 4 * g + 4) if kt >= 0]

                    def win_a(kt):
                        (sa, ska), (sb2, skb) = scA[kt % 2], scB[kt % 2]
                        (pa, pka), (pb2, pkb) = pTA[kt % 2], pTB[kt % 2]
                        ro = (kt % 8) * 128
                        mm(sa[:], kwT[:, ro:ro + 128], qcat[:, 0, :], True, True, ["kwT", "qcat"], [ska])
                        mm(sb2[:], kwT[:, ro:ro + 128], qcat[:, 2, :], True, True, ["kwT", "qcat"], [skb])
                        P.op("act", lambda e: e.activation(out=pa, in_=sa[:], func=AF.Exp), [ska], [pka])
                        P.op("act", lambda e: e.activation(out=pb2, in_=sb2[:], func=AF.Exp), [skb], [pkb])
                        rel = kt - 4 * g
                        for p_, k_ in ((pa, pka), (pb2, pkb)):
                            if rel >= 0:
                                pmask(p_, k_, -128 * rel, -1, 1)
                            else:
                                pmask(p_, k_, 511 + 128 * rel, 1, -1)

                    win_a(kts[0])
                    for ii, kt in enumerate(kts):
                        if ii + 1 < len(kts):
                            win_a(kts[ii + 1])
                        mm(oa[0][0:65, :], vwA[:, kt % 8, :], pTA[kt % 2][0], ii == 0, ii == len(kts) - 1, ["vwA", pTA[kt % 2][1]], ["oa0"])
                        mm(oa[1][0:65, :], vwA[:, kt % 8, :], pTB[kt % 2][0], ii == 0, ii == len(kts) - 1, ["vwA", pTB[kt % 2][1]], ["oa1"])
                    combine_pair(pr, 2, False)
                    P.phase = "nsa_sel"

                P.phase = "nsa"
                o_, w_ = PK["nsag"]
                for pr in range(2):
                    src = acc[:, pr, :]
                    P.op("act", lambda e, src=src: e.activation(out=hb0[:, 0:G], in_=src, func=AF.Square), ["acc"], ["hb0"])
                    mm(mi[:], blkavg[:], hb0[:, 0:G], True, True, ["blkavg", "hb0"], ["mi"])
                    P.op("act", lambda e: e.activation(out=coef[:], in_=mi[:], func=AF.Ln, bias=epsc[:]), ["mi", "epsc"], ["coef"])
                    P.op("act", lambda e: e.activation(out=coef[:], in_=coef[:], func=AF.Exp, scale=-0.5), ["coef"], ["coef"])
                    P.op("dve", lambda e, src=src, pr=pr, o_=o_: e.scalar_tensor_tensor(out=coef[:], in0=coef[:], scalar=pk[:, o_ + pr:o_ + pr + 1], in1=src, op0=ALU.mult, op1=ALU.mult), ["coef", "acc", "pk"], ["coef"])
                    P.op("dve", lambda e, pr=pr: e.tensor_tensor(out=yT[:, 4 + pr, :], in0=coef[:], in1=gn[:, pr, :], op=ALU.mult), ["coef", "gn"], ["yT"])
                P.phase = ""
                stop_at(7)
                for i in range(4):
                    xb, xk = xt[i % 2], "xt%d" % (i % 2)
                    P.dma("sp", lambda e, i=i, t0=t0, xb=xb: e.dma_start(out=xb[:], in_=x_d[t0 + i * 128:t0 + (i + 1) * 128, :]), writes=[xk])
                    for half in range(2):
                        pb = pj[half]
                        for k in range(8):
                            mm(pb[:], yT[:, k, i * 128:(i + 1) * 128], woutb[:, k, half * 512:(half + 1) * 512], k == 0, k == 7, ["yT", "woutb"], ["pj%d" % half])
                        P.op("dve", lambda e, pb=pb, xb=xb, half=half: e.tensor_tensor(out=xb[:, half * 512:(half + 1) * 512], in0=xb[:, half * 512:(half + 1) * 512], in1=pb[:], op=ALU.add),
                             ["pj%d" % half, xk], [xk])
                    sk = "st%d" % (8 + i)
                    ssq = st[:, 8 + i:9 + i]
                    hjk = hb[i % 2]
                    P.op("act", lambda e, xb=xb, ssq=ssq, hjk=hjk: e.activation(out=hjk[:], in_=xb[:], func=AF.Square, accum_out=ssq), [xk], ["hb0", sk])
                    P.op("dve", lambda e, ssq=ssq: e.tensor_scalar(out=ssq, in0=ssq, scalar1=1.0 / D, scalar2=1e-6, op0=ALU.mult, op1=ALU.add), [sk], [sk])
                    P.op("dve", lambda e, ssq=ssq: e.reciprocal(out=ssq, in_=ssq), [sk], [sk])
                    P.op("act", lambda e, ssq=ssq: e.activation(out=ssq, in_=ssq, func=AF.Sqrt), [sk], [sk])
                    P.op("dve", lambda e, xb=xb, ssq=ssq: e.scalar_tensor_tensor(out=xb[:], in0=xb[:], scalar=ssq, in1=gfin[:], op0=ALU.mult, op1=ALU.mult), [xk, sk, "gfin"], [xk])
                    P.dma("pool", lambda e, i=i, t0=t0, xb=xb: e.dma_start(out=out_d[t0 + i * 128:t0 + (i + 1) * 128, :], in_=xb[:]), reads=[xk], is_out=True)
        except _Stop:
            P.dma("pool", lambda e: e.dma_start(out=out_d[0:128, :], in_=xt[0][:]), reads=["xt0"], is_out=True)
        P.finish("sp")
        P.emit(block, sems, dsems)
    return nc


def make_in_maps(inputs, T, batches):
    ci = _colidx()
    w_in = np.asarray(inputs["w_in"][0])
    wcat = np.ascontiguousarray(w_in[:, ci].reshape(8, 128, NCOL))
    wout = np.ascontiguousarray(np.asarray(inputs["w_out"][0]).reshape(8, 128, D))
    wmkv = np.ascontiguousarray(np.asarray(inputs["w_mem_kv"][0]).reshape(8, 128, 512))
    pk = host_params({k: np.asarray(v) for k, v in inputs.items()})
    w1k = np.asarray(inputs["nsa_cmp_k_w1"][0]).reshape(32, 64, 64).transpose(1, 0, 2)
    w1v = np.asarray(inputs["nsa_cmp_v_w1"][0]).reshape(32, 64, 64).transpose(1, 0, 2)
    w1 = np.ascontiguousarray(np.concatenate([w1k, w1v], 0).reshape(128, 2048))
    w2k = np.asarray(inputs["nsa_cmp_k_w2"][0]); w2v = np.asarray(inputs["nsa_cmp_v_w2"][0])
    w2 = np.zeros((128, 192), np.float32)
    w2[0:64] = np.concatenate([w2k, w2k, w2v], 1)
    lora = np.ascontiguousarray(np.concatenate([np.asarray(inputs["rwkv_w_up"][0]), np.asarray(inputs["rwkv_a_up"][0])], 0))
    w0row = np.ascontiguousarray(np.asarray(inputs["rwkv_w0"][0]).reshape(1, 512))
    consts = host_consts(T)
    maps = []
    for b in batches:
        m = {
            "x": np.ascontiguousarray(np.asarray(inputs["x"][b][:T])),
            "mem": np.ascontiguousarray(np.asarray(inputs["mem"][b])),
            "wcat": wcat, "wout": wout, "wmkv": wmkv, "pk": pk, "w1": w1, "w2": w2, "lora": lora, "w0row": w0row,
            "gfin": np.ascontiguousarray(np.asarray(inputs["norm_final_g"]).reshape(1, D)),
        }
        m.update(consts)
        maps.append(m)
    return maps


_NC_CACHE = {}


def kernel(**inputs):
    T = inputs["x"].shape[1]
    B = inputs["x"].shape[0]
    if T not in _NC_CACHE:
        _NC_CACHE[T] = build_nc(T)
    nc = _NC_CACHE[T]
    batches = [i % B for i in range(8)]
    maps = make_in_maps(inputs, T, batches)
    res = run_bass_kernel_spmd(nc, maps, core_ids=list(range(8)))
    out = np.stack([res.results[b]["out"] for b in range(B)], axis=0)
    return out.astype(np.float32)
```

```python
import numpy as np
import ml_dtypes
from contextlib import ExitStack
import concourse.bass as bass
import concourse.mybir as mybir
from concourse.bass_utils import run_bass_kernel_spmd

F32 = mybir.dt.float32
BF16 = mybir.dt.bfloat16
ALU = mybir.AluOpType
AF = mybir.ActivationFunctionType
AX = mybir.AxisListType

ENGS = ("pe", "act", "dve", "pool", "sp")
N_DMA_SEMS = 24
import os as _os0
SAME_ENG_SYNC = _os0.environ.get("KSAME", "1") == "1"
NEG = -30000.0

D = 1024
NCOL = 29 * 128 + 12
G = 512
PSUM_PREFIX = ("pj", "sc", "oa", "mi", "tp")


class Prog:
    def __init__(self, nc):
        self.nc = nc
        self.lists = {e: [] for e in ENGS}
        self.cnt = {e: 0 for e in ENGS}
        self.seen = {e: {} for e in ENGS}
        self.clock_at = {e: [None] for e in ENGS}
        self.buf = {}
        self.dma_val = [0] * N_DMA_SEMS
        self.dma_clock = [[] for _ in range(N_DMA_SEMS)]
        self.dma_rr = 0
        self.out_toks = []
        self.phase = ""
        self.skip = set()
        self.children = {}

    def _deps(self, eng, reads, writes):
        need = {}

        def add(tok):
            if tok is None:
                return
            src, val = tok
            if src == eng and (eng == "pe" or not SAME_ENG_SYNC):
                return
            if need.get(src, 0) < val:
                need[src] = val

        def related(k):
            ks = [k]
            if "/" in k:
                ks.append(k.split("/")[0])
            else:
                ks.extend(self.children.get(k, ()))
            return ks

        for k0 in reads:
            for k in related(k0):
                st = self.buf.get(k)
                if st:
                    add(st[0])
                    if k[:2] in PSUM_PREFIX:
                        for r in st[1]:
                            if r[0] != eng:
                                add(r)
        for k0 in writes:
            for k in related(k0):
                st = self.buf.get(k)
                if st:
                    add(st[0])
                    for r in st[1]:
                        add(r)
        seen = self.seen[eng]
        waits = []
        for src, val in need.items():
            if seen.get(src, 0) >= val:
                continue
            waits.append((src, val))
            if isinstance(src, str):
                snap = self.clock_at[src][val]
            else:
                snap = self.dma_clock[src[1]][val // 16 - 1]
            for s2, v2 in snap.items():
                if seen.get(s2, 0) < v2:
                    seen[s2] = v2
            seen[src] = val
        return waits

    def _mark(self, tok, reads, writes):
        for k in list(reads) + list(writes):
            if "/" in k:
                self.children.setdefault(k.split("/")[0], set()).add(k)
        for k in reads:
            st = self.buf.setdefault(k, [None, []])
            st[1].append(tok)
            if len(st[1]) > 64:
                st[1] = st[1][-64:]
        for k in writes:
            self.buf[k] = [tok, []]
            if "/" not in k:
                for ch in self.children.get(k, ()):
                    self.buf[ch] = [tok, []]

    def op(self, eng, fn, reads=(), writes=(), self_wait=None):
        if self.phase in self.skip:
            return
        waits = self._deps(eng, reads, writes)
        if self_wait is not None and self.seen[eng].get(eng, 0) < self_wait:
            waits.append((eng, self_wait))
            self.seen[eng][eng] = self_wait
        self.cnt[eng] += 1
        c = self.cnt[eng]
        snap = dict(self.seen[eng])
        snap[eng] = c
        self.clock_at[eng].append(snap)
        self.lists[eng].append(("op", fn, waits, None))
        self._mark((eng, c), reads, writes)

    def dma(self, eng, fn, reads=(), writes=(), is_out=False):
        if self.phase in self.skip:
            return None
        s = self.dma_rr
        self.dma_rr = (self.dma_rr + 1) % N_DMA_SEMS
        waits = self._deps(eng, reads, writes)
        src = ("dma", s)
        prev = self.dma_val[s]
        if prev and self.seen[eng].get(src, 0) < prev:
            waits.append((src, prev))
            self.seen[eng][src] = prev
        val = prev + 16
        self.dma_val[s] = val
        self.dma_clock[s].append(dict(self.seen[eng]))
        self.lists[eng].append(("dma", fn, waits, (s, val)))
        self._mark((src, val), reads, writes)
        if is_out:
            self.out_toks.append((src, val))
        return (src, val)

    def finish(self, eng="sp"):
        waits = []
        for s in range(N_DMA_SEMS):
            if self.dma_val[s]:
                waits.append((("dma", s), self.dma_val[s]))
        self.lists[eng].append(("wait", None, waits, None))

    def emit(self, block, sems, dsems):
        engobj = {"pe": "tensor", "act": "scalar", "dve": "vector", "pool": "gpsimd", "sp": "sync"}

        def semof(src):
            return sems[src] if isinstance(src, str) else dsems[src[1]]

        def make(ename):
            lst = self.lists[ename]

            def body(e):
                for kind, fn, waits, extra in lst:
                    for src, val in waits:
                        e.wait_ge(semof(src), val)
                    if kind == "op":
                        fn(e).then_inc(sems[ename], 1)
                    elif kind == "dma":
                        fn(e).then_inc(dsems[extra[0]], 16)

            return body

        for ename in ENGS:
            if self.lists[ename]:
                getattr(block, engobj[ename])(make(ename))


RW, NSAB, MEMB = 0, 2176, 3084


def _colidx():
    idx = []
    for hp in range(4):
        for base in (0, 512, 1024, 1536):
            idx += list(range(base + 128 * hp, base + 128 * hp + 128))
    idx += list(range(2048, 2176))
    q0, g0 = NSAB, NSAB + 256
    idx += list(range(q0, q0 + 256))
    idx += list(range(g0, g0 + 256))
    kc, vc, ks, vs, kw, vw = (NSAB + 524 + 64 * i for i in range(6))
    idx += list(range(kc, kc + 64)) + list(range(vc, vc + 64))
    idx += list(range(ks, ks + 64)) * 2
    idx += list(range(kw, kw + 64)) * 2
    idx += list(range(vs, vs + 64)) + list(range(vw, vw + 64))
    idx += list(range(MEMB, MEMB + 512))
    idx += list(range(NSAB + 512, NSAB + 524))
    assert len(idx) == NCOL
    return np.array(idx)


def _bf(a):
    return np.ascontiguousarray(a.astype(ml_dtypes.bfloat16))


def host_consts(T):
    c = {}
    c["ident"] = _bf(np.eye(128, dtype=np.float32))
    half = 8
    inv = (np.float32(500000.0) ** (-np.arange(half, dtype=np.float32) / np.float32(half))).astype(np.float32)
    ang = np.arange(T, dtype=np.float32)[None, :] * inv[:, None]
    C = np.ones((64, T), np.float32)
    S = np.zeros((64, T), np.float32)
    C[0:8] = np.cos(ang); C[8:16] = np.cos(ang)
    S[0:8] = -np.sin(ang); S[8:16] = np.sin(ang)
    c["ropeC"] = np.ascontiguousarray(np.concatenate([C, C], 0))
    c["ropeS"] = np.ascontiguousarray(np.concatenate([S, S], 0))
    pm = np.zeros((128, 128), np.float32)
    for hb in (0, 64):
        for d in range(8):
            pm[hb + d + 8, hb + d] = 1.0
            pm[hb + d, hb + d + 8] = 1.0
    c["pm"] = _bf(pm)
    ovl = np.zeros((512, 129), np.float32)
    for i in range(511):
        for sblk in range(128):
            o = min(16 * i + 32, 64 * sblk + 64) - max(16 * i, 64 * sblk)
            if o > 0:
                ovl[i, sblk] = o / 32.0
    ovl[:, 128] = 1.0
    c["ovl"] = _bf(ovl.reshape(4, 128, 129).transpose(1, 0, 2))
    addw = np.zeros((128, 256), np.float32)
    for pp in range(128):
        cur = 1 if pp >= 64 else 0
        for u in range(256):
            sp = u - 126
            valid = sp <= cur
            forced = sp in (cur, cur - 1)
            addw[pp, u] = (0.0 if valid else -1.0) + (1e4 if forced else 0.0)
    c["addw"] = addw
    selpat = np.zeros((64, 32, 128), np.float32)
    for j in range(32):
        selpat[2 * j, j, 0:64] = 1.0
        selpat[2 * j + 1, j, 64:128] = 1.0
    c["selpat"] = _bf(selpat.reshape(64, 4096))
    gselp = np.zeros((12, 6, 128), np.float32)
    for pr_ in range(2):
        for b_ in range(3):
            gselp[3 * (2 * pr_) + b_, pr_ * 3 + b_, 0:64] = 1.0
            gselp[3 * (2 * pr_ + 1) + b_, pr_ * 3 + b_, 64:128] = 1.0
    c["gselp"] = _bf(gselp)
    dsel = np.zeros((33, 128), np.float32)
    dsel[0, 0:64] = 1.0
    dsel[32, 64:128] = 1.0
    c["dsel"] = _bf(dsel)
    pp = np.arange(128)[:, None]
    ff = np.arange(128)[None, :]
    for nm, m in (("maskL2", pp > ff), ("maskU2", pp < ff), ("maskUI2", pp <= ff), ("ident2", pp == ff)):
        m = m.astype(np.float32)
        c[nm] = _bf(np.stack([m, m], 1))
    blk = (pp // 64 == ff // 64).astype(np.float32)
    c["blk1"] = _bf(blk)
    c["blkavg"] = _bf(blk / 64.0)
    return c


PK = {}
_o = 0
for _n, _w in (("gin", 8), ("gmem", 8), ("mu", 17), ("memg", 2), ("nsag", 2), ("gateb", 1), ("posT", 32), ("w0", 4), ("a0", 4), ("kk", 4), ("ka", 4), ("rk", 4), ("lnw", 4), ("lnb", 4)):
    PK[_n] = (_o, _w)
    _o += _w
NPK = _o


def host_params(inp):
    pk = np.zeros((128, NPK), np.float32)

    def put(name, arr):
        o, w = PK[name]
        pk[:, o:o + w] = arr

    put("gin", inp["norm_in_g"][0].reshape(8, 128).T)
    put("gmem", inp["mem_norm_g"][0].reshape(8, 128).T)
    ci = _colidx()
    put("mu", inp["rwkv_mu"][0][ci[:17 * 128]].reshape(17, 128).T)
    put("memg", inp["mem_out_g"][0].reshape(2, 128).T)
    put("nsag", inp["nsa_out_g"][0].reshape(2, 128).T)
    gb = np.zeros((128, 1), np.float32)
    gb[0:12, 0] = inp["nsa_gate_b"][0]
    put("gateb", gb)
    pt = inp["nsa_cmp_pos"][0].T
    put("posT", np.concatenate([pt, pt], 0))
    for nm, key in (("w0", "rwkv_w0"), ("a0", "rwkv_a0"), ("kk", "rwkv_k_k"), ("ka", "rwkv_k_a"), ("rk", "rwkv_r_k"), ("lnw", "rwkv_ln_w"), ("lnb", "rwkv_ln_b")):
        put(nm, inp[key][0].reshape(4, 128).T)
    return pk


def build_nc(T, dbg=None):
    NG = T // G
    NT = T // 128
    nc = bass.Bass("TRN2", target_bir_lowering=False)
    dram = {}

    def din(name, shape, dt=F32):
        dram[name] = nc.dram_tensor(name, list(shape), dt, kind="ExternalInput").ap()
        return dram[name]

    x_d = din("x", [T, D])
    mem_d = din("mem", [256, D])
    wcat_d = din("wcat", [8, 128, NCOL])
    wout_d = din("wout", [8, 128, D])
    wmkv_d = din("wmkv", [8, 128, 512])
    pk_d = din("pk", [128, NPK])
    gfin_d = din("gfin", [1, D])
    ident_d = din("ident", [128, 128], BF16)
    ropeC_d = din("ropeC", [128, T])
    ropeS_d = din("ropeS", [128, T])
    pm_d = din("pm", [128, 128], BF16)
    ovl_d = din("ovl", [128, 4, 129], BF16)
    addw_d = din("addw", [128, 256])
    selpat_d = din("selpat", [64, 4096], BF16)
    gselp_d = din("gselp", [12, 6, 128], BF16)
    dsel_d = din("dsel", [33, 128], BF16)
    w1_d = din("w1", [128, 2048])
    w2_d = din("w2", [128, 192])
    lora_d = din("lora", [128, 512])
    w0row_d = din("w0row", [1, 512])
    maskL2_d = din("maskL2", [128, 2, 128], BF16)
    maskU2_d = din("maskU2", [128, 2, 128], BF16)
    maskUI2_d = din("maskUI2", [128, 2, 128], BF16)
    ident2_d = din("ident2", [128, 2, 128], BF16)
    blk1_d = din("blk1", [128, 128], BF16)
    blkavg_d = din("blkavg", [128, 128], BF16)
    out_d = nc.dram_tensor("out", [T, D], F32, kind="ExternalOutput").ap()
    dbg_d = {}
    if dbg:
        for n, (shape, dt) in dbg.items():
            dbg_d[n] = nc.dram_tensor("dbg_" + n, list(shape), dt, kind="ExternalOutput").ap()

    with ExitStack() as es:
        def sb(name, shape, dt=F32):
            return es.enter_context(nc.sbuf_tensor("sb_" + name, list(shape), dt))

        def ps(name, shape, dt=F32):
            return es.enter_context(nc.psum_tensor("ps_" + name, list(shape), dt))

        wb = sb("wb", [128, 8, NCOL], BF16)
        woutb = sb("woutb", [128, 8, D], BF16)
        wmkvb = sb("wmkvb", [128, 8, 512], BF16)
        pk = sb("pk", [128, NPK])
        gfin = sb("gfin", [128, D])
        ident = sb("ident", [128, 128], BF16)
        xt = [sb("xt%d" % i, [128, D]) for i in range(2)]
        hb0 = sb("hb0", [128, D], BF16)
        hb = [hb0, hb0]
        hT = sb("hT", [128, 8, G], BF16)
        st = sb("st", [128, 16])
        yT = sb("yT", [128, 8, G], BF16)
        memkT = sb("memkT", [128, 2, 256], BF16)
        memvA = sb("memvA", [128, 2, 4, 65], BF16)
        qm = sb("qm", [128, 2, G], BF16)
        gm = sb("gm", [128, 2, G], BF16)
        pT = [sb("pT%d" % i, [128, G], BF16) for i in range(2)]
        oaug = sb("oaug", [128, G], BF16)
        osq = sb("osq", [65, G], BF16)
        cvec = sb("cvec", [65, 128], BF16)
        pm = sb("pm", [128, 128], BF16)
        ovl = sb("ovl", [128, 4, 129], BF16)
        addw = sb("addw", [128, 256])
        qcat = sb("qcat", [128, 4, G], BF16)
        gselp = sb("gselp", [12, 6, 128], BF16)
        dsel = sb("dsel", [33, 128], BF16)
        epsc = sb("epsc", [128, 1])
        e64 = sb("e64", [65, 128], BF16)
        w1b = sb("w1b", [128, 32, 64], BF16)
        w2b = sb("w2b", [64, 192], BF16)
        posTb = sb("posTb", [128, 32], BF16)
        ccst = sb("ccst", [64, 2])
        ropeC = sb("ropeC", [128, G])
        ropeS = sb("ropeS", [128, G])
        qn = sb("qn", [128, 2, G], BF16)
        qr = sb("qr", [128, 2, G], BF16)
        gn = sb("gn", [128, 2, G], BF16)
        kcvc = sb("kcvc", [128, 16 + G], BF16)
        kraw = sb("kraw", [128, G], BF16)
        ksT = sb("ksT", [128, T], BF16)
        kwT = sb("kwT", [128, 1024], BF16)
        vsA = sb("vsA", [128, NT, 65], BF16)
        vwA = sb("vwA", [128, 8, 65], BF16)
        kcmpT = sb("kcmpT", [128, 512], BF16)
        vcmpA = sb("vcmpA", [128, 4, 65], BF16)
        vstg = sb("vstg", [32, 65], BF16)
        h1k = sb("h1k", [64, 32], BF16)
        h1v = sb("h1v", [64, 32], BF16)
        gsig = sb("gsig", [12, G], BF16)
        impS = sb("impS", [128, 4, 128])
        score = sb("score", [128, 128])
        stmp = sb("stmp", [128, 128])
        m8 = sb("m8", [128, 16])
        nbq = sb("nbq", [128, 128], BF16)
        nb = sb("nb", [128, G], BF16)
        oab = sb("oab", [65, G], BF16)
        coef = sb("coef", [128, G])
        scl = coef
        rtmp = coef
        acc = sb("acc", [128, 2, G])
        ltb = [sb("lt%d" % i, [128, G + 2], BF16) for i in range(2)]
        mu1m = sb("mu1m", [128, 17])
        prevcol = sb("prevcol", [128, 17])
        loraup = sb("loraup", [128, 512], BF16)
        w0hi = sb("w0hi", [1, 512], BF16)
        w0lo = sb("w0lo", [1, 512], BF16)
        onesrow = sb("onesrow", [1, 128], BF16)
        ka1m = sb("ka1m", [128, 4])
        maskL2 = sb("maskL2", [128, 2, 128], BF16)
        maskU2 = sb("maskU2", [128, 2, 128], BF16)
        maskUI2 = sb("maskUI2", [128, 2, 128], BF16)
        ident2 = sb("ident2", [128, 2, 128], BF16)
        blk1 = sb("blk1", [128, 128], BF16)
        blkavg = sb("blkavg", [128, 128], BF16)
        sgtok = sb("sgtok", [128, 4, 128], BF16)
        AakT4 = sb("AakT4", [128, 2, G], BF16)
        AkV4 = sb("AkV4", [128, G], BF16)
        STb = sb("STb", [64, 8, 64], BF16)
        STn = sb("STn", [64, 2, 64], BF16)
        ncL = sb("ncL", [128, 4])
        WL = sb("WL", [128, 4])
        WL0 = sb("WL0", [64, 8])
        pj = [ps("pj%d" % i, [128, 512]) for i in range(2)]
        sc = [ps("sc%d" % i, [128, 512]) for i in range(2)]
        oa = [ps("oa%d" % i, [128, 512]) for i in range(2)]
        mi = ps("mi", [128, 512])
        tp = ps("tp", [128, 1024], BF16)

        pTc = wmkvb[:, 0:4, :]
        sems = {e: es.enter_context(nc.semaphore("s_" + e)) for e in ENGS}
        dsems = [es.enter_context(nc.semaphore("d%d" % i)) for i in range(N_DMA_SEMS)]
        block = es.enter_context(nc.Block())
        P = Prog(nc)
        rr = {"ev": 0}

        def ev_eng():
            rr["ev"] ^= 1
            return "act" if rr["ev"] else "dve"

        def copy(eng, out, in_, reads, writes):
            if eng == "act":
                P.op("act", lambda e: e.copy(out=out, in_=in_), reads, writes)
            else:
                P.op(eng, lambda e: e.tensor_copy(out=out, in_=in_), reads, writes)

        pe_last = {}

        def mm(out, lhsT, rhs, start, stop, reads, writes):
            base, rows = lhsT.base_partition(), lhsT.shape[0]
            sw = None
            for k in writes:
                last = pe_last.get(k)
                if last is not None:
                    c0, b0, r0 = last
                    if b0 + r0 <= base or base + rows <= b0:
                        sw = max(sw or 0, c0)
            P.op("pe", lambda e: e.matmul(out, lhsT, rhs, start=start, stop=stop), reads, writes, self_wait=sw)
            for k in writes:
                pe_last[k] = (P.cnt["pe"], base, rows)

        def dbg_out(name, ap, key):
            if dbg and name in dbg_d:
                P.dma("sp", lambda e: e.dma_start(out=dbg_d[name], in_=ap), reads=[key])

        import os as _os
        KSTOP = int(_os.environ.get("KSTOP", "99"))
        KSKIP = _os.environ.get("KSKIP", "")
        P.skip = set(KSKIP.split(",")) if KSKIP else set()

        class _Stop(Exception):
            pass

        def stop_at(n):
            if KSTOP == n:
                raise _Stop()

        try:
            P.dma("sp", lambda e: e.dma_start(out=pk[:], in_=pk_d[:, :]), writes=["pk"])
            P.dma("sp", lambda e: e.dma_start(out=ident[:], in_=ident_d[:, :]), writes=["ident"])
            P.dma("sp", lambda e: e.dma_start(out=gfin[:], in_=gfin_d[0:1, :].broadcast_to([128, D])), writes=["gfin"])

            def pkc(name, j=0, rows=128):
                o, w = PK[name]
                return pk[0:rows, o + j:o + j + 1]

            def load_cast(dst3, src_d, ncols, gname, key):
                i = 0
                for k in range(8):
                    for c0 in range(0, ncols, 1024):
                        cw = min(1024, ncols - c0)
                        stg = xt[i % 2]
                        skey = "xt%d" % (i % 2)
                        P.dma("sp" if i % 2 == 0 else "pool",
                              lambda e, stg=stg, k=k, c0=c0, cw=cw: e.dma_start(out=stg[:, 0:cw], in_=src_d[k, :, c0:c0 + cw]),
                              writes=[skey])
                        eng = ev_eng()
                        o = dst3[:, k, c0:c0 + cw]
                        if gname is None:
                            copy(eng, o, stg[:, 0:cw], [skey], [key])
                        elif eng == "act":
                            P.op("act", lambda e, o=o, stg=stg, cw=cw, k=k: e.activation(out=o, in_=stg[:, 0:cw], func=AF.Copy, scale=pkc(gname, k)),
                                 [skey, "pk"], [key])
                        else:
                            P.op("dve", lambda e, o=o, stg=stg, cw=cw, k=k: e.tensor_scalar(out=o, in0=stg[:, 0:cw], scalar1=pkc(gname, k), scalar2=None, op0=ALU.mult),
                                 [skey, "pk"], [key])
                        i += 1

            stop_at(1)
            load_cast(wmkvb, wmkv_d, 512, "gmem", "wmkvb")
            stop_at(2)
            load_cast(wb, wcat_d, NCOL, "gin", "wb")
            load_cast(woutb, wout_d, D, None, "woutb")

            P.op("pool", lambda e: e.memset(cvec[0:64, :], 1.0 / 64), writes=["cvec"])
            P.op("pool", lambda e: e.memset(cvec[64:65, :], 1e-6), writes=["cvec"])

            def norm_transpose(src_tile, skey, dstT, dkey, col0, slot):
                h = hb[slot]
                hk = "hb0"
                ssq = st[:, slot:slot + 1]
                P.op("act", lambda e: e.activation(out=h[:], in_=src_tile[:], func=AF.Square, accum_out=ssq), [skey], [hk, "st%d" % slot])
                P.op("dve", lambda e: e.tensor_scalar(out=ssq, in0=ssq, scalar1=1.0 / D, scalar2=1e-6, op0=ALU.mult, op1=ALU.add), ["st%d" % slot], ["st%d" % slot])
                P.op("dve", lambda e: e.reciprocal(out=ssq, in_=ssq), ["st%d" % slot], ["st%d" % slot])
                P.op("act", lambda e: e.activation(out=ssq, in_=ssq, func=AF.Sqrt), ["st%d" % slot], ["st%d" % slot])
                P.op("dve", lambda e: e.tensor_scalar(out=h[:], in0=src_tile[:], scalar1=ssq, scalar2=None, op0=ALU.mult), [skey, "st%d" % slot], [hk])
                for k in range(8):
                    P.op("pe", lambda e, k=k: e.transpose(out=tp[:, k * 128:(k + 1) * 128], in_=h[:, k * 128:(k + 1) * 128], identity=ident[:]), [hk, "ident"], ["tp"])
                copy(ev_eng(), dstT[:, :, col0:col0 + 128], tp[:].rearrange("p (k t) -> p k t", k=8), ["tp"], [dkey])


            for nm, dst, src in (("pm", pm, pm_d), ("ovl", ovl, ovl_d), ("addw", addw, addw_d),
                                 ):
                P.dma("sp", lambda e, dst=dst, src=src: e.dma_start(out=dst[:], in_=src), writes=[nm])
            P.dma("sp", lambda e: e.dma_start(out=gselp[:], in_=gselp_d), writes=["gselp"])
            P.dma("sp", lambda e: e.dma_start(out=dsel[:], in_=dsel_d), writes=["dsel"])
            P.op("pool", lambda e: e.memset(epsc[:], 1e-6), writes=["epsc"])
            P.op("pool", lambda e: e.memset(oab[:], 0.0), writes=["oab"])
            P.op("pool", lambda e: e.memset(e64[:], 0.0), writes=["e64"])
            P.op("pool", lambda e: e.memset(e64[64:65, :], 1.0), writes=["e64"])
            P.op("pool", lambda e: e.memset(kcmpT[:], 0.0), writes=["kcmpT"])
            P.op("pool", lambda e: e.memset(vcmpA[:], 0.0), writes=["vcmpA"])
            P.op("pool", lambda e: e.memset(vsA[:], 1.0), writes=["vsA"])
            P.op("pool", lambda e: e.memset(vwA[:], 1.0), writes=["vwA"])
            P.op("pool", lambda e: e.memset(kwT[:], 0.0), writes=["kwT"])
            for c0 in range(0, T, 4096):
                cw = min(4096, T - c0)
                P.dma("sp", lambda e, c0=c0, cw=cw: e.dma_start(out=ksT[64:128, c0:c0 + cw], in_=selpat_d[:, 0:cw]), writes=["ksT"])
            P.op("pool", lambda e: e.memset(vstg[:], 1.0), writes=["vstg"])
            P.op("pool", lambda e: e.memset(kcvc[:], 0.0), writes=["kcvc"])
            for hlf in range(2):
                P.dma("sp", lambda e, hlf=hlf: e.dma_start(out=xt[hlf][:], in_=w1_d[:, hlf * 1024:(hlf + 1) * 1024]), writes=["xt%d" % hlf])
                copy(ev_eng(), w1b[:, hlf * 16:(hlf + 1) * 16, :], xt[hlf][:].rearrange("p (l e) -> p l e", e=64), ["xt%d" % hlf], ["w1b"])
            P.dma("sp", lambda e: e.dma_start(out=xt[0][:, 0:192], in_=w2_d[:, :]), writes=["xt0"])
            copy("dve", w2b[:], xt[0][0:64, 0:192], ["xt0"], ["w2b"])
            o_, w_ = PK["posT"]
            copy("dve", posTb[:], pk[:, o_:o_ + 32], ["pk"], ["posTb"])
            for kv in range(2):
                b0 = kv * 64
                for l in range(32):
                    mm(mi[0:64, kv:kv + 1], w1b[b0:b0 + 64, l, :], posTb[b0:b0 + 64, l:l + 1], l == 0, l == 31, ["w1b", "posTb"], ["mi"])
                copy("dve", ccst[:, kv:kv + 1], mi[0:64, kv:kv + 1], ["mi"], ["ccst"])

            for nm, dst, src in (("maskL2", maskL2, maskL2_d), ("maskU2", maskU2, maskU2_d), ("maskUI2", maskUI2, maskUI2_d),
                                 ("ident2", ident2, ident2_d), ("blk1", blk1, blk1_d), ("blkavg", blkavg, blkavg_d)):
                P.dma("sp", lambda e, dst=dst, src=src: e.dma_start(out=dst[:], in_=src), writes=[nm])
            P.dma("sp", lambda e: e.dma_start(out=xt[1][:, 0:512], in_=lora_d[:, :]), writes=["xt1"])
            copy("dve", loraup[:], xt[1][:, 0:512], ["xt1"], ["loraup"])
            P.dma("sp", lambda e: e.dma_start(out=coef[0:1, 0:512], in_=w0row_d[:, :]), writes=["coef"])
            copy("dve", w0hi[:], coef[0:1, 0:512], ["coef"], ["w0hi"])
            P.op("dve", lambda e: e.tensor_tensor(out=coef[0:1, 0:512], in0=coef[0:1, 0:512], in1=w0hi[:], op=ALU.subtract), ["coef", "w0hi"], ["coef"])
            copy("dve", w0lo[:], coef[0:1, 0:512], ["coef"], ["w0lo"])
            o_, w_ = PK["mu"]
            P.op("dve", lambda e, o_=o_: e.tensor_scalar(out=mu1m[:], in0=pk[:, o_:o_ + 17], scalar1=-1.0, scalar2=1.0, op0=ALU.mult, op1=ALU.add), ["pk"], ["mu1m"])
            P.op("pool", lambda e: e.memset(onesrow[:], 1.0), writes=["onesrow"])
            P.op("pool", lambda e: e.memset(prevcol[:], 0.0), writes=["prevcol"])
            P.op("pool", lambda e: e.memset(STb[:], 0.0), writes=["STb"])
            o_, w_ = PK["ka"]
            P.op("dve", lambda e, o_=o_: e.tensor_scalar(out=ka1m[:], in0=pk[:, o_:o_ + 4], scalar1=-1.0, scalar2=1.0, op0=ALU.mult, op1=ALU.add), ["pk"], ["ka1m"])
            tokall = wmkvb[:, 4:8, :].rearrange("p c (w f) -> p c w f", w=4)
            stop_at(3)
            for mt in range(2):
                P.dma("sp", lambda e, mt=mt: e.dma_start(out=xt[mt][:], in_=mem_d[mt * 128:(mt + 1) * 128, :]), writes=["xt%d" % mt])
                norm_transpose(xt[mt], "xt%d" % mt, hT, "hT", mt * 128, mt)
            stop_at(4)
            for pr in range(2):
                for k in range(8):
                    mm(pj[0][:, 0:256], wmkvb[:, k, pr * 128:(pr + 1) * 128], hT[:, k, 0:256], k == 0, k == 7, ["wmkvb", "hT"], ["pj0"])
                copy(ev_eng(), memkT[:, pr, :], pj[0][:, 0:256], ["pj0"], ["memkT"])
            P.op("pool", lambda e: e.memset(memvA[:], 1.0), writes=["memvA"])
            for mt in range(2):
                for k in range(8):
                    mm(pj[1][:, 0:256], hT[:, k, mt * 128:(mt + 1) * 128], wmkvb[:, k, 256:512], k == 0, k == 7, ["wmkvb", "hT"], ["pj1"])
                copy(ev_eng(), memvA[:, mt, :, 0:64], pj[1][:, 0:256].rearrange("p (h d) -> p h d", h=4), ["pj1"], ["memvA"])

            def finalize_head(o_ps, okey, use_den, hb_, gcol_ap, gate_ap, gate_key, dst_ap, dst_key, from_sbuf=False):
                sl = slice(hb_, hb_ + 64)
                if from_sbuf:
                    copy("dve", oaug[sl, :], o_ps, [okey], ["oaug"])
                    P.op("act", lambda e: e.activation(out=osq[0:64, :], in_=o_ps, func=AF.Square), [okey], ["osq"])
                    P.op("pool", lambda e: e.memset(osq[64:65, :], 1.0), [], ["osq"])
                elif use_den:
                    copy("dve", oaug[sl, :], o_ps[0:64, :], [okey], ["oaug"])
                    P.op("act", lambda e: e.activation(out=osq[:], in_=o_ps[0:65, :], func=AF.Square), [okey], ["osq"])
                else:
                    copy("dve", oaug[sl, :], o_ps[0:64, :], [okey], ["oaug"])
                    P.op("act", lambda e: e.activation(out=osq[0:64, :], in_=o_ps[0:64, :], func=AF.Square), [okey], ["osq"])
                    P.op("pool", lambda e: e.memset(osq[64:65, :], 1.0), [], ["osq"])
                mm(mi[:], cvec[:], osq[:], True, True, ["cvec", "osq"], ["mi"])
                P.op("act", lambda e: e.activation(out=scl[sl, :], in_=mi[sl, :], func=AF.Ln), ["mi"], ["coef"])
                P.op("act", lambda e: e.activation(out=scl[sl, :], in_=scl[sl, :], func=AF.Exp, scale=-0.5), ["coef"], ["coef"])
                P.op("dve", lambda e: e.scalar_tensor_tensor(out=scl[sl, :], in0=scl[sl, :], scalar=gcol_ap, in1=oaug[sl, :], op0=ALU.mult, op1=ALU.mult), ["coef", "oaug", "pk"], ["coef"])
                P.op("dve", lambda e: e.tensor_tensor(out=dst_ap, in0=scl[sl, :], in1=gate_ap, op=ALU.mult), ["coef", gate_key], [dst_key])

            P.op("pool", lambda e: e.memset(wmkvb[:, 4:8, :], 0.0), ["wmkvb"], ["wmkvb", "tokall"])
            stop_at(5)
            for g in range(NG):
                t0 = g * G
                for i in range(4):
                    P.dma("sp", lambda e, i=i, t0=t0: e.dma_start(out=xt[i % 2][:], in_=x_d[t0 + i * 128:t0 + (i + 1) * 128, :]), writes=["xt%d" % (i % 2)])
                    norm_transpose(xt[i % 2], "xt%d" % (i % 2), hT, "hT", i * 128, i % 2)

                pj_rot = {"i": 0}

                def project(chunk, width=128):
                    if P.phase == "rwkv":
                        banks = ((pj[0], "pj0"), (pj[1], "pj1"), (sc[0], "sc0"), (sc[1], "sc1"))
                    elif P.phase in ("nsa", "mem"):
                        banks = ((pj[0], "pj0"), (pj[1], "pj1"), (sc[0], "sc0"), (sc[1], "sc1"))
                    else:
                        banks = ((pj[0], "pj0"), (pj[1], "pj1"))
                    pb, pkey = banks[pj_rot["i"] % len(banks)]
                    pj_rot["i"] += 1
                    for k in range(8):
                        mm(pb[0:width, :], wb[:, k, chunk * 128:chunk * 128 + width], hT[:, k, :], k == 0, k == 7, ["wb", "hT"], [pkey])
                    return pb, pkey


                P.phase = "rwkv"
                C0 = float(np.exp(-0.5))

                def tt(eng, out, in0, in1, op, reads, writes):
                    P.op(eng, lambda e: e.tensor_tensor(out=out, in0=in0, in1=in1, op=op), reads, writes)

                def ts(eng, out, in0, s1, s2, op0, op1, reads, writes):
                    if s2 is None:
                        P.op(eng, lambda e: e.tensor_scalar(out=out, in0=in0, scalar1=s1, scalar2=None, op0=op0), reads, writes)
                    else:
                        P.op(eng, lambda e: e.tensor_scalar(out=out, in0=in0, scalar1=s1, scalar2=s2, op0=op0, op1=op1), reads, writes)

                def stt(eng, out, in0, scalar, in1, op0, op1, reads, writes):
                    P.op(eng, lambda e: e.scalar_tensor_tensor(out=out, in0=in0, scalar=scalar, in1=in1, op0=op0, op1=op1), reads, writes)

                def actf(out, in_, func, reads, writes, bias=None, scale=None):
                    kw = {}
                    if bias is not None:
                        kw["bias"] = bias
                    if scale is not None:
                        kw["scale"] = scale
                    P.op("act", lambda e: e.activation(out=out, in_=in_, func=func, **kw), reads, writes)

                def pcol(name, j):
                    o, w = PK[name]
                    return pk[:, o + j:o + j + 1]

                def lerp_chunk(cidx, dst_ap, dkey):
                    pb, pkey = project(cidx)
                    lt, lk = ltb[cidx % 2], "lt%d" % (cidx % 2)
                    actf(lt[:, 1:G + 1], pb[:], AF.Copy, [pkey, "pk"], [lk], scale=pcol("mu", cidx))
                    copy("dve", lt[:, 0:1], prevcol[:, cidx:cidx + 1], ["prevcol"], [lk])
                    copy("dve", prevcol[:, cidx:cidx + 1], lt[:, G:G + 1], [lk], ["prevcol"])
                    stt("dve", dst_ap, pb[:], mu1m[:, cidx:cidx + 1], lt[:, 0:G], ALU.mult, ALU.add, [pkey, "mu1m", lk], [dkey])

                rT, kmT = qn[:, 0, :], qn[:, 1, :]
                vT, gT = qr[:, 0, :], qr[:, 1, :]
                bT, At = gn[:, 0, :], gn[:, 1, :]
                Rt, Bt = qm[:, 0, :], qm[:, 1, :]
                Kt, Bb = gm[:, 0, :], gm[:, 1, :]
                Kb, kkn = pT[0][:], pT[1][:]
                aT, lin = kraw[:], nb[:]
                kf, yraw = acc[:, 0, :], acc[:, 1, :]
                Etmp, Esq = hb0[:, 0:G], hb0[:, G:2 * G]

                lerp_chunk(16, kf, "acc")
                actf(lin[0:64, :], kf[0:64, :], AF.Tanh, ["acc"], ["nb"])
                copy("dve", lin[64:128, :], kf[64:128, :], ["acc"], ["nb"])

                stop_at(40)
                for hp in range(4):
                    hc = slice(hp * 128, (hp + 1) * 128)
                    mm(oa[0][:], loraup[64:128, hc], lin[64:128, :], True, True, ["nb", "loraup"], ["oa0"])
                    for c in range(4):
                        reg = mi[:, c * 128:(c + 1) * 128]
                        mm(reg, lin[0:64, c * 128:(c + 1) * 128], loraup[0:64, hc], True, False, ["nb", "loraup"], ["mi"])
                        mm(reg, onesrow[:], w0hi[0:1, hc], False, False, ["onesrow", "w0hi"], ["mi"])
                        mm(reg, onesrow[:], w0lo[0:1, hc], False, True, ["onesrow", "w0lo"], ["mi"])
                    actf(sgtok[:].rearrange("p c f -> p (c f)"), mi[:], AF.Sigmoid, ["mi"], ["sgtok"])
                    actf(aT, oa[0][:], AF.Sigmoid, ["oa0", "pk"], ["kraw"], bias=pcol("a0", hp))
                    stop_at(41)
                    lerp_chunk(4 * hp + 0, rT, "qn")
                    lerp_chunk(4 * hp + 1, kf, "acc")
                    lerp_chunk(4 * hp + 2, vT, "qr")
                    lerp_chunk(4 * hp + 3, gT, "qr")
                    actf(Etmp, gT, AF.Sigmoid, ["qr"], ["hb0"])
                    tt("dve", gT, gT, Etmp, ALU.mult, ["qr", "hb0"], ["qr"])
                    for c in range(4):
                        mm(sc[0][:, c * 128:(c + 1) * 128], sgtok[:, c, :], maskUI2[:, 0, :], True, True, ["sgtok", "maskUI2"], ["sc0"])
                    for c in range(4):
                        mm(sc[1][:, c * 128:(c + 1) * 128], sgtok[:, c, :], maskU2[:, 0, :], True, True, ["sgtok", "maskU2"], ["sc1"])
                    stop_at(42)
                    ts("dve", coef[:], kf, pcol("kk", hp), None, ALU.mult, None, ["acc", "pk"], ["coef"])
                    actf(Esq, coef[:], AF.Square, ["coef"], ["hb0"])
                    mm(oa[1][:], blk1[:], Esq, True, True, ["blk1", "hb0"], ["oa1"])
                    ts("dve", yraw, oa[1][:], 1e-24, None, ALU.max, None, ["oa1"], ["acc/1"])
                    actf(yraw, yraw, AF.Ln, ["acc/1"], ["acc/1"])
                    actf(yraw, yraw, AF.Exp, ["acc/1"], ["acc/1"], scale=-0.5)
                    tt("dve", kkn, coef[:], yraw, ALU.mult, ["coef", "acc/1"], ["pT1"])
                    ts("dve", coef[:], aT, pcol("ka", hp), ka1m[:, hp:hp + 1], ALU.mult, ALU.add, ["kraw", "pk", "ka1m"], ["coef"])
                    tt("dve", kmT, kf, coef[:], ALU.mult, ["acc", "coef"], ["qn"])
                    tt("pool", bT, kkn, aT, ALU.mult, ["pT1", "kraw"], ["gn"])
                    if g == 0 and hp == 0:
                        dbg_out("rT", rT, "qn"); dbg_out("kmT", kmT, "qn"); dbg_out("vT", vT, "qr"); dbg_out("aT", aT, "kraw"); dbg_out("kkn", kkn, "pT1"); dbg_out("bT", bT, "gn")
                        copy("dve", coef[:], sc[0][:], ["sc0"], ["coef"]); dbg_out("cumI", coef[:], "coef")
                        copy("dve", coef[:], sc[1][:], ["sc1"], ["coef"]); dbg_out("cumE", coef[:], "coef")
                    stop_at(43)
                    actf(Etmp, sc[1][:], AF.Exp, ["sc1"], ["hb0"], scale=-C0)
                    stt("dve", At, Etmp, -1.0, kkn, ALU.mult, ALU.mult, ["hb0", "pT1"], ["gn"])
                    actf(Etmp, sc[0][:], AF.Exp, ["sc0"], ["hb0"], scale=-C0)
                    tt("dve", Rt, rT, Etmp, ALU.mult, ["qn", "hb0"], ["qm"])
                    actf(Esq, sc[0][:], AF.Exp, ["sc0"], ["hb0"], scale=C0)
                    tt("dve", Bt, bT, Esq, ALU.mult, ["gn", "hb0"], ["qm"])
                    tt("pool", Kt, kmT, Esq, ALU.mult, ["qn", "hb0"], ["gm"])
                    ts("dve", ncL[:], sc[0][:, 127:G:128], -C0, None, ALU.mult, None, ["sc0"], ["ncL"])
                    actf(WL[:], ncL[:], AF.Exp, ["ncL"], ["WL"])
                    copy("dve", WL0[:, 0:4], WL[0:64, :], ["WL"], ["WL0"])
                    copy("dve", WL0[:, 4:8], WL[64:128, :], ["WL"], ["WL0"])
                    for c in range(4):
                        actf(Etmp[:, c * 128:(c + 1) * 128], sc[0][:, c * 128:(c + 1) * 128], AF.Exp, ["sc0", "ncL"], ["hb0"], bias=ncL[:, c:c + 1], scale=C0)
                    tt("dve", Bb, bT, Etmp, ALU.mult, ["gn", "hb0"], ["gm"])
                    tt("pool", Kb, kmT, Etmp, ALU.mult, ["qn", "hb0"], ["pT0"])
                    stop_at(44)
                    for c in range(4):
                        cs = slice(c * 128, (c + 1) * 128)
                        half = (c % 2) * 512
                        for wi, (src, skey) in enumerate(((At, "gn"), (vT, "qr"), (Bb, "gm"), (Kb, "pT0"))):
                            P.op("pe", lambda e, src=src, cs=cs, half=half, wi=wi: e.transpose(out=tp[:, half + wi * 128:half + (wi + 1) * 128], in_=src[:, cs], identity=ident[:]), [skey, "ident"], ["tp"])
                        copy(ev_eng(), tokall[:, c, :, :], tp[:, half:half + 512].rearrange("p (w f) -> p w f", w=4), ["tp"], ["tokall"])

                    P.phase = "rwkv_chain"
                    stop_at(45)

                    def hs(e):
                        return slice(64 * e, 64 * e + 64)

                    def v4(ap):
                        return ap.rearrange("p (c f) -> p c f", c=4)

                    def bc4(m2):
                        return m2[:, 0:1, :].broadcast_to([128, 4, 128])

                    PmH, PmK = [hb0[:, 0:G], hb0[:, G:2 * G]], ["hb0/0", "hb0/1"]
                    PTH, PTK = [pT[0][:], pT[1][:]], ["pT0", "pT1"]
                    XTH, XTK = [gn[:, 0, :], kraw[:]], ["gn/0", "kraw"]
                    bkP = [(oa[0], "oa0"), (oa[1], "oa1")]
                    bkT = [(pj[0], "pj0"), (pj[1], "pj1")]
                    bkX = [(mi, "mi"), (sc[0], "sc0")]

                    def rg(bank, c):
                        return bank[:, c * 128:(c + 1) * 128]

                    def csl(c):
                        return slice(c * 128, (c + 1) * 128)

                    for c in range(4):
                        for e in range(2):
                            mm(rg(bkP[e][0], c), At[hs(e), csl(c)], Bt[hs(e), csl(c)], True, True, ["gn/1", "qm"], [bkP[e][1]])
                    for c in range(4):
                        for e in range(2):
                            mm(rg(bkT[e][0], c), Bt[hs(e), csl(c)], At[hs(e), csl(c)], True, True, ["gn/1", "qm"], [bkT[e][1]])
                    for e in range(2):
                        tt("dve", v4(PmH[e]), v4(bkP[e][0][:]), bc4(maskL2), ALU.mult, [bkP[e][1], "maskL2"], [PmK[e]])
                        tt("dve", v4(PTH[e]), v4(bkT[e][0][:]), bc4(maskU2), ALU.mult, [bkT[e][1], "maskU2"], [PTK[e]])
                        tt("pool", v4(XTH[e]), v4(PTH[e]), bc4(ident2), ALU.add, [PTK[e], "ident2"], [XTK[e]])
                    for lvl in range(1, 7):
                        for e in range(2):
                            for c in range(4):
                                mm(rg(bkP[e][0], c), v4(PTH[e])[:, c, :], v4(PmH[e])[:, c, :], True, True, [PTK[e], PmK[e]], [bkP[e][1]])
                            if lvl < 6:
                                for c in range(4):
                                    mm(rg(bkT[e][0], c), v4(PmH[e])[:, c, :], v4(PTH[e])[:, c, :], True, True, [PTK[e], PmK[e]], [bkT[e][1]])
                        for e in range(2):
                            copy("act", PmH[e], bkP[e][0][:], [bkP[e][1]], [PmK[e]])
                            if lvl < 6:
                                copy("dve" if e == 0 else "act", PTH[e], bkT[e][0][:], [bkT[e][1]], [PTK[e]])
                        for e in range(2):
                            for c in range(4):
                                mm(rg(bkX[e][0], c), v4(PmH[e])[:, c, :], v4(XTH[e])[:, c, :], True, True, [PmK[e], XTK[e]], [bkX[e][1]])
                        for e in range(2):
                            tt("dve", XTH[e], XTH[e], bkX[e][0][:], ALU.add, [XTK[e], bkX[e][1]], [XTK[e]])

                    stop_at(46)
                    def v8(ap):
                        return ap.rearrange("p (c e f) -> p c e f", c=4, e=2)

                    ArbH, ArbK = [hb0[:, 0:G], hb0[:, G:2 * G]], ["hb0/0", "hb0/1"]
                    ArkH, ArkK = [pT[0][:], pT[1][:]], ["pT0", "pT1"]
                    AakH, AakK = [AakT4[:, 0, :], AakT4[:, 1, :]], ["AakT4/0", "AakT4/1"]
                    Ahat4, AhK = v8(gm[:, 1, :]), "gm/1"
                    Uhat4, UhK = v8(sgtok[:].rearrange("p c f -> p (c f)")), "sgtok"
                    AkV4v = v8(AkV4[:])
                    RhH, RhK = [oab[0:64, :], osq[0:64, :]], ["oab", "osq"]
                    Msb4 = v8(qcat[0:64, 0, :])

                    for c in range(4):
                        for e in range(2):
                            mm(rg(bkP[e][0], c), Kt[hs(e), csl(c)], At[hs(e), csl(c)], True, True, ["gm/0", "gn/1"], [bkP[e][1]])
                    for c in range(4):
                        for e in range(2):
                            mm(rg(bkT[e][0], c), Bt[hs(e), csl(c)], Rt[hs(e), csl(c)], True, True, ["qm"], [bkT[e][1]])
                    for c in range(4):
                        for e in range(2):
                            mm(rg(bkX[e][0], c), Kt[hs(e), csl(c)], Rt[hs(e), csl(c)], True, True, ["gm/0", "qm"], [bkX[e][1]])
                    for e in range(2):
                        tt("dve", v4(AakH[e]), v4(bkP[e][0][:]), bc4(maskU2), ALU.mult, [bkP[e][1], "maskU2"], [AakK[e]])
                        tt("dve", v4(ArbH[e]), v4(bkT[e][0][:]), bc4(maskUI2), ALU.mult, [bkT[e][1], "maskUI2"], [ArbK[e]])
                        tt("dve", v4(ArkH[e]), v4(bkX[e][0][:]), bc4(maskUI2), ALU.mult, [bkX[e][1], "maskUI2"], [ArkK[e]])
                    for c in range(4):
                        for e in range(2):
                            o = (c * 2 + e) * 64
                            mm(oa[0][:, o:o + 64], v4(AakH[e])[:, c, :], tokall[:, c, 1, hs(e)], True, True, [AakK[e], "tokall"], ["oa0"])
                    copy("act", AkV4[:], oa[0][:], ["oa0"], ["AkV4"])
                    for c in range(4):
                        for e in range(2):
                            o = (c * 2 + e) * 64
                            mm(oa[1][:, o:o + 64], v4(XTH[e])[:, c, :], tokall[:, c, 0, hs(e)], True, True, [XTK[e], "tokall"], ["oa1"])
                    copy("dve", gm[:, 1, :], oa[1][:], ["oa1"], [AhK])
                    for c in range(4):
                        for e in range(2):
                            o = (c * 2 + e) * 64
                            mm(pj[0][:, o:o + 64], v4(XTH[e])[:, c, :], AkV4v[:, c, e, :], True, True, [XTK[e], "AkV4"], ["pj0"])
                    copy("act", sgtok[:].rearrange("p c f -> p (c f)"), pj[0][:], ["pj0"], [UhK])
                    for e in range(2):
                        for c in range(4):
                            reg = rg(bkX[e][0], c)[0:64, :]
                            mm(reg, Ahat4[:, c, e, :], v4(ArbH[e])[:, c, :], True, False, [AhK, ArbK[e]], [bkX[e][1]])
                            mm(reg, ident[hs(e), hs(e)], Rt[hs(e), csl(c)], False, True, ["ident", "qm"], [bkX[e][1]])
                        copy("dve" if e == 0 else "act", RhH[e], bkX[e][0][0:64, :], [bkX[e][1]], [RhK[e]])
                    for c in range(4):
                        for e in range(2):
                            o = (c * 2 + e) * 64
                            mm(pj[1][0:64, o:o + 64], Ahat4[:, c, e, :], tokall[:, c, 2, hs(e)], True, True, [AhK, "tokall"], ["pj1"])
                    copy("act", qcat[0:64, 0, :], pj[1][0:64, :], ["pj1"], ["qcat"])
                    stop_at(47)
                    for c in range(4):
                        cs = slice(c * 128, (c + 1) * 128)

                        def s_src(e):
                            return (STb[:, 2 * hp + e, :], "STb") if c % 2 == 0 else (STn[:, e, :], "STn")

                        def s_dst(e):
                            return (STn[:, e, :], "STn") if c % 2 == 0 else (STb[:, 2 * hp + e, :], "STb")

                        ob_, obk = oa[c % 2], "oa%d" % (c % 2)
                        for e in range(2):
                            reg = ob_[0:64, e * 64:(e + 1) * 64]
                            mm(reg, tokall[:, c, 2, hs(e)], Uhat4[:, c, e, :], True, False, ["tokall", UhK], [obk])
                            mm(reg, tokall[:, c, 3, hs(e)], tokall[:, c, 1, hs(e)], False, False, ["tokall"], [obk])
                            mm(reg, Msb4[:, c, e, :], s_src(e)[0], False, True, ["qcat", s_src(e)[1]], [obk])
                        for e in range(2):
                            stt("dve", s_dst(e)[0], s_src(e)[0], WL0[:, 4 * e + c:4 * e + c + 1], ob_[0:64, e * 64:(e + 1) * 64], ALU.mult, ALU.add, [s_src(e)[1], "WL0", obk], [s_dst(e)[1]])
                        for e in range(2):
                            reg = sc[1][0:64, e * 128:(e + 1) * 128]
                            mm(reg, Uhat4[:, c, e, :], v4(ArbH[e])[:, c, :], True, False, [UhK, ArbK[e]], ["sc1"])
                            mm(reg, tokall[:, c, 1, hs(e)], v4(ArkH[e])[:, c, :], False, False, ["tokall", ArkK[e]], ["sc1"])
                            mm(reg, s_src(e)[0], v4(RhH[e])[:, c, :], False, True, [s_src(e)[1], RhK[e]], ["sc1"])
                        copy("act", yraw[0:64, cs], sc[1][0:64, 0:128], ["sc1"], ["acc"])
                        copy("act", yraw[64:128, cs], sc[1][0:64, 128:256], ["sc1"], ["acc"])

                    if g == 0 and hp == 0:
                        dbg_out("yraw", yraw, "acc")
                    P.phase = "rwkv"
                    stop_at(49)
                    copy("act", Etmp, yraw, ["acc"], ["hb0"])
                    actf(Esq, yraw, AF.Square, ["acc"], ["hb0"])
                    mm(sc[0][:], blkavg[:], Etmp, True, True, ["blkavg", "hb0"], ["sc0"])
                    mm(sc[1][:], blkavg[:], Esq, True, True, ["blkavg", "hb0"], ["sc1"])
                    actf(coef[:], sc[0][:], AF.Square, ["sc0"], ["coef"])
                    tt("dve", coef[:], sc[1][:], coef[:], ALU.subtract, ["sc1", "coef"], ["coef"])
                    ts("dve", coef[:], coef[:], 64e-5, None, ALU.add, None, ["coef"], ["coef"])
                    actf(coef[:], coef[:], AF.Ln, ["coef"], ["coef"])
                    actf(coef[:], coef[:], AF.Exp, ["coef"], ["coef"], scale=-0.5)
                    tt("dve", yraw, yraw, sc[0][:], ALU.subtract, ["acc", "sc0"], ["acc"])
                    tt("dve", yraw, yraw, coef[:], ALU.mult, ["acc", "coef"], ["acc"])
                    ts("dve", yraw, yraw, pcol("lnw", hp), pcol("lnb", hp), ALU.mult, ALU.add, ["acc", "pk"], ["acc"])
                    tt("pool", Etmp, rT, kmT, ALU.mult, ["qn"], ["hb0"])
                    ts("dve", Esq, Etmp, pcol("rk", hp), None, ALU.mult, None, ["hb0", "pk"], ["hb0"])
                    mm(sc[0][:], blk1[:], Esq, True, True, ["blk1", "hb0"], ["sc0"])
                    tt("dve", coef[:], sc[0][:], vT, ALU.mult, ["sc0", "qr"], ["coef"])
                    tt("dve", yraw, yraw, coef[:], ALU.add, ["acc", "coef"], ["acc"])
                    tt("dve", yT[:, hp, :], yraw, gT, ALU.mult, ["acc", "qr"], ["yT"])

                P.phase = "mem"
                stop_at(6)
                for pr in range(2):
                    pb, pkey = project(25 + pr)
                    P.op("act", lambda e, pb=pb, pr=pr: e.activation(out=qm[:, pr, :], in_=pb[:], func=AF.Copy, scale=0.125), [pkey], ["qm"])
                    pb, pkey = project(27 + pr)
                    P.op("act", lambda e, pb=pb, pr=pr: e.activation(out=gm[:, pr, :], in_=pb[:], func=AF.Silu), [pkey], ["gm"])
                stop_at(61)
                for h in range(4):
                    pr, hb_ = h // 2, (h % 2) * 64
                    ob = oa[h % 2]
                    okey = "oa%d" % (h % 2)
                    for mc in range(2):
                        sb_ = sc[mc]
                        mm(sb_[:], memkT[hb_:hb_ + 64, pr, mc * 128:(mc + 1) * 128], qm[hb_:hb_ + 64, pr, :], True, True, ["memkT", "qm"], ["sc%d" % mc])
                        P.op("act", lambda e, sb_=sb_, mc=mc: e.activation(out=pT[mc][:], in_=sb_[:], func=AF.Exp), ["sc%d" % mc], ["pT%d" % mc])
                    stop_at(62 if h == 0 else 66)
                    for mc in range(2):
                        mm(ob[0:65, :], memvA[:, mc, h, :], pT[mc][:], mc == 0, mc == 1, ["memvA", "pT%d" % mc], [okey])
                    stop_at(63 if h == 0 else 67)
                    o, w = PK["memg"]
                    finalize_head(ob, okey, True, hb_, pk[hb_:hb_ + 64, o + pr:o + pr + 1], gm[hb_:hb_ + 64, pr, :], "gm", yT[hb_:hb_ + 64, 6 + pr, :], "yT")


                P.phase = "nsa"
                P.dma("sp", lambda e, t0=t0: e.dma_start(out=ropeC[:], in_=ropeC_d[:, t0:t0 + G]), writes=["ropeC"])
                P.dma("sp", lambda e, t0=t0: e.dma_start(out=ropeS[:], in_=ropeS_d[:, t0:t0 + G]), writes=["ropeS"])

                def rope(src_t, skey, dst_ap, dkey, nr=128):
                    mm(mi[:], pm[:], src_t, True, True, ["pm", skey], ["mi"])
                    P.op("dve", lambda e: e.tensor_tensor(out=rtmp[0:nr, :], in0=mi[0:nr, :], in1=ropeS[0:nr, :], op=ALU.mult), ["mi", "ropeS"], ["coef"])
                    P.op("pool", lambda e: e.tensor_tensor(out=dst_ap, in0=src_t[0:nr, :], in1=ropeC[0:nr, :], op=ALU.mult), [skey, "ropeC"], [dkey])
                    P.op("dve", lambda e: e.tensor_tensor(out=dst_ap, in0=dst_ap, in1=rtmp[0:nr, :], op=ALU.add), [dkey, "coef"], [dkey])

                for pr in range(2):
                    pb, pkey = project(17 + pr)
                    P.op("act", lambda e, pb=pb, pr=pr: e.activation(out=qn[:, pr, :], in_=pb[:], func=AF.Copy, scale=0.125), [pkey], ["qn"])
                    rope(qn[:, pr, :], "qn", qr[:, pr, :], "qr")
                    pb, pkey = project(19 + pr)
                    P.op("act", lambda e, pb=pb, pr=pr: e.activation(out=gn[:, pr, :], in_=pb[:], func=AF.Silu), [pkey], ["gn"])
                copy("dve", kcvc[:, 0:16], kcvc[:, G:G + 16], ["kcvc"], ["kcvc"])
                pb, pkey = project(21)
                copy("act", kcvc[:, 16:16 + G], pb[:], [pkey], ["kcvc"])
                pb, pkey = project(22)
                copy("act", kraw[:], pb[:], [pkey], ["kraw"])
                rope(kraw[:], "kraw", ksT[0:64, t0:t0 + G], "ksT", 64)
                pb, pkey = project(23)
                copy("act", kraw[:], pb[:], [pkey], ["kraw"])
                wo = (g % 2) * G
                rope(kraw[:], "kraw", kwT[0:64, wo:wo + G], "kwT", 64)
                for i in range(4):
                    pbv = pj[i % 2]
                    for k in range(8):
                        mm(pbv[:, 0:128], hT[:, k, i * 128:(i + 1) * 128], wb[:, k, 24 * 128:25 * 128], k == 0, k == 7, ["hT", "wb"], ["pj%d" % (i % 2)])
                    kt = 4 * g + i
                    copy("dve", vsA[:, kt, 0:64], pbv[:, 0:64], ["pj%d" % (i % 2)], ["vsA"])
                    copy("act", vwA[:, kt % 8, 0:64], pbv[:, 64:128], ["pj%d" % (i % 2)], ["vwA"])
                pb, pkey = project(29, 12)
                o_, w_ = PK["gateb"]
                P.op("act", lambda e, pb=pb, o_=o_: e.activation(out=gsig[:], in_=pb[0:12, :], func=AF.Sigmoid, bias=pk[0:12, o_:o_ + 1]), [pkey, "pk"], ["gsig"])

                lo, hi = max(0, 32 * g - 1), 32 * g + 30
                n = hi - lo + 1
                c0 = 16 * lo + 16 - t0
                for kv in range(2):
                    b0 = kv * 64
                    reg = mi[0:64, kv * 64:kv * 64 + n]
                    for l in range(32):
                        mm(reg, w1b[b0:b0 + 64, l, :], kcvc[b0:b0 + 64, c0 + l:c0 + l + 16 * (n - 1) + 1:16], l == 0, l == 31, ["w1b", "kcvc"], ["mi"])
                    dsth = (h1k, h1v)[kv]
                    P.op("act", lambda e, reg=reg, dsth=dsth, kv=kv, n=n: e.activation(out=dsth[:, 0:n], in_=reg, func=AF.Silu, bias=ccst[:, kv:kv + 1]), ["mi", "ccst"], ["h1%d" % kv])
                mm(sc[0][:, 0:n], w2b[:, 0:128], h1k[:, 0:n], True, True, ["w2b", "h10"], ["sc0"])
                copy("dve", kcmpT[:, lo:hi + 1], sc[0][:, 0:n], ["sc0"], ["kcmpT"])
                mm(sc[1][0:n, 0:64], h1v[:, 0:n], w2b[:, 128:192], True, True, ["w2b", "h11"], ["sc1"])
                copy("dve", vstg[0:n, 0:64], sc[1][0:n, 0:64], ["sc1"], ["vstg"])
                i0 = lo
                while i0 <= hi:
                    i1 = min(hi, (i0 // 128) * 128 + 127)
                    P.dma("sp", lambda e, i0=i0, i1=i1, lo=lo: e.dma_start(out=vcmpA[i0 % 128:i1 % 128 + 1, i0 // 128, :], in_=vstg[i0 - lo:i1 - lo + 1, :]), reads=["vstg"], writes=["vcmpA"])
                    i0 = i1 + 1

                def combine_pair(pr, b, first):
                    copy("act", oaug[0:64, :], oa[0][0:64, :], ["oa0"], ["oaug"])
                    copy("dve", oaug[64:128, :], oa[1][0:64, :], ["oa1"], ["oaug"])
                    copy("act", oab[0:1, :], oa[0][64:65, :], ["oa0"], ["oab"])
                    copy("dve", oab[32:33, :], oa[1][64:65, :], ["oa1"], ["oab"])
                    mm(pj[0][:], dsel[:], oab[0:33, :], True, True, ["dsel", "oab"], ["pj0"])
                    mm(pj[1][:], gselp[:, pr * 3 + b, :], gsig[:], True, True, ["gselp", "gsig"], ["pj1"])
                    P.op("dve", lambda e: e.tensor_scalar(out=coef[:], in0=pj[0][:], scalar1=1e-30, scalar2=None, op0=ALU.max), ["pj0"], ["coef"])
                    P.op("act", lambda e: e.activation(out=coef[:], in_=coef[:], func=AF.Ln), ["coef"], ["coef"])
                    P.op("act", lambda e: e.activation(out=coef[:], in_=coef[:], func=AF.Exp, scale=-1.0), ["coef"], ["coef"])
                    P.op("dve", lambda e: e.tensor_tensor(out=coef[:], in0=coef[:], in1=pj[1][:], op=ALU.mult), ["coef", "pj1"], ["coef"])
                    if first:
                        P.op("dve", lambda e: e.tensor_tensor(out=acc[:, pr, :], in0=coef[:], in1=oaug[:], op=ALU.mult), ["coef", "oaug"], ["acc"])
                    else:
                        P.op("dve", lambda e: e.tensor_tensor(out=coef[:], in0=coef[:], in1=oaug[:], op=ALU.mult), ["coef", "oaug"], ["coef"])
                        P.op("dve", lambda e: e.tensor_tensor(out=acc[:, pr, :], in0=acc[:, pr, :], in1=coef[:], op=ALU.add), ["coef", "acc"], ["acc"])

                def pmask(ap, key, base, cm, step):
                    P.op("pool", lambda e: e.affine_select(out=ap, in_=ap, pattern=[[step, G]], compare_op=ALU.is_ge, fill=0.0, base=base, channel_multiplier=cm), [key], [key])

                def pmask(ap, key, base, cm, step):
                    P.op("pool", lambda e: e.affine_select(out=ap, in_=ap, pattern=[[step, G]], compare_op=ALU.is_ge, fill=0.0, base=base, channel_multiplier=cm), [key], [key])

                P.op("pool", lambda e: e.memset(impS[:], 0.0), [], ["impS"])
                ncc = g // 4 + 1
                for h in range(4):
                    pr, hb_ = h // 2, (h % 2) * 64
                    ob, okey = oa[h % 2], "oa%d" % (h % 2)
                    for ci in range(ncc):
                        dl = 2048 * ci - 512 * g
                        sb_, skey = sc[ci % 2], "sc%d" % (ci % 2)
                        mm(sb_[:], kcmpT[hb_:hb_ + 64, ci * 128:(ci + 1) * 128], qn[hb_:hb_ + 64, pr, :], True, True, ["kcmpT", "qn"], [skey])
                        P.op("act", lambda e, sb_=sb_, ci=ci: e.activation(out=pTc[:, ci, :], in_=sb_[:], func=AF.Exp), [skey], ["wmkvb"])
                        if dl >= -2048:
                            pmask(pTc[:, ci, :], "wmkvb", -31 - dl, -16, 1)
                        mm(ob[0:65, :], vcmpA[:, ci, :], pTc[:, ci, :], ci == 0, ci == ncc - 1, ["vcmpA", "wmkvb"], [okey])
                    for j in range(4):
                        reg = mi[:, (j % 2) * 256:(j % 2) * 256 + 129]
                        for ci in range(ncc):
                            mm(reg, pTc[:, ci, j * 128:(j + 1) * 128], ovl[:, ci, :], ci == 0, ci == ncc - 1, ["wmkvb", "ovl"], ["mi"])
                        P.op("dve", lambda e, reg=reg: e.tensor_scalar(out=m8[:, 0:1], in0=reg[:, 128:129], scalar1=1e-30, scalar2=None, op0=ALU.max), ["mi"], ["m8"])
                        P.op("dve", lambda e: e.reciprocal(out=m8[:, 0:1], in_=m8[:, 0:1]), ["m8"], ["m8"])
                        P.op("dve", lambda e, reg=reg, j=j: e.scalar_tensor_tensor(out=impS[:, j, :], in0=reg[:, 0:128], scalar=m8[:, 0:1], in1=impS[:, j, :], op0=ALU.mult, op1=ALU.add), ["mi", "m8", "impS"], ["impS"])
                    if h % 2 == 1:
                        combine_pair(h // 2, 0, True)

                for j in range(4):
                    qt = 4 * g + j
                    u0 = 126 - 2 * qt
                    P.op("dve", lambda e, j=j, u0=u0: e.tensor_tensor(out=score[:], in0=impS[:, j, :], in1=addw[:, u0:u0 + 128], op=ALU.add), ["impS", "addw"], ["score"])
                    P.op("dve", lambda e: e.tensor_scalar(out=score[:, 0:1], in0=score[:, 0:1], scalar1=1e4, scalar2=None, op0=ALU.add), ["score"], ["score"])
                    P.op("dve", lambda e: e.max(out=m8[:, 0:8], in_=score[:]), ["score"], ["m8"])
                    P.op("dve", lambda e: e.match_replace(out=stmp[:], in_to_replace=m8[:, 0:8], in_values=score[:], imm_value=-2.0), ["score", "m8"], ["stmp"])
                    P.op("dve", lambda e: e.max(out=m8[:, 8:16], in_=stmp[:]), ["stmp"], ["m8"])
                    P.op("dve", lambda e: e.tensor_reduce(out=m8[:, 0:1], in_=m8[:, 8:16], axis=AX.X, op=ALU.min), ["m8"], ["m8"])
                    P.op("dve", lambda e: e.tensor_scalar(out=stmp[:], in0=score[:], scalar1=m8[:, 0:1], scalar2=None, op0=ALU.is_ge), ["score", "m8"], ["stmp"])
                    P.op("dve", lambda e: e.tensor_scalar(out=nbq[:], in0=stmp[:], scalar1=1.0, scalar2=-NEG, op0=ALU.subtract, op1=ALU.mult), ["stmp"], ["nbq"])
                    P.op("pe", lambda e: e.transpose(out=tp[:, 0:128], in_=nbq[:], identity=ident[:]), ["nbq", "ident"], ["tp"])
                    copy("dve", nb[:, j * 128:(j + 1) * 128], tp[:, 0:128], ["tp"], ["nb"])

                P.phase = "nsa_sel"
                scA = [(sc[0], "sc0"), (sc[1], "sc1")]
                scB = [(pj[0], "pj0"), (pj[1], "pj1")]
                pTA = [(pT[0][:], "pT0"), (pT[1][:], "pT1")]
                pTB = [(AakT4[:, 0, :], "AakT4/0"), (AakT4[:, 1, :], "AakT4/1")]
                nslab = (4 * g + 3) // 32 + 1
                for pr in range(2):
                    for hl in range(2):
                        for M in range(nslab):
                            copy(("act", "dve")[(hl + M) % 2], qcat[0:64, 2 * hl + M, :], qr[64 * hl:64 * hl + 64, pr, :], ["qr"], ["qcat"])
                            copy(("dve", "pool")[hl], qcat[64:128, 2 * hl + M, :], nb[64 * M:64 * M + 64, :], ["nb"], ["qcat"])
                    nkt = 4 * g + 4

                    def sel_a(kt):
                        (sa, ska), (sb2, skb) = scA[kt % 2], scB[kt % 2]
                        (pa, pka), (pb2, pkb) = pTA[kt % 2], pTB[kt % 2]
                        M = kt // 32
                        ks_ = slice(kt * 128, (kt + 1) * 128)
                        mm(sa[:], ksT[:, ks_], qcat[:, M, :], True, True, ["ksT", "qcat"], [ska])
                        mm(sb2[:], ksT[:, ks_], qcat[:, 2 + M, :], True, True, ["ksT", "qcat"], [skb])
                        P.op("act", lambda e: e.activation(out=pa, in_=sa[:], func=AF.Exp), [ska], [pka])
                        P.op("act", lambda e: e.activation(out=pb2, in_=sb2[:], func=AF.Exp), [skb], [pkb])
                        if kt >= 4 * g:
                            pmask(pa, pka, -128 * (kt - 4 * g), -1, 1)
                            pmask(pb2, pkb, -128 * (kt - 4 * g), -1, 1)

                    sel_a(0)
                    for kt in range(nkt):
                        if kt + 1 < nkt:
                            sel_a(kt + 1)
                        mm(oa[0][0:65, :], vsA[:, kt, :], pTA[kt % 2][0], kt == 0, kt == nkt - 1, ["vsA", pTA[kt % 2][1]], ["oa0"])
                        mm(oa[1][0:65, :], vsA[:, kt, :], pTB[kt % 2][0], kt == 0, kt == nkt - 1, ["vsA", pTB[kt % 2][1]], ["oa1"])
                    combine_pair(pr, 1, False)

                    P.phase = "nsa_win"
                    kts = [kt for kt in range(4 * g - 4, 4 * g + 4) if kt >= 0]

                    def win_a(kt):
                        (sa, ska), (sb2, skb) = scA[kt % 2], scB[kt % 2]
                        (pa, pka), (pb2, pkb) = pTA[kt % 2], pTB[kt % 2]
                        ro = (kt % 8) * 128
                        mm(sa[:], kwT[:, ro:ro + 128], qcat[:, 0, :], True, True, ["kwT", "qcat"], [ska])
                        mm(sb2[:], kwT[:, ro:ro + 128], qcat[:, 2, :], True, True, ["kwT", "qcat"], [skb])
                        P.op("act", lambda e: e.activation(out=pa, in_=sa[:], func=AF.Exp), [ska], [pka])
                        P.op("act", lambda e: e.activation(out=pb2, in_=sb2[:], func=AF.Exp), [skb], [pkb])
                        rel = kt - 4 * g
                        for p_, k_ in ((pa, pka), (pb2, pkb)):
                            if rel >= 0:
                                pmask(p_, k_, -128 * rel, -1, 1)
                            else:
                                pmask(p_, k_, 511 + 128 * rel, 1, -1)

                    win_a(kts[0])
                    for ii, kt in enumerate(kts):
                        if ii + 1 < len(kts):
                            win_a(kts[ii + 1])
                        mm(oa[0][0:65, :], vwA[:, kt % 8, :], pTA[kt % 2][0], ii == 0, ii == len(kts) - 1, ["vwA", pTA[kt % 2][1]], ["oa0"])
                        mm(oa[1][0:65, :], vwA[:, kt % 8, :], pTB[kt % 2][0], ii == 0, ii == len(kts) - 1, ["vwA", pTB[kt % 2][1]], ["oa1"])
                    combine_pair(pr, 2, False)
                    P.phase = "nsa_sel"

                P.phase = "nsa"
                o_, w_ = PK["nsag"]
                for pr in range(2):
                    src = acc[:, pr, :]
                    P.op("act", lambda e, src=src: e.activation(out=hb0[:, 0:G], in_=src, func=AF.Square), ["acc"], ["hb0"])
                    mm(mi[:], blkavg[:], hb0[:, 0:G], True, True, ["blkavg", "hb0"], ["mi"])
                    P.op("act", lambda e: e.activation(out=coef[:], in_=mi[:], func=AF.Ln, bias=epsc[:]), ["mi", "epsc"], ["coef"])
                    P.op("act", lambda e: e.activation(out=coef[:], in_=coef[:], func=AF.Exp, scale=-0.5), ["coef"], ["coef"])
                    P.op("dve", lambda e, src=src, pr=pr, o_=o_: e.scalar_tensor_tensor(out=coef[:], in0=coef[:], scalar=pk[:, o_ + pr:o_ + pr + 1], in1=src, op0=ALU.mult, op1=ALU.mult), ["coef", "acc", "pk"], ["coef"])
                    P.op("dve", lambda e, pr=pr: e.tensor_tensor(out=yT[:, 4 + pr, :], in0=coef[:], in1=gn[:, pr, :], op=ALU.mult), ["coef", "gn"], ["yT"])
                P.phase = ""
                stop_at(7)
                for i in range(4):
                    xb, xk = xt[i % 2], "xt%d" % (i % 2)
                    P.dma("sp", lambda e, i=i, t0=t0, xb=xb: e.dma_start(out=xb[:], in_=x_d[t0 + i * 128:t0 + (i + 1) * 128, :]), writes=[xk])
                    for half in range(2):
                        pb = pj[half]
                        for k in range(8):
                            mm(pb[:], yT[:, k, i * 128:(i + 1) * 128], woutb[:, k, half * 512:(half + 1) * 512], k == 0, k == 7, ["yT", "woutb"], ["pj%d" % half])
                        P.op("dve", lambda e, pb=pb, xb=xb, half=half: e.tensor_tensor(out=xb[:, half * 512:(half + 1) * 512], in0=xb[:, half * 512:(half + 1) * 512], in1=pb[:], op=ALU.add),
                             ["pj%d" % half, xk], [xk])
                    sk = "st%d" % (8 + i)
                    ssq = st[:, 8 + i:9 + i]
                    hjk = hb[i % 2]
                    P.op("act", lambda e, xb=xb, ssq=ssq, hjk=hjk: e.activation(out=hjk[:], in_=xb[:], func=AF.Square, accum_out=ssq), [xk], ["hb0", sk])
                    P.op("dve", lambda e, ssq=ssq: e.tensor_scalar(out=ssq, in0=ssq, scalar1=1.0 / D, scalar2=1e-6, op0=ALU.mult, op1=ALU.add), [sk], [sk])
                    P.op("dve", lambda e, ssq=ssq: e.reciprocal(out=ssq, in_=ssq), [sk], [sk])
                    P.op("act", lambda e, ssq=ssq: e.activation(out=ssq, in_=ssq, func=AF.Sqrt), [sk], [sk])
                    P.op("dve", lambda e, xb=xb, ssq=ssq: e.scalar_tensor_tensor(out=xb[:], in0=xb[:], scalar=ssq, in1=gfin[:], op0=ALU.mult, op1=ALU.mult), [xk, sk, "gfin"], [xk])
                    P.dma("pool", lambda e, i=i, t0=t0, xb=xb: e.dma_start(out=out_d[t0 + i * 128:t0 + (i + 1) * 128, :], in_=xb[:]), reads=[xk], is_out=True)
        except _Stop:
            P.dma("pool", lambda e: e.dma_start(out=out_d[0:128, :], in_=xt[0][:]), reads=["xt0"], is_out=True)
        P.finish("sp")
        P.emit(block, sems, dsems)
    return nc


def make_in_maps(inputs, T, batches):
    ci = _colidx()
    w_in = np.asarray(inputs["w_in"][0])
    wcat = np.ascontiguousarray(w_in[:, ci].reshape(8, 128, NCOL))
    wout = np.ascontiguousarray(np.asarray(inputs["w_out"][0]).reshape(8, 128, D))
    wmkv = np.ascontiguousarray(np.asarray(inputs["w_mem_kv"][0]).reshape(8, 128, 512))
    pk = host_params({k: np.asarray(v) for k, v in inputs.items()})
    w1k = np.asarray(inputs["nsa_cmp_k_w1"][0]).reshape(32, 64, 64).transpose(1, 0, 2)
    w1v = np.asarray(inputs["nsa_cmp_v_w1"][0]).reshape(32, 64, 64).transpose(1, 0, 2)
    w1 = np.ascontiguousarray(np.concatenate([w1k, w1v], 0).reshape(128, 2048))
    w2k = np.asarray(inputs["nsa_cmp_k_w2"][0]); w2v = np.asarray(inputs["nsa_cmp_v_w2"][0])
    w2 = np.zeros((128, 192), np.float32)
    w2[0:64] = np.concatenate([w2k, w2k, w2v], 1)
    lora = np.ascontiguousarray(np.concatenate([np.asarray(inputs["rwkv_w_up"][0]), np.asarray(inputs["rwkv_a_up"][0])], 0))
    w0row = np.ascontiguousarray(np.asarray(inputs["rwkv_w0"][0]).reshape(1, 512))
    consts = host_consts(T)
    maps = []
    for b in batches:
        m = {
            "x": np.ascontiguousarray(np.asarray(inputs["x"][b][:T])),
            "mem": np.ascontiguousarray(np.asarray(inputs["mem"][b])),
            "wcat": wcat, "wout": wout, "wmkv": wmkv, "pk": pk, "w1": w1, "w2": w2, "lora": lora, "w0row": w0row,
            "gfin": np.ascontiguousarray(np.asarray(inputs["norm_final_g"]).reshape(1, D)),
        }
        m.update(consts)
        maps.append(m)
    return maps


_NC_CACHE = {}


def kernel(**inputs):
    T = inputs["x"].shape[1]
    B = inputs["x"].shape[0]
    if T not in _NC_CACHE:
        _NC_CACHE[T] = build_nc(T)
    nc = _NC_CACHE[T]
    batches = [i % B for i in range(8)]
    maps = make_in_maps(inputs, T, batches)
    res = run_bass_kernel_spmd(nc, maps, core_ids=list(range(8)))
    out = np.stack([res.results[b]["out"] for b in range(B)], axis=0)
    return out.astype(np.float32)
```

```python
import numpy as np
import ml_dtypes
from contextlib import ExitStack
import concourse.bass as bass
import concourse.mybir as mybir
from concourse.bass_utils import run_bass_kernel_spmd

F32 = mybir.dt.float32
BF16 = mybir.dt.bfloat16
ALU = mybir.AluOpType
AF = mybir.ActivationFunctionType
AX = mybir.AxisListType

ENGS = ("pe", "act", "dve", "pool", "sp")
N_DMA_SEMS = 24
import os as _os0
SAME_ENG_SYNC = _os0.environ.get("KSAME", "1") == "1"
NEG = -30000.0

D = 1024
NCOL = 29 * 128 + 12
G = 512
PSUM_PREFIX = ("pj", "sc", "oa", "mi", "tp")


class Prog:
    def __init__(self, nc):
        self.nc = nc
        self.lists = {e: [] for e in ENGS}
        self.cnt = {e: 0 for e in ENGS}
        self.seen = {e: {} for e in ENGS}
        self.clock_at = {e: [None] for e in ENGS}
        self.buf = {}
        self.dma_val = [0] * N_DMA_SEMS
        self.dma_clock = [[] for _ in range(N_DMA_SEMS)]
        self.dma_rr = 0
        self.out_toks = []
        self.phase = ""
        self.skip = set()
        self.children = {}

    def _deps(self, eng, reads, writes):
        need = {}

        def add(tok):
            if tok is None:
                return
            src, val = tok
            if src == eng and (eng == "pe" or not SAME_ENG_SYNC):
                return
            if need.get(src, 0) < val:
                need[src] = val

        def related(k):
            ks = [k]
            if "/" in k:
                ks.append(k.split("/")[0])
            else:
                ks.extend(self.children.get(k, ()))
            return ks

        for k0 in reads:
            for k in related(k0):
                st = self.buf.get(k)
                if st:
                    add(st[0])
                    if k[:2] in PSUM_PREFIX:
                        for r in st[1]:
                            if r[0] != eng:
                                add(r)
        for k0 in writes:
            for k in related(k0):
                st = self.buf.get(k)
                if st:
                    add(st[0])
                    for r in st[1]:
                        add(r)
        seen = self.seen[eng]
        waits = []
        for src, val in need.items():
            if seen.get(src, 0) >= val:
                continue
            waits.append((src, val))
            if isinstance(src, str):
                snap = self.clock_at[src][val]
            else:
                snap = self.dma_clock[src[1]][val // 16 - 1]
            for s2, v2 in snap.items():
                if seen.get(s2, 0) < v2:
                    seen[s2] = v2
            seen[src] = val
        return waits

    def _mark(self, tok, reads, writes):
        for k in list(reads) + list(writes):
            if "/" in k:
                self.children.setdefault(k.split("/")[0], set()).add(k)
        for k in reads:
            st = self.buf.setdefault(k, [None, []])
            st[1].append(tok)
            if len(st[1]) > 64:
                st[1] = st[1][-64:]
        for k in writes:
            self.buf[k] = [tok, []]
            if "/" not in k:
                for ch in self.children.get(k, ()):
                    self.buf[ch] = [tok, []]

    def op(self, eng, fn, reads=(), writes=(), self_wait=None):
        if self.phase in self.skip:
            return
        waits = self._deps(eng, reads, writes)
        if self_wait is not None and self.seen[eng].get(eng, 0) < self_wait:
            waits.append((eng, self_wait))
            self.seen[eng][eng] = self_wait
        self.cnt[eng] += 1
        c = self.cnt[eng]
        snap = dict(self.seen[eng])
        snap[eng] = c
        self.clock_at[eng].append(snap)
        self.lists[eng].append(("op", fn, waits, None))
        self._mark((eng, c), reads, writes)

    def dma(self, eng, fn, reads=(), writes=(), is_out=False):
        if self.phase in self.skip:
            return None
        s = self.dma_rr
        self.dma_rr = (self.dma_rr + 1) % N_DMA_SEMS
        waits = self._deps(eng, reads, writes)
        src = ("dma", s)
        prev = self.dma_val[s]
        if prev and self.seen[eng].get(src, 0) < prev:
            waits.append((src, prev))
            self.seen[eng][src] = prev
        val = prev + 16
        self.dma_val[s] = val
        self.dma_clock[s].append(dict(self.seen[eng]))
        self.lists[eng].append(("dma", fn, waits, (s, val)))
        self._mark((src, val), reads, writes)
        if is_out:
            self.out_toks.append((src, val))
        return (src, val)

    def finish(self, eng="sp"):
        waits = []
        for s in range(N_DMA_SEMS):
            if self.dma_val[s]:
                waits.append((("dma", s), self.dma_val[s]))
        self.lists[eng].append(("wait", None, waits, None))

    def emit(self, block, sems, dsems):
        engobj = {"pe": "tensor", "act": "scalar", "dve": "vector", "pool": "gpsimd", "sp": "sync"}

        def semof(src):
            return sems[src] if isinstance(src, str) else dsems[src[1]]

        def make(ename):
            lst = self.lists[ename]

            def body(e):
                for kind, fn, waits, extra in lst:
                    for src, val in waits:
                        e.wait_ge(semof(src), val)
                    if kind == "op":
                        fn(e).then_inc(sems[ename], 1)
                    elif kind == "dma":
                        fn(e).then_inc(dsems[extra[0]], 16)

            return body

        for ename in ENGS:
            if self.lists[ename]:
                getattr(block, engobj[ename])(make(ename))


RW, NSAB, MEMB = 0, 2176, 3084


def _colidx():
    idx = []
    for hp in range(4):
        for base in (0, 512, 1024, 1536):
            idx += list(range(base + 128 * hp, base + 128 * hp + 128))
    idx += list(range(2048, 2176))
    q0, g0 = NSAB, NSAB + 256
    idx += list(range(q0, q0 + 256))
    idx += list(range(g0, g0 + 256))
    kc, vc, ks, vs, kw, vw = (NSAB + 524 + 64 * i for i in range(6))
    idx += list(range(kc, kc + 64)) + list(range(vc, vc + 64))
    idx += list(range(ks, ks + 64)) * 2
    idx += list(range(kw, kw + 64)) * 2
    idx += list(range(vs, vs + 64)) + list(range(vw, vw + 64))
    idx += list(range(MEMB, MEMB + 512))
    idx += list(range(NSAB + 512, NSAB + 524))
    assert len(idx) == NCOL
    return np.array(idx)


def _bf(a):
    return np.ascontiguousarray(a.astype(ml_dtypes.bfloat16))


def host_consts(T):
    c = {}
    c["ident"] = _bf(np.eye(128, dtype=np.float32))
    half = 8
    inv = (np.float32(500000.0) ** (-np.arange(half, dtype=np.float32) / np.float32(half))).astype(np.float32)
    ang = np.arange(T, dtype=np.float32)[None, :] * inv[:, None]
    C = np.ones((64, T), np.float32)
    S = np.zeros((64, T), np.float32)
    C[0:8] = np.cos(ang); C[8:16] = np.cos(ang)
    S[0:8] = -np.sin(ang); S[8:16] = np.sin(ang)
    c["ropeC"] = np.ascontiguousarray(np.concatenate([C, C], 0))
    c["ropeS"] = np.ascontiguousarray(np.concatenate([S, S], 0))
    pm = np.zeros((128, 128), np.float32)
    for hb in (0, 64):
        for d in range(8):
            pm[hb + d + 8, hb + d] = 1.0
            pm[hb + d, hb + d + 8] = 1.0
    c["pm"] = _bf(pm)
    ovl = np.zeros((512, 129), np.float32)
    for i in range(511):
        for sblk in range(128):
            o = min(16 * i + 32, 64 * sblk + 64) - max(16 * i, 64 * sblk)
            if o > 0:
                ovl[i, sblk] = o / 32.0
    ovl[:, 128] = 1.0
    c["ovl"] = _bf(ovl.reshape(4, 128, 129).transpose(1, 0, 2))
    addw = np.zeros((128, 256), np.float32)
    for pp in range(128):
        cur = 1 if pp >= 64 else 0
        for u in range(256):
            sp = u - 126
            valid = sp <= cur
            forced = sp in (cur, cur - 1)
            addw[pp, u] = (0.0 if valid else -1.0) + (1e4 if forced else 0.0)
    c["addw"] = addw
    selpat = np.zeros((64, 32, 128), np.float32)
    for j in range(32):
        selpat[2 * j, j, 0:64] = 1.0
        selpat[2 * j + 1, j, 64:128] = 1.0
    c["selpat"] = _bf(selpat.reshape(64, 4096))
    gselp = np.zeros((12, 6, 128), np.float32)
    for pr_ in range(2):
        for b_ in range(3):
            gselp[3 * (2 * pr_) + b_, pr_ * 3 + b_, 0:64] = 1.0
            gselp[3 * (2 * pr_ + 1) + b_, pr_ * 3 + b_, 64:128] = 1.0
    c["gselp"] = _bf(gselp)
    dsel = np.zeros((33, 128), np.float32)
    dsel[0, 0:64] = 1.0
    dsel[32, 64:128] = 1.0
    c["dsel"] = _bf(dsel)
    pp = np.arange(128)[:, None]
    ff = np.arange(128)[None, :]
    for nm, m in (("maskL2", pp > ff), ("maskU2", pp < ff), ("maskUI2", pp <= ff), ("ident2", pp == ff)):
        m = m.astype(np.float32)
        c[nm] = _bf(np.stack([m, m], 1))
    blk = (pp // 64 == ff // 64).astype(np.float32)
    c["blk1"] = _bf(blk)
    c["blkavg"] = _bf(blk / 64.0)
    return c


PK = {}
_o = 0
for _n, _w in (("gin", 8), ("gmem", 8), ("mu", 17), ("memg", 2), ("nsag", 2), ("gateb", 1), ("posT", 32), ("w0", 4), ("a0", 4), ("kk", 4), ("ka", 4), ("rk", 4), ("lnw", 4), ("lnb", 4)):
    PK[_n] = (_o, _w)
    _o += _w
NPK = _o


def host_params(inp):
    pk = np.zeros((128, NPK), np.float32)

    def put(name, arr):
        o, w = PK[name]
        pk[:, o:o + w] = arr

    put("gin", inp["norm_in_g"][0].reshape(8, 128).T)
    put("gmem", inp["mem_norm_g"][0].reshape(8, 128).T)
    ci = _colidx()
    put("mu", inp["rwkv_mu"][0][ci[:17 * 128]].reshape(17, 128).T)
    put("memg", inp["mem_out_g"][0].reshape(2, 128).T)
    put("nsag", inp["nsa_out_g"][0].reshape(2, 128).T)
    gb = np.zeros((128, 1), np.float32)
    gb[0:12, 0] = inp["nsa_gate_b"][0]
    put("gateb", gb)
    pt = inp["nsa_cmp_pos"][0].T
    put("posT", np.concatenate([pt, pt], 0))
    for nm, key in (("w0", "rwkv_w0"), ("a0", "rwkv_a0"), ("kk", "rwkv_k_k"), ("ka", "rwkv_k_a"), ("rk", "rwkv_r_k"), ("lnw", "rwkv_ln_w"), ("lnb", "rwkv_ln_b")):
        put(nm, inp[key][0].reshape(4, 128).T)
    return pk


def build_nc(T, dbg=None):
    NG = T // G
    NT = T // 128
    nc = bass.Bass("TRN2", target_bir_lowering=False)
    dram = {}

    def din(name, shape, dt=F32):
        dram[name] = nc.dram_tensor(name, list(shape), dt, kind="ExternalInput").ap()
        return dram[name]

    x_d = din("x", [T, D])
    mem_d = din("mem", [256, D])
    wcat_d = din("wcat", [8, 128, NCOL])
    wout_d = din("wout", [8, 128, D])
    wmkv_d = din("wmkv", [8, 128, 512])
    pk_d = din("pk", [128, NPK])
    gfin_d = din("gfin", [1, D])
    ident_d = din("ident", [128, 128], BF16)
    ropeC_d = din("ropeC", [128, T])
    ropeS_d = din("ropeS", [128, T])
    pm_d = din("pm", [128, 128], BF16)
    ovl_d = din("ovl", [128, 4, 129], BF16)
    addw_d = din("addw", [128, 256])
    selpat_d = din("selpat", [64, 4096], BF16)
    gselp_d = din("gselp", [12, 6, 128], BF16)
    dsel_d = din("dsel", [33, 128], BF16)
    w1_d = din("w1", [128, 2048])
    w2_d = din("w2", [128, 192])
    lora_d = din("lora", [128, 512])
    w0row_d = din("w0row", [1, 512])
    maskL2_d = din("maskL2", [128, 2, 128], BF16)
    maskU2_d = din("maskU2", [128, 2, 128], BF16)
    maskUI2_d = din("maskUI2", [128, 2, 128], BF16)
    ident2_d = din("ident2", [128, 2, 128], BF16)
    blk1_d = din("blk1", [128, 128], BF16)
    blkavg_d = din("blkavg", [128, 128], BF16)
    out_d = nc.dram_tensor("out", [T, D], F32, kind="ExternalOutput").ap()
    dbg_d = {}
    if dbg:
        for n, (shape, dt) in dbg.items():
            dbg_d[n] = nc.dram_tensor("dbg_" + n, list(shape), dt, kind="ExternalOutput").ap()

    with ExitStack() as es:
        def sb(name, shape, dt=F32):
            return es.enter_context(nc.sbuf_tensor("sb_" + name, list(shape), dt))

        def ps(name, shape, dt=F32):
            return es.enter_context(nc.psum_tensor("ps_" + name, list(shape), dt))

        wb = sb("wb", [128, 8, NCOL], BF16)
        woutb = sb("woutb", [128, 8, D], BF16)
        wmkvb = sb("wmkvb", [128, 8, 512], BF16)
        pk = sb("pk", [128, NPK])
        gfin = sb("gfin", [128, D])
        ident = sb("ident", [128, 128], BF16)
        xt = [sb("xt%d" % i, [128, D]) for i in range(2)]
        hb0 = sb("hb0", [128, D], BF16)
        hb = [hb0, hb0]
        hT = sb("hT", [128, 8, G], BF16)
        st = sb("st", [128, 16])
        yT = sb("yT", [128, 8, G], BF16)
        memkT = sb("memkT", [128, 2, 256], BF16)
        memvA = sb("memvA", [128, 2, 4, 65], BF16)
        qm = sb("qm", [128, 2, G], BF16)
        gm = sb("gm", [128, 2, G], BF16)
        pT = [sb("pT%d" % i, [128, G], BF16) for i in range(2)]
        oaug = sb("oaug", [128, G], BF16)
        osq = sb("osq", [65, G], BF16)
        cvec = sb("cvec", [65, 128], BF16)
        pm = sb("pm", [128, 128], BF16)
        ovl = sb("ovl", [128, 4, 129], BF16)
        addw = sb("addw", [128, 256])
        qcat = sb("qcat", [128, 4, G], BF16)
        gselp = sb("gselp", [12, 6, 128], BF16)
        dsel = sb("dsel", [33, 128], BF16)
        epsc = sb("epsc", [128, 1])
        e64 = sb("e64", [65, 128], BF16)
        w1b = sb("w1b", [128, 32, 64], BF16)
        w2b = sb("w2b", [64, 192], BF16)
        posTb = sb("posTb", [128, 32], BF16)
        ccst = sb("ccst", [64, 2])
        ropeC = sb("ropeC", [128, G])
        ropeS = sb("ropeS", [128, G])
        qn = sb("qn", [128, 2, G], BF16)
        qr = sb("qr", [128, 2, G], BF16)
        gn = sb("gn", [128, 2, G], BF16)
        kcvc = sb("kcvc", [128, 16 + G], BF16)
        kraw = sb("kraw", [128, G], BF16)
        ksT = sb("ksT", [128, T], BF16)
        kwT = sb("kwT", [128, 1024], BF16)
        vsA = sb("vsA", [128, NT, 65], BF16)
        vwA = sb("vwA", [128, 8, 65], BF16)
        kcmpT = sb("kcmpT", [128, 512], BF16)
        vcmpA = sb("vcmpA", [128, 4, 65], BF16)
        vstg = sb("vstg", [32, 65], BF16)
        h1k = sb("h1k", [64, 32], BF16)
        h1v = sb("h1v", [64, 32], BF16)
        gsig = sb("gsig", [12, G], BF16)
        impS = sb("impS", [128, 4, 128])
        score = sb("score", [128, 128])
        stmp = sb("stmp", [128, 128])
        m8 = sb("m8", [128, 16])
        nbq = sb("nbq", [128, 128], BF16)
        nb = sb("nb", [128, G], BF16)
        oab = sb("oab", [65, G], BF16)
        coef = sb("coef", [128, G])
        scl = coef
        rtmp = coef
        acc = sb("acc", [128, 2, G])
        ltb = [sb("lt%d" % i, [128, G + 2], BF16) for i in range(2)]
        mu1m = sb("mu1m", [128, 17])
        prevcol = sb("prevcol", [128, 17])
        loraup = sb("loraup", [128, 512], BF16)
        w0hi = sb("w0hi", [1, 512], BF16)
        w0lo = sb("w0lo", [1, 512], BF16)
        onesrow = sb("onesrow", [1, 128], BF16)
        ka1m = sb("ka1m", [128, 4])
        maskL2 = sb("maskL2", [128, 2, 128], BF16)
        maskU2 = sb("maskU2", [128, 2, 128], BF16)
        maskUI2 = sb("maskUI2", [128, 2, 128], BF16)
        ident2 = sb("ident2", [128, 2, 128], BF16)
        blk1 = sb("blk1", [128, 128], BF16)
        blkavg = sb("blkavg", [128, 128], BF16)
        sgtok = sb("sgtok", [128, 4, 128], BF16)
        AakT4 = sb("AakT4", [128, 2, G], BF16)
        AkV4 = sb("AkV4", [128, G], BF16)
        STb = sb("STb", [64, 8, 64], BF16)
        STn = sb("STn", [64, 2, 64], BF16)
        ncL = sb("ncL", [128, 4])
        WL = sb("WL", [128, 4])
        WL0 = sb("WL0", [64, 8])
        pj = [ps("pj%d" % i, [128, 512]) for i in range(2)]
        sc = [ps("sc%d" % i, [128, 512]) for i in range(2)]
        oa = [ps("oa%d" % i, [128, 512]) for i in range(2)]
        mi = ps("mi", [128, 512])
        tp = ps("tp", [128, 1024], BF16)

        pTc = wmkvb[:, 0:4, :]
        sems = {e: es.enter_context(nc.semaphore("s_" + e)) for e in ENGS}
        dsems = [es.enter_context(nc.semaphore("d%d" % i)) for i in range(N_DMA_SEMS)]
        block = es.enter_context(nc.Block())
        P = Prog(nc)
        rr = {"ev": 0}

        def ev_eng():
            rr["ev"] ^= 1
            return "act" if rr["ev"] else "dve"

        def copy(eng, out, in_, reads, writes):
            if eng == "act":
                P.op("act", lambda e: e.copy(out=out, in_=in_), reads, writes)
            else:
                P.op(eng, lambda e: e.tensor_copy(out=out, in_=in_), reads, writes)

        pe_last = {}

        def mm(out, lhsT, rhs, start, stop, reads, writes):
            base, rows = lhsT.base_partition(), lhsT.shape[0]
            sw = None
            for k in writes:
                last = pe_last.get(k)
                if last is not None:
                    c0, b0, r0 = last
                    if b0 + r0 <= base or base + rows <= b0:
                        sw = max(sw or 0, c0)
            P.op("pe", lambda e: e.matmul(out, lhsT, rhs, start=start, stop=stop), reads, writes, self_wait=sw)
            for k in writes:
                pe_last[k] = (P.cnt["pe"], base, rows)

        def dbg_out(name, ap, key):
            if dbg and name in dbg_d:
                P.dma("sp", lambda e: e.dma_start(out=dbg_d[name], in_=ap), reads=[key])

        import os as _os
        KSTOP = int(_os.environ.get("KSTOP", "99"))
        KSKIP = _os.environ.get("KSKIP", "")
        P.skip = set(KSKIP.split(",")) if KSKIP else set()

        class _Stop(Exception):
            pass

        def stop_at(n):
            if KSTOP == n:
                raise _Stop()

        try:
            P.dma("sp", lambda e: e.dma_start(out=pk[:], in_=pk_d[:, :]), writes=["pk"])
            P.dma("sp", lambda e: e.dma_start(out=ident[:], in_=ident_d[:, :]), writes=["ident"])
            P.dma("sp", lambda e: e.dma_start(out=gfin[:], in_=gfin_d[0:1, :].broadcast_to([128, D])), writes=["gfin"])

            def pkc(name, j=0, rows=128):
                o, w = PK[name]
                return pk[0:rows, o + j:o + j + 1]

            def load_cast(dst3, src_d, ncols, gname, key):
                i = 0
                for k in range(8):
                    for c0 in range(0, ncols, 1024):
                        cw = min(1024, ncols - c0)
                        stg = xt[i % 2]
                        skey = "xt%d" % (i % 2)
                        P.dma("sp" if i % 2 == 0 else "pool",
                              lambda e, stg=stg, k=k, c0=c0, cw=cw: e.dma_start(out=stg[:, 0:cw], in_=src_d[k, :, c0:c0 + cw]),
                              writes=[skey])
                        eng = ev_eng()
                        o = dst3[:, k, c0:c0 + cw]
                        if gname is None:
                            copy(eng, o, stg[:, 0:cw], [skey], [key])
                        elif eng == "act":
                            P.op("act", lambda e, o=o, stg=stg, cw=cw, k=k: e.activation(out=o, in_=stg[:, 0:cw], func=AF.Copy, scale=pkc(gname, k)),
                                 [skey, "pk"], [key])
                        else:
                            P.op("dve", lambda e, o=o, stg=stg, cw=cw, k=k: e.tensor_scalar(out=o, in0=stg[:, 0:cw], scalar1=pkc(gname, k), scalar2=None, op0=ALU.mult),
                                 [skey, "pk"], [key])
                        i += 1

            stop_at(1)
            load_cast(wmkvb, wmkv_d, 512, "gmem", "wmkvb")
            stop_at(2)
            load_cast(wb, wcat_d, NCOL, "gin", "wb")
            load_cast(woutb, wout_d, D, None, "woutb")

            P.op("pool", lambda e: e.memset(cvec[0:64, :], 1.0 / 64), writes=["cvec"])
            P.op("pool", lambda e: e.memset(cvec[64:65, :], 1e-6), writes=["cvec"])

            def norm_transpose(src_tile, skey, dstT, dkey, col0, slot):
                h = hb[slot]
                hk = "hb0"
                ssq = st[:, slot:slot + 1]
                P.op("act", lambda e: e.activation(out=h[:], in_=src_tile[:], func=AF.Square, accum_out=ssq), [skey], [hk, "st%d" % slot])
                P.op("dve", lambda e: e.tensor_scalar(out=ssq, in0=ssq, scalar1=1.0 / D, scalar2=1e-6, op0=ALU.mult, op1=ALU.add), ["st%d" % slot], ["st%d" % slot])
                P.op("dve", lambda e: e.reciprocal(out=ssq, in_=ssq), ["st%d" % slot], ["st%d" % slot])
                P.op("act", lambda e: e.activation(out=ssq, in_=ssq, func=AF.Sqrt), ["st%d" % slot], ["st%d" % slot])
                P.op("dve", lambda e: e.tensor_scalar(out=h[:], in0=src_tile[:], scalar1=ssq, scalar2=None, op0=ALU.mult), [skey, "st%d" % slot], [hk])
                for k in range(8):
                    P.op("pe", lambda e, k=k: e.transpose(out=tp[:, k * 128:(k + 1) * 128], in_=h[:, k * 128:(k + 1) * 128], identity=ident[:]), [hk, "ident"], ["tp"])
                copy(ev_eng(), dstT[:, :, col0:col0 + 128], tp[:].rearrange("p (k t) -> p k t", k=8), ["tp"], [dkey])


            for nm, dst, src in (("pm", pm, pm_d), ("ovl", ovl, ovl_d), ("addw", addw, addw_d),
                                 ):
                P.dma("sp", lambda e, dst=dst, src=src: e.dma_start(out=dst[:], in_=src), writes=[nm])
            P.dma("sp", lambda e: e.dma_start(out=gselp[:], in_=gselp_d), writes=["gselp"])
            P.dma("sp", lambda e: e.dma_start(out=dsel[:], in_=dsel_d), writes=["dsel"])
            P.op("pool", lambda e: e.memset(epsc[:], 1e-6), writes=["epsc"])
            P.op("pool", lambda e: e.memset(oab[:], 0.0), writes=["oab"])
            P.op("pool", lambda e: e.memset(e64[:], 0.0), writes=["e64"])
            P.op("pool", lambda e: e.memset(e64[64:65, :], 1.0), writes=["e64"])
            P.op("pool", lambda e: e.memset(kcmpT[:], 0.0), writes=["kcmpT"])
            P.op("pool", lambda e: e.memset(vcmpA[:], 0.0), writes=["vcmpA"])
            P.op("pool", lambda e: e.memset(vsA[:], 1.0), writes=["vsA"])
            P.op("pool", lambda e: e.memset(vwA[:], 1.0), writes=["vwA"])
            P.op("pool", lambda e: e.memset(kwT[:], 0.0), writes=["kwT"])
            for c0 in range(0, T, 4096):
                cw = min(4096, T - c0)
                P.dma("sp", lambda e, c0=c0, cw=cw: e.dma_start(out=ksT[64:128, c0:c0 + cw], in_=selpat_d[:, 0:cw]), writes=["ksT"])
            P.op("pool", lambda e: e.memset(vstg[:], 1.0), writes=["vstg"])
            P.op("pool", lambda e: e.memset(kcvc[:], 0.0), writes=["kcvc"])
            for hlf in range(2):
                P.dma("sp", lambda e, hlf=hlf: e.dma_start(out=xt[hlf][:], in_=w1_d[:, hlf * 1024:(hlf + 1) * 1024]), writes=["xt%d" % hlf])
                copy(ev_eng(), w1b[:, hlf * 16:(hlf + 1) * 16, :], xt[hlf][:].rearrange("p (l e) -> p l e", e=64), ["xt%d" % hlf], ["w1b"])
            P.dma("sp", lambda e: e.dma_start(out=xt[0][:, 0:192], in_=w2_d[:, :]), writes=["xt0"])
            copy("dve", w2b[:], xt[0][0:64, 0:192], ["xt0"], ["w2b"])
            o_, w_ = PK["posT"]
            copy("dve", posTb[:], pk[:, o_:o_ + 32], ["pk"], ["posTb"])
            for kv in range(2):
                b0 = kv * 64
                for l in range(32):
                    mm(mi[0:64, kv:kv + 1], w1b[b0:b0 + 64, l, :], posTb[b0:b0 + 64, l:l + 1], l == 0, l == 31, ["w1b", "posTb"], ["mi"])
                copy("dve", ccst[:, kv:kv + 1], mi[0:64, kv:kv + 1], ["mi"], ["ccst"])

            for nm, dst, src in (("maskL2", maskL2, maskL2_d), ("maskU2", maskU2, maskU2_d), ("maskUI2", maskUI2, maskUI2_d),
                                 ("ident2", ident2, ident2_d), ("blk1", blk1, blk1_d), ("blkavg", blkavg, blkavg_d)):
                P.dma("sp", lambda e, dst=dst, src=src: e.dma_start(out=dst[:], in_=src), writes=[nm])
            P.dma("sp", lambda e: e.dma_start(out=xt[1][:, 0:512], in_=lora_d[:, :]), writes=["xt1"])
            copy("dve", loraup[:], xt[1][:, 0:512], ["xt1"], ["loraup"])
            P.dma("sp", lambda e: e.dma_start(out=coef[0:1, 0:512], in_=w0row_d[:, :]), writes=["coef"])
            copy("dve", w0hi[:], coef[0:1, 0:512], ["coef"], ["w0hi"])
            P.op("dve", lambda e: e.tensor_tensor(out=coef[0:1, 0:512], in0=coef[0:1, 0:512], in1=w0hi[:], op=ALU.subtract), ["coef", "w0hi"], ["coef"])
            copy("dve", w0lo[:], coef[0:1, 0:512], ["coef"], ["w0lo"])
            o_, w_ = PK["mu"]
            P.op("dve", lambda e, o_=o_: e.tensor_scalar(out=mu1m[:], in0=pk[:, o_:o_ + 17], scalar1=-1.0, scalar2=1.0, op0=ALU.mult, op1=ALU.add), ["pk"], ["mu1m"])
            P.op("pool", lambda e: e.memset(onesrow[:], 1.0), writes=["onesrow"])
            P.op("pool", lambda e: e.memset(prevcol[:], 0.0), writes=["prevcol"])
            P.op("pool", lambda e: e.memset(STb[:], 0.0), writes=["STb"])
            o_, w_ = PK["ka"]
            P.op("dve", lambda e, o_=o_: e.tensor_scalar(out=ka1m[:], in0=pk[:, o_:o_ + 4], scalar1=-1.0, scalar2=1.0, op0=ALU.mult, op1=ALU.add), ["pk"], ["ka1m"])
            tokall = wmkvb[:, 4:8, :].rearrange("p c (w f) -> p c w f", w=4)
            stop_at(3)
            for mt in range(2):
                P.dma("sp", lambda e, mt=mt: e.dma_start(out=xt[mt][:], in_=mem_d[mt * 128:(mt + 1) * 128, :]), writes=["xt%d" % mt])
                norm_transpose(xt[mt], "xt%d" % mt, hT, "hT", mt * 128, mt)
            stop_at(4)
            for pr in range(2):
                for k in range(8):
                    mm(pj[0][:, 0:256], wmkvb[:, k, pr * 128:(pr + 1) * 128], hT[:, k, 0:256], k == 0, k == 7, ["wmkvb", "hT"], ["pj0"])
                copy(ev_eng(), memkT[:, pr, :], pj[0][:, 0:256], ["pj0"], ["memkT"])
            P.op("pool", lambda e: e.memset(memvA[:], 1.0), writes=["memvA"])
            for mt in range(2):
                for k in range(8):
                    mm(pj[1][:, 0:256], hT[:, k, mt * 128:(mt + 1) * 128], wmkvb[:, k, 256:512], k == 0, k == 7, ["wmkvb", "hT"], ["pj1"])
                copy(ev_eng(), memvA[:, mt, :, 0:64], pj[1][:, 0:256].rearrange("p (h d) -> p h d", h=4), ["pj1"], ["memvA"])

            def finalize_head(o_ps, okey, use_den, hb_, gcol_ap, gate_ap, gate_key, dst_ap, dst_key, from_sbuf=False):
                sl = slice(hb_, hb_ + 64)
                if from_sbuf:
                    copy("dve", oaug[sl, :], o_ps, [okey], ["oaug"])
                    P.op("act", lambda e: e.activation(out=osq[0:64, :], in_=o_ps, func=AF.Square), [okey], ["osq"])
                    P.op("pool", lambda e: e.memset(osq[64:65, :], 1.0), [], ["osq"])
                elif use_den:
                    copy("dve", oaug[sl, :], o_ps[0:64, :], [okey], ["oaug"])
                    P.op("act", lambda e: e.activation(out=osq[:], in_=o_ps[0:65, :], func=AF.Square), [okey], ["osq"])
                else:
                    copy("dve", oaug[sl, :], o_ps[0:64, :], [okey], ["oaug"])
                    P.op("act", lambda e: e.activation(out=osq[0:64, :], in_=o_ps[0:64, :], func=AF.Square), [okey], ["osq"])
                    P.op("pool", lambda e: e.memset(osq[64:65, :], 1.0), [], ["osq"])
                mm(mi[:], cvec[:], osq[:], True, True, ["cvec", "osq"], ["mi"])
                P.op("act", lambda e: e.activation(out=scl[sl, :], in_=mi[sl, :], func=AF.Ln), ["mi"], ["coef"])
                P.op("act", lambda e: e.activation(out=scl[sl, :], in_=scl[sl, :], func=AF.Exp, scale=-0.5), ["coef"], ["coef"])
                P.op("dve", lambda e: e.scalar_tensor_tensor(out=scl[sl, :], in0=scl[sl, :], scalar=gcol_ap, in1=oaug[sl, :], op0=ALU.mult, op1=ALU.mult), ["coef", "oaug", "pk"], ["coef"])
                P.op("dve", lambda e: e.tensor_tensor(out=dst_ap, in0=scl[sl, :], in1=gate_ap, op=ALU.mult), ["coef", gate_key], [dst_key])

            P.op("pool", lambda e: e.memset(wmkvb[:, 4:8, :], 0.0), ["wmkvb"], ["wmkvb", "tokall"])
            stop_at(5)
            for g in range(NG):
                t0 = g * G
                for i in range(4):
                    P.dma("sp", lambda e, i=i, t0=t0: e.dma_start(out=xt[i % 2][:], in_=x_d[t0 + i * 128:t0 + (i + 1) * 128, :]), writes=["xt%d" % (i % 2)])
                    norm_transpose(xt[i % 2], "xt%d" % (i % 2), hT, "hT", i * 128, i % 2)

                pj_rot = {"i": 0}

                def project(chunk, width=128):
                    if P.phase == "rwkv":
                        banks = ((pj[0], "pj0"), (pj[1], "pj1"), (sc[0], "sc0"), (sc[1], "sc1"))
                    elif P.phase in ("nsa", "mem"):
                        banks = ((pj[0], "pj0"), (pj[1], "pj1"), (sc[0], "sc0"), (sc[1], "sc1"))
                    else:
                        banks = ((pj[0], "pj0"), (pj[1], "pj1"))
                    pb, pkey = banks[pj_rot["i"] % len(banks)]
                    pj_rot["i"] += 1
                    for k in range(8):
                        mm(pb[0:width, :], wb[:, k, chunk * 128:chunk * 128 + width], hT[:, k, :], k == 0, k == 7, ["wb", "hT"], [pkey])
                    return pb, pkey


                P.phase = "rwkv"
                C0 = float(np.exp(-0.5))

                def tt(eng, out, in0, in1, op, reads, writes):
                    P.op(eng, lambda e: e.tensor_tensor(out=out, in0=in0, in1=in1, op=op), reads, writes)

                def ts(eng, out, in0, s1, s2, op0, op1, reads, writes):
                    if s2 is None:
                        P.op(eng, lambda e: e.tensor_scalar(out=out, in0=in0, scalar1=s1, scalar2=None, op0=op0), reads, writes)
                    else:
                        P.op(eng, lambda e: e.tensor_scalar(out=out, in0=in0, scalar1=s1, scalar2=s2, op0=op0, op1=op1), reads, writes)

                def stt(eng, out, in0, scalar, in1, op0, op1, reads, writes):
                    P.op(eng, lambda e: e.scalar_tensor_tensor(out=out, in0=in0, scalar=scalar, in1=in1, op0=op0, op1=op1), reads, writes)

                def actf(out, in_, func, reads, writes, bias=None, scale=None):
                    kw = {}
                    if bias is not None:
                        kw["bias"] = bias
                    if scale is not None:
                        kw["scale"] = scale
                    P.op("act", lambda e: e.activation(out=out, in_=in_, func=func, **kw), reads, writes)

                def pcol(name, j):
                    o, w = PK[name]
                    return pk[:, o + j:o + j + 1]

                def lerp_chunk(cidx, dst_ap, dkey):
                    pb, pkey = project(cidx)
                    lt, lk = ltb[cidx % 2], "lt%d" % (cidx % 2)
                    actf(lt[:, 1:G + 1], pb[:], AF.Copy, [pkey, "pk"], [lk], scale=pcol("mu", cidx))
                    copy("dve", lt[:, 0:1], prevcol[:, cidx:cidx + 1], ["prevcol"], [lk])
                    copy("dve", prevcol[:, cidx:cidx + 1], lt[:, G:G + 1], [lk], ["prevcol"])
                    stt("dve", dst_ap, pb[:], mu1m[:, cidx:cidx + 1], lt[:, 0:G], ALU.mult, ALU.add, [pkey, "mu1m", lk], [dkey])

                rT, kmT = qn[:, 0, :], qn[:, 1, :]
                vT, gT = qr[:, 0, :], qr[:, 1, :]
                bT, At = gn[:, 0, :], gn[:, 1, :]
                Rt, Bt = qm[:, 0, :], qm[:, 1, :]
                Kt, Bb = gm[:, 0, :], gm[:, 1, :]
                Kb, kkn = pT[0][:], pT[1][:]
                aT, lin = kraw[:], nb[:]
                kf, yraw = acc[:, 0, :], acc[:, 1, :]
                Etmp, Esq = hb0[:, 0:G], hb0[:, G:2 * G]

                lerp_chunk(16, kf, "acc")
                actf(lin[0:64, :], kf[0:64, :], AF.Tanh, ["acc"], ["nb"])
                copy("dve", lin[64:128, :], kf[64:128, :], ["acc"], ["nb"])

                stop_at(40)
                for hp in range(4):
                    hc = slice(hp * 128, (hp + 1) * 128)
                    mm(oa[0][:], loraup[64:128, hc], lin[64:128, :], True, True, ["nb", "loraup"], ["oa0"])
                    for c in range(4):
                        reg = mi[:, c * 128:(c + 1) * 128]
                        mm(reg, lin[0:64, c * 128:(c + 1) * 128], loraup[0:64, hc], True, False, ["nb", "loraup"], ["mi"])
                        mm(reg, onesrow[:], w0hi[0:1, hc], False, False, ["onesrow", "w0hi"], ["mi"])
                        mm(reg, onesrow[:], w0lo[0:1, hc], False, True, ["onesrow", "w0lo"], ["mi"])
                    actf(sgtok[:].rearrange("p c f -> p (c f)"), mi[:], AF.Sigmoid, ["mi"], ["sgtok"])
                    actf(aT, oa[0][:], AF.Sigmoid, ["oa0", "pk"], ["kraw"], bias=pcol("a0", hp))
                    stop_at(41)
                    lerp_chunk(4 * hp + 0, rT, "qn")
                    lerp_chunk(4 * hp + 1, kf, "acc")
                    lerp_chunk(4 * hp + 2, vT, "qr")
                    lerp_chunk(4 * hp + 3, gT, "qr")
                    actf(Etmp, gT, AF.Sigmoid, ["qr"], ["hb0"])
                    tt("dve", gT, gT, Etmp, ALU.mult, ["qr", "hb0"], ["qr"])
                    for c in range(4):
                        mm(sc[0][:, c * 128:(c + 1) * 128], sgtok[:, c, :], maskUI2[:, 0, :], True, True, ["sgtok", "maskUI2"], ["sc0"])
                    for c in range(4):
                        mm(sc[1][:, c * 128:(c + 1) * 128], sgtok[:, c, :], maskU2[:, 0, :], True, True, ["sgtok", "maskU2"], ["sc1"])
                    stop_at(42)
                    ts("dve", coef[:], kf, pcol("kk", hp), None, ALU.mult, None, ["acc", "pk"], ["coef"])
                    actf(Esq, coef[:], AF.Square, ["coef"], ["hb0"])
                    mm(oa[1][:], blk1[:], Esq, True, True, ["blk1", "hb0"], ["oa1"])
                    ts("dve", yraw, oa[1][:], 1e-24, None, ALU.max, None, ["oa1"], ["acc/1"])
                    actf(yraw, yraw, AF.Ln, ["acc/1"], ["acc/1"])
                    actf(yraw, yraw, AF.Exp, ["acc/1"], ["acc/1"], scale=-0.5)
                    tt("dve", kkn, coef[:], yraw, ALU.mult, ["coef", "acc/1"], ["pT1"])
                    ts("dve", coef[:], aT, pcol("ka", hp), ka1m[:, hp:hp + 1], ALU.mult, ALU.add, ["kraw", "pk", "ka1m"], ["coef"])
                    tt("dve", kmT, kf, coef[:], ALU.mult, ["acc", "coef"], ["qn"])
                    tt("pool", bT, kkn, aT, ALU.mult, ["pT1", "kraw"], ["gn"])
                    if g == 0 and hp == 0:
                        dbg_out("rT", rT, "qn"); dbg_out("kmT", kmT, "qn"); dbg_out("vT", vT, "qr"); dbg_out("aT", aT, "kraw"); dbg_out("kkn", kkn, "pT1"); dbg_out("bT", bT, "gn")
                        copy("dve", coef[:], sc[0][:], ["sc0"], ["coef"]); dbg_out("cumI", coef[:], "coef")
                        copy("dve", coef[:], sc[1][:], ["sc1"], ["coef"]); dbg_out("cumE", coef[:], "coef")
                    stop_at(43)
                    actf(Etmp, sc[1][:], AF.Exp, ["sc1"], ["hb0"], scale=-C0)
                    stt("dve", At, Etmp, -1.0, kkn, ALU.mult, ALU.mult, ["hb0", "pT1"], ["gn"])
                    actf(Etmp, sc[0][:], AF.Exp, ["sc0"], ["hb0"], scale=-C0)
                    tt("dve", Rt, rT, Etmp, ALU.mult, ["qn", "hb0"], ["qm"])
                    actf(Esq, sc[0][:], AF.Exp, ["sc0"], ["hb0"], scale=C0)
                    tt("dve", Bt, bT, Esq, ALU.mult, ["gn", "hb0"], ["qm"])
                    tt("pool", Kt, kmT, Esq, ALU.mult, ["qn", "hb0"], ["gm"])
                    ts("dve", ncL[:], sc[0][:, 127:G:128], -C0, None, ALU.mult, None, ["sc0"], ["ncL"])
                    actf(WL[:], ncL[:], AF.Exp, ["ncL"], ["WL"])
                    copy("dve", WL0[:, 0:4], WL[0:64, :], ["WL"], ["WL0"])
                    copy("dve", WL0[:, 4:8], WL[64:128, :], ["WL"], ["WL0"])
                    for c in range(4):
                        actf(Etmp[:, c * 128:(c + 1) * 128], sc[0][:, c * 128:(c + 1) * 128], AF.Exp, ["sc0", "ncL"], ["hb0"], bias=ncL[:, c:c + 1], scale=C0)
                    tt("dve", Bb, bT, Etmp, ALU.mult, ["gn", "hb0"], ["gm"])
                    tt("dve", Kb, kmT, Etmp, ALU.mult, ["qn", "hb0"], ["pT0"])
                    stop_at(44)
                    for c in range(4):
                        cs = slice(c * 128, (c + 1) * 128)
                        half = (c % 2) * 512
                        for wi, (src, skey) in enumerate(((At, "gn"), (vT, "qr"), (Bb, "gm"), (Kb, "pT0"))):
                            P.op("pe", lambda e, src=src, cs=cs, half=half, wi=wi: e.transpose(out=tp[:, half + wi * 128:half + (wi + 1) * 128], in_=src[:, cs], identity=ident[:]), [skey, "ident"], ["tp"])
                        copy(ev_eng(), tokall[:, c, :, :], tp[:, half:half + 512].rearrange("p (w f) -> p w f", w=4), ["tp"], ["tokall"])

                    P.phase = "rwkv_chain"
                    stop_at(45)

                    def hs(e):
                        return slice(64 * e, 64 * e + 64)

                    def v4(ap):
                        return ap.rearrange("p (c f) -> p c f", c=4)

                    def bc4(m2):
                        return m2[:, 0:1, :].broadcast_to([128, 4, 128])

                    PmH, PmK = [hb0[:, 0:G], hb0[:, G:2 * G]], ["hb0/0", "hb0/1"]
                    PTH, PTK = [pT[0][:], pT[1][:]], ["pT0", "pT1"]
                    XTH, XTK = [gn[:, 0, :], kraw[:]], ["gn/0", "kraw"]
                    bkP = [(oa[0], "oa0"), (oa[1], "oa1")]
                    bkT = [(pj[0], "pj0"), (pj[1], "pj1")]
                    bkX = [(mi, "mi"), (sc[0], "sc0")]

                    def rg(bank, c):
                        return bank[:, c * 128:(c + 1) * 128]

                    def csl(c):
                        return slice(c * 128, (c + 1) * 128)

                    for c in range(4):
                        for e in range(2):
                            mm(rg(bkP[e][0], c), At[hs(e), csl(c)], Bt[hs(e), csl(c)], True, True, ["gn/1", "qm"], [bkP[e][1]])
                    for c in range(4):
                        for e in range(2):
                            mm(rg(bkT[e][0], c), Bt[hs(e), csl(c)], At[hs(e), csl(c)], True, True, ["gn/1", "qm"], [bkT[e][1]])
                    for e in range(2):
                        tt("dve", v4(PmH[e]), v4(bkP[e][0][:]), bc4(maskL2), ALU.mult, [bkP[e][1], "maskL2"], [PmK[e]])
                        tt("dve", v4(PTH[e]), v4(bkT[e][0][:]), bc4(maskU2), ALU.mult, [bkT[e][1], "maskU2"], [PTK[e]])
                        tt("pool", v4(XTH[e]), v4(PTH[e]), bc4(ident2), ALU.add, [PTK[e], "ident2"], [XTK[e]])
                    for lvl in range(1, 7):
                        for e in range(2):
                            for c in range(4):
                                mm(rg(bkP[e][0], c), v4(PTH[e])[:, c, :], v4(PmH[e])[:, c, :], True, True, [PTK[e], PmK[e]], [bkP[e][1]])
                            if lvl < 6:
                                for c in range(4):
                                    mm(rg(bkT[e][0], c), v4(PmH[e])[:, c, :], v4(PTH[e])[:, c, :], True, True, [PTK[e], PmK[e]], [bkT[e][1]])
                        for e in range(2):
                            copy("act", PmH[e], bkP[e][0][:], [bkP[e][1]], [PmK[e]])
                            if lvl < 6:
                                copy("dve" if e == 0 else "act", PTH[e], bkT[e][0][:], [bkT[e][1]], [PTK[e]])
                        for e in range(2):
                            for c in range(4):
                                mm(rg(bkX[e][0], c), v4(PmH[e])[:, c, :], v4(XTH[e])[:, c, :], True, True, [PmK[e], XTK[e]], [bkX[e][1]])
                        for e in range(2):
                            tt("dve", XTH[e], XTH[e], bkX[e][0][:], ALU.add, [XTK[e], bkX[e][1]], [XTK[e]])

                    stop_at(46)
                    def v8(ap):
                        return ap.rearrange("p (c e f) -> p c e f", c=4, e=2)

                    ArbH, ArbK = [hb0[:, 0:G], hb0[:, G:2 * G]], ["hb0/0", "hb0/1"]
                    ArkH, ArkK = [pT[0][:], pT[1][:]], ["pT0", "pT1"]
                    AakH, AakK = [AakT4[:, 0, :], AakT4[:, 1, :]], ["AakT4/0", "AakT4/1"]
                    Ahat4, AhK = v8(gm[:, 1, :]), "gm/1"
                    Uhat4, UhK = v8(sgtok[:].rearrange("p c f -> p (c f)")), "sgtok"
                    AkV4v = v8(AkV4[:])
                    RhH, RhK = [oab[0:64, :], osq[0:64, :]], ["oab", "osq"]
                    Msb4 = v8(qcat[0:64, 0, :])

                    for c in range(4):
                        for e in range(2):
                            mm(rg(bkP[e][0], c), Kt[hs(e), csl(c)], At[hs(e), csl(c)], True, True, ["gm/0", "gn/1"], [bkP[e][1]])
                    for c in range(4):
                        for e in range(2):
                            mm(rg(bkT[e][0], c), Bt[hs(e), csl(c)], Rt[hs(e), csl(c)], True, True, ["qm"], [bkT[e][1]])
                    for c in range(4):
                        for e in range(2):
                            mm(rg(bkX[e][0], c), Kt[hs(e), csl(c)], Rt[hs(e), csl(c)], True, True, ["gm/0", "qm"], [bkX[e][1]])
                    for dstH, dstK, bk, msk, mkey in ((AakH, AakK, bkP, maskU2, "maskU2"), (ArbH, ArbK, bkT, maskUI2, "maskUI2"), (ArkH, ArkK, bkX, maskUI2, "maskUI2")):
                        tt("dve", v4(dstH[0]), v4(bk[0][0][:]), bc4(msk), ALU.mult, [bk[0][1], mkey], [dstK[0]])
                        copy("act", dstH[1], bk[1][0][:], [bk[1][1]], [dstK[1]])
                        tt("pool", v4(dstH[1]), v4(dstH[1]), bc4(msk), ALU.mult, [dstK[1], mkey], [dstK[1]])
                    for c in range(4):
                        for e in range(2):
                            o = (c * 2 + e) * 64
                            mm(oa[0][:, o:o + 64], v4(AakH[e])[:, c, :], tokall[:, c, 1, hs(e)], True, True, [AakK[e], "tokall"], ["oa0"])
                    copy("act", AkV4[:], oa[0][:], ["oa0"], ["AkV4"])
                    for c in range(4):
                        for e in range(2):
                            o = (c * 2 + e) * 64
                            mm(oa[1][:, o:o + 64], v4(XTH[e])[:, c, :], tokall[:, c, 0, hs(e)], True, True, [XTK[e], "tokall"], ["oa1"])
                    copy("dve", gm[:, 1, :], oa[1][:], ["oa1"], [AhK])
                    for c in range(4):
                        for e in range(2):
                            o = (c * 2 + e) * 64
                            mm(pj[0][:, o:o + 64], v4(XTH[e])[:, c, :], AkV4v[:, c, e, :], True, True, [XTK[e], "AkV4"], ["pj0"])
                    copy("act", sgtok[:].rearrange("p c f -> p (c f)"), pj[0][:], ["pj0"], [UhK])
                    for e in range(2):
                        for c in range(4):
                            reg = rg(bkX[e][0], c)[0:64, :]
                            mm(reg, Ahat4[:, c, e, :], v4(ArbH[e])[:, c, :], True, False, [AhK, ArbK[e]], [bkX[e][1]])
                            mm(reg, ident[hs(e), hs(e)], Rt[hs(e), csl(c)], False, True, ["ident", "qm"], [bkX[e][1]])
                        copy("dve" if e == 0 else "act", RhH[e], bkX[e][0][0:64, :], [bkX[e][1]], [RhK[e]])
                    for c in range(4):
                        for e in range(2):
                            o = (c * 2 + e) * 64
                            mm(pj[1][0:64, o:o + 64], Ahat4[:, c, e, :], tokall[:, c, 2, hs(e)], True, True, [AhK, "tokall"], ["pj1"])
                    copy("act", qcat[0:64, 0, :], pj[1][0:64, :], ["pj1"], ["qcat"])
                    stop_at(47)
                    for c in range(4):
                        cs = slice(c * 128, (c + 1) * 128)

                        def s_src(e):
                            return (STb[:, 2 * hp + e, :], "STb") if c % 2 == 0 else (STn[:, e, :], "STn")

                        def s_dst(e):
                            return (STn[:, e, :], "STn") if c % 2 == 0 else (STb[:, 2 * hp + e, :], "STb")

                        ob_, obk = oa[c % 2], "oa%d" % (c % 2)
                        for e in range(2):
                            reg = ob_[0:64, e * 64:(e + 1) * 64]
                            mm(reg, tokall[:, c, 2, hs(e)], Uhat4[:, c, e, :], True, False, ["tokall", UhK], [obk])
                            mm(reg, tokall[:, c, 3, hs(e)], tokall[:, c, 1, hs(e)], False, False, ["tokall"], [obk])
                            mm(reg, Msb4[:, c, e, :], s_src(e)[0], False, True, ["qcat", s_src(e)[1]], [obk])
                        for e in range(2):
                            stt("dve", s_dst(e)[0], s_src(e)[0], WL0[:, 4 * e + c:4 * e + c + 1], ob_[0:64, e * 64:(e + 1) * 64], ALU.mult, ALU.add, [s_src(e)[1], "WL0", obk], [s_dst(e)[1]])
                        for e in range(2):
                            reg = sc[1][0:64, e * 128:(e + 1) * 128]
                            mm(reg, Uhat4[:, c, e, :], v4(ArbH[e])[:, c, :], True, False, [UhK, ArbK[e]], ["sc1"])
                            mm(reg, tokall[:, c, 1, hs(e)], v4(ArkH[e])[:, c, :], False, False, ["tokall", ArkK[e]], ["sc1"])
                            mm(reg, s_src(e)[0], v4(RhH[e])[:, c, :], False, True, [s_src(e)[1], RhK[e]], ["sc1"])
                        copy("act", yraw[0:64, cs], sc[1][0:64, 0:128], ["sc1"], ["acc"])
                        copy("act", yraw[64:128, cs], sc[1][0:64, 128:256], ["sc1"], ["acc"])

                    if g == 0 and hp == 0:
                        dbg_out("yraw", yraw, "acc")
                    P.phase = "rwkv"
                    stop_at(49)
                    copy("act", Etmp, yraw, ["acc"], ["hb0"])
                    actf(Esq, yraw, AF.Square, ["acc"], ["hb0"])
                    mm(sc[0][:], blkavg[:], Etmp, True, True, ["blkavg", "hb0"], ["sc0"])
                    mm(sc[1][:], blkavg[:], Esq, True, True, ["blkavg", "hb0"], ["sc1"])
                    actf(coef[:], sc[0][:], AF.Square, ["sc0"], ["coef"])
                    tt("dve", coef[:], sc[1][:], coef[:], ALU.subtract, ["sc1", "coef"], ["coef"])
                    ts("dve", coef[:], coef[:], 64e-5, None, ALU.add, None, ["coef"], ["coef"])
                    actf(coef[:], coef[:], AF.Ln, ["coef"], ["coef"])
                    actf(coef[:], coef[:], AF.Exp, ["coef"], ["coef"], scale=-0.5)
                    tt("dve", yraw, yraw, sc[0][:], ALU.subtract, ["acc", "sc0"], ["acc"])
                    tt("dve", yraw, yraw, coef[:], ALU.mult, ["acc", "coef"], ["acc"])
                    ts("dve", yraw, yraw, pcol("lnw", hp), pcol("lnb", hp), ALU.mult, ALU.add, ["acc", "pk"], ["acc"])
                    tt("dve", Etmp, rT, kmT, ALU.mult, ["qn"], ["hb0"])
                    ts("dve", Esq, Etmp, pcol("rk", hp), None, ALU.mult, None, ["hb0", "pk"], ["hb0"])
                    mm(sc[0][:], blk1[:], Esq, True, True, ["blk1", "hb0"], ["sc0"])
                    tt("dve", coef[:], sc[0][:], vT, ALU.mult, ["sc0", "qr"], ["coef"])
                    tt("dve", yraw, yraw, coef[:], ALU.add, ["acc", "coef"], ["acc"])
                    tt("dve", yT[:, hp, :], yraw, gT, ALU.mult, ["acc", "qr"], ["yT"])

                P.phase = "mem"
                stop_at(6)
                for pr in range(2):
                    pb, pkey = project(25 + pr)
                    P.op("act", lambda e, pb=pb, pr=pr: e.activation(out=qm[:, pr, :], in_=pb[:], func=AF.Copy, scale=0.125), [pkey], ["qm"])
                    pb, pkey = project(27 + pr)
                    P.op("act", lambda e, pb=pb, pr=pr: e.activation(out=gm[:, pr, :], in_=pb[:], func=AF.Silu), [pkey], ["gm"])
                stop_at(61)
                for h in range(4):
                    pr, hb_ = h // 2, (h % 2) * 64
                    ob = oa[h % 2]
                    okey = "oa%d" % (h % 2)
                    for mc in range(2):
                        sb_ = sc[mc]
                        mm(sb_[:], memkT[hb_:hb_ + 64, pr, mc * 128:(mc + 1) * 128], qm[hb_:hb_ + 64, pr, :], True, True, ["memkT", "qm"], ["sc%d" % mc])
                        P.op("act", lambda e, sb_=sb_, mc=mc: e.activation(out=pT[mc][:], in_=sb_[:], func=AF.Exp), ["sc%d" % mc], ["pT%d" % mc])
                    stop_at(62 if h == 0 else 66)
                    for mc in range(2):
                        mm(ob[0:65, :], memvA[:, mc, h, :], pT[mc][:], mc == 0, mc == 1, ["memvA", "pT%d" % mc], [okey])
                    stop_at(63 if h == 0 else 67)
                    o, w = PK["memg"]
                    finalize_head(ob, okey, True, hb_, pk[hb_:hb_ + 64, o + pr:o + pr + 1], gm[hb_:hb_ + 64, pr, :], "gm", yT[hb_:hb_ + 64, 6 + pr, :], "yT")


                P.phase = "nsa"
                P.dma("sp", lambda e, t0=t0: e.dma_start(out=ropeC[:], in_=ropeC_d[:, t0:t0 + G]), writes=["ropeC"])
                P.dma("sp", lambda e, t0=t0: e.dma_start(out=ropeS[:], in_=ropeS_d[:, t0:t0 + G]), writes=["ropeS"])

                def rope(src_t, skey, dst_ap, dkey, nr=128):
                    mm(mi[:], pm[:], src_t, True, True, ["pm", skey], ["mi"])
                    P.op("dve", lambda e: e.tensor_tensor(out=rtmp[0:nr, :], in0=mi[0:nr, :], in1=ropeS[0:nr, :], op=ALU.mult), ["mi", "ropeS"], ["coef"])
                    P.op("pool", lambda e: e.tensor_tensor(out=dst_ap, in0=src_t[0:nr, :], in1=ropeC[0:nr, :], op=ALU.mult), [skey, "ropeC"], [dkey])
                    P.op("dve", lambda e: e.tensor_tensor(out=dst_ap, in0=dst_ap, in1=rtmp[0:nr, :], op=ALU.add), [dkey, "coef"], [dkey])

                for pr in range(2):
                    pb, pkey = project(17 + pr)
                    P.op("act", lambda e, pb=pb, pr=pr: e.activation(out=qn[:, pr, :], in_=pb[:], func=AF.Copy, scale=0.125), [pkey], ["qn"])
                    rope(qn[:, pr, :], "qn", qr[:, pr, :], "qr")
                    pb, pkey = project(19 + pr)
                    P.op("act", lambda e, pb=pb, pr=pr: e.activation(out=gn[:, pr, :], in_=pb[:], func=AF.Silu), [pkey], ["gn"])
                copy("dve", kcvc[:, 0:16], kcvc[:, G:G + 16], ["kcvc"], ["kcvc"])
                pb, pkey = project(21)
                copy("act", kcvc[:, 16:16 + G], pb[:], [pkey], ["kcvc"])
                pb, pkey = project(22)
                copy("act", kraw[:], pb[:], [pkey], ["kraw"])
                rope(kraw[:], "kraw", ksT[0:64, t0:t0 + G], "ksT", 64)
                pb, pkey = project(23)
                copy("act", kraw[:], pb[:], [pkey], ["kraw"])
                wo = (g % 2) * G
                rope(kraw[:], "kraw", kwT[0:64, wo:wo + G], "kwT", 64)
                for i in range(4):
                    pbv = pj[i % 2]
                    for k in range(8):
                        mm(pbv[:, 0:128], hT[:, k, i * 128:(i + 1) * 128], wb[:, k, 24 * 128:25 * 128], k == 0, k == 7, ["hT", "wb"], ["pj%d" % (i % 2)])
                    kt = 4 * g + i
                    copy("dve", vsA[:, kt, 0:64], pbv[:, 0:64], ["pj%d" % (i % 2)], ["vsA"])
                    copy("act", vwA[:, kt % 8, 0:64], pbv[:, 64:128], ["pj%d" % (i % 2)], ["vwA"])
                pb, pkey = project(29, 12)
                o_, w_ = PK["gateb"]
                P.op("act", lambda e, pb=pb, o_=o_: e.activation(out=gsig[:], in_=pb[0:12, :], func=AF.Sigmoid, bias=pk[0:12, o_:o_ + 1]), [pkey, "pk"], ["gsig"])

                lo, hi = max(0, 32 * g - 1), 32 * g + 30
                n = hi - lo + 1
                c0 = 16 * lo + 16 - t0
                for kv in range(2):
                    b0 = kv * 64
                    reg = mi[0:64, kv * 64:kv * 64 + n]
                    for l in range(32):
                        mm(reg, w1b[b0:b0 + 64, l, :], kcvc[b0:b0 + 64, c0 + l:c0 + l + 16 * (n - 1) + 1:16], l == 0, l == 31, ["w1b", "kcvc"], ["mi"])
                    dsth = (h1k, h1v)[kv]
                    P.op("act", lambda e, reg=reg, dsth=dsth, kv=kv, n=n: e.activation(out=dsth[:, 0:n], in_=reg, func=AF.Silu, bias=ccst[:, kv:kv + 1]), ["mi", "ccst"], ["h1%d" % kv])
                mm(sc[0][:, 0:n], w2b[:, 0:128], h1k[:, 0:n], True, True, ["w2b", "h10"], ["sc0"])
                copy("dve", kcmpT[:, lo:hi + 1], sc[0][:, 0:n], ["sc0"], ["kcmpT"])
                mm(sc[1][0:n, 0:64], h1v[:, 0:n], w2b[:, 128:192], True, True, ["w2b", "h11"], ["sc1"])
                copy("dve", vstg[0:n, 0:64], sc[1][0:n, 0:64], ["sc1"], ["vstg"])
                i0 = lo
                while i0 <= hi:
                    i1 = min(hi, (i0 // 128) * 128 + 127)
                    P.dma("sp", lambda e, i0=i0, i1=i1, lo=lo: e.dma_start(out=vcmpA[i0 % 128:i1 % 128 + 1, i0 // 128, :], in_=vstg[i0 - lo:i1 - lo + 1, :]), reads=["vstg"], writes=["vcmpA"])
                    i0 = i1 + 1

                def combine_pair(pr, b, first):
                    copy("act", oaug[0:64, :], oa[0][0:64, :], ["oa0"], ["oaug"])
                    copy("dve", oaug[64:128, :], oa[1][0:64, :], ["oa1"], ["oaug"])
                    copy("act", oab[0:1, :], oa[0][64:65, :], ["oa0"], ["oab"])
                    copy("dve", oab[32:33, :], oa[1][64:65, :], ["oa1"], ["oab"])
                    mm(pj[0][:], dsel[:], oab[0:33, :], True, True, ["dsel", "oab"], ["pj0"])
                    mm(pj[1][:], gselp[:, pr * 3 + b, :], gsig[:], True, True, ["gselp", "gsig"], ["pj1"])
                    P.op("dve", lambda e: e.tensor_scalar(out=coef[:], in0=pj[0][:], scalar1=1e-30, scalar2=None, op0=ALU.max), ["pj0"], ["coef"])
                    P.op("act", lambda e: e.activation(out=coef[:], in_=coef[:], func=AF.Ln), ["coef"], ["coef"])
                    P.op("act", lambda e: e.activation(out=coef[:], in_=coef[:], func=AF.Exp, scale=-1.0), ["coef"], ["coef"])
                    P.op("dve", lambda e: e.tensor_tensor(out=coef[:], in0=coef[:], in1=pj[1][:], op=ALU.mult), ["coef", "pj1"], ["coef"])
                    if first:
                        P.op("dve", lambda e: e.tensor_tensor(out=acc[:, pr, :], in0=coef[:], in1=oaug[:], op=ALU.mult), ["coef", "oaug"], ["acc"])
                    else:
                        P.op("dve", lambda e: e.tensor_tensor(out=coef[:], in0=coef[:], in1=oaug[:], op=ALU.mult), ["coef", "oaug"], ["coef"])
                        P.op("dve", lambda e: e.tensor_tensor(out=acc[:, pr, :], in0=acc[:, pr, :], in1=coef[:], op=ALU.add), ["coef", "acc"], ["acc"])

                def pmask(ap, key, base, cm, step):
                    P.op("pool", lambda e: e.affine_select(out=ap, in_=ap, pattern=[[step, G]], compare_op=ALU.is_ge, fill=0.0, base=base, channel_multiplier=cm), [key], [key])

                def pmask(ap, key, base, cm, step):
                    P.op("pool", lambda e: e.affine_select(out=ap, in_=ap, pattern=[[step, G]], compare_op=ALU.is_ge, fill=0.0, base=base, channel_multiplier=cm), [key], [key])

                P.op("pool", lambda e: e.memset(impS[:], 0.0), [], ["impS"])
                ncc = g // 4 + 1
                for h in range(4):
                    pr, hb_ = h // 2, (h % 2) * 64
                    ob, okey = oa[h % 2], "oa%d" % (h % 2)
                    for ci in range(ncc):
                        dl = 2048 * ci - 512 * g
                        sb_, skey = sc[ci % 2], "sc%d" % (ci % 2)
                        mm(sb_[:], kcmpT[hb_:hb_ + 64, ci * 128:(ci + 1) * 128], qn[hb_:hb_ + 64, pr, :], True, True, ["kcmpT", "qn"], [skey])
                        P.op("act", lambda e, sb_=sb_, ci=ci: e.activation(out=pTc[:, ci, :], in_=sb_[:], func=AF.Exp), [skey], ["wmkvb"])
                        if dl >= -2048:
                            pmask(pTc[:, ci, :], "wmkvb", -31 - dl, -16, 1)
                        mm(ob[0:65, :], vcmpA[:, ci, :], pTc[:, ci, :], ci == 0, ci == ncc - 1, ["vcmpA", "wmkvb"], [okey])
                    ibk = [(mi, "mi"), (pj[0], "pj0")]
                    for j in range(4):
                        bnk, bkey = ibk[j // 2]
                        reg = bnk[:, (j % 2) * 256:(j % 2) * 256 + 129]
                        for ci in range(ncc):
                            mm(reg, pTc[:, ci, j * 128:(j + 1) * 128], ovl[:, ci, :], ci == 0, ci == ncc - 1, ["wmkvb", "ovl"], [bkey])
                    P.op("dve", lambda e: e.tensor_scalar(out=m8[:, 0:2], in0=mi[:, 128:385:256], scalar1=1e-30, scalar2=None, op0=ALU.max), ["mi"], ["m8"])
                    P.op("dve", lambda e: e.tensor_scalar(out=m8[:, 2:4], in0=pj[0][:, 128:385:256], scalar1=1e-30, scalar2=None, op0=ALU.max), ["pj0"], ["m8"])
                    P.op("dve", lambda e: e.reciprocal(out=m8[:, 0:4], in_=m8[:, 0:4]), ["m8"], ["m8"])
                    for j in range(4):
                        bnk, bkey = ibk[j // 2]
                        reg = bnk[:, (j % 2) * 256:(j % 2) * 256 + 129]
                        P.op("dve", lambda e, reg=reg, j=j: e.scalar_tensor_tensor(out=impS[:, j, :], in0=reg[:, 0:128], scalar=m8[:, j:j + 1], in1=impS[:, j, :], op0=ALU.mult, op1=ALU.add), [bkey, "m8", "impS"], ["impS"])
                    if h % 2 == 1:
                        combine_pair(h // 2, 0, True)

                for j in range(4):
                    qt = 4 * g + j
                    u0 = 126 - 2 * qt
                    P.op("dve", lambda e, j=j, u0=u0: e.tensor_tensor(out=score[:], in0=impS[:, j, :], in1=addw[:, u0:u0 + 128], op=ALU.add), ["impS", "addw"], ["score"])
                    P.op("dve", lambda e: e.tensor_scalar(out=score[:, 0:1], in0=score[:, 0:1], scalar1=1e4, scalar2=None, op0=ALU.add), ["score"], ["score"])
                    P.op("dve", lambda e: e.max(out=m8[:, 0:8], in_=score[:]), ["score"], ["m8"])
                    P.op("dve", lambda e: e.match_replace(out=stmp[:], in_to_replace=m8[:, 0:8], in_values=score[:], imm_value=-2.0), ["score", "m8"], ["stmp"])
                    P.op("dve", lambda e: e.max(out=m8[:, 8:16], in_=stmp[:]), ["stmp"], ["m8"])
                    P.op("dve", lambda e: e.tensor_reduce(out=m8[:, 0:1], in_=m8[:, 8:16], axis=AX.X, op=ALU.min), ["m8"], ["m8"])
                    P.op("dve", lambda e: e.tensor_scalar(out=stmp[:], in0=score[:], scalar1=m8[:, 0:1], scalar2=None, op0=ALU.is_ge), ["score", "m8"], ["stmp"])
                    P.op("dve", lambda e: e.tensor_scalar(out=nbq[:], in0=stmp[:], scalar1=1.0, scalar2=-NEG, op0=ALU.subtract, op1=ALU.mult), ["stmp"], ["nbq"])
                    P.op("pe", lambda e: e.transpose(out=tp[:, 0:128], in_=nbq[:], identity=ident[:]), ["nbq", "ident"], ["tp"])
                    copy("dve", nb[:, j * 128:(j + 1) * 128], tp[:, 0:128], ["tp"], ["nb"])

                P.phase = "nsa_sel"
                scA = [(sc[0], "sc0"), (sc[1], "sc1")]
                scB = [(pj[0], "pj0"), (pj[1], "pj1")]
                pTA = [(pT[0][:], "pT0"), (pT[1][:], "pT1")]
                pTB = [(AakT4[:, 0, :], "AakT4/0"), (AakT4[:, 1, :], "AakT4/1")]
                nslab = (4 * g + 3) // 32 + 1
                for pr in range(2):
                    for hl in range(2):
                        for M in range(nslab):
                            copy(("act", "dve")[(hl + M) % 2], qcat[0:64, 2 * hl + M, :], qr[64 * hl:64 * hl + 64, pr, :], ["qr"], ["qcat"])
                            copy(("dve", "pool")[hl], qcat[64:128, 2 * hl + M, :], nb[64 * M:64 * M + 64, :], ["nb"], ["qcat"])
                    nkt = 4 * g + 4

                    def sel_a(kt):
                        (sa, ska), (sb2, skb) = scA[kt % 2], scB[kt % 2]
                        (pa, pka), (pb2, pkb) = pTA[kt % 2], pTB[kt % 2]
                        M = kt // 32
                        ks_ = slice(kt * 128, (kt + 1) * 128)
                        mm(sa[:], ksT[:, ks_], qcat[:, M, :], True, True, ["ksT", "qcat"], [ska])
                        mm(sb2[:], ksT[:, ks_], qcat[:, 2 + M, :], True, True, ["ksT", "qcat"], [skb])
                        P.op("act", lambda e: e.activation(out=pa, in_=sa[:], func=AF.Exp), [ska], [pka])
                        P.op("act", lambda e: e.activation(out=pb2, in_=sb2[:], func=AF.Exp), [skb], [pkb])
                        if kt >= 4 * g:
                            pmask(pa, pka, -128 * (kt - 4 * g), -1, 1)
                            pmask(pb2, pkb, -128 * (kt - 4 * g), -1, 1)

                    sel_a(0)
                    for kt in range(nkt):
                        if kt + 1 < nkt:
                            sel_a(kt + 1)
                        mm(oa[0][0:65, :], vsA[:, kt, :], pTA[kt % 2][0], kt == 0, kt == nkt - 1, ["vsA", pTA[kt % 2][1]], ["oa0"])
                        mm(oa[1][0:65, :], vsA[:, kt, :], pTB[kt % 2][0], kt == 0, kt == nkt - 1, ["vsA", pTB[kt % 2][1]], ["oa1"])
                    combine_pair(pr, 1, False)

                    P.phase = "nsa_win"
                    kts = [kt for kt in range(4 * g - 4, 4 * g + 4) if kt >= 0]

                    def win_a(kt):
                        (sa, ska), (sb2, skb) = scA[kt % 2], scB[kt % 2]
                        (pa, pka), (pb2, pkb) = pTA[kt % 2], pTB[kt % 2]
                        ro = (kt % 8) * 128
                        mm(sa[:], kwT[:, ro:ro + 128], qcat[:, 0, :], True, True, ["kwT", "qcat"], [ska])
                        mm(sb2[:], kwT[:, ro:ro + 128], qcat[:, 2, :], True, True, ["kwT", "qcat"], [skb])
                        P.op("act", lambda e: e.activation(out=pa, in_=sa[:], func=AF.Exp), [ska], [pka])
                        P.op("act", lambda e: e.activation(out=pb2, in_=sb2[:], func=AF.Exp), [skb], [pkb])
                        rel = kt - 4 * g
                        for p_, k_ in ((pa, pka), (pb2, pkb)):
                            if rel >= 0:
                                pmask(p_, k_, -128 * rel, -1, 1)
                            else:
                                pmask(p_, k_, 511 + 128 * rel, 1, -1)

                    win_a(kts[0])
                    for ii, kt in enumerate(kts):
                        if ii + 1 < len(kts):
                            win_a(kts[ii + 1])
                        mm(oa[0][0:65, :], vwA[:, kt % 8, :], pTA[kt % 2][0], ii == 0, ii == len(kts) - 1, ["vwA", pTA[kt % 2][1]], ["oa0"])
                        mm(oa[1][0:65, :], vwA[:, kt % 8, :], pTB[kt % 2][0], ii == 0, ii == len(kts) - 1, ["vwA", pTB[kt % 2][1]], ["oa1"])
                    combine_pair(pr, 2, False)
                    P.phase = "nsa_sel"

                P.phase = "nsa"
                o_, w_ = PK["nsag"]
                for pr in range(2):
                    src = acc[:, pr, :]
                    P.op("act", lambda e, src=src: e.activation(out=hb0[:, 0:G], in_=src, func=AF.Square), ["acc"], ["hb0"])
                    mm(mi[:], blkavg[:], hb0[:, 0:G], True, True, ["blkavg", "hb0"], ["mi"])
                    P.op("act", lambda e: e.activation(out=coef[:], in_=mi[:], func=AF.Ln, bias=epsc[:]), ["mi", "epsc"], ["coef"])
                    P.op("act", lambda e: e.activation(out=coef[:], in_=coef[:], func=AF.Exp, scale=-0.5), ["coef"], ["coef"])
                    P.op("dve", lambda e, src=src, pr=pr, o_=o_: e.scalar_tensor_tensor(out=coef[:], in0=coef[:], scalar=pk[:, o_ + pr:o_ + pr + 1], in1=src, op0=ALU.mult, op1=ALU.mult), ["coef", "acc", "pk"], ["coef"])
                    P.op("dve", lambda e, pr=pr: e.tensor_tensor(out=yT[:, 4 + pr, :], in0=coef[:], in1=gn[:, pr, :], op=ALU.mult), ["coef", "gn"], ["yT"])
                P.phase = ""
                stop_at(7)
                for i in range(4):
                    xb, xk = xt[i % 2], "xt%d" % (i % 2)
                    P.dma("sp", lambda e, i=i, t0=t0, xb=xb: e.dma_start(out=xb[:], in_=x_d[t0 + i * 128:t0 + (i + 1) * 128, :]), writes=[xk])
                    for half in range(2):
                        pb = pj[half]
                        for k in range(8):
                            mm(pb[:], yT[:, k, i * 128:(i + 1) * 128], woutb[:, k, half * 512:(half + 1) * 512], k == 0, k == 7, ["yT", "woutb"], ["pj%d" % half])
                        P.op("dve", lambda e, pb=pb, xb=xb, half=half: e.tensor_tensor(out=xb[:, half * 512:(half + 1) * 512], in0=xb[:, half * 512:(half + 1) * 512], in1=pb[:], op=ALU.add),
                             ["pj%d" % half, xk], [xk])
                    sk = "st%d" % (8 + i)
                    ssq = st[:, 8 + i:9 + i]
                    hjk = hb[i % 2]
                    P.op("act", lambda e, xb=xb, ssq=ssq, hjk=hjk: e.activation(out=hjk[:], in_=xb[:], func=AF.Square, accum_out=ssq), [xk], ["hb0", sk])
                    P.op("dve", lambda e, ssq=ssq: e.tensor_scalar(out=ssq, in0=ssq, scalar1=1.0 / D, scalar2=1e-6, op0=ALU.mult, op1=ALU.add), [sk], [sk])
                    P.op("dve", lambda e, ssq=ssq: e.reciprocal(out=ssq, in_=ssq), [sk], [sk])
                    P.op("act", lambda e, ssq=ssq: e.activation(out=ssq, in_=ssq, func=AF.Sqrt), [sk], [sk])
                    P.op("dve", lambda e, xb=xb, ssq=ssq: e.scalar_tensor_tensor(out=xb[:], in0=xb[:], scalar=ssq, in1=gfin[:], op0=ALU.mult, op1=ALU.mult), [xk, sk, "gfin"], [xk])
                    P.dma("pool", lambda e, i=i, t0=t0, xb=xb: e.dma_start(out=out_d[t0 + i * 128:t0 + (i + 1) * 128, :], in_=xb[:]), reads=[xk], is_out=True)
        except _Stop:
            P.dma("pool", lambda e: e.dma_start(out=out_d[0:128, :], in_=xt[0][:]), reads=["xt0"], is_out=True)
        P.finish("sp")
        P.emit(block, sems, dsems)
    return nc


def make_in_maps(inputs, T, batches):
    ci = _colidx()
    w_in = np.asarray(inputs["w_in"][0])
    wcat = np.ascontiguousarray(w_in[:, ci].reshape(8, 128, NCOL))
    wout = np.ascontiguousarray(np.asarray(inputs["w_out"][0]).reshape(8, 128, D))
    wmkv = np.ascontiguousarray(np.asarray(inputs["w_mem_kv"][0]).reshape(8, 128, 512))
    pk = host_params({k: np.asarray(v) for k, v in inputs.items()})
    w1k = np.asarray(inputs["nsa_cmp_k_w1"][0]).reshape(32, 64, 64).transpose(1, 0, 2)
    w1v = np.asarray(inputs["nsa_cmp_v_w1"][0]).reshape(32, 64, 64).transpose(1, 0, 2)
    w1 = np.ascontiguousarray(np.concatenate([w1k, w1v], 0).reshape(128, 2048))
    w2k = np.asarray(inputs["nsa_cmp_k_w2"][0]); w2v = np.asarray(inputs["nsa_cmp_v_w2"][0])
    w2 = np.zeros((128, 192), np.float32)
    w2[0:64] = np.concatenate([w2k, w2k, w2v], 1)
    lora = np.ascontiguousarray(np.concatenate([np.asarray(inputs["rwkv_w_up"][0]), np.asarray(inputs["rwkv_a_up"][0])], 0))
    w0row = np.ascontiguousarray(np.asarray(inputs["rwkv_w0"][0]).reshape(1, 512))
    consts = host_consts(T)
    maps = []
    for b in batches:
        m = {
            "x": np.ascontiguousarray(np.asarray(inputs["x"][b][:T])),
            "mem": np.ascontiguousarray(np.asarray(inputs["mem"][b])),
            "wcat": wcat, "wout": wout, "wmkv": wmkv, "pk": pk, "w1": w1, "w2": w2, "lora": lora, "w0row": w0row,
            "gfin": np.ascontiguousarray(np.asarray(inputs["norm_final_g"]).reshape(1, D)),
        }
        m.update(consts)
        maps.append(m)
    return maps


_NC_CACHE = {}


def kernel(**inputs):
    T = inputs["x"].shape[1]
    B = inputs["x"].shape[0]
    if T not in _NC_CACHE:
        _NC_CACHE[T] = build_nc(T)
    nc = _NC_CACHE[T]
    batches = [i % B for i in range(8)]
    maps = make_in_maps(inputs, T, batches)
    res = run_bass_kernel_spmd(nc, maps, core_ids=list(range(8)))
    out = np.stack([res.results[b]["out"] for b in range(B)], axis=0)
    return out.astype(np.float32)
```

```python
import numpy as np
import ml_dtypes
from contextlib import ExitStack
import concourse.bass as bass
import concourse.mybir as mybir
from concourse.bass_utils import run_bass_kernel_spmd

F32 = mybir.dt.float32
BF16 = mybir.dt.bfloat16
ALU = mybir.AluOpType
AF = mybir.ActivationFunctionType
AX = mybir.AxisListType

ENGS = ("pe", "act", "dve", "pool", "sp")
N_DMA_SEMS = 24
import os as _os0
SAME_ENG_SYNC = _os0.environ.get("KSAME", "1") == "1"
NEG = -30000.0

D = 1024
NCOL = 29 * 128 + 12
G = 512
PSUM_PREFIX = ("pj", "sc", "oa", "mi", "tp")


class Prog:
    def __init__(self, nc):
        self.nc = nc
        self.lists = {e: [] for e in ENGS}
        self.cnt = {e: 0 for e in ENGS}
        self.seen = {e: {} for e in ENGS}
        self.clock_at = {e: [None] for e in ENGS}
        self.buf = {}
        self.dma_val = [0] * N_DMA_SEMS
        self.dma_clock = [[] for _ in range(N_DMA_SEMS)]
        self.dma_rr = 0
        self.out_toks = []
        self.phase = ""
        self.skip = set()
        self.children = {}

    def _deps(self, eng, reads, writes):
        need = {}

        def add(tok):
            if tok is None:
                return
            src, val = tok
            if src == eng and (eng == "pe" or not SAME_ENG_SYNC):
                return
            if need.get(src, 0) < val:
                need[src] = val

        def related(k):
            ks = [k]
            if "/" in k:
                ks.append(k.split("/")[0])
            else:
                ks.extend(self.children.get(k, ()))
            return ks

        for k0 in reads:
            for k in related(k0):
                st = self.buf.get(k)
                if st:
                    add(st[0])
                    if k[:2] in PSUM_PREFIX:
                        for r in st[1]:
                            if r[0] != eng:
                                add(r)
        for k0 in writes:
            for k in related(k0):
                st = self.buf.get(k)
                if st:
                    add(st[0])
                    for r in st[1]:
                        add(r)
        seen = self.seen[eng]
        waits = []
        for src, val in need.items():
            if seen.get(src, 0) >= val:
                continue
            waits.append((src, val))
            if isinstance(src, str):
                snap = self.clock_at[src][val]
            else:
                snap = self.dma_clock[src[1]][val // 16 - 1]
            for s2, v2 in snap.items():
                if seen.get(s2, 0) < v2:
                    seen[s2] = v2
            seen[src] = val
        return waits

    def _mark(self, tok, reads, writes):
        for k in list(reads) + list(writes):
            if "/" in k:
                self.children.setdefault(k.split("/")[0], set()).add(k)
        for k in reads:
            st = self.buf.setdefault(k, [None, []])
            st[1].append(tok)
            if len(st[1]) > 64:
                st[1] = st[1][-64:]
        for k in writes:
            self.buf[k] = [tok, []]
            if "/" not in k:
                for ch in self.children.get(k, ()):
                    self.buf[ch] = [tok, []]

    def op(self, eng, fn, reads=(), writes=(), self_wait=None):
        if self.phase in self.skip:
            return
        waits = self._deps(eng, reads, writes)
        if self_wait is not None and self.seen[eng].get(eng, 0) < self_wait:
            waits.append((eng, self_wait))
            self.seen[eng][eng] = self_wait
        self.cnt[eng] += 1
        c = self.cnt[eng]
        snap = dict(self.seen[eng])
        snap[eng] = c
        self.clock_at[eng].append(snap)
        self.lists[eng].append(("op", fn, waits, None))
        self._mark((eng, c), reads, writes)

    def dma(self, eng, fn, reads=(), writes=(), is_out=False):
        if self.phase in self.skip:
            return None
        s = self.dma_rr
        self.dma_rr = (self.dma_rr + 1) % N_DMA_SEMS
        waits = self._deps(eng, reads, writes)
        src = ("dma", s)
        prev = self.dma_val[s]
        if prev and self.seen[eng].get(src, 0) < prev:
            waits.append((src, prev))
            self.seen[eng][src] = prev
        val = prev + 16
        self.dma_val[s] = val
        self.dma_clock[s].append(dict(self.seen[eng]))
        self.lists[eng].append(("dma", fn, waits, (s, val)))
        self._mark((src, val), reads, writes)
        if is_out:
            self.out_toks.append((src, val))
        return (src, val)

    def finish(self, eng="sp"):
        waits = []
        for s in range(N_DMA_SEMS):
            if self.dma_val[s]:
                waits.append((("dma", s), self.dma_val[s]))
        self.lists[eng].append(("wait", None, waits, None))

    def emit(self, block, sems, dsems):
        engobj = {"pe": "tensor", "act": "scalar", "dve": "vector", "pool": "gpsimd", "sp": "sync"}

        def semof(src):
            return sems[src] if isinstance(src, str) else dsems[src[1]]

        def make(ename):
            lst = self.lists[ename]

            def body(e):
                for kind, fn, waits, extra in lst:
                    for src, val in waits:
                        e.wait_ge(semof(src), val)
                    if kind == "op":
                        fn(e).then_inc(sems[ename], 1)
                    elif kind == "dma":
                        fn(e).then_inc(dsems[extra[0]], 16)

            return body

        for ename in ENGS:
            if self.lists[ename]:
                getattr(block, engobj[ename])(make(ename))


RW, NSAB, MEMB = 0, 2176, 3084


def _colidx():
    idx = []
    for hp in range(4):
        for base in (0, 512, 1024, 1536):
            idx += list(range(base + 128 * hp, base + 128 * hp + 128))
    idx += list(range(2048, 2176))
    q0, g0 = NSAB, NSAB + 256
    idx += list(range(q0, q0 + 256))
    idx += list(range(g0, g0 + 256))
    kc, vc, ks, vs, kw, vw = (NSAB + 524 + 64 * i for i in range(6))
    idx += list(range(kc, kc + 64)) + list(range(vc, vc + 64))
    idx += list(range(ks, ks + 64)) * 2
    idx += list(range(kw, kw + 64)) * 2
    idx += list(range(vs, vs + 64)) + list(range(vw, vw + 64))
    idx += list(range(MEMB, MEMB + 512))
    idx += list(range(NSAB + 512, NSAB + 524))
    assert len(idx) == NCOL
    return np.array(idx)


def _bf(a):
    return np.ascontiguousarray(a.astype(ml_dtypes.bfloat16))


def host_consts(T):
    c = {}
    c["ident"] = _bf(np.eye(128, dtype=np.float32))
    half = 8
    inv = (np.float32(500000.0) ** (-np.arange(half, dtype=np.float32) / np.float32(half))).astype(np.float32)
    ang = np.arange(T, dtype=np.float32)[None, :] * inv[:, None]
    C = np.ones((64, T), np.float32)
    S = np.zeros((64, T), np.float32)
    C[0:8] = np.cos(ang); C[8:16] = np.cos(ang)
    S[0:8] = -np.sin(ang); S[8:16] = np.sin(ang)
    c["ropeC"] = np.ascontiguousarray(np.concatenate([C, C], 0))
    c["ropeS"] = np.ascontiguousarray(np.concatenate([S, S], 0))
    pm = np.zeros((128, 128), np.float32)
    for hb in (0, 64):
        for d in range(8):
            pm[hb + d + 8, hb + d] = 1.0
            pm[hb + d, hb + d + 8] = 1.0
    c["pm"] = _bf(pm)
    ovl = np.zeros((512, 129), np.float32)
    for i in range(511):
        for sblk in range(128):
            o = min(16 * i + 32, 64 * sblk + 64) - max(16 * i, 64 * sblk)
            if o > 0:
                ovl[i, sblk] = o / 32.0
    ovl[:, 128] = 1.0
    c["ovl"] = _bf(ovl.reshape(4, 128, 129).transpose(1, 0, 2))
    addw = np.zeros((128, 256), np.float32)
    for pp in range(128):
        cur = 1 if pp >= 64 else 0
        for u in range(256):
            sp = u - 126
            valid = sp <= cur
            forced = sp in (cur, cur - 1)
            addw[pp, u] = (0.0 if valid else -1.0) + (1e4 if forced else 0.0)
    c["addw"] = addw
    selpat = np.zeros((64, 32, 128), np.float32)
    for j in range(32):
        selpat[2 * j, j, 0:64] = 1.0
        selpat[2 * j + 1, j, 64:128] = 1.0
    c["selpat"] = _bf(selpat.reshape(64, 4096))
    gselp = np.zeros((12, 6, 128), np.float32)
    for pr_ in range(2):
        for b_ in range(3):
            gselp[3 * (2 * pr_) + b_, pr_ * 3 + b_, 0:64] = 1.0
            gselp[3 * (2 * pr_ + 1) + b_, pr_ * 3 + b_, 64:128] = 1.0
    c["gselp"] = _bf(gselp)
    dsel = np.zeros((33, 128), np.float32)
    dsel[0, 0:64] = 1.0
    dsel[32, 64:128] = 1.0
    c["dsel"] = _bf(dsel)
    pp = np.arange(128)[:, None]
    ff = np.arange(128)[None, :]
    for nm, m in (("maskL2", pp > ff), ("maskU2", pp < ff), ("maskUI2", pp <= ff), ("ident2", pp == ff)):
        m = m.astype(np.float32)
        c[nm] = _bf(np.stack([m, m], 1))
    blk = (pp // 64 == ff // 64).astype(np.float32)
    c["blk1"] = _bf(blk)
    c["blkavg"] = _bf(blk / 64.0)
    return c


PK = {}
_o = 0
for _n, _w in (("gin", 8), ("gmem", 8), ("mu", 17), ("memg", 2), ("nsag", 2), ("gateb", 1), ("posT", 32), ("w0", 4), ("a0", 4), ("kk", 4), ("ka", 4), ("rk", 4), ("lnw", 4), ("lnb", 4)):
    PK[_n] = (_o, _w)
    _o += _w
NPK = _o


def host_params(inp):
    pk = np.zeros((128, NPK), np.float32)

    def put(name, arr):
        o, w = PK[name]
        pk[:, o:o + w] = arr

    put("gin", inp["norm_in_g"][0].reshape(8, 128).T)
    put("gmem", inp["mem_norm_g"][0].reshape(8, 128).T)
    ci = _colidx()
    put("mu", inp["rwkv_mu"][0][ci[:17 * 128]].reshape(17, 128).T)
    put("memg", inp["mem_out_g"][0].reshape(2, 128).T)
    put("nsag", inp["nsa_out_g"][0].reshape(2, 128).T)
    gb = np.zeros((128, 1), np.float32)
    gb[0:12, 0] = inp["nsa_gate_b"][0]
    put("gateb", gb)
    pt = inp["nsa_cmp_pos"][0].T
    put("posT", np.concatenate([pt, pt], 0))
    for nm, key in (("w0", "rwkv_w0"), ("a0", "rwkv_a0"), ("kk", "rwkv_k_k"), ("ka", "rwkv_k_a"), ("rk", "rwkv_r_k"), ("lnw", "rwkv_ln_w"), ("lnb", "rwkv_ln_b")):
        put(nm, inp[key][0].reshape(4, 128).T)
    return pk


def build_nc(T, dbg=None):
    NG = T // G
    NT = T // 128
    nc = bass.Bass("TRN2", target_bir_lowering=False)
    dram = {}

    def din(name, shape, dt=F32):
        dram[name] = nc.dram_tensor(name, list(shape), dt, kind="ExternalInput").ap()
        return dram[name]

    x_d = din("x", [T, D])
    mem_d = din("mem", [256, D])
    wcat_d = din("wcat", [8, 128, NCOL])
    wout_d = din("wout", [8, 128, D])
    wmkv_d = din("wmkv", [8, 128, 512])
    pk_d = din("pk", [128, NPK])
    gfin_d = din("gfin", [1, D])
    ident_d = din("ident", [128, 128], BF16)
    ropeC_d = din("ropeC", [128, T])
    ropeS_d = din("ropeS", [128, T])
    pm_d = din("pm", [128, 128], BF16)
    ovl_d = din("ovl", [128, 4, 129], BF16)
    addw_d = din("addw", [128, 256])
    selpat_d = din("selpat", [64, 4096], BF16)
    gselp_d = din("gselp", [12, 6, 128], BF16)
    dsel_d = din("dsel", [33, 128], BF16)
    w1_d = din("w1", [128, 2048])
    w2_d = din("w2", [128, 192])
    lora_d = din("lora", [128, 512])
    w0row_d = din("w0row", [1, 512])
    maskL2_d = din("maskL2", [128, 2, 128], BF16)
    maskU2_d = din("maskU2", [128, 2, 128], BF16)
    maskUI2_d = din("maskUI2", [128, 2, 128], BF16)
    ident2_d = din("ident2", [128, 2, 128], BF16)
    blk1_d = din("blk1", [128, 128], BF16)
    blkavg_d = din("blkavg", [128, 128], BF16)
    out_d = nc.dram_tensor("out", [T, D], F32, kind="ExternalOutput").ap()
    dbg_d = {}
    if dbg:
        for n, (shape, dt) in dbg.items():
            dbg_d[n] = nc.dram_tensor("dbg_" + n, list(shape), dt, kind="ExternalOutput").ap()

    with ExitStack() as es:
        def sb(name, shape, dt=F32):
            return es.enter_context(nc.sbuf_tensor("sb_" + name, list(shape), dt))

        def ps(name, shape, dt=F32):
            return es.enter_context(nc.psum_tensor("ps_" + name, list(shape), dt))

        wb = sb("wb", [128, 8, NCOL], BF16)
        woutb = sb("woutb", [128, 8, D], BF16)
        wmkvb = sb("wmkvb", [128, 8, 512], BF16)
        pk = sb("pk", [128, NPK])
        gfin = sb("gfin", [128, D])
        ident = sb("ident", [128, 128], BF16)
        xt = [sb("xt%d" % i, [128, D]) for i in range(2)]
        hb0 = sb("hb0", [128, D], BF16)
        hb = [hb0, hb0]
        hT = sb("hT", [128, 8, G], BF16)
        st = sb("st", [128, 16])
        yT = sb("yT", [128, 8, G], BF16)
        memkT = sb("memkT", [128, 2, 256], BF16)
        memvA = sb("memvA", [128, 2, 4, 65], BF16)
        qm = sb("qm", [128, 2, G], BF16)
        gm = sb("gm", [128, 2, G], BF16)
        pT = [sb("pT%d" % i, [128, G], BF16) for i in range(2)]
        oaug = sb("oaug", [128, G], BF16)
        osq = sb("osq", [65, G], BF16)
        cvec = sb("cvec", [65, 128], BF16)
        pm = sb("pm", [128, 128], BF16)
        ovl = sb("ovl", [128, 4, 129], BF16)
        addw = sb("addw", [128, 256])
        qcat = sb("qcat", [128, 4, G], BF16)
        gselp = sb("gselp", [12, 6, 128], BF16)
        dsel = sb("dsel", [33, 128], BF16)
        epsc = sb("epsc", [128, 1])
        e64 = sb("e64", [65, 128], BF16)
        w1b = sb("w1b", [128, 32, 64], BF16)
        w2b = sb("w2b", [64, 192], BF16)
        posTb = sb("posTb", [128, 32], BF16)
        ccst = sb("ccst", [64, 2])
        ropeC = sb("ropeC", [128, G])
        ropeS = sb("ropeS", [128, G])
        qn = sb("qn", [128, 2, G], BF16)
        qr = sb("qr", [128, 2, G], BF16)
        gn = sb("gn", [128, 2, G], BF16)
        kcvc = sb("kcvc", [128, 16 + G], BF16)
        kraw = sb("kraw", [128, G], BF16)
        ksT = sb("ksT", [128, T], BF16)
        kwT = sb("kwT", [128, 1024], BF16)
        vsA = sb("vsA", [128, NT, 65], BF16)
        vwA = sb("vwA", [128, 8, 65], BF16)
        kcmpT = sb("kcmpT", [128, 512], BF16)
        vcmpA = sb("vcmpA", [128, 4, 65], BF16)
        vstg = sb("vstg", [32, 65], BF16)
        h1k = sb("h1k", [64, 32], BF16)
        h1v = sb("h1v", [64, 32], BF16)
        gsig = sb("gsig", [12, G], BF16)
        impS = sb("impS", [128, 4, 128])
        score = sb("score", [128, 128])
        stmp = sb("stmp", [128, 128])
        m8 = sb("m8", [128, 16])
        nbq = sb("nbq", [128, 128], BF16)
        nb = sb("nb", [128, G], BF16)
        oab = sb("oab", [65, G], BF16)
        coef = sb("coef", [128, G])
        scl = coef
        rtmp = coef
        acc = sb("acc", [128, 2, G])
        ltb = [sb("lt%d" % i, [128, G + 2], BF16) for i in range(2)]
        mu1m = sb("mu1m", [128, 17])
        prevcol = sb("prevcol", [128, 17])
        loraup = sb("loraup", [128, 512], BF16)
        w0hi = sb("w0hi", [1, 512], BF16)
        w0lo = sb("w0lo", [1, 512], BF16)
        onesrow = sb("onesrow", [1, 128], BF16)
        ka1m = sb("ka1m", [128, 4])
        maskL2 = sb("maskL2", [128, 2, 128], BF16)
        maskU2 = sb("maskU2", [128, 2, 128], BF16)
        maskUI2 = sb("maskUI2", [128, 2, 128], BF16)
        ident2 = sb("ident2", [128, 2, 128], BF16)
        blk1 = sb("blk1", [128, 128], BF16)
        blkavg = sb("blkavg", [128, 128], BF16)
        sgtok = sb("sgtok", [128, 4, 128], BF16)
        AakT4 = sb("AakT4", [128, 2, G], BF16)
        AkV4 = sb("AkV4", [128, G], BF16)
        STb = sb("STb", [64, 8, 64], BF16)
        STn = sb("STn", [64, 2, 64], BF16)
        ncL = sb("ncL", [128, 4])
        WL = sb("WL", [128, 4])
        WL0 = sb("WL0", [64, 8])
        pj = [ps("pj%d" % i, [128, 512]) for i in range(2)]
        sc = [ps("sc%d" % i, [128, 512]) for i in range(2)]
        oa = [ps("oa%d" % i, [128, 512]) for i in range(2)]
        mi = ps("mi", [128, 512])
        tp = ps("tp", [128, 1024], BF16)

        pTc = wmkvb[:, 0:4, :]
        sems = {e: es.enter_context(nc.semaphore("s_" + e)) for e in ENGS}
        dsems = [es.enter_context(nc.semaphore("d%d" % i)) for i in range(N_DMA_SEMS)]
        block = es.enter_context(nc.Block())
        P = Prog(nc)
        rr = {"ev": 0}

        def ev_eng():
            rr["ev"] ^= 1
            return "act" if rr["ev"] else "dve"

        def copy(eng, out, in_, reads, writes):
            if eng == "act":
                P.op("act", lambda e: e.copy(out=out, in_=in_), reads, writes)
            else:
                P.op(eng, lambda e: e.tensor_copy(out=out, in_=in_), reads, writes)

        pe_last = {}

        def mm(out, lhsT, rhs, start, stop, reads, writes):
            base, rows = lhsT.base_partition(), lhsT.shape[0]
            sw = None
            for k in writes:
                last = pe_last.get(k)
                if last is not None:
                    c0, b0, r0 = last
                    if b0 + r0 <= base or base + rows <= b0:
                        sw = max(sw or 0, c0)
            P.op("pe", lambda e: e.matmul(out, lhsT, rhs, start=start, stop=stop), reads, writes, self_wait=sw)
            for k in writes:
                pe_last[k] = (P.cnt["pe"], base, rows)

        def dbg_out(name, ap, key):
            if dbg and name in dbg_d:
                P.dma("sp", lambda e: e.dma_start(out=dbg_d[name], in_=ap), reads=[key])

        import os as _os
        KSTOP = int(_os.environ.get("KSTOP", "99"))
        KSKIP = _os.environ.get("KSKIP", "")
        P.skip = set(KSKIP.split(",")) if KSKIP else set()

        class _Stop(Exception):
            pass

        def stop_at(n):
            if KSTOP == n:
                raise _Stop()

        try:
            P.dma("sp", lambda e: e.dma_start(out=pk[:], in_=pk_d[:, :]), writes=["pk"])
            P.dma("sp", lambda e: e.dma_start(out=ident[:], in_=ident_d[:, :]), writes=["ident"])
            P.dma("sp", lambda e: e.dma_start(out=gfin[:], in_=gfin_d[0:1, :].broadcast_to([128, D])), writes=["gfin"])

            def pkc(name, j=0, rows=128):
                o, w = PK[name]
                return pk[0:rows, o + j:o + j + 1]

            def load_cast(dst3, src_d, ncols, gname, key):
                i = 0
                for k in range(8):
                    for c0 in range(0, ncols, 1024):
                        cw = min(1024, ncols - c0)
                        stg = xt[i % 2]
                        skey = "xt%d" % (i % 2)
                        P.dma("sp" if i % 2 == 0 else "pool",
                              lambda e, stg=stg, k=k, c0=c0, cw=cw: e.dma_start(out=stg[:, 0:cw], in_=src_d[k, :, c0:c0 + cw]),
                              writes=[skey])
                        eng = ev_eng()
                        o = dst3[:, k, c0:c0 + cw]
                        if gname is None:
                            copy(eng, o, stg[:, 0:cw], [skey], [key])
                        elif eng == "act":
                            P.op("act", lambda e, o=o, stg=stg, cw=cw, k=k: e.activation(out=o, in_=stg[:, 0:cw], func=AF.Copy, scale=pkc(gname, k)),
                                 [skey, "pk"], [key])
                        else:
                            P.op("dve", lambda e, o=o, stg=stg, cw=cw, k=k: e.tensor_scalar(out=o, in0=stg[:, 0:cw], scalar1=pkc(gname, k), scalar2=None, op0=ALU.mult),
                                 [skey, "pk"], [key])
                        i += 1

            stop_at(1)
            load_cast(wmkvb, wmkv_d, 512, "gmem", "wmkvb")
            stop_at(2)
            load_cast(wb, wcat_d, NCOL, "gin", "wb")
            load_cast(woutb, wout_d, D, None, "woutb")

            P.op("pool", lambda e: e.memset(cvec[0:64, :], 1.0 / 64), writes=["cvec"])
            P.op("pool", lambda e: e.memset(cvec[64:65, :], 1e-6), writes=["cvec"])

            def norm_transpose(src_tile, skey, dstT, dkey, col0, slot):
                h = hb[slot]
                hk = "hb0"
                ssq = st[:, slot:slot + 1]
                P.op("act", lambda e: e.activation(out=h[:], in_=src_tile[:], func=AF.Square, accum_out=ssq), [skey], [hk, "st%d" % slot])
                P.op("dve", lambda e: e.tensor_scalar(out=ssq, in0=ssq, scalar1=1.0 / D, scalar2=1e-6, op0=ALU.mult, op1=ALU.add), ["st%d" % slot], ["st%d" % slot])
                P.op("dve", lambda e: e.reciprocal(out=ssq, in_=ssq), ["st%d" % slot], ["st%d" % slot])
                P.op("act", lambda e: e.activation(out=ssq, in_=ssq, func=AF.Sqrt), ["st%d" % slot], ["st%d" % slot])
                P.op("dve", lambda e: e.tensor_scalar(out=h[:], in0=src_tile[:], scalar1=ssq, scalar2=None, op0=ALU.mult), [skey, "st%d" % slot], [hk])
                for k in range(8):
                    P.op("pe", lambda e, k=k: e.transpose(out=tp[:, k * 128:(k + 1) * 128], in_=h[:, k * 128:(k + 1) * 128], identity=ident[:]), [hk, "ident"], ["tp"])
                copy(ev_eng(), dstT[:, :, col0:col0 + 128], tp[:].rearrange("p (k t) -> p k t", k=8), ["tp"], [dkey])


            for nm, dst, src in (("pm", pm, pm_d), ("ovl", ovl, ovl_d), ("addw", addw, addw_d),
                                 ):
                P.dma("sp", lambda e, dst=dst, src=src: e.dma_start(out=dst[:], in_=src), writes=[nm])
            P.dma("sp", lambda e: e.dma_start(out=gselp[:], in_=gselp_d), writes=["gselp"])
            P.dma("sp", lambda e: e.dma_start(out=dsel[:], in_=dsel_d), writes=["dsel"])
            P.op("pool", lambda e: e.memset(epsc[:], 1e-6), writes=["epsc"])
            P.op("pool", lambda e: e.memset(oab[:], 0.0), writes=["oab"])
            P.op("pool", lambda e: e.memset(e64[:], 0.0), writes=["e64"])
            P.op("pool", lambda e: e.memset(e64[64:65, :], 1.0), writes=["e64"])
            P.op("pool", lambda e: e.memset(kcmpT[:], 0.0), writes=["kcmpT"])
            P.op("pool", lambda e: e.memset(vcmpA[:], 0.0), writes=["vcmpA"])
            P.op("pool", lambda e: e.memset(vsA[:], 1.0), writes=["vsA"])
            P.op("pool", lambda e: e.memset(vwA[:], 1.0), writes=["vwA"])
            P.op("pool", lambda e: e.memset(kwT[:], 0.0), writes=["kwT"])
            for c0 in range(0, T, 4096):
                cw = min(4096, T - c0)
                P.dma("sp", lambda e, c0=c0, cw=cw: e.dma_start(out=ksT[64:128, c0:c0 + cw], in_=selpat_d[:, 0:cw]), writes=["ksT"])
            P.op("pool", lambda e: e.memset(vstg[:], 1.0), writes=["vstg"])
            P.op("pool", lambda e: e.memset(kcvc[:], 0.0), writes=["kcvc"])
            for hlf in range(2):
                P.dma("sp", lambda e, hlf=hlf: e.dma_start(out=xt[hlf][:], in_=w1_d[:, hlf * 1024:(hlf + 1) * 1024]), writes=["xt%d" % hlf])
                copy(ev_eng(), w1b[:, hlf * 16:(hlf + 1) * 16, :], xt[hlf][:].rearrange("p (l e) -> p l e", e=64), ["xt%d" % hlf], ["w1b"])
            P.dma("sp", lambda e: e.dma_start(out=xt[0][:, 0:192], in_=w2_d[:, :]), writes=["xt0"])
            copy("dve", w2b[:], xt[0][0:64, 0:192], ["xt0"], ["w2b"])
            o_, w_ = PK["posT"]
            copy("dve", posTb[:], pk[:, o_:o_ + 32], ["pk"], ["posTb"])
            for kv in range(2):
                b0 = kv * 64
                for l in range(32):
                    mm(mi[0:64, kv:kv + 1], w1b[b0:b0 + 64, l, :], posTb[b0:b0 + 64, l:l + 1], l == 0, l == 31, ["w1b", "posTb"], ["mi"])
                copy("dve", ccst[:, kv:kv + 1], mi[0:64, kv:kv + 1], ["mi"], ["ccst"])

            for nm, dst, src in (("maskL2", maskL2, maskL2_d), ("maskU2", maskU2, maskU2_d), ("maskUI2", maskUI2, maskUI2_d),
                                 ("ident2", ident2, ident2_d), ("blk1", blk1, blk1_d), ("blkavg", blkavg, blkavg_d)):
                P.dma("sp", lambda e, dst=dst, src=src: e.dma_start(out=dst[:], in_=src), writes=[nm])
            P.dma("sp", lambda e: e.dma_start(out=xt[1][:, 0:512], in_=lora_d[:, :]), writes=["xt1"])
            copy("dve", loraup[:], xt[1][:, 0:512], ["xt1"], ["loraup"])
            P.dma("sp", lambda e: e.dma_start(out=coef[0:1, 0:512], in_=w0row_d[:, :]), writes=["coef"])
            copy("dve", w0hi[:], coef[0:1, 0:512], ["coef"], ["w0hi"])
            P.op("dve", lambda e: e.tensor_tensor(out=coef[0:1, 0:512], in0=coef[0:1, 0:512], in1=w0hi[:], op=ALU.subtract), ["coef", "w0hi"], ["coef"])
            copy("dve", w0lo[:], coef[0:1, 0:512], ["coef"], ["w0lo"])
            o_, w_ = PK["mu"]
            P.op("dve", lambda e, o_=o_: e.tensor_scalar(out=mu1m[:], in0=pk[:, o_:o_ + 17], scalar1=-1.0, scalar2=1.0, op0=ALU.mult, op1=ALU.add), ["pk"], ["mu1m"])
            P.op("pool", lambda e: e.memset(onesrow[:], 1.0), writes=["onesrow"])
            P.op("pool", lambda e: e.memset(prevcol[:], 0.0), writes=["prevcol"])
            P.op("pool", lambda e: e.memset(STb[:], 0.0), writes=["STb"])
            o_, w_ = PK["ka"]
            P.op("dve", lambda e, o_=o_: e.tensor_scalar(out=ka1m[:], in0=pk[:, o_:o_ + 4], scalar1=-1.0, scalar2=1.0, op0=ALU.mult, op1=ALU.add), ["pk"], ["ka1m"])
            tokall = wmkvb[:, 4:8, :].rearrange("p c (w f) -> p c w f", w=4)
            stop_at(3)
            for mt in range(2):
                P.dma("sp", lambda e, mt=mt: e.dma_start(out=xt[mt][:], in_=mem_d[mt * 128:(mt + 1) * 128, :]), writes=["xt%d" % mt])
                norm_transpose(xt[mt], "xt%d" % mt, hT, "hT", mt * 128, mt)
            stop_at(4)
            for pr in range(2):
                for k in range(8):
                    mm(pj[0][:, 0:256], wmkvb[:, k, pr * 128:(pr + 1) * 128], hT[:, k, 0:256], k == 0, k == 7, ["wmkvb", "hT"], ["pj0"])
                copy(ev_eng(), memkT[:, pr, :], pj[0][:, 0:256], ["pj0"], ["memkT"])
            P.op("pool", lambda e: e.memset(memvA[:], 1.0), writes=["memvA"])
            for mt in range(2):
                for k in range(8):
                    mm(pj[1][:, 0:256], hT[:, k, mt * 128:(mt + 1) * 128], wmkvb[:, k, 256:512], k == 0, k == 7, ["wmkvb", "hT"], ["pj1"])
                copy(ev_eng(), memvA[:, mt, :, 0:64], pj[1][:, 0:256].rearrange("p (h d) -> p h d", h=4), ["pj1"], ["memvA"])

            def finalize_head(o_ps, okey, use_den, hb_, gcol_ap, gate_ap, gate_key, dst_ap, dst_key, from_sbuf=False):
                sl = slice(hb_, hb_ + 64)
                if from_sbuf:
                    copy("dve", oaug[sl, :], o_ps, [okey], ["oaug"])
                    P.op("act", lambda e: e.activation(out=osq[0:64, :], in_=o_ps, func=AF.Square), [okey], ["osq"])
                    P.op("pool", lambda e: e.memset(osq[64:65, :], 1.0), [], ["osq"])
                elif use_den:
                    copy("dve", oaug[sl, :], o_ps[0:64, :], [okey], ["oaug"])
                    P.op("act", lambda e: e.activation(out=osq[:], in_=o_ps[0:65, :], func=AF.Square), [okey], ["osq"])
                else:
                    copy("dve", oaug[sl, :], o_ps[0:64, :], [okey], ["oaug"])
                    P.op("act", lambda e: e.activation(out=osq[0:64, :], in_=o_ps[0:64, :], func=AF.Square), [okey], ["osq"])
                    P.op("pool", lambda e: e.memset(osq[64:65, :], 1.0), [], ["osq"])
                mm(mi[:], cvec[:], osq[:], True, True, ["cvec", "osq"], ["mi"])
                P.op("act", lambda e: e.activation(out=scl[sl, :], in_=mi[sl, :], func=AF.Ln), ["mi"], ["coef"])
                P.op("act", lambda e: e.activation(out=scl[sl, :], in_=scl[sl, :], func=AF.Exp, scale=-0.5), ["coef"], ["coef"])
                P.op("dve", lambda e: e.scalar_tensor_tensor(out=scl[sl, :], in0=scl[sl, :], scalar=gcol_ap, in1=oaug[sl, :], op0=ALU.mult, op1=ALU.mult), ["coef", "oaug", "pk"], ["coef"])
                P.op("dve", lambda e: e.tensor_tensor(out=dst_ap, in0=scl[sl, :], in1=gate_ap, op=ALU.mult), ["coef", gate_key], [dst_key])

            P.op("pool", lambda e: e.memset(wmkvb[:, 4:8, :], 0.0), ["wmkvb"], ["wmkvb", "tokall"])
            stop_at(5)
            for g in range(NG):
                t0 = g * G
                for i in range(4):
                    P.dma("sp", lambda e, i=i, t0=t0: e.dma_start(out=xt[i % 2][:], in_=x_d[t0 + i * 128:t0 + (i + 1) * 128, :]), writes=["xt%d" % (i % 2)])
                    norm_transpose(xt[i % 2], "xt%d" % (i % 2), hT, "hT", i * 128, i % 2)

                pj_rot = {"i": 0}

                def project(chunk, width=128):
                    if P.phase == "rwkv":
                        banks = ((pj[0], "pj0"), (pj[1], "pj1"), (sc[0], "sc0"), (sc[1], "sc1"))
                    elif P.phase in ("nsa", "mem"):
                        banks = ((pj[0], "pj0"), (pj[1], "pj1"), (sc[0], "sc0"), (sc[1], "sc1"))
                    else:
                        banks = ((pj[0], "pj0"), (pj[1], "pj1"))
                    pb, pkey = banks[pj_rot["i"] % len(banks)]
                    pj_rot["i"] += 1
                    for k in range(8):
                        mm(pb[0:width, :], wb[:, k, chunk * 128:chunk * 128 + width], hT[:, k, :], k == 0, k == 7, ["wb", "hT"], [pkey])
                    return pb, pkey


                P.phase = "rwkv"
                C0 = float(np.exp(-0.5))

                def tt(eng, out, in0, in1, op, reads, writes):
                    P.op(eng, lambda e: e.tensor_tensor(out=out, in0=in0, in1=in1, op=op), reads, writes)

                def ts(eng, out, in0, s1, s2, op0, op1, reads, writes):
                    if s2 is None:
                        P.op(eng, lambda e: e.tensor_scalar(out=out, in0=in0, scalar1=s1, scalar2=None, op0=op0), reads, writes)
                    else:
                        P.op(eng, lambda e: e.tensor_scalar(out=out, in0=in0, scalar1=s1, scalar2=s2, op0=op0, op1=op1), reads, writes)

                def stt(eng, out, in0, scalar, in1, op0, op1, reads, writes):
                    P.op(eng, lambda e: e.scalar_tensor_tensor(out=out, in0=in0, scalar=scalar, in1=in1, op0=op0, op1=op1), reads, writes)

                def actf(out, in_, func, reads, writes, bias=None, scale=None):
                    kw = {}
                    if bias is not None:
                        kw["bias"] = bias
                    if scale is not None:
                        kw["scale"] = scale
                    P.op("act", lambda e: e.activation(out=out, in_=in_, func=func, **kw), reads, writes)

                def pcol(name, j):
                    o, w = PK[name]
                    return pk[:, o + j:o + j + 1]

                def lerp_chunk(cidx, dst_ap, dkey):
                    pb, pkey = project(cidx)
                    lt, lk = ltb[cidx % 2], "lt%d" % (cidx % 2)
                    actf(lt[:, 1:G + 1], pb[:], AF.Copy, [pkey, "pk"], [lk], scale=pcol("mu", cidx))
                    copy("dve", lt[:, 0:1], prevcol[:, cidx:cidx + 1], ["prevcol"], [lk])
                    copy("dve", prevcol[:, cidx:cidx + 1], lt[:, G:G + 1], [lk], ["prevcol"])
                    stt("dve", dst_ap, pb[:], mu1m[:, cidx:cidx + 1], lt[:, 0:G], ALU.mult, ALU.add, [pkey, "mu1m", lk], [dkey])

                rT, kmT = qn[:, 0, :], qn[:, 1, :]
                vT, gT = qr[:, 0, :], qr[:, 1, :]
                bT, At = gn[:, 0, :], gn[:, 1, :]
                Rt, Bt = qm[:, 0, :], qm[:, 1, :]
                Kt, Bb = gm[:, 0, :], gm[:, 1, :]
                Kb, kkn = pT[0][:], pT[1][:]
                aT, lin = kraw[:], nb[:]
                kf, yraw = acc[:, 0, :], acc[:, 1, :]
                Etmp, Esq = hb0[:, 0:G], hb0[:, G:2 * G]

                lerp_chunk(16, kf, "acc")
                actf(lin[0:64, :], kf[0:64, :], AF.Tanh, ["acc"], ["nb"])
                copy("dve", lin[64:128, :], kf[64:128, :], ["acc"], ["nb"])

                stop_at(40)
                for hp in range(4):
                    hc = slice(hp * 128, (hp + 1) * 128)
                    mm(oa[0][:], loraup[64:128, hc], lin[64:128, :], True, True, ["nb", "loraup"], ["oa0"])
                    for c in range(4):
                        reg = mi[:, c * 128:(c + 1) * 128]
                        mm(reg, lin[0:64, c * 128:(c + 1) * 128], loraup[0:64, hc], True, False, ["nb", "loraup"], ["mi"])
                        mm(reg, onesrow[:], w0hi[0:1, hc], False, False, ["onesrow", "w0hi"], ["mi"])
                        mm(reg, onesrow[:], w0lo[0:1, hc], False, True, ["onesrow", "w0lo"], ["mi"])
                    actf(sgtok[:].rearrange("p c f -> p (c f)"), mi[:], AF.Sigmoid, ["mi"], ["sgtok"])
                    actf(aT, oa[0][:], AF.Sigmoid, ["oa0", "pk"], ["kraw"], bias=pcol("a0", hp))
                    stop_at(41)
                    lerp_chunk(4 * hp + 0, rT, "qn")
                    lerp_chunk(4 * hp + 1, kf, "acc")
                    lerp_chunk(4 * hp + 2, vT, "qr")
                    lerp_chunk(4 * hp + 3, gT, "qr")
                    actf(Etmp, gT, AF.Sigmoid, ["qr"], ["hb0"])
                    tt("dve", gT, gT, Etmp, ALU.mult, ["qr", "hb0"], ["qr"])
                    for c in range(4):
                        mm(sc[0][:, c * 128:(c + 1) * 128], sgtok[:, c, :], maskUI2[:, 0, :], True, True, ["sgtok", "maskUI2"], ["sc0"])
                    for c in range(4):
                        mm(sc[1][:, c * 128:(c + 1) * 128], sgtok[:, c, :], maskU2[:, 0, :], True, True, ["sgtok", "maskU2"], ["sc1"])
                    stop_at(42)
                    ts("dve", coef[:], kf, pcol("kk", hp), None, ALU.mult, None, ["acc", "pk"], ["coef"])
                    actf(Esq, coef[:], AF.Square, ["coef"], ["hb0"])
                    mm(oa[1][:], blk1[:], Esq, True, True, ["blk1", "hb0"], ["oa1"])
                    ts("dve", yraw, oa[1][:], 1e-24, None, ALU.max, None, ["oa1"], ["acc/1"])
                    actf(yraw, yraw, AF.Ln, ["acc/1"], ["acc/1"])
                    actf(yraw, yraw, AF.Exp, ["acc/1"], ["acc/1"], scale=-0.5)
                    tt("dve", kkn, coef[:], yraw, ALU.mult, ["coef", "acc/1"], ["pT1"])
                    ts("dve", coef[:], aT, pcol("ka", hp), ka1m[:, hp:hp + 1], ALU.mult, ALU.add, ["kraw", "pk", "ka1m"], ["coef"])
                    tt("dve", kmT, kf, coef[:], ALU.mult, ["acc", "coef"], ["qn"])
                    tt("pool", bT, kkn, aT, ALU.mult, ["pT1", "kraw"], ["gn"])
                    if g == 0 and hp == 0:
                        dbg_out("rT", rT, "qn"); dbg_out("kmT", kmT, "qn"); dbg_out("vT", vT, "qr"); dbg_out("aT", aT, "kraw"); dbg_out("kkn", kkn, "pT1"); dbg_out("bT", bT, "gn")
                        copy("dve", coef[:], sc[0][:], ["sc0"], ["coef"]); dbg_out("cumI", coef[:], "coef")
                        copy("dve", coef[:], sc[1][:], ["sc1"], ["coef"]); dbg_out("cumE", coef[:], "coef")
                    stop_at(43)
                    actf(Etmp, sc[1][:], AF.Exp, ["sc1"], ["hb0"], scale=-C0)
                    stt("dve", At, Etmp, -1.0, kkn, ALU.mult, ALU.mult, ["hb0", "pT1"], ["gn"])
                    actf(Etmp, sc[0][:], AF.Exp, ["sc0"], ["hb0"], scale=-C0)
                    tt("dve", Rt, rT, Etmp, ALU.mult, ["qn", "hb0"], ["qm"])
                    actf(Esq, sc[0][:], AF.Exp, ["sc0"], ["hb0"], scale=C0)
                    tt("dve", Bt, bT, Esq, ALU.mult, ["gn", "hb0"], ["qm"])
                    tt("pool", Kt, kmT, Esq, ALU.mult, ["qn", "hb0"], ["gm"])
                    ts("dve", ncL[:], sc[0][:, 127:G:128], -C0, None, ALU.mult, None, ["sc0"], ["ncL"])
                    actf(WL[:], ncL[:], AF.Exp, ["ncL"], ["WL"])
                    copy("dve", WL0[:, 0:4], WL[0:64, :], ["WL"], ["WL0"])
                    copy("dve", WL0[:, 4:8], WL[64:128, :], ["WL"], ["WL0"])
                    for c in range(4):
                        actf(Etmp[:, c * 128:(c + 1) * 128], sc[0][:, c * 128:(c + 1) * 128], AF.Exp, ["sc0", "ncL"], ["hb0"], bias=ncL[:, c:c + 1], scale=C0)
                    tt("dve", Bb, bT, Etmp, ALU.mult, ["gn", "hb0"], ["gm"])
                    tt("dve", Kb, kmT, Etmp, ALU.mult, ["qn", "hb0"], ["pT0"])
                    stop_at(44)
                    tt("dve", Etmp, rT, kmT, ALU.mult, ["qn", "hb0"], ["hb0"])
                    ts("dve", Esq, Etmp, pcol("rk", hp), None, ALU.mult, None, ["hb0", "pk"], ["hb0"])
                    mm(sc[0][:], blk1[:], Esq, True, True, ["blk1", "hb0"], ["sc0"])
                    tt("dve", kf, sc[0][:], vT, ALU.mult, ["sc0", "qr"], ["acc/0"])
                    for c in range(4):
                        cs = slice(c * 128, (c + 1) * 128)
                        half = (c % 2) * 512
                        for wi, (src, skey) in enumerate(((At, "gn"), (vT, "qr"), (Bb, "gm"), (Kb, "pT0"))):
                            P.op("pe", lambda e, src=src, cs=cs, half=half, wi=wi: e.transpose(out=tp[:, half + wi * 128:half + (wi + 1) * 128], in_=src[:, cs], identity=ident[:]), [skey, "ident"], ["tp"])
                        copy(ev_eng(), tokall[:, c, :, :], tp[:, half:half + 512].rearrange("p (w f) -> p w f", w=4), ["tp"], ["tokall"])

                    P.phase = "rwkv_chain"
                    stop_at(45)

                    def hs(e):
                        return slice(64 * e, 64 * e + 64)

                    def v4(ap):
                        return ap.rearrange("p (c f) -> p c f", c=4)

                    def bc4(m2):
                        return m2[:, 0:1, :].broadcast_to([128, 4, 128])

                    PmH, PmK = [hb0[:, 0:G], hb0[:, G:2 * G]], ["hb0/0", "hb0/1"]
                    PTH, PTK = [pT[0][:], pT[1][:]], ["pT0", "pT1"]
                    XTH, XTK = [gn[:, 0, :], kraw[:]], ["gn/0", "kraw"]
                    bkP = [(oa[0], "oa0"), (oa[1], "oa1")]
                    bkT = [(pj[0], "pj0"), (pj[1], "pj1")]
                    bkX = [(mi, "mi"), (sc[0], "sc0")]

                    def rg(bank, c):
                        return bank[:, c * 128:(c + 1) * 128]

                    def csl(c):
                        return slice(c * 128, (c + 1) * 128)

                    for c in range(4):
                        for e in range(2):
                            mm(rg(bkP[e][0], c), At[hs(e), csl(c)], Bt[hs(e), csl(c)], True, True, ["gn/1", "qm"], [bkP[e][1]])
                    for c in range(4):
                        for e in range(2):
                            mm(rg(bkT[e][0], c), Bt[hs(e), csl(c)], At[hs(e), csl(c)], True, True, ["gn/1", "qm"], [bkT[e][1]])
                    for e in range(2):
                        tt("dve", v4(PmH[e]), v4(bkP[e][0][:]), bc4(maskL2), ALU.mult, [bkP[e][1], "maskL2"], [PmK[e]])
                        tt("dve", v4(PTH[e]), v4(bkT[e][0][:]), bc4(maskU2), ALU.mult, [bkT[e][1], "maskU2"], [PTK[e]])
                        tt("pool", v4(XTH[e]), v4(PTH[e]), bc4(ident2), ALU.add, [PTK[e], "ident2"], [XTK[e]])
                    for lvl in range(1, 7):
                        for e in range(2):
                            for c in range(4):
                                mm(rg(bkP[e][0], c), v4(PTH[e])[:, c, :], v4(PmH[e])[:, c, :], True, True, [PTK[e], PmK[e]], [bkP[e][1]])
                            if lvl < 6:
                                for c in range(4):
                                    mm(rg(bkT[e][0], c), v4(PmH[e])[:, c, :], v4(PTH[e])[:, c, :], True, True, [PTK[e], PmK[e]], [bkT[e][1]])
                        for e in range(2):
                            copy("act", PmH[e], bkP[e][0][:], [bkP[e][1]], [PmK[e]])
                            if lvl < 6:
                                copy("dve" if e == 0 else "act", PTH[e], bkT[e][0][:], [bkT[e][1]], [PTK[e]])
                        for e in range(2):
                            for c in range(4):
                                mm(rg(bkX[e][0], c), v4(PmH[e])[:, c, :], v4(XTH[e])[:, c, :], True, True, [PmK[e], XTK[e]], [bkX[e][1]])
                        for e in range(2):
                            tt("dve", XTH[e], XTH[e], bkX[e][0][:], ALU.add, [XTK[e], bkX[e][1]], [XTK[e]])

                    stop_at(46)
                    def v8(ap):
                        return ap.rearrange("p (c e f) -> p c e f", c=4, e=2)

                    ArbH, ArbK = [hb0[:, 0:G], hb0[:, G:2 * G]], ["hb0/0", "hb0/1"]
                    ArkH, ArkK = [pT[0][:], pT[1][:]], ["pT0", "pT1"]
                    AakH, AakK = [AakT4[:, 0, :], AakT4[:, 1, :]], ["AakT4/0", "AakT4/1"]
                    Ahat4, AhK = v8(gm[:, 1, :]), "gm/1"
                    Uhat4, UhK = v8(sgtok[:].rearrange("p c f -> p (c f)")), "sgtok"
                    AkV4v = v8(AkV4[:])
                    RhH, RhK = [oab[0:64, :], osq[0:64, :]], ["oab", "osq"]
                    Msb4 = v8(qcat[0:64, 0, :])

                    for c in range(4):
                        for e in range(2):
                            mm(rg(bkP[e][0], c), Kt[hs(e), csl(c)], At[hs(e), csl(c)], True, True, ["gm/0", "gn/1"], [bkP[e][1]])
                    for c in range(4):
                        for e in range(2):
                            mm(rg(bkT[e][0], c), Bt[hs(e), csl(c)], Rt[hs(e), csl(c)], True, True, ["qm"], [bkT[e][1]])
                    for c in range(4):
                        for e in range(2):
                            mm(rg(bkX[e][0], c), Kt[hs(e), csl(c)], Rt[hs(e), csl(c)], True, True, ["gm/0", "qm"], [bkX[e][1]])
                    for e in range(2):
                        tt("dve", v4(AakH[e]), v4(bkP[e][0][:]), bc4(maskU2), ALU.mult, [bkP[e][1], "maskU2"], [AakK[e]])
                        tt("dve", v4(ArbH[e]), v4(bkT[e][0][:]), bc4(maskUI2), ALU.mult, [bkT[e][1], "maskUI2"], [ArbK[e]])
                        tt("dve", v4(ArkH[e]), v4(bkX[e][0][:]), bc4(maskUI2), ALU.mult, [bkX[e][1], "maskUI2"], [ArkK[e]])
                    for c in range(4):
                        for e in range(2):
                            o = (c * 2 + e) * 64
                            mm(oa[0][:, o:o + 64], v4(AakH[e])[:, c, :], tokall[:, c, 1, hs(e)], True, True, [AakK[e], "tokall"], ["oa0"])
                    copy("act", AkV4[:], oa[0][:], ["oa0"], ["AkV4"])
                    for c in range(4):
                        for e in range(2):
                            o = (c * 2 + e) * 64
                            mm(oa[1][:, o:o + 64], v4(XTH[e])[:, c, :], tokall[:, c, 0, hs(e)], True, True, [XTK[e], "tokall"], ["oa1"])
                    copy("dve", gm[:, 1, :], oa[1][:], ["oa1"], [AhK])
                    for c in range(4):
                        for e in range(2):
                            o = (c * 2 + e) * 64
                            mm(pj[0][:, o:o + 64], v4(XTH[e])[:, c, :], AkV4v[:, c, e, :], True, True, [XTK[e], "AkV4"], ["pj0"])
                    copy("act", sgtok[:].rearrange("p c f -> p (c f)"), pj[0][:], ["pj0"], [UhK])
                    for e in range(2):
                        for c in range(4):
                            reg = rg(bkX[e][0], c)[0:64, :]
                            mm(reg, Ahat4[:, c, e, :], v4(ArbH[e])[:, c, :], True, False, [AhK, ArbK[e]], [bkX[e][1]])
                            mm(reg, ident[hs(e), hs(e)], Rt[hs(e), csl(c)], False, True, ["ident", "qm"], [bkX[e][1]])
                        copy("dve" if e == 0 else "act", RhH[e], bkX[e][0][0:64, :], [bkX[e][1]], [RhK[e]])
                    for c in range(4):
                        for e in range(2):
                            o = (c * 2 + e) * 64
                            mm(pj[1][0:64, o:o + 64], Ahat4[:, c, e, :], tokall[:, c, 2, hs(e)], True, True, [AhK, "tokall"], ["pj1"])
                    copy("act", qcat[0:64, 0, :], pj[1][0:64, :], ["pj1"], ["qcat"])
                    stop_at(47)
                    for c in range(4):
                        cs = slice(c * 128, (c + 1) * 128)

                        def s_src(e):
                            return (STb[:, 2 * hp + e, :], "STb") if c % 2 == 0 else (STn[:, e, :], "STn")

                        def s_dst(e):
                            return (STn[:, e, :], "STn") if c % 2 == 0 else (STb[:, 2 * hp + e, :], "STb")

                        ob_, obk = oa[c % 2], "oa%d" % (c % 2)
                        for e in range(2):
                            reg = ob_[0:64, e * 64:(e + 1) * 64]
                            mm(reg, tokall[:, c, 2, hs(e)], Uhat4[:, c, e, :], True, False, ["tokall", UhK], [obk])
                            mm(reg, tokall[:, c, 3, hs(e)], tokall[:, c, 1, hs(e)], False, False, ["tokall"], [obk])
                            mm(reg, Msb4[:, c, e, :], s_src(e)[0], False, True, ["qcat", s_src(e)[1]], [obk])
                        for e in range(2):
                            stt("dve", s_dst(e)[0], s_src(e)[0], WL0[:, 4 * e + c:4 * e + c + 1], ob_[0:64, e * 64:(e + 1) * 64], ALU.mult, ALU.add, [s_src(e)[1], "WL0", obk], [s_dst(e)[1]])
                        for e in range(2):
                            reg = sc[1][0:64, e * 128:(e + 1) * 128]
                            mm(reg, Uhat4[:, c, e, :], v4(ArbH[e])[:, c, :], True, False, [UhK, ArbK[e]], ["sc1"])
                            mm(reg, tokall[:, c, 1, hs(e)], v4(ArkH[e])[:, c, :], False, False, ["tokall", ArkK[e]], ["sc1"])
                            mm(reg, s_src(e)[0], v4(RhH[e])[:, c, :], False, True, [s_src(e)[1], RhK[e]], ["sc1"])
                        copy("act", yraw[0:64, cs], sc[1][0:64, 0:128], ["sc1"], ["acc"])
                        copy("act", yraw[64:128, cs], sc[1][0:64, 128:256], ["sc1"], ["acc"])

                    if g == 0 and hp == 0:
                        dbg_out("yraw", yraw, "acc")
                    P.phase = "rwkv"
                    stop_at(49)
                    copy("act", Etmp, yraw, ["acc"], ["hb0"])
                    actf(Esq, yraw, AF.Square, ["acc"], ["hb0"])
                    mm(sc[0][:], blkavg[:], Etmp, True, True, ["blkavg", "hb0"], ["sc0"])
                    mm(sc[1][:], blkavg[:], Esq, True, True, ["blkavg", "hb0"], ["sc1"])
                    actf(coef[:], sc[0][:], AF.Square, ["sc0"], ["coef"])
                    tt("dve", coef[:], sc[1][:], coef[:], ALU.subtract, ["sc1", "coef"], ["coef"])
                    ts("dve", coef[:], coef[:], 64e-5, None, ALU.add, None, ["coef"], ["coef"])
                    actf(coef[:], coef[:], AF.Ln, ["coef"], ["coef"])
                    actf(coef[:], coef[:], AF.Exp, ["coef"], ["coef"], scale=-0.5)
                    tt("dve", yraw, yraw, sc[0][:], ALU.subtract, ["acc", "sc0"], ["acc"])
                    tt("dve", yraw, yraw, coef[:], ALU.mult, ["acc", "coef"], ["acc"])
                    ts("dve", yraw, yraw, pcol("lnw", hp), pcol("lnb", hp), ALU.mult, ALU.add, ["acc", "pk"], ["acc"])
                    tt("dve", yraw, yraw, kf, ALU.add, ["acc"], ["acc"])
                    tt("dve", yT[:, hp, :], yraw, gT, ALU.mult, ["acc", "qr"], ["yT"])

                P.phase = "mem"
                stop_at(6)
                for pr in range(2):
                    pb, pkey = project(25 + pr)
                    P.op("act", lambda e, pb=pb, pr=pr: e.activation(out=qm[:, pr, :], in_=pb[:], func=AF.Copy, scale=0.125), [pkey], ["qm"])
                    pb, pkey = project(27 + pr)
                    P.op("act", lambda e, pb=pb, pr=pr: e.activation(out=gm[:, pr, :], in_=pb[:], func=AF.Silu), [pkey], ["gm"])
                stop_at(61)
                for h in range(4):
                    pr, hb_ = h // 2, (h % 2) * 64
                    ob = oa[h % 2]
                    okey = "oa%d" % (h % 2)
                    for mc in range(2):
                        sb_ = sc[mc]
                        mm(sb_[:], memkT[hb_:hb_ + 64, pr, mc * 128:(mc + 1) * 128], qm[hb_:hb_ + 64, pr, :], True, True, ["memkT", "qm"], ["sc%d" % mc])
                        P.op("act", lambda e, sb_=sb_, mc=mc: e.activation(out=pT[mc][:], in_=sb_[:], func=AF.Exp), ["sc%d" % mc], ["pT%d" % mc])
                    stop_at(62 if h == 0 else 66)
                    for mc in range(2):
                        mm(ob[0:65, :], memvA[:, mc, h, :], pT[mc][:], mc == 0, mc == 1, ["memvA", "pT%d" % mc], [okey])
                    stop_at(63 if h == 0 else 67)
                    o, w = PK["memg"]
                    finalize_head(ob, okey, True, hb_, pk[hb_:hb_ + 64, o + pr:o + pr + 1], gm[hb_:hb_ + 64, pr, :], "gm", yT[hb_:hb_ + 64, 6 + pr, :], "yT")


                P.phase = "nsa"
                P.dma("sp", lambda e, t0=t0: e.dma_start(out=ropeC[:], in_=ropeC_d[:, t0:t0 + G]), writes=["ropeC"])
                P.dma("sp", lambda e, t0=t0: e.dma_start(out=ropeS[:], in_=ropeS_d[:, t0:t0 + G]), writes=["ropeS"])

                def rope(src_t, skey, dst_ap, dkey, nr=128):
                    mm(mi[:], pm[:], src_t, True, True, ["pm", skey], ["mi"])
                    P.op("dve", lambda e: e.tensor_tensor(out=rtmp[0:nr, :], in0=mi[0:nr, :], in1=ropeS[0:nr, :], op=ALU.mult), ["mi", "ropeS"], ["coef"])
                    P.op("pool", lambda e: e.tensor_tensor(out=dst_ap, in0=src_t[0:nr, :], in1=ropeC[0:nr, :], op=ALU.mult), [skey, "ropeC"], [dkey])
                    P.op("dve", lambda e: e.tensor_tensor(out=dst_ap, in0=dst_ap, in1=rtmp[0:nr, :], op=ALU.add), [dkey, "coef"], [dkey])

                for pr in range(2):
                    pb, pkey = project(17 + pr)
                    P.op("act", lambda e, pb=pb, pr=pr: e.activation(out=qn[:, pr, :], in_=pb[:], func=AF.Copy, scale=0.125), [pkey], ["qn"])
                    rope(qn[:, pr, :], "qn", qr[:, pr, :], "qr")
                    pb, pkey = project(19 + pr)
                    P.op("act", lambda e, pb=pb, pr=pr: e.activation(out=gn[:, pr, :], in_=pb[:], func=AF.Silu), [pkey], ["gn"])
                copy("dve", kcvc[:, 0:16], kcvc[:, G:G + 16], ["kcvc"], ["kcvc"])
                pb, pkey = project(21)
                copy("act", kcvc[:, 16:16 + G], pb[:], [pkey], ["kcvc"])
                pb, pkey = project(22)
                copy("act", kraw[:], pb[:], [pkey], ["kraw"])
                rope(kraw[:], "kraw", ksT[0:64, t0:t0 + G], "ksT", 64)
                pb, pkey = project(23)
                copy("act", kraw[:], pb[:], [pkey], ["kraw"])
                wo = (g % 2) * G
                rope(kraw[:], "kraw", kwT[0:64, wo:wo + G], "kwT", 64)
                for i in range(4):
                    pbv = pj[i % 2]
                    for k in range(8):
                        mm(pbv[:, 0:128], hT[:, k, i * 128:(i + 1) * 128], wb[:, k, 24 * 128:25 * 128], k == 0, k == 7, ["hT", "wb"], ["pj%d" % (i % 2)])
                    kt = 4 * g + i
                    copy("dve", vsA[:, kt, 0:64], pbv[:, 0:64], ["pj%d" % (i % 2)], ["vsA"])
                    copy("act", vwA[:, kt % 8, 0:64], pbv[:, 64:128], ["pj%d" % (i % 2)], ["vwA"])
                pb, pkey = project(29, 12)
                o_, w_ = PK["gateb"]
                P.op("act", lambda e, pb=pb, o_=o_: e.activation(out=gsig[:], in_=pb[0:12, :], func=AF.Sigmoid, bias=pk[0:12, o_:o_ + 1]), [pkey, "pk"], ["gsig"])

                lo, hi = max(0, 32 * g - 1), 32 * g + 30
                n = hi - lo + 1
                c0 = 16 * lo + 16 - t0
                for kv in range(2):
                    b0 = kv * 64
                    reg = mi[0:64, kv * 64:kv * 64 + n]
                    for l in range(32):
                        mm(reg, w1b[b0:b0 + 64, l, :], kcvc[b0:b0 + 64, c0 + l:c0 + l + 16 * (n - 1) + 1:16], l == 0, l == 31, ["w1b", "kcvc"], ["mi"])
                    dsth = (h1k, h1v)[kv]
                    P.op("act", lambda e, reg=reg, dsth=dsth, kv=kv, n=n: e.activation(out=dsth[:, 0:n], in_=reg, func=AF.Silu, bias=ccst[:, kv:kv + 1]), ["mi", "ccst"], ["h1%d" % kv])
                mm(sc[0][:, 0:n], w2b[:, 0:128], h1k[:, 0:n], True, True, ["w2b", "h10"], ["sc0"])
                copy("dve", kcmpT[:, lo:hi + 1], sc[0][:, 0:n], ["sc0"], ["kcmpT"])
                mm(sc[1][0:n, 0:64], h1v[:, 0:n], w2b[:, 128:192], True, True, ["w2b", "h11"], ["sc1"])
                copy("dve", vstg[0:n, 0:64], sc[1][0:n, 0:64], ["sc1"], ["vstg"])
                i0 = lo
                while i0 <= hi:
                    i1 = min(hi, (i0 // 128) * 128 + 127)
                    P.dma("sp", lambda e, i0=i0, i1=i1, lo=lo: e.dma_start(out=vcmpA[i0 % 128:i1 % 128 + 1, i0 // 128, :], in_=vstg[i0 - lo:i1 - lo + 1, :]), reads=["vstg"], writes=["vcmpA"])
                    i0 = i1 + 1

                def combine_pair(pr, b, first):
                    copy("act", oaug[0:64, :], oa[0][0:64, :], ["oa0"], ["oaug"])
                    copy("dve", oaug[64:128, :], oa[1][0:64, :], ["oa1"], ["oaug"])
                    copy("act", oab[0:1, :], oa[0][64:65, :], ["oa0"], ["oab"])
                    copy("dve", oab[32:33, :], oa[1][64:65, :], ["oa1"], ["oab"])
                    mm(pj[0][:], dsel[:], oab[0:33, :], True, True, ["dsel", "oab"], ["pj0"])
                    mm(pj[1][:], gselp[:, pr * 3 + b, :], gsig[:], True, True, ["gselp", "gsig"], ["pj1"])
                    P.op("dve", lambda e: e.tensor_scalar(out=coef[:], in0=pj[0][:], scalar1=1e-30, scalar2=None, op0=ALU.max), ["pj0"], ["coef"])
                    P.op("act", lambda e: e.activation(out=coef[:], in_=coef[:], func=AF.Ln), ["coef"], ["coef"])
                    P.op("act", lambda e: e.activation(out=coef[:], in_=coef[:], func=AF.Exp, scale=-1.0), ["coef"], ["coef"])
                    P.op("dve", lambda e: e.tensor_tensor(out=coef[:], in0=coef[:], in1=pj[1][:], op=ALU.mult), ["coef", "pj1"], ["coef"])
                    if first:
                        P.op("dve", lambda e: e.tensor_tensor(out=acc[:, pr, :], in0=coef[:], in1=oaug[:], op=ALU.mult), ["coef", "oaug"], ["acc"])
                    else:
                        P.op("dve", lambda e: e.tensor_tensor(out=coef[:], in0=coef[:], in1=oaug[:], op=ALU.mult), ["coef", "oaug"], ["coef"])
                        P.op("dve", lambda e: e.tensor_tensor(out=acc[:, pr, :], in0=acc[:, pr, :], in1=coef[:], op=ALU.add), ["coef", "acc"], ["acc"])

                def pmask(ap, key, base, cm, step):
                    P.op("pool", lambda e: e.affine_select(out=ap, in_=ap, pattern=[[step, G]], compare_op=ALU.is_ge, fill=0.0, base=base, channel_multiplier=cm), [key], [key])

                def pmask(ap, key, base, cm, step):
                    P.op("pool", lambda e: e.affine_select(out=ap, in_=ap, pattern=[[step, G]], compare_op=ALU.is_ge, fill=0.0, base=base, channel_multiplier=cm), [key], [key])

                P.op("pool", lambda e: e.memset(impS[:], 0.0), [], ["impS"])
                ncc = g // 4 + 1
                for h in range(4):
                    pr, hb_ = h // 2, (h % 2) * 64
                    ob, okey = oa[h % 2], "oa%d" % (h % 2)
                    for ci in range(ncc):
                        dl = 2048 * ci - 512 * g
                        sb_, skey = sc[ci % 2], "sc%d" % (ci % 2)
                        mm(sb_[:], kcmpT[hb_:hb_ + 64, ci * 128:(ci + 1) * 128], qn[hb_:hb_ + 64, pr, :], True, True, ["kcmpT", "qn"], [skey])
                        P.op("act", lambda e, sb_=sb_, ci=ci: e.activation(out=pTc[:, ci, :], in_=sb_[:], func=AF.Exp), [skey], ["wmkvb"])
                        if dl >= -2048:
                            pmask(pTc[:, ci, :], "wmkvb", -31 - dl, -16, 1)
                        mm(ob[0:65, :], vcmpA[:, ci, :], pTc[:, ci, :], ci == 0, ci == ncc - 1, ["vcmpA", "wmkvb"], [okey])
                    ibk = [(mi, "mi"), (pj[0], "pj0")]
                    for j in range(4):
                        bnk, bkey = ibk[j // 2]
                        reg = bnk[:, (j % 2) * 256:(j % 2) * 256 + 129]
                        for ci in range(ncc):
                            mm(reg, pTc[:, ci, j * 128:(j + 1) * 128], ovl[:, ci, :], ci == 0, ci == ncc - 1, ["wmkvb", "ovl"], [bkey])
                    P.op("dve", lambda e: e.tensor_scalar(out=m8[:, 0:2], in0=mi[:, 128:385:256], scalar1=1e-30, scalar2=None, op0=ALU.max), ["mi"], ["m8"])
                    P.op("dve", lambda e: e.tensor_scalar(out=m8[:, 2:4], in0=pj[0][:, 128:385:256], scalar1=1e-30, scalar2=None, op0=ALU.max), ["pj0"], ["m8"])
                    P.op("dve", lambda e: e.reciprocal(out=m8[:, 0:4], in_=m8[:, 0:4]), ["m8"], ["m8"])
                    for j in range(4):
                        bnk, bkey = ibk[j // 2]
                        reg = bnk[:, (j % 2) * 256:(j % 2) * 256 + 129]
                        P.op("dve", lambda e, reg=reg, j=j: e.scalar_tensor_tensor(out=impS[:, j, :], in0=reg[:, 0:128], scalar=m8[:, j:j + 1], in1=impS[:, j, :], op0=ALU.mult, op1=ALU.add), [bkey, "m8", "impS"], ["impS"])
                    if h % 2 == 1:
                        combine_pair(h // 2, 0, True)

                for j in range(4):
                    qt = 4 * g + j
                    u0 = 126 - 2 * qt
                    P.op("dve", lambda e, j=j, u0=u0: e.tensor_tensor(out=score[:], in0=impS[:, j, :], in1=addw[:, u0:u0 + 128], op=ALU.add), ["impS", "addw"], ["score"])
                    P.op("dve", lambda e: e.tensor_scalar(out=score[:, 0:1], in0=score[:, 0:1], scalar1=1e4, scalar2=None, op0=ALU.add), ["score"], ["score"])
                    P.op("dve", lambda e: e.max(out=m8[:, 0:8], in_=score[:]), ["score"], ["m8"])
                    P.op("dve", lambda e: e.match_replace(out=stmp[:], in_to_replace=m8[:, 0:8], in_values=score[:], imm_value=-2.0), ["score", "m8"], ["stmp"])
                    P.op("dve", lambda e: e.max(out=m8[:, 8:16], in_=stmp[:]), ["stmp"], ["m8"])
                    P.op("dve", lambda e: e.tensor_reduce(out=m8[:, 0:1], in_=m8[:, 8:16], axis=AX.X, op=ALU.min), ["m8"], ["m8"])
                    P.op("dve", lambda e: e.tensor_scalar(out=stmp[:], in0=score[:], scalar1=m8[:, 0:1], scalar2=None, op0=ALU.is_ge), ["score", "m8"], ["stmp"])
                    P.op("dve", lambda e: e.tensor_scalar(out=nbq[:], in0=stmp[:], scalar1=1.0, scalar2=-NEG, op0=ALU.subtract, op1=ALU.mult), ["stmp"], ["nbq"])
                    P.op("pe", lambda e: e.transpose(out=tp[:, 0:128], in_=nbq[:], identity=ident[:]), ["nbq", "ident"], ["tp"])
                    copy("dve", nb[:, j * 128:(j + 1) * 128], tp[:, 0:128], ["tp"], ["nb"])

                P.phase = "nsa_sel"
                scA = [(sc[0], "sc0"), (sc[1], "sc1")]
                scB = [(pj[0], "pj0"), (pj[1], "pj1")]
                pTA = [(pT[0][:], "pT0"), (pT[1][:], "pT1")]
                pTB = [(AakT4[:, 0, :], "AakT4/0"), (AakT4[:, 1, :], "AakT4/1")]
                nslab = (4 * g + 3) // 32 + 1
                for pr in range(2):
                    for hl in range(2):
                        for M in range(nslab):
                            copy(("act", "dve")[(hl + M) % 2], qcat[0:64, 2 * hl + M, :], qr[64 * hl:64 * hl + 64, pr, :], ["qr"], ["qcat"])
                            copy(("dve", "pool")[hl], qcat[64:128, 2 * hl + M, :], nb[64 * M:64 * M + 64, :], ["nb"], ["qcat"])
                    nkt = 4 * g + 4

                    def sel_a(kt):
                        (sa, ska), (sb2, skb) = scA[kt % 2], scB[kt % 2]
                        (pa, pka), (pb2, pkb) = pTA[kt % 2], pTB[kt % 2]
                        M = kt // 32
                        ks_ = slice(kt * 128, (kt + 1) * 128)
                        mm(sa[:], ksT[:, ks_], qcat[:, M, :], True, True, ["ksT", "qcat"], [ska])
                        mm(sb2[:], ksT[:, ks_], qcat[:, 2 + M, :], True, True, ["ksT", "qcat"], [skb])
                        P.op("act", lambda e: e.activation(out=pa, in_=sa[:], func=AF.Exp), [ska], [pka])
                        P.op("act", lambda e: e.activation(out=pb2, in_=sb2[:], func=AF.Exp), [skb], [pkb])
                        if kt >= 4 * g:
                            pmask(pa, pka, -128 * (kt - 4 * g), -1, 1)
                            pmask(pb2, pkb, -128 * (kt - 4 * g), -1, 1)

                    sel_a(0)
                    for kt in range(nkt):
                        if kt + 1 < nkt:
                            sel_a(kt + 1)
                        mm(oa[0][0:65, :], vsA[:, kt, :], pTA[kt % 2][0], kt == 0, kt == nkt - 1, ["vsA", pTA[kt % 2][1]], ["oa0"])
                        mm(oa[1][0:65, :], vsA[:, kt, :], pTB[kt % 2][0], kt == 0, kt == nkt - 1, ["vsA", pTB[kt % 2][1]], ["oa1"])
                    combine_pair(pr, 1, False)

                    P.phase = "nsa_win"
                    kts = [kt for kt in range(4 * g - 4, 4 * g + 4) if kt >= 0]

                    def win_a(kt):
                        (sa, ska), (sb2, skb) = scA[kt % 2], scB[kt % 2]
                        (pa, pka), (pb2, pkb) = pTA[kt % 2], pTB[kt % 2]
                        ro = (kt % 8) * 128
                        mm(sa[:], kwT[:, ro:ro + 128], qcat[:, 0, :], True, True, ["kwT", "qcat"], [ska])
                        mm(sb2[:], kwT[:, ro:ro + 128], qcat[:, 2, :], True, True, ["kwT", "qcat"], [skb])
                        P.op("act", lambda e: e.activation(out=pa, in_=sa[:], func=AF.Exp), [ska], [pka])
                        P.op("act", lambda e: e.activation(out=pb2, in_=sb2[:], func=AF.Exp), [skb], [pkb])
                        rel = kt - 4 * g
                        for p_, k_ in ((pa, pka), (pb2, pkb)):
                            if rel >= 0:
                                pmask(p_, k_, -128 * rel, -1, 1)
                            else:
                                pmask(p_, k_, 511 + 128 * rel, 1, -1)

                    win_a(kts[0])
                    for ii, kt in enumerate(kts):
                        if ii + 1 < len(kts):
                            win_a(kts[ii + 1])
                        mm(oa[0][0:65, :], vwA[:, kt % 8, :], pTA[kt % 2][0], ii == 0, ii == len(kts) - 1, ["vwA", pTA[kt % 2][1]], ["oa0"])
                        mm(oa[1][0:65, :], vwA[:, kt % 8, :], pTB[kt % 2][0], ii == 0, ii == len(kts) - 1, ["vwA", pTB[kt % 2][1]], ["oa1"])
                    combine_pair(pr, 2, False)
                    P.phase = "nsa_sel"

                P.phase = "nsa"
                o_, w_ = PK["nsag"]
                for pr in range(2):
                    src = acc[:, pr, :]
                    P.op("act", lambda e, src=src: e.activation(out=hb0[:, 0:G], in_=src, func=AF.Square), ["acc"], ["hb0"])
                    mm(mi[:], blkavg[:], hb0[:, 0:G], True, True, ["blkavg", "hb0"], ["mi"])
                    P.op("act", lambda e: e.activation(out=coef[:], in_=mi[:], func=AF.Ln, bias=epsc[:]), ["mi", "epsc"], ["coef"])
                    P.op("act", lambda e: e.activation(out=coef[:], in_=coef[:], func=AF.Exp, scale=-0.5), ["coef"], ["coef"])
                    P.op("dve", lambda e, src=src, pr=pr, o_=o_: e.scalar_tensor_tensor(out=coef[:], in0=coef[:], scalar=pk[:, o_ + pr:o_ + pr + 1], in1=src, op0=ALU.mult, op1=ALU.mult), ["coef", "acc", "pk"], ["coef"])
                    P.op("dve", lambda e, pr=pr: e.tensor_tensor(out=yT[:, 4 + pr, :], in0=coef[:], in1=gn[:, pr, :], op=ALU.mult), ["coef", "gn"], ["yT"])
                P.phase = ""
                stop_at(7)
                for i in range(4):
                    xb, xk = xt[i % 2], "xt%d" % (i % 2)
                    P.dma("sp", lambda e, i=i, t0=t0, xb=xb: e.dma_start(out=xb[:], in_=x_d[t0 + i * 128:t0 + (i + 1) * 128, :]), writes=[xk])
                    for half in range(2):
                        pb = pj[half]
                        for k in range(8):
                            mm(pb[:], yT[:, k, i * 128:(i + 1) * 128], woutb[:, k, half * 512:(half + 1) * 512], k == 0, k == 7, ["yT", "woutb"], ["pj%d" % half])
                        P.op("dve", lambda e, pb=pb, xb=xb, half=half: e.tensor_tensor(out=xb[:, half * 512:(half + 1) * 512], in0=xb[:, half * 512:(half + 1) * 512], in1=pb[:], op=ALU.add),
                             ["pj%d" % half, xk], [xk])
                    sk = "st%d" % (8 + i)
                    ssq = st[:, 8 + i:9 + i]
                    hjk = hb[i % 2]
                    P.op("act", lambda e, xb=xb, ssq=ssq, hjk=hjk: e.activation(out=hjk[:], in_=xb[:], func=AF.Square, accum_out=ssq), [xk], ["hb0", sk])
                    P.op("dve", lambda e, ssq=ssq: e.tensor_scalar(out=ssq, in0=ssq, scalar1=1.0 / D, scalar2=1e-6, op0=ALU.mult, op1=ALU.add), [sk], [sk])
                    P.op("dve", lambda e, ssq=ssq: e.reciprocal(out=ssq, in_=ssq), [sk], [sk])
                    P.op("act", lambda e, ssq=ssq: e.activation(out=ssq, in_=ssq, func=AF.Sqrt), [sk], [sk])
                    P.op("dve", lambda e, xb=xb, ssq=ssq: e.scalar_tensor_tensor(out=xb[:], in0=xb[:], scalar=ssq, in1=gfin[:], op0=ALU.mult, op1=ALU.mult), [xk, sk, "gfin"], [xk])
                    P.dma("pool", lambda e, i=i, t0=t0, xb=xb: e.dma_start(out=out_d[t0 + i * 128:t0 + (i + 1) * 128, :], in_=xb[:]), reads=[xk], is_out=True)
        except _Stop:
            P.dma("pool", lambda e: e.dma_start(out=out_d[0:128, :], in_=xt[0][:]), reads=["xt0"], is_out=True)
        P.finish("sp")
        P.emit(block, sems, dsems)
    return nc


def make_in_maps(inputs, T, batches):
    ci = _colidx()
    w_in = np.asarray(inputs["w_in"][0])
    wcat = np.ascontiguousarray(w_in[:, ci].reshape(8, 128, NCOL))
    wout = np.ascontiguousarray(np.asarray(inputs["w_out"][0]).reshape(8, 128, D))
    wmkv = np.ascontiguousarray(np.asarray(inputs["w_mem_kv"][0]).reshape(8, 128, 512))
    pk = host_params({k: np.asarray(v) for k, v in inputs.items()})
    w1k = np.asarray(inputs["nsa_cmp_k_w1"][0]).reshape(32, 64, 64).transpose(1, 0, 2)
    w1v = np.asarray(inputs["nsa_cmp_v_w1"][0]).reshape(32, 64, 64).transpose(1, 0, 2)
    w1 = np.ascontiguousarray(np.concatenate([w1k, w1v], 0).reshape(128, 2048))
    w2k = np.asarray(inputs["nsa_cmp_k_w2"][0]); w2v = np.asarray(inputs["nsa_cmp_v_w2"][0])
    w2 = np.zeros((128, 192), np.float32)
    w2[0:64] = np.concatenate([w2k, w2k, w2v], 1)
    lora = np.ascontiguousarray(np.concatenate([np.asarray(inputs["rwkv_w_up"][0]), np.asarray(inputs["rwkv_a_up"][0])], 0))
    w0row = np.ascontiguousarray(np.asarray(inputs["rwkv_w0"][0]).reshape(1, 512))
    consts = host_consts(T)
    maps = []
    for b in batches:
        m = {
            "x": np.ascontiguousarray(np.asarray(inputs["x"][b][:T])),
            "mem": np.ascontiguousarray(np.asarray(inputs["mem"][b])),
            "wcat": wcat, "wout": wout, "wmkv": wmkv, "pk": pk, "w1": w1, "w2": w2, "lora": lora, "w0row": w0row,
            "gfin": np.ascontiguousarray(np.asarray(inputs["norm_final_g"]).reshape(1, D)),
        }
        m.update(consts)
        maps.append(m)
    return maps


_NC_CACHE = {}


def kernel(**inputs):
    T = inputs["x"].shape[1]
    B = inputs["x"].shape[0]
    if T not in _NC_CACHE:
        _NC_CACHE[T] = build_nc(T)
    nc = _NC_CACHE[T]
    batches = [i % B for i in range(8)]
    maps = make_in_maps(inputs, T, batches)
    res = run_bass_kernel_spmd(nc, maps, core_ids=list(range(8)))
    out = np.stack([res.results[b]["out"] for b in range(B)], axis=0)
    return out.astype(np.float32)
```
